# Optimizing a Trainium2 kernel written in Bass

```python
import numpy as np
import jax
import jax.numpy as jnp
from jax import lax

D_MODEL = 1024
BATCH = 8
SEQ = 4096
DEPTH = 2

ROPE_THETA = 10000.0
NORM_EPS = 1e-6
NEG_INF = -1e30
TINY = 1e-30
D_FF = 2816
Q_BLOCK = 128

MLA_HEADS = 8
MLA_NOPE = 64
MLA_ROPE = 32
MLA_V = 64
MLA_Q_LORA = 256
MLA_KV_LORA = 128

GLA_HEADS = 4
GLA_DK = 64
GLA_DV = 128
GLA_GATE_RANK = 16
GLA_GATE_TAU = 16.0
GLA_CHUNK = 64

NSA_HEADS = 16
NSA_KV_GROUPS = 4
NSA_HEAD_DIM = 64
NSA_CMP_LEN = 32
NSA_CMP_STRIDE = 16
NSA_CMP_HIDDEN = 128
NSA_SEL_LEN = 64
NSA_TOP_N = 16
NSA_WINDOW = 512
NSA_Q_BLOCK = 64
NSA_FORCE_SCORE = 1e4

HY_SPLITS = (MLA_Q_LORA, MLA_KV_LORA, MLA_ROPE, GLA_HEADS * GLA_DK, GLA_HEADS * GLA_DK,
             GLA_HEADS * GLA_DV, GLA_GATE_RANK, GLA_HEADS * GLA_DV)
HY_IN = sum(HY_SPLITS)
HY_OUT = MLA_HEADS * MLA_V + GLA_HEADS * GLA_DV
NSA_KV_WIDTH = NSA_KV_GROUPS * NSA_HEAD_DIM
NSA_SPLITS = (NSA_HEADS * NSA_HEAD_DIM,) + (NSA_KV_WIDTH,) * 6 + (NSA_HEADS * 3,)
NSA_IN = sum(NSA_SPLITS)
NSA_OUT = NSA_HEADS * NSA_HEAD_DIM
N_EVEN = (DEPTH + 1) // 2
N_ODD = DEPTH // 2

kernel_name = 'hybrid_mla_gla_nsa_macaron'


def split_cols(h, sizes):
    return jnp.split(h, np.cumsum(sizes)[:-1].tolist(), axis=-1)


def rms_norm(x, g):
    xf = x.astype(jnp.float32)
    y = xf * lax.rsqrt(jnp.mean(xf * xf, axis=-1, keepdims=True) + NORM_EPS)
    return (y * g.astype(jnp.float32)).astype(x.dtype)


def rope(x, pos):
    d = x.shape[-1]
    inv_freq = jnp.power(ROPE_THETA, -jnp.arange(d // 2, dtype=jnp.float32) * (2.0 / d))
    ang = pos.astype(jnp.float32)[..., None] * inv_freq
    cos = jnp.cos(ang)[:, :, None, :]
    sin = jnp.sin(ang)[:, :, None, :]
    xf = x.astype(jnp.float32)
    x1, x2 = xf[..., : d // 2], xf[..., d // 2:]
    return jnp.concatenate([x1 * cos - x2 * sin, x2 * cos + x1 * sin], axis=-1).astype(x.dtype)


def masked_softmax(s, mask):
    s = jnp.where(mask, s.astype(jnp.float32), NEG_INF)
    m = jnp.max(s, axis=-1, keepdims=True)
    p = jnp.where(mask, jnp.exp(s - m), 0.0)
    return p / jnp.maximum(jnp.sum(p, axis=-1, keepdims=True), TINY)


def swiglu(x, w_gate, w_up, w_down):
    return (jax.nn.silu(x @ w_gate) * (x @ w_up)) @ w_down


def causal_block_attention(q, k, v, scale):
    B, S, H, Dk = q.shape
    nb = S // Q_BLOCK
    q_blocks = q.reshape(B, nb, Q_BLOCK, H, Dk).transpose(1, 0, 2, 3, 4)
    key_pos = jnp.arange(S)

    def one_block(args):
        i, qb = args
        s = jnp.einsum('bqhd,bkhd->bhqk', qb, k, preferred_element_type=jnp.float32) * scale
        q_pos = i * Q_BLOCK + jnp.arange(Q_BLOCK)
        p = masked_softmax(s, key_pos[None, :] <= q_pos[:, None])
        return jnp.einsum('bhqk,bkhd->bqhd', p.astype(v.dtype), v)

    out = lax.map(one_block, (jnp.arange(nb), q_blocks))
    return out.transpose(1, 0, 2, 3, 4).reshape(B, S, H, v.shape[-1])


def mla_heads(c_q, c_kv, k_r, positions, q_norm, w_uq, kv_norm, w_ukv):
    B, S, _ = c_q.shape
    q = (rms_norm(c_q, q_norm) @ w_uq).reshape(B, S, MLA_HEADS, MLA_NOPE + MLA_ROPE)
    q_nope, q_rot = q[..., :MLA_NOPE], rope(q[..., MLA_NOPE:], positions)
    kv = (rms_norm(c_kv, kv_norm) @ w_ukv).reshape(B, S, MLA_HEADS, MLA_NOPE + MLA_V)
    k_nope, v = kv[..., :MLA_NOPE], kv[..., MLA_NOPE:]
    k_rot = rope(k_r.reshape(B, S, 1, MLA_ROPE), positions)
    q_full = jnp.concatenate([q_nope, q_rot], axis=-1)
    k_full = jnp.concatenate([k_nope, jnp.broadcast_to(k_rot, (B, S, MLA_HEADS, MLA_ROPE))], axis=-1)
    o = causal_block_attention(q_full, k_full, v, (MLA_NOPE + MLA_ROPE) ** -0.5)
    return o.reshape(B, S, MLA_HEADS * MLA_V)


def gla_heads(h_q, h_k, h_v, h_a, h_r, w_a2, b_a, out_norm):
    B, S, _ = h_q.shape
    H, DK, DV, C = GLA_HEADS, GLA_DK, GLA_DV, GLA_CHUNK
    nc = S // C
    f32 = jnp.float32
    q = h_q.reshape(B, nc, C, H, DK).astype(f32) * DK ** -0.5
    k = h_k.reshape(B, nc, C, H, DK).astype(f32)
    v = h_v.reshape(B, nc, C, H, DV).astype(f32)
    log_a = jax.nn.log_sigmoid((h_a @ w_a2 + b_a).astype(f32)) / GLA_GATE_TAU
    b = jnp.cumsum(log_a.reshape(B, nc, C, H, DK), axis=2)
    b_last = b[:, :, -1:]
    k_dec = k * jnp.exp(b_last - b)
    q_inter = q * jnp.exp(b)
    q_intra = q * jnp.exp(b - b_last)
    causal = jnp.tril(jnp.ones((C, C), dtype=bool))
    A = jnp.where(causal, jnp.einsum('bnihd,bnjhd->bnhij', q_intra, k_dec), 0.0)
    o_intra = jnp.einsum('bnhij,bnjhv->bnihv', A, v)
    kv_chunk = jnp.einsum('bnjhd,bnjhv->nbhdv', k_dec, v)
    decay_chunk = jnp.exp(b[:, :, -1]).transpose(1, 0, 2, 3)

    def step(state, inp):
        dec, kv = inp
        return state * dec[..., None] + kv, state

    _, s_prev = lax.scan(step, jnp.zeros((B, H, DK, DV), f32), (decay_chunk, kv_chunk))
    o_inter = jnp.einsum('bnihd,nbhdv->bnihv', q_inter, s_prev)
    o = rms_norm((o_intra + o_inter).reshape(B, S, H, DV), out_norm)
    o = o * jax.nn.silu(h_r.reshape(B, S, H, DV).astype(f32))
    return o.reshape(B, S, H * DV).astype(h_q.dtype)


def mla_gla_mixer(u, positions, w_in, q_norm, w_uq, kv_norm, w_ukv, w_a2, b_a, out_norm, w_out):
    c_q, c_kv, k_r, g_q, g_k, g_v, g_a, g_r = split_cols(u @ w_in, HY_SPLITS)
    o_mla = mla_heads(c_q, c_kv, k_r, positions, q_norm, w_uq, kv_norm, w_ukv)
    o_gla = gla_heads(g_q, g_k, g_v, g_a, g_r, w_a2, b_a, out_norm)
    return jnp.concatenate([o_mla, o_gla.astype(o_mla.dtype)], axis=-1) @ w_out


def nsa_mixer(u, positions, w_in, pos_k, pos_v, ck_w1, ck_w2, cv_w1, cv_w2, w_out):
    B, S, _ = u.shape
    H, G, Dh = NSA_HEADS, NSA_KV_GROUPS, NSA_HEAD_DIM
    Hg = H // G
    dt = u.dtype
    q, k_c, v_c, k_s, v_s, k_w, v_w, g = split_cols(u @ w_in, NSA_SPLITS)
    q = rope(q.reshape(B, S, H, Dh), positions)
    gates = jax.nn.sigmoid(g.reshape(B, S, H, 3))

    n_cmp = (S - NSA_CMP_LEN) // NSA_CMP_STRIDE + 1
    cmp_start = np.arange(n_cmp) * NSA_CMP_STRIDE
    cmp_end = cmp_start + NSA_CMP_LEN - 1
    cmp_idx = cmp_start[:, None] + np.arange(NSA_CMP_LEN)[None, :]

    def compress(t, pos_emb, w1, w2):
        blocks = t.reshape(B, S, G, Dh)[:, cmp_idx] + pos_emb[None, None, :, None, :]
        flat = blocks.transpose(0, 1, 3, 2, 4).reshape(B, n_cmp, G, NSA_CMP_LEN * Dh)
        return jax.nn.silu(flat @ w1) @ w2

    k_cmp = rope(compress(k_c, pos_k, ck_w1, ck_w2), positions[:, cmp_end])
    v_cmp = compress(v_c, pos_v, cv_w1, cv_w2)
    cmp_end_j = jnp.asarray(cmp_end)

    n_blk = S // NSA_SEL_LEN
    n_pad = max(n_blk, NSA_TOP_N)

    def to_blocks(t):
        tb = t.reshape(B, n_blk, NSA_SEL_LEN, G, Dh).transpose(0, 3, 1, 2, 4)
        return jnp.pad(tb, ((0, 0), (0, 0), (0, n_pad - n_blk), (0, 0), (0, 0)))

    k_sel_all = to_blocks(rope(k_s.reshape(B, S, G, Dh), positions))
    v_sel_all = to_blocks(v_s.reshape(B, S, G, Dh))
    blk_start = np.arange(n_pad) * NSA_SEL_LEN
    ov = np.minimum(cmp_start[:, None] + NSA_CMP_LEN, blk_start[None, :] + NSA_SEL_LEN) - np.maximum(cmp_start[:, None], blk_start[None, :])
    overlap = jnp.asarray(np.clip(ov, 0, None) / NSA_CMP_LEN, dtype=jnp.float32)

    pad_w = ((0, 0), (NSA_WINDOW, 0), (0, 0), (0, 0))
    k_win = jnp.pad(rope(k_w.reshape(B, S, G, Dh), positions), pad_w)
    v_win = jnp.pad(v_w.reshape(B, S, G, Dh), pad_w)

    nq = S // NSA_Q_BLOCK
    q_blocks = q.reshape(B, nq, NSA_Q_BLOCK, G, Hg, Dh).transpose(1, 0, 2, 3, 4, 5)
    g_blocks = gates.reshape(B, nq, NSA_Q_BLOCK, G, Hg, 3).transpose(1, 0, 2, 3, 4, 5)
    bi = jnp.arange(B)[:, None, None, None]
    gi = jnp.arange(G)[None, :, None, None]
    scale = Dh ** -0.5
    span = NSA_Q_BLOCK + NSA_WINDOW

    def one_block(args):
        i, qb, gb = args
        t = i * NSA_Q_BLOCK + jnp.arange(NSA_Q_BLOCK)
        s_c = jnp.einsum('bqghd,bngd->bghqn', qb, k_cmp, preferred_element_type=jnp.float32) * scale
        p_c = masked_softmax(s_c, cmp_end_j[None, :] <= t[:, None])
        o_c = jnp.einsum('bghqn,bngd->bqghd', p_c.astype(dt), v_cmp)
        imp = jnp.einsum('bghqn,nm->bgqm', p_c, overlap)
        blk = jnp.arange(n_pad)[None, :]
        cur = (t // NSA_SEL_LEN)[:, None]
        forced = (blk == 0) | (blk == cur) | (blk == cur - 1)
        score = jnp.where(blk <= cur, jnp.where(forced, NSA_FORCE_SCORE, imp), NEG_INF)
        _, sel = lax.top_k(score, NSA_TOP_N)
        k_sel = k_sel_all[bi, gi, sel].reshape(B, G, NSA_Q_BLOCK, NSA_TOP_N * NSA_SEL_LEN, Dh)
        v_sel = v_sel_all[bi, gi, sel].reshape(B, G, NSA_Q_BLOCK, NSA_TOP_N * NSA_SEL_LEN, Dh)
        kpos = (sel[..., None] * NSA_SEL_LEN + jnp.arange(NSA_SEL_LEN)).reshape(B, G, NSA_Q_BLOCK, -1)
        s_s = jnp.einsum('bqghd,bgqmd->bghqm', qb, k_sel, preferred_element_type=jnp.float32) * scale
        p_s = masked_softmax(s_s, (kpos <= t[None, None, :, None])[:, :, None])
        o_s = jnp.einsum('bghqm,bgqmd->bqghd', p_s.astype(dt), v_sel)
        start = i * NSA_Q_BLOCK
        k_wb = lax.dynamic_slice_in_dim(k_win, start, span, axis=1)
        v_wb = lax.dynamic_slice_in_dim(v_win, start, span, axis=1)
        wpos = (start - NSA_WINDOW + jnp.arange(span))[None, :]
        m_w = (wpos <= t[:, None]) & (wpos > t[:, None] - NSA_WINDOW) & (wpos >= 0)
        s_w = jnp.einsum('bqghd,bkgd->bghqk', qb, k_wb, preferred_element_type=jnp.float32) * scale
        p_w = masked_softmax(s_w, m_w)
        o_w = jnp.einsum('bghqk,bkgd->bqghd', p_w.astype(dt), v_wb)
        return gb[..., 0:1] * o_c + gb[..., 1:2] * o_s + gb[..., 2:3] * o_w

    out = lax.map(one_block, (jnp.arange(nq), q_blocks, g_blocks))
    return out.transpose(1, 0, 2, 3, 4, 5).reshape(B, S, NSA_OUT) @ w_out


def setup_inputs(seed: int = 0) -> dict:
    key = jax.random.key(seed)
    ks = iter(jax.random.split(key, 40))
    f32 = jnp.float32

    def w(shape, fan_in):
        return jax.random.normal(next(ks), shape, f32) * fan_in ** -0.5

    def gain(shape):
        return 1.0 + 0.02 * jax.random.normal(next(ks), shape, f32)

    def small(shape, s):
        return s * jax.random.normal(next(ks), shape, f32)

    x = jax.random.normal(next(ks), (BATCH, SEQ, D_MODEL), f32)
    offsets = jax.random.randint(next(ks), (BATCH, 1), 0, 1024)
    positions = (offsets + jnp.arange(SEQ)[None, :]).astype(jnp.int32)
    return {
        'x': x,
        'positions': positions,
        'ffn_norm': gain((DEPTH, 2, D_MODEL)),
        'ffn_w_gate': w((DEPTH, 2, D_MODEL, D_FF), D_MODEL),
        'ffn_w_up': w((DEPTH, 2, D_MODEL, D_FF), D_MODEL),
        'ffn_w_down': w((DEPTH, 2, D_FF, D_MODEL), D_FF),
        'mix_norm': gain((DEPTH, D_MODEL)),
        'hy_w_in': w((N_EVEN, D_MODEL, HY_IN), D_MODEL),
        'mla_q_norm': gain((N_EVEN, MLA_Q_LORA)),
        'mla_w_uq': w((N_EVEN, MLA_Q_LORA, MLA_HEADS * (MLA_NOPE + MLA_ROPE)), MLA_Q_LORA),
        'mla_kv_norm': gain((N_EVEN, MLA_KV_LORA)),
        'mla_w_ukv': w((N_EVEN, MLA_KV_LORA, MLA_HEADS * (MLA_NOPE + MLA_V)), MLA_KV_LORA),
        'gla_w_a2': w((N_EVEN, GLA_GATE_RANK, GLA_HEADS * GLA_DK), GLA_GATE_RANK),
        'gla_b_a': small((N_EVEN, GLA_HEADS * GLA_DK), 0.1),
        'gla_out_norm': gain((N_EVEN, GLA_DV)),
        'hy_w_out': w((N_EVEN, HY_OUT, D_MODEL), HY_OUT),
        'nsa_w_in': w((N_ODD, D_MODEL, NSA_IN), D_MODEL),
        'nsa_pos_k': small((N_ODD, NSA_CMP_LEN, NSA_HEAD_DIM), 0.1),
        'nsa_pos_v': small((N_ODD, NSA_CMP_LEN, NSA_HEAD_DIM), 0.1),
        'nsa_ck_w1': w((N_ODD, NSA_CMP_LEN * NSA_HEAD_DIM, NSA_CMP_HIDDEN), NSA_CMP_LEN * NSA_HEAD_DIM),
        'nsa_ck_w2': w((N_ODD, NSA_CMP_HIDDEN, NSA_HEAD_DIM), NSA_CMP_HIDDEN),
        'nsa_cv_w1': w((N_ODD, NSA_CMP_LEN * NSA_HEAD_DIM, NSA_CMP_HIDDEN), NSA_CMP_LEN * NSA_HEAD_DIM),
        'nsa_cv_w2': w((N_ODD, NSA_CMP_HIDDEN, NSA_HEAD_DIM), NSA_CMP_HIDDEN),
        'nsa_w_out': w((N_ODD, NSA_OUT, D_MODEL), NSA_OUT),
        'final_norm': gain((D_MODEL,)),
    }


def reference(x, positions, ffn_norm, ffn_w_gate, ffn_w_up, ffn_w_down, mix_norm,
              hy_w_in, mla_q_norm, mla_w_uq, mla_kv_norm, mla_w_ukv, gla_w_a2, gla_b_a,
              gla_out_norm, hy_w_out, nsa_w_in, nsa_pos_k, nsa_pos_v, nsa_ck_w1, nsa_ck_w2,
              nsa_cv_w1, nsa_cv_w2, nsa_w_out, final_norm):
    h = x
    for layer in range(DEPTH):
        j = layer // 2
        h = h + 0.5 * swiglu(rms_norm(h, ffn_norm[layer, 0]), ffn_w_gate[layer, 0], ffn_w_up[layer, 0], ffn_w_down[layer, 0])
        u = rms_norm(h, mix_norm[layer])
        if layer % 2 == 0:
            mixed = mla_gla_mixer(u, positions, hy_w_in[j], mla_q_norm[j], mla_w_uq[j], mla_kv_norm[j],
                                  mla_w_ukv[j], gla_w_a2[j], gla_b_a[j], gla_out_norm[j], hy_w_out[j])
        else:
            mixed = nsa_mixer(u, positions, nsa_w_in[j], nsa_pos_k[j], nsa_pos_v[j], nsa_ck_w1[j],
                              nsa_ck_w2[j], nsa_cv_w1[j], nsa_cv_w2[j], nsa_w_out[j])
        h = h + mixed.astype(h.dtype)
        h = h + 0.5 * swiglu(rms_norm(h, ffn_norm[layer, 1]), ffn_w_gate[layer, 1], ffn_w_up[layer, 1], ffn_w_down[layer, 1])
    return rms_norm(h, final_norm)
```

```python
import numpy as np
from contextlib import ExitStack
import concourse.bass as bass
import concourse.mybir as mybir
from concourse.bass_utils import run_bass_kernel_spmd

F32 = mybir.dt.float32
BF16 = mybir.dt.bfloat16
I32 = mybir.dt.int32
ALU = mybir.AluOpType
AF = mybir.ActivationFunctionType
AX = mybir.AxisListType

S = 4096
D = 1024
DFF = 2816
NFC = 22
NG = 8
TG = 512
EPS = 1e-6
HY_IN = 1968
NSA_IN = 2608
NEGB = -30000.0

ENGINES = ["tensor", "vector", "scalar", "gpsimd", "sync"]
class Buf:
    __slots__ = ("name", "last_w", "readers")

    def __init__(self, name=""):
        self.name = name
        self.last_w = None
        self.readers = []


class Op:
    __slots__ = ("eng", "fn", "raw", "oth", "is_dma", "sig", "sem", "semval", "prev_semval", "gidx")


class Prog:
    def __init__(self, nc, n_dma_sems=12):
        self.nc = nc
        self.ops = {e: [] for e in ENGINES}
        self.n = 0
        self.n_dma_sems = n_dma_sems
        self.pending_barrier = {}
        self.dma_last = {}
        self.dma_cnt = {}

    def op(self, eng, fn, reads=(), writes=(), dma=False):
        o = Op()
        o.eng = eng
        o.fn = fn
        o.is_dma = dma
        o.raw = set()
        o.oth = set()
        o.sig = False
        o.sem = None
        o.semval = 0
        o.prev_semval = 0
        o.gidx = self.n
        self.n += 1
        for b in reads:
            if b.last_w is not None:
                o.raw.add(b.last_w)
        for b in writes:
            if b.last_w is not None:
                o.oth.add(b.last_w)
            for r in b.readers:
                o.oth.add(r)
        for b in reads:
            b.readers.append(o)
        for b in writes:
            b.last_w = o
            b.readers = []
        o.raw.discard(o)
        o.oth.discard(o)
        if eng in self.pending_barrier:
            for d in self.pending_barrier.pop(eng):
                o.raw.add(d)
        self.ops[eng].append(o)
        if dma:
            k = self.dma_cnt.get(eng, 0)
            self.dma_cnt[eng] = k + 1
            self.dma_last[(eng, k % self.n_dma_sems)] = o
        return o

    def barrier(self):
        deps = []
        for e in ENGINES:
            comp = [x for x in self.ops[e] if not x.is_dma]
            if comp:
                deps.append(comp[-1])
        deps.extend(self.dma_last.values())
        for e in ENGINES:
            self.pending_barrier[e] = list(deps) + self.pending_barrier.get(e, [])

    def mm(self, fn, reads=(), writes=()):
        return self.op("tensor", fn, reads, writes)

    def dve(self, fn, reads=(), writes=()):
        return self.op("vector", fn, reads, writes)

    def act(self, fn, reads=(), writes=()):
        return self.op("scalar", fn, reads, writes)

    def pool(self, fn, reads=(), writes=()):
        return self.op("gpsimd", fn, reads, writes)

    def load(self, out, in_, reads=(), writes=(), eng="sync", **kw):
        return self.op(eng, lambda e: e.dma_start(out=out, in_=in_, **kw), reads, writes, dma=True)

    def needed_deps(self, o):
        res = []
        for d in o.raw:
            if d.eng == o.eng and not d.is_dma and not o.is_dma:
                if o.eng == "tensor":
                    continue
                res.append(d)
            else:
                res.append(d)
        for d in o.oth:
            if d.eng == o.eng and not d.is_dma and not o.is_dma:
                continue
            res.append(d)
        return res

    def finalize(self, stack):
        nc = self.nc
        for e in ENGINES:
            for o in self.ops[e]:
                if o.is_dma:
                    o.sig = True
                for d in self.needed_deps(o):
                    d.sig = True
        csem = {}
        for e in ["tensor", "vector", "scalar", "gpsimd"]:
            csem[e] = stack.enter_context(nc.semaphore("c_" + e))
        dpool = {}
        for e in ["sync", "gpsimd", "scalar"]:
            if any(o.is_dma for o in self.ops[e]):
                dpool[e] = [stack.enter_context(nc.semaphore("d_%s_%d" % (e, i))) for i in range(self.n_dma_sems)]
        for e in ENGINES:
            cnt = 0
            k = 0
            uses = {}
            for o in self.ops[e]:
                if o.is_dma:
                    s = dpool[e][k % self.n_dma_sems]
                    k += 1
                    o.sem = s
                    o.prev_semval = uses.get(id(s), 0)
                    o.semval = o.prev_semval + 16
                    uses[id(s)] = o.semval
                elif o.sig:
                    cnt += 1
                    o.sem = csem[e]
                    o.semval = cnt
        self.final_dma = []
        for e in dpool:
            last = {}
            for o in self.ops[e]:
                if o.is_dma:
                    last[id(o.sem)] = (o.sem, o.semval)
            self.final_dma.append((e, list(last.values())))
        block = stack.enter_context(nc.Block())
        prog = self

        def make(e):
            def body(eng):
                waited = {}
                for o in prog.ops[e]:
                    need = {}
                    for d in prog.needed_deps(o):
                        key = id(d.sem)
                        if key not in need or need[key][1] < d.semval:
                            need[key] = (d.sem, d.semval)
                    if o.is_dma and o.prev_semval > 0:
                        key = id(o.sem)
                        if key not in need or need[key][1] < o.prev_semval:
                            need[key] = (o.sem, o.prev_semval)
                    for key, (s, v) in need.items():
                        if waited.get(key, 0) >= v:
                            continue
                        eng.wait_ge(s, v)
                        waited[key] = v
                    ins = o.fn(eng)
                    if o.sig:
                        ins.then_inc(o.sem, 16 if o.is_dma else 1)
                for (ee, lst) in prog.final_dma:
                    if ee == e:
                        for (s, v) in lst:
                            if waited.get(id(s), 0) < v:
                                eng.wait_ge(s, v)
            return body

        for e in ENGINES:
            if not self.ops[e]:
                continue
            getattr(block, e)(make(e))


CB = {}
_off = 0
for _n, _w in [("ident", 128), ("ones", 128), ("cpen", 128), ("bpen", 128), ("tri", 64), ("scanmask", 512),
               ("freq", 4), ("wide", 128), ("col0", 64), ("sel48", 48 * 64 + 1), ("E", 32 * 128)]:
    CB[_n] = (_off, _w)
    _off += _w
CB_W = _off


def make_consts():
    c = np.zeros((128, CB_W), np.float32)
    p = np.arange(128)[:, None]

    def put(n, a):
        o, w = CB[n]
        c[: a.shape[0], o:o + a.shape[1]] = a
    put("ident", np.eye(128, dtype=np.float32))
    put("ones", np.ones((128, 128), np.float32))
    f = np.arange(128)[None, :]
    put("cpen", np.where(p <= f, 0.0, NEGB).astype(np.float32))
    put("bpen", np.where(p > f, 0.0, NEGB).astype(np.float32))
    j = np.arange(64)[:, None]
    i = np.arange(64)[None, :]
    put("tri", (j <= i).astype(np.float32))
    sm = np.ones((128, 512), np.float32)
    sm[:, ::64] = 0.0
    put("scanmask", sm)
    fr = np.zeros((128, 4), np.float32)
    r = np.arange(128)
    im = (r - 64) % 16
    fr[:, 0] = (10000.0 ** (-im * (2.0 / 32))) / (2 * np.pi)
    fr[:, 1] = np.where(((r - 64) % 32) < 16, -1.0, 1.0) * (2 * np.pi * (1 - 1e-6))
    i2 = r % 32
    fr[:, 2] = (10000.0 ** (-i2 * (2.0 / 64))) / (2 * np.pi)
    fr[:, 3] = np.where((r % 64) < 32, -1.0, 1.0) * (2 * np.pi * (1 - 1e-6))
    put("freq", fr)
    x = np.arange(128)[None, :]
    rel = x - 64 - (p >= 64)
    wide = np.where(rel > 0, -1e30, np.where(rel >= -1, 1e4, 0.0)).astype(np.float32)
    put("wide", wide)
    c0 = np.full((128, 64), -3e38, np.float32)
    c0[:, 0] = 1e4
    put("col0", c0)
    sel = np.zeros((128, 48 * 64 + 1), np.float32)
    for rr in range(48):
        sel[rr, 1 + rr * 64:1 + (rr + 1) * 64] = 1.0
    put("sel48", sel)
    E = np.zeros((128, 32 * 128), np.float32)
    for kt in range(32):
        for key in range(128):
            E[2 * kt + (key >= 64), kt * 128 + key] = -NEGB
    put("E", E)
    return c


def make_cmp_consts():
    n = np.arange(256)[:, None]
    t = np.arange(S)[None, :]
    cm = np.where((16 * n + 31 <= t) & (n < 255), 0.0, NEGB).astype(np.float32)
    ncmp = np.arange(255)
    cs_ = ncmp * 16
    bs_ = np.arange(64) * 64
    ov = np.minimum(cs_[:, None] + 32, bs_[None, :] + 64) - np.maximum(cs_[:, None], bs_[None, :])
    ovl = np.zeros((256, 65), np.float32)
    ovl[:255, :64] = np.clip(ov, 0, None) / 32.0
    ovl[:255, 64] = 1.0
    return cm, ovl


class KB:
    def __init__(self, debug_phases=None):
        self.nc = bass.Bass("TRN2", target_bir_lowering=False)
        self.P = Prog(self.nc)
        self.debug_phases = debug_phases
        self.uid = 0
        self.gdeps = []

    def name(self, p):
        self.uid += 1
        return "%s_%d" % (p, self.uid)

    def dram(self, name, shape, dt, kind="Internal"):
        return self.nc.dram_tensor(name, list(shape), dt, kind=kind).ap()

    def sb(self, st, shape, dt, name="t"):
        return st.enter_context(self.nc.sbuf_tensor(self.name(name), list(shape), dt))

    def ps(self, st, shape, dt=F32, name="p"):
        return st.enter_context(self.nc.psum_tensor(self.name(name), list(shape), dt))

    def declare(self):
        d = self.dram
        I = {}
        I["x"] = d("x", [S, D], F32, "ExternalInput")
        I["positions"] = d("positions", [1, S], I32, "ExternalInput")
        I["consts"] = d("consts", [128, CB_W], F32, "ExternalInput")
        I["cmpmask"] = d("cmpmask", [256, S], F32, "ExternalInput")
        I["ovl"] = d("ovl", [256, 65], F32, "ExternalInput")
        for n, shp in [("ffn_norm", [4, D]), ("ffn_w_gate", [4, D, DFF]), ("ffn_w_up", [4, D, DFF]),
                       ("ffn_w_down", [4, DFF, D]), ("mix_norm", [2, D]), ("hy_w_in", [D, HY_IN]),
                       ("mla_q_norm", [1, 256]), ("mla_w_uq", [256, 768]), ("mla_kv_norm", [1, 128]),
                       ("mla_w_ukv", [128, 1024]), ("gla_w_a2", [16, 256]), ("gla_b_a", [1, 256]),
                       ("gla_out_norm", [1, 128]), ("hy_w_out", [D, D]), ("nsa_w_in", [D, NSA_IN]),
                       ("nsa_pos_k", [32, 64]), ("nsa_pos_v", [32, 64]), ("nsa_ck_w1", [2048, 128]),
                       ("nsa_ck_w2", [128, 64]), ("nsa_cv_w1", [2048, 128]), ("nsa_cv_w2", [128, 64]),
                       ("nsa_w_out", [D, D]), ("final_norm", [1, D])]:
            I[n] = d(n, shp, F32, "ExternalInput")
        self.I = I
        self.out = d("out", [S, D], F32, "ExternalOutput")
        self.hT = [d("hT%d" % i, [D, S], F32) for i in range(2)]
        self.hbuf = [Buf("hT0"), Buf("hT1")]
        self.wgu = d("wgu", [4, NFC, 128, 2 * 8 * 128], BF16)
        self.wdn = d("wdn", [4, 8, 128, NFC * 128], BF16)
        self.wgu_b = {}
        self.wdn_b = {}
        self.prepq = []

    def prep_ffn(self, f):
        I = self.I
        items = []
        self.wgu_b[f] = {}
        self.wdn_b[f] = []
        for c in range(NFC):
            for which, src in enumerate([I["ffn_w_gate"], I["ffn_w_up"]]):
                bf_ = Buf("wgu")
                self.wgu_b[f][(which, c)] = bf_
                dst = self.wgu[f, c].rearrange("p (w k j) -> p w k j", w=2, k=8, j=128)[:, which, :, :]
                s_ = src[f, :, c * 128:(c + 1) * 128].rearrange("(k p) j -> p k j", p=128)
                items.append((dst, s_, bf_))
        for c in range(NFC):
            bf_ = Buf("wdn")
            self.wdn_b[f].append(bf_)
            dst = self.wdn[f].rearrange("fc p (c j) -> p fc c j", c=NFC, j=128)[:, :, c, :]
            s_ = I["ffn_w_down"][f, c * 128:(c + 1) * 128, :].rearrange("p (fc j) -> p fc j", j=128)
            items.append((dst, s_, bf_))
        self.prepq.extend(items)

    def pump(self, n):
        for _ in range(n):
            if not self.prepq:
                return
            dst, s_, bf_ = self.prepq.pop(0)
            self.P.load(dst, s_, writes=[bf_], eng="gpsimd")

    def load_consts(self, st):
        P = self.P
        self.c32 = self.sb(st, [128, CB["E"][0]], F32, "c32")
        self.cb = Buf("c32")
        P.load(self.c32[:], self.I["consts"][:, 0:CB["E"][0]], writes=[self.cb])
        self.ident_bf = self.sb(st, [128, 128], BF16, "identbf")
        self.ones_bf = self.sb(st, [128, 128], BF16, "onesbf")
        self.cpen_bf = self.sb(st, [128, 128], BF16, "cpenbf")
        self.bpen_bf = self.sb(st, [128, 128], BF16, "bpenbf")
        self.cbf = Buf("cbf")
        for t, n in [(self.ident_bf, "ident"), (self.ones_bf, "ones"), (self.cpen_bf, "cpen"), (self.bpen_bf, "bpen")]:
            o, w = CB[n]
            P.dve(lambda e, t=t, o=o, w=w: e.tensor_copy(out=t[:], in_=self.c32[:, o:o + w]), reads=[self.cb], writes=[self.cbf])
        self.gcol = self.sb(st, [128, 7, 8], F32, "gcol")
        self.gb = Buf("gcol")
        with self.nc.allow_non_contiguous_dma("tiny gain vectors"):
            for j, src in [(0, self.I["ffn_norm"][0:1, :]), (1, self.I["ffn_norm"][1:2, :]), (2, self.I["ffn_norm"][2:3, :]),
                           (3, self.I["ffn_norm"][3:4, :]), (4, self.I["mix_norm"][0:1, :]), (5, self.I["mix_norm"][1:2, :]),
                           (6, self.I["final_norm"][0:1, :])]:
                P.load(self.gcol[:, j, :], src.rearrange("o (fc p) -> p (o fc)", p=128), writes=[self.gb], allow_slow_non_contiguous=True)

    def cst(self, n, rows=128, c0=0, c1=None):
        o, w = CB[n]
        if c1 is None:
            c1 = w
        return self.c32[0:rows, o + c0:o + c1]

    def transpose_in(self):
        P = self.P
        with ExitStack() as st:
            xt = [self.sb(st, [128, D], F32, "xt") for _ in range(4)]
            xb = [Buf("xt%d" % i) for i in range(4)]
            tp = [self.ps(st, [128, 512], F32, "tp") for _ in range(2)]
            tb = [Buf("tp0"), Buf("tp1")]
            stg = [self.sb(st, [128, 8, 512], F32, "stg") for _ in range(2)]
            sgb = [Buf("stg0"), Buf("stg1")]
            ident = self.cst("ident")
            k = 0
            for g in range(NG):
                for t in range(4):
                    r0 = g * TG + t * 128
                    P.load(xt[t][:], self.I["x"][r0:r0 + 128, :], writes=[xb[t]])
                so = stg[g % 2]
                for fc in range(8):
                    pb = tp[k % 2]
                    for t in range(4):
                        P.mm(lambda e, pb=pb, t=t, fc=fc: e.transpose(out=pb[:, t * 128:(t + 1) * 128], in_=xt[t][:, fc * 128:(fc + 1) * 128], identity=ident),
                             reads=[xb[t], self.cb], writes=[tb[k % 2]])
                    if fc % 2 == 0:
                        P.act(lambda e, pb=pb, fc=fc, so=so: e.copy(out=so[:, fc, :], in_=pb[:]), reads=[tb[k % 2]], writes=[sgb[g % 2]])
                    else:
                        P.dve(lambda e, pb=pb, fc=fc, so=so: e.tensor_copy(out=so[:, fc, :], in_=pb[:]), reads=[tb[k % 2]], writes=[sgb[g % 2]])
                    k += 1
                P.load(self.hT[0].rearrange("(fc p) t -> p fc t", p=128)[:, :, g * TG:(g + 1) * TG], so[:],
                       reads=[sgb[g % 2]], writes=[self.hbuf[0]], eng="gpsimd")
        P.barrier()

    def norm_slab(self, hs, hsb, nfc, gcols, ss_ps, ssb, sq, sqb, rstd, rstdb, uT, uTb, inv_n, psum_src=False):
        P = self
        Pg = self.P
        for fc in range(nfc):
            s_ = sq[fc % len(sq)]
            sb_ = sqb[fc % len(sq)]
            Pg.act(lambda e, s_=s_, fc=fc: e.activation(out=s_[:], in_=hs[:, fc, :], func=AF.Square), reads=[hsb], writes=[sb_])
            Pg.mm(lambda e, s_=s_, fc=fc: e.matmul(ss_ps[:], lhsT=self.ones_bf[:], rhs=s_[:], start=(fc == 0), stop=(fc == nfc - 1)),
                  reads=[sb_, self.cbf], writes=[ssb])
        Pg.act(lambda e: e.activation(out=rstd[:], in_=ss_ps[:], func=AF.Ln, scale=float(inv_n), bias=float(EPS)), reads=[ssb], writes=[rstdb])
        Pg.act(lambda e: e.activation(out=rstd[:], in_=rstd[:], func=AF.Exp, scale=-0.5), reads=[rstdb], writes=[rstdb])
        for fc in range(nfc):
            Pg.dve(lambda e, fc=fc: e.scalar_tensor_tensor(out=uT[:, fc, :], in0=hs[:, fc, :], scalar=gcols[fc], in1=rstd[:],
                                                           op0=ALU.mult, op1=ALU.mult), reads=[hsb, rstdb, self.gb], writes=[uTb])

    def ffn(self, f, src, dst):
        P = self.P
        hin, hinb = self.hT[src], self.hbuf[src]
        hout, houtb = self.hT[dst], self.hbuf[dst]
        hin_v = hin.rearrange("(fc p) t -> p fc t", p=128)
        hout_v = hout.rearrange("(fc p) t -> p fc t", p=128)
        with ExitStack() as st:
            hs = [self.sb(st, [128, 8, TG], F32, "hs") for _ in range(2)]
            hsb = [Buf("hs0"), Buf("hs1")]
            uT = [self.sb(st, [128, 8, TG], BF16, "uT") for _ in range(2)]
            uTb = [Buf("uT0"), Buf("uT1")]
            sq = [self.sb(st, [128, TG], BF16, "sq") for _ in range(2)]
            sqb = [Buf("sq0"), Buf("sq1")]
            rstd = self.sb(st, [128, TG], F32, "rstd")
            rstdb = Buf("rstd")
            actT = [self.sb(st, [128, NFC, TG], BF16, "actT") for _ in range(2)]
            actb = [Buf("act0"), Buf("act1")]
            NW = 6
            wgu = [self.sb(st, [128, 2, 8, 128], BF16, "wgu") for _ in range(NW)]
            wgub = [Buf("wgu%d" % i) for i in range(NW)]
            NWD = 3
            wdn = [self.sb(st, [128, NFC, 128], BF16, "wdn") for _ in range(NWD)]
            wdnb = [Buf("wdn%d" % i) for i in range(NWD)]
            sg = [self.sb(st, [128, TG], BF16, "sg") for _ in range(2)]
            sgb = [Buf("sg0"), Buf("sg1")]
            ho = [self.sb(st, [128, TG], F32, "ho") for _ in range(3)]
            hob = [Buf("ho%d" % i) for i in range(3)]
            ss_ps = self.ps(st, [128, TG], F32, "ss")
            ssb = Buf("ss")
            g_ps = [self.ps(st, [128, TG], F32, "gps") for _ in range(2)]
            gpb = [Buf("g0"), Buf("g1")]
            u_ps = [self.ps(st, [128, TG], F32, "ups") for _ in range(2)]
            upb = [Buf("u0"), Buf("u1")]
            o_ps = [self.ps(st, [128, TG], F32, "ops") for _ in range(2)]
            opb = [Buf("o0"), Buf("o1")]
            gcols = [self.gcol[:, f, fc:fc + 1] for fc in range(8)]
            cnt = {"w": 0, "d": 0, "c": 0, "o": 0}

            def stage_load(g):
                P.load(hs[g % 2][:], hin_v[:, :, g * TG:(g + 1) * TG], reads=[hinb], writes=[hsb[g % 2]])

            def stage_norm(g):
                self.norm_slab(hs[g % 2], hsb[g % 2], 8, gcols, ss_ps, ssb, sq, sqb, rstd, rstdb, uT[g % 2], uTb[g % 2], 1.0 / D)

            def stage_gu(g):
                for c in range(NFC):
                    w = cnt["w"] % NW
                    cnt["w"] += 1
                    P.load(wgu[w][:], self.wgu[f, c].rearrange("p (w k j) -> p w k j", w=2, k=8), reads=[self.wgu_b[f][(0, c)], self.wgu_b[f][(1, c)]], writes=[wgub[w]])
                    b = cnt["c"] % 2
                    cnt["c"] += 1
                    for which, (pt, pbf) in enumerate([(g_ps[b], gpb[b]), (u_ps[b], upb[b])]):
                        for kc in range(8):
                            P.mm(lambda e, pt=pt, w=w, which=which, kc=kc: e.matmul(pt[:], lhsT=wgu[w][:, which, kc, :], rhs=uT[g % 2][:, kc, :],
                                                                                    start=(kc == 0), stop=(kc == 7)),
                                 reads=[wgub[w], uTb[g % 2]], writes=[pbf])
                    P.act(lambda e, b=b: e.activation(out=sg[b][:], in_=g_ps[b][:], func=AF.Silu), reads=[gpb[b]], writes=[sgb[b]])
                    P.dve(lambda e, b=b, c=c: e.tensor_tensor(out=actT[g % 2][:, c, :], in0=sg[b][:], in1=u_ps[b][:], op=ALU.mult),
                          reads=[sgb[b], upb[b]], writes=[actb[g % 2]])

            def stage_down(g):
                for fc in range(8):
                    w = cnt["d"] % NWD
                    cnt["d"] += 1
                    P.load(wdn[w][:], self.wdn[f, fc].rearrange("p (c j) -> p c j", j=128), reads=self.wdn_b[f], writes=[wdnb[w]])
                    b = fc % 2
                    for c in range(NFC):
                        P.mm(lambda e, b=b, w=w, c=c: e.matmul(o_ps[b][:], lhsT=wdn[w][:, c, :], rhs=actT[g % 2][:, c, :], start=(c == 0), stop=(c == NFC - 1)),
                             reads=[wdnb[w], actb[g % 2]], writes=[opb[b]])
                    k = cnt["o"] % 3
                    cnt["o"] += 1
                    P.dve(lambda e, b=b, k=k, fc=fc: e.scalar_tensor_tensor(out=ho[k][:], in0=o_ps[b][:], scalar=0.5, in1=hs[g % 2][:, fc, :],
                                                                            op0=ALU.mult, op1=ALU.add), reads=[opb[b], hsb[g % 2]], writes=[hob[k]])
                    P.load(hout_v[:, fc, g * TG:(g + 1) * TG], ho[k][:], reads=[hob[k]], writes=[houtb], eng="gpsimd")
                    self.pump(5)

            stage_load(0)
            stage_norm(0)
            for g in range(NG):
                if g + 1 < NG:
                    stage_load(g + 1)
                stage_gu(g)
                if g + 1 < NG:
                    stage_norm(g + 1)
                stage_down(g)
        P.barrier()

    def final(self, src, do_norm=True):
        P = self.P
        hin_v = self.hT[src].rearrange("(fc p) t -> p fc t", p=128)
        hinb = self.hbuf[src]
        outb = Buf("out")
        with ExitStack() as st:
            hs = [self.sb(st, [128, 8, TG], F32, "hs") for _ in range(2)]
            hsb = [Buf("hs0"), Buf("hs1")]
            yT = [self.sb(st, [128, 8, TG], F32, "yT") for _ in range(2)]
            yTb = [Buf("y0"), Buf("y1")]
            sq = [self.sb(st, [128, TG], BF16, "sq") for _ in range(2)]
            sqb = [Buf("sq0"), Buf("sq1")]
            rstd = self.sb(st, [128, TG], F32, "rstd")
            rstdb = Buf("rstd")
            ss_ps = self.ps(st, [128, TG], F32, "ss")
            ssb = Buf("ss")
            tp = [self.ps(st, [128, 512], F32, "tp") for _ in range(4)]
            tb = [Buf("tp%d" % i) for i in range(4)]
            ot = [self.sb(st, [128, D], F32, "ot") for _ in range(3)]
            otb = [Buf("ot%d" % i) for i in range(3)]
            gcols = [self.gcol[:, 6, fc:fc + 1] for fc in range(8)]
            ident = self.cst("ident")
            k = 0
            n = 0
            for g in range(NG):
                P.load(hs[g % 2][:], hin_v[:, :, g * TG:(g + 1) * TG], reads=[hinb], writes=[hsb[g % 2]])
                if do_norm:
                    self.norm_slab(hs[g % 2], hsb[g % 2], 8, gcols, ss_ps, ssb, sq, sqb, rstd, rstdb, yT[g % 2], yTb[g % 2], 1.0 / D)
                    y, yb = yT[g % 2], yTb[g % 2]
                else:
                    y, yb = hs[g % 2], hsb[g % 2]
                for t in range(4):
                    o_ = ot[n % 3]
                    ob_ = otb[n % 3]
                    n += 1
                    for half in range(2):
                        pb = tp[k % 4]
                        pbb = tb[k % 4]
                        k += 1
                        for j in range(4):
                            fc = half * 4 + j
                            P.mm(lambda e, pb=pb, j=j, fc=fc, t=t, y=y: e.transpose(out=pb[:, j * 128:(j + 1) * 128], in_=y[:, fc, t * 128:(t + 1) * 128], identity=ident),
                                 reads=[yb, self.cb], writes=[pbb])
                        if half == 0:
                            P.act(lambda e, pb=pb, o_=o_: e.copy(out=o_[:, 0:512], in_=pb[:]), reads=[pbb], writes=[ob_])
                        else:
                            P.dve(lambda e, pb=pb, o_=o_: e.tensor_copy(out=o_[:, 512:1024], in_=pb[:]), reads=[pbb], writes=[ob_])
                    r0 = g * TG + t * 128
                    P.load(self.out[r0:r0 + 128, :], o_[:], reads=[ob_], writes=[outb], eng="gpsimd")


def build(nphase=99, final_norm=True, only=None):
    import os
    kb = KB()
    kb.declare()
    P = kb.P
    with ExitStack() as st:
        kb.load_consts(st)
        cur = 0
        if only is None:
            kb.prep_ffn(0)
            kb.pump(10 ** 6)
        kb.transpose_in()
        if only == "mix1":
            kb.declare_mix1()
            kb.inproj1(cur)
            kb.nsa_attn()
            kb.outproj(cur, 1 - cur, kb.I["nsa_w_out"], kb.ONT, kb.m1b["ONT"], 16)
            cur = 1 - cur
        elif only is None:
            if nphase >= 1:
                kb.prep_ffn(1)
                kb.ffn(0, cur, 1 - cur)
                cur = 1 - cur
            if nphase >= 2:
                kb.declare_mix0()
                kb.inproj0(cur)
                kb.mla()
                kb.gla()
                kb.outproj(cur, 1 - cur, kb.I["hy_w_out"], kb.OTm, kb.m0b["OTm"], 8, kb.OTg, kb.m0b["OTg"])
                cur = 1 - cur
            if nphase >= 3:
                kb.prep_ffn(2)
                kb.ffn(1, cur, 1 - cur)
                cur = 1 - cur
                kb.prep_ffn(3)
                kb.ffn(2, cur, 1 - cur)
                cur = 1 - cur
            if nphase >= 4:
                kb.declare_mix1()
                kb.inproj1(cur)
                kb.nsa_attn()
                kb.outproj(cur, 1 - cur, kb.I["nsa_w_out"], kb.ONT, kb.m1b["ONT"], 16)
                cur = 1 - cur
            if nphase >= 5:
                kb.ffn(3, cur, 1 - cur)
                cur = 1 - cur
        kb.pump(10 ** 6)
        kb.final(cur, do_norm=final_norm)
        P.finalize(st)
    return kb.nc


WNAMES = ["ffn_norm", "ffn_w_gate", "ffn_w_up", "ffn_w_down", "mix_norm", "hy_w_in", "mla_q_norm", "mla_w_uq", "mla_kv_norm",
          "mla_w_ukv", "gla_w_a2", "gla_b_a", "gla_out_norm", "hy_w_out", "nsa_w_in", "nsa_pos_k", "nsa_pos_v", "nsa_ck_w1",
          "nsa_ck_w2", "nsa_cv_w1", "nsa_cv_w2", "nsa_w_out", "final_norm"]


def make_in_maps(inputs, ncores=8):
    consts = make_consts()
    cm, ovl = make_cmp_consts()
    shared = {"consts": consts, "cmpmask": cm, "ovl": ovl}
    f32 = lambda a: np.ascontiguousarray(np.asarray(a), dtype=np.float32)
    shared["ffn_norm"] = f32(inputs["ffn_norm"]).reshape(4, D)
    shared["ffn_w_gate"] = f32(inputs["ffn_w_gate"]).reshape(4, D, DFF)
    shared["ffn_w_up"] = f32(inputs["ffn_w_up"]).reshape(4, D, DFF)
    shared["ffn_w_down"] = f32(inputs["ffn_w_down"]).reshape(4, DFF, D)
    shared["mix_norm"] = f32(inputs["mix_norm"])
    for n in ["hy_w_in", "mla_w_uq", "mla_w_ukv", "gla_w_a2", "hy_w_out", "nsa_w_in", "nsa_pos_k", "nsa_pos_v", "nsa_ck_w1",
              "nsa_ck_w2", "nsa_cv_w1", "nsa_cv_w2", "nsa_w_out"]:
        shared[n] = f32(inputs[n])[0]
    for n in ["mla_q_norm", "mla_kv_norm", "gla_b_a", "gla_out_norm"]:
        shared[n] = f32(inputs[n]).reshape(1, -1)
    shared["final_norm"] = f32(inputs["final_norm"]).reshape(1, D)
    x = f32(inputs["x"])
    pos = np.ascontiguousarray(np.asarray(inputs["positions"]), dtype=np.int32)
    maps = []
    for c in range(ncores):
        m = dict(shared)
        m["x"] = x[c]
        m["positions"] = pos[c:c + 1]
        maps.append(m)
    return maps


def kernel(**inputs):
    nc = build()
    maps = make_in_maps(inputs)
    res = run_bass_kernel_spmd(nc, maps, core_ids=list(range(8)))
    return np.stack([r["out"] for r in res.results], axis=0)


class Tl:
    def __init__(self, t, name):
        self.t = t
        self.b = Buf(name)

    def __getitem__(self, k):
        return self.t[k]


def _kb_tile(self, st, shape, dt, name="t"):
    return Tl(self.sb(st, shape, dt, name), name)


def _kb_ptile(self, st, shape=(128, 512), dt=F32, name="p"):
    return Tl(self.ps(st, list(shape), dt, name), name)


def _bl(xs):
    return [x.b if isinstance(x, Tl) else x for x in xs]


def _MM(self, out, lhsT, rhs, start, stop, r, w):
    return self.P.mm(lambda e: e.matmul(out, lhsT=lhsT, rhs=rhs, start=start, stop=stop), reads=_bl(r), writes=_bl(w))


def _PROJ(self, out, pairs, r, w):
    n = len(pairs)
    for i, (l, rh) in enumerate(pairs):
        self.MM(out, l, rh, i == 0, i == n - 1, r, w)


def _TR(self, out, in_, ident, r, w):
    return self.P.mm(lambda e: e.transpose(out=out, in_=in_, identity=ident), reads=_bl(r), writes=_bl(w))


def _ACT(self, out, in_, func, r, w, **kw):
    return self.P.act(lambda e: e.activation(out=out, in_=in_, func=func, **kw), reads=_bl(r), writes=_bl(w))


def _TT(self, out, a, b, op, r, w, eng="vector"):
    return self.P.op(eng, lambda e: e.tensor_tensor(out=out, in0=a, in1=b, op=op), reads=_bl(r), writes=_bl(w))


def _STT(self, out, in0, scalar, in1, op0, op1, r, w):
    return self.P.dve(lambda e: e.scalar_tensor_tensor(out=out, in0=in0, scalar=scalar, in1=in1, op0=op0, op1=op1), reads=_bl(r), writes=_bl(w))


def _TS(self, out, in0, s1, s2, op0, op1, r, w, eng="vector"):
    if s2 is None:
        return self.P.op(eng, lambda e: e.tensor_scalar(out=out, in0=in0, scalar1=s1, scalar2=None, op0=op0), reads=_bl(r), writes=_bl(w))
    return self.P.op(eng, lambda e: e.tensor_scalar(out=out, in0=in0, scalar1=s1, scalar2=s2, op0=op0, op1=op1), reads=_bl(r), writes=_bl(w))


def _CP(self, out, in_, r, w, eng="vector"):
    if eng == "scalar":
        return self.P.act(lambda e: e.copy(out=out, in_=in_), reads=_bl(r), writes=_bl(w))
    return self.P.op(eng, lambda e: e.tensor_copy(out=out, in_=in_), reads=_bl(r), writes=_bl(w))


def _LD(self, out, in_, r, w, eng="sync", **kw):
    return self.P.load(out, in_, reads=_bl(r), writes=_bl(w), eng=eng, **kw)


def _MS(self, ap, val, w, eng="gpsimd"):
    return self.P.op(eng, lambda e: e.memset(ap, val), reads=[], writes=_bl(w))


for _n, _f in [("tile", _kb_tile), ("ptile", _kb_ptile), ("MM", _MM), ("PROJ", _PROJ), ("TR", _TR), ("ACT", _ACT), ("TT", _TT),
               ("STT", _STT), ("TS", _TS), ("CP", _CP), ("LD", _LD), ("MS", _MS)]:
    setattr(KB, _n, _f)


def _norm_ps(self, srcs, gcols, inv_n, ss, sq, rstd, outs, out_b):
    n = len(srcs)
    for i, (ap, tl) in enumerate(srcs):
        q = sq[i % len(sq)]
        self.ACT(q[:], ap, AF.Square, [tl], [q])
        self.MM(ss[:], self.ones_bf[:], q[:], i == 0, i == n - 1, [q, self.cbf], [ss])
    self.ACT(rstd[:], ss[:], AF.Ln, [ss], [rstd], scale=float(inv_n), bias=float(EPS))
    self.ACT(rstd[:], rstd[:], AF.Exp, [rstd], [rstd], scale=-0.5)
    for i, (ap, tl) in enumerate(srcs):
        self.STT(outs[i], ap, gcols[i], rstd[:], ALU.mult, ALU.mult, [tl, rstd] + self.gdeps, [out_b])


KB.norm_ps = _norm_ps


def _rope_tables(self, st, rows, r0, fcol, scol, name, pos_ap=None, S=S):
    C = self.tile(st, [rows, S], F32, name + "C")
    Sg = self.tile(st, [rows, S], F32, name + "S")
    with ExitStack() as st2:
        pi_ = self.tile(st2, [rows, S], I32, "posi")
        t = self.tile(st2, [rows, S], F32, "rt")
        u = self.tile(st2, [rows, S], F32, "ru")
        ti = self.tile(st2, [rows, S], I32, "rti")
        rs = slice(r0, rows)
        fo = CB["freq"][0]
        if pos_ap is None:
            pos_ap = self.I["positions"]
        with self.nc.allow_non_contiguous_dma("positions"):
            self.LD(pi_[rs, :], pos_ap.partition_broadcast(rows - r0).rearrange("p o s -> p (o s)"), [], [pi_], allow_slow_non_contiguous=True)
        self.CP(t[rs, :], pi_[rs, :], [pi_], [t])
        self.TS(t[rs, :], t[rs, :], self.c32[rs, fo + fcol:fo + fcol + 1], None, ALU.mult, None, [t, self.cb], [t])
        for tab, shift, scale in [(Sg, 0.0, self.c32[rs, fo + scol:fo + scol + 1]), (C, 0.25, float(2 * np.pi * (1 - 1e-6)))]:
            if shift:
                self.TS(u[rs, :], t[rs, :], shift, None, ALU.add, None, [t], [u])
            else:
                self.CP(u[rs, :], t[rs, :], [t], [u])
            self.CP(ti[rs, :], u[rs, :], [u], [ti])
            self.CP(tab[rs, :], ti[rs, :], [ti], [tab])
            self.TT(u[rs, :], u[rs, :], tab[rs, :], ALU.subtract, [u, tab], [u])
            self.ACT(tab[rs, :], u[rs, :], AF.Sin, [u, self.cb], [tab], scale=scale)
        self.P.barrier()
    return C, Sg


KB.rope_tables = _rope_tables


def _declare_mix0(self):
    d = self.dram
    self.QT = d("QT", [8, 96, S], BF16)
    self.KT = d("KT", [8, 96, S], BF16)
    self.Vs = d("Vs", [S, 520], BF16)
    self.qintra = d("qintra", [256, S], BF16)
    self.qinter = d("qinter", [256, S], BF16)
    self.kdec = d("kdec", [256, S], BF16)
    self.kdtok = d("kdtok", [S, 256], BF16)
    self.gv = d("gv", [S, 512], BF16)
    self.grs = d("grs", [512, S], BF16)
    self.decd = d("decd", [256, 64], F32)
    self.OTm = d("OTm", [8, 64, S], BF16)
    self.OTg = d("OTg", [4, 128, S], BF16)
    self.m0b = {n: Buf(n) for n in ["QT", "KT", "Vs", "qintra", "qinter", "kdec", "kdtok", "gv", "grs", "decd", "OTm", "OTg"]}


KB.declare_mix0 = _declare_mix0


def _inproj0(self, src):
    P, I = self.P, self.I
    hin_v = self.hT[src].rearrange("(fc p) t -> p fc t", p=128)
    hinb = self.hbuf[src]
    mb = self.m0b
    with ExitStack() as st:
        T = lambda shape, dt, n: self.tile(st, shape, dt, n)
        w_in = T([128, 8, HY_IN], BF16, "w_in")
        for kc in range(8):
            self.LD(w_in[:, kc, :], I["hy_w_in"][kc * 128:(kc + 1) * 128, :], [], [w_in], eng="gpsimd")
        w_uq = T([128, 2, 768], BF16, "w_uq")
        w_uqs = T([128, 2, 768], BF16, "w_uqs")
        uqsrc = I["mla_w_uq"].rearrange("(k p) n -> p k n", p=128)
        self.LD(w_uq[:], uqsrc, [], [w_uq], eng="gpsimd")
        self.LD(w_uqs[:], uqsrc, [], [w_uqs], eng="gpsimd")
        v4 = lambda ap: ap.rearrange("p k (h d) -> p k h d", d=96)
        with self.nc.allow_non_contiguous_dma("small swapped weight blocks"):
            for kc in range(2):
                self.LD(v4(w_uqs[:])[:, kc, :, 64:80], v4(uqsrc)[:, kc, :, 80:96], [], [w_uqs], eng="gpsimd")
                self.LD(v4(w_uqs[:])[:, kc, :, 80:96], v4(uqsrc)[:, kc, :, 64:80], [], [w_uqs], eng="gpsimd")
        w_ukv = T([128, 1024], BF16, "w_ukv")
        self.LD(w_ukv[:], I["mla_w_ukv"], [], [w_ukv], eng="gpsimd")
        wkrs = T([128, 8, 96], BF16, "wkrs")
        insrc = I["hy_w_in"].rearrange("(k p) n -> p k n", p=128)
        with self.nc.allow_non_contiguous_dma("small swapped weight blocks"):
            self.LD(wkrs[:, :, 0:64], insrc[:, :, 320:384], [], [wkrs], eng="gpsimd")
            self.LD(wkrs[:, :, 64:80], insrc[:, :, 400:416], [], [wkrs], eng="gpsimd")
            self.LD(wkrs[:, :, 80:96], insrc[:, :, 384:400], [], [wkrs], eng="gpsimd")
        w_a2 = T([16, 256], BF16, "w_a2")
        self.LD(w_a2[:], I["gla_w_a2"], [], [w_a2], eng="gpsimd")
        cols = T([128, 8], F32, "cols")
        with self.nc.allow_non_contiguous_dma("tiny vectors"):
            self.LD(cols[:, 0:2], I["mla_q_norm"].rearrange("o (k p) -> p (o k)", p=128), [], [cols], allow_slow_non_contiguous=True)
            self.LD(cols[:, 2:3], I["mla_kv_norm"].rearrange("o (k p) -> p (o k)", p=128), [], [cols], allow_slow_non_contiguous=True)
            self.LD(cols[:, 3:5], I["gla_b_a"].rearrange("o (k p) -> p (o k)", p=128), [], [cols], allow_slow_non_contiguous=True)
        self.TS(cols[:, 5:7], cols[:, 3:5], -1.0, None, ALU.mult, None, [cols], [cols])
        C, Sg = self.rope_tables(st, 96, 64, 0, 1, "mla")
        hs = [T([128, 8, TG], F32, "hs") for _ in range(2)]
        uT = T([128, 8, TG], BF16, "uT")
        sq = [T([128, TG], BF16, "sq") for _ in range(2)]
        rstd = T([128, TG], F32, "rstd")
        cqn = T([128, 2, TG], BF16, "cqn")
        ckvn = T([128, TG], BF16, "ckvn")
        qst = [T([96, TG], BF16, "qst") for _ in range(2)]
        t1 = [T([96, TG], F32, "t1") for _ in range(2)]
        t2 = [T([96, TG], F32, "t2") for _ in range(2)]
        kst = T([96, 8, TG], BF16, "kst")
        krot = T([96, TG], F32, "krot")
        vst = T([128, 4, 8, 65], BF16, "vst")
        self.MS(vst[:], 1.0, [vst])
        ga = T([16, TG], BF16, "ga")
        lt = T([128, TG], F32, "lt")
        cs = T([128, TG], F32, "cs")
        dd = T([128, TG], F32, "dd")
        E1 = T([128, TG], F32, "E1")
        E2 = T([128, TG], F32, "E2")
        E3 = T([128, TG], F32, "E3")
        qia = T([128, 2, TG], BF16, "qia")
        qie = T([128, 2, TG], BF16, "qie")
        kde = T([128, 2, TG], BF16, "kde")
        dec = T([128, 2, 64], F32, "dec")
        kdt = T([128, 4, 256], BF16, "kdt")
        gvs = T([128, 4, 512], BF16, "gvs")
        grt = T([128, 4, TG], BF16, "grt")
        ss = self.ptile(st, name="ss")
        A = [self.ptile(st, name="A") for _ in range(2)]
        Bp = [self.ptile(st, name="B") for _ in range(2)]
        Tp = [self.ptile(st, name="T") for _ in range(2)]
        Tb = self.ptile(st, [128, 1024], BF16, name="Tb")
        self.gdeps = [self.gb, cols.b]
        gm = [self.gcol[:, 4, fc:fc + 1] for fc in range(8)]
        scanmask = self.cst("scanmask")
        for g in range(NG):
            gs = slice(g * TG, (g + 1) * TG)
            h_ = hs[g % 2]
            self.LD(h_[:], hin_v[:, :, gs], [hinb], [h_])
            self.norm_ps([(h_[:, fc, :], h_) for fc in range(8)], gm, 1.0 / D, ss, sq, rstd, [uT[:, fc, :] for fc in range(8)], uT)
            for ch in range(2):
                self.PROJ(A[ch][:], [(w_in[:, kc, ch * 128:(ch + 1) * 128], uT[:, kc, :]) for kc in range(8)], [w_in, uT], [A[ch]])
            self.norm_ps([(A[ch][:], A[ch]) for ch in range(2)], [cols[:, ch:ch + 1] for ch in range(2)], 1.0 / 256, ss, sq, rstd,
                         [cqn[:, ch, :] for ch in range(2)], cqn)
            for h in range(8):
                a, b = A[h % 2], Bp[h % 2]
                q_, x1, x2 = qst[h % 2], t1[h % 2], t2[h % 2]
                self.PROJ(a[0:96, :], [(w_uq[:, kc, h * 96:(h + 1) * 96], cqn[:, kc, :]) for kc in range(2)], [w_uq, cqn], [a])
                self.PROJ(b[0:96, :], [(w_uqs[:, kc, h * 96:(h + 1) * 96], cqn[:, kc, :]) for kc in range(2)], [w_uqs, cqn], [b])
                self.CP(q_[0:64, :], a[0:64, :], [a], [q_], eng="scalar")
                self.TT(x1[64:96, :], a[64:96, :], C[64:96, gs], ALU.mult, [a, C], [x1])
                self.TT(x2[64:96, :], b[64:96, :], Sg[64:96, gs], ALU.mult, [b, Sg], [x2])
                self.TT(q_[64:96, :], x1[64:96, :], x2[64:96, :], ALU.add, [x1, x2], [q_], eng="gpsimd")
                self.LD(self.QT[h, :, gs], q_[:], [q_], [mb["QT"]], eng="gpsimd")
            self.PROJ(A[0][:], [(w_in[:, kc, 256:384], uT[:, kc, :]) for kc in range(8)], [w_in, uT], [A[0]])
            self.norm_ps([(A[0][:], A[0])], [cols[:, 2:3]], 1.0 / 128, ss, sq, rstd, [ckvn[:]], ckvn)
            for h in range(8):
                a = A[h % 2]
                self.MM(a[0:64, :], w_ukv[:, h * 128:h * 128 + 64], ckvn[:], True, True, [w_ukv, ckvn], [a])
                self.CP(kst[0:64, h, :], a[0:64, :], [a], [kst], eng=("scalar" if h % 2 else "vector"))
            self.PROJ(A[0][0:96, :], [(w_in[:, kc, 320:416], uT[:, kc, :]) for kc in range(8)], [w_in, uT], [A[0]])
            self.PROJ(Bp[0][0:96, :], [(wkrs[:, kc, :], uT[:, kc, :]) for kc in range(8)], [wkrs, uT], [Bp[0]])
            self.TT(t1[0][64:96, :], A[0][64:96, :], C[64:96, gs], ALU.mult, [A[0], C], [t1[0]])
            self.TT(t2[0][64:96, :], Bp[0][64:96, :], Sg[64:96, gs], ALU.mult, [Bp[0], Sg], [t2[0]])
            self.TT(krot[64:96, :], t1[0][64:96, :], t2[0][64:96, :], ALU.add, [t1[0], t2[0]], [krot], eng="gpsimd")
            self.CP(kst[64:96, :, :], krot[64:96, :].unsqueeze(1).broadcast_to([32, 8, TG]), [krot], [kst], eng="gpsimd")
            self.LD(self.KT[:, :, gs].rearrange("h r t -> r h t"), kst[:], [kst], [mb["KT"]], eng="gpsimd")
            wv = w_ukv[:].rearrange("p (h t d) -> p h t d", t=2, d=64)[:, :, 1, :]
            for t in range(4):
                tp = Tp[t % 2]
                self.MM(tp[:].rearrange("p (h d) -> p h d", d=64), ckvn[:, t * 128:(t + 1) * 128], wv, True, True, [ckvn, w_ukv], [tp])
                self.CP(vst[:, t, :, 0:64], tp[:].rearrange("p (h d) -> p h d", d=64), [tp], [vst], eng=("scalar" if t % 2 else "vector"))
            self.LD(self.Vs[gs, :].rearrange("(t p) f -> p t f", p=128), vst[:].rearrange("p t h d -> p t (h d)"), [vst], [mb["Vs"]], eng="gpsimd")
            for ch in range(2):
                self.PROJ(A[ch][:], [(w_in[:, kc, 416 + ch * 128:416 + (ch + 1) * 128], uT[:, kc, :]) for kc in range(8)], [w_in, uT], [A[ch]])
                self.PROJ(Bp[ch][:], [(w_in[:, kc, 672 + ch * 128:672 + (ch + 1) * 128], uT[:, kc, :]) for kc in range(8)], [w_in, uT], [Bp[ch]])
            self.PROJ(Tp[0][0:16, :], [(w_in[:, kc, 1440:1456], uT[:, kc, :]) for kc in range(8)], [w_in, uT], [Tp[0]])
            self.CP(ga[:], Tp[0][0:16, :], [Tp[0]], [ga])
            for ch in range(2):
                tp = Tp[1]
                self.MM(tp[:], w_a2[0:16, ch * 128:(ch + 1) * 128], ga[0:16, :], True, True, [w_a2, ga], [tp])
                self.ACT(lt[:], tp[:], AF.Exp, [tp, cols], [lt], scale=-1.0, bias=cols[:, 5 + ch:6 + ch])
                self.ACT(lt[:], lt[:], AF.Ln, [lt], [lt], scale=1.0, bias=1.0)
                self.P.dve(lambda e: e.tensor_tensor_scan(out=cs[:], data0=scanmask, data1=lt[:], initial=0.0, op0=ALU.mult, op1=ALU.add),
                           reads=[lt.b, self.cb], writes=[cs.b])
                cs3 = cs[:].rearrange("p (c k) -> p c k", k=64)
                self.TT(dd[:].rearrange("p (c k) -> p c k", k=64), cs3, cs3[:, :, 63:64].broadcast_to([128, 8, 64]), ALU.subtract, [cs], [dd])
                self.ACT(E1[:], dd[:], AF.Exp, [dd], [E1], scale=-1.0 / 16)
                self.ACT(E2[:], dd[:], AF.Exp, [dd], [E2], scale=1.0 / 16)
                self.ACT(E3[:], cs[:], AF.Exp, [cs], [E3], scale=-1.0 / 16)
                self.STT(qia[:, ch, :], A[ch][:], 0.125, E1[:], ALU.mult, ALU.mult, [A[ch], E1], [qia])
                self.STT(qie[:, ch, :], A[ch][:], 0.125, E3[:], ALU.mult, ALU.mult, [A[ch], E3], [qie])
                self.TT(kde[:, ch, :], Bp[ch][:], E2[:], ALU.mult, [Bp[ch], E2], [kde])
                self.CP(dec[:, ch, g * 8:(g + 1) * 8], E3[:].rearrange("p (c k) -> p c k", k=64)[:, :, 63], [E3], [dec], eng="gpsimd")
            fm = lambda dr: dr.rearrange("(c p) t -> p c t", p=128)[:, :, gs]
            self.LD(fm(self.qintra), qia[:], [qia], [mb["qintra"]], eng="gpsimd")
            self.LD(fm(self.qinter), qie[:], [qie], [mb["qinter"]], eng="gpsimd")
            self.LD(fm(self.kdec), kde[:], [kde], [mb["kdec"]], eng="gpsimd")
            for t in range(4):
                for ch in range(2):
                    self.TR(Tb[:, (t % 4) * 256 + ch * 128:(t % 4) * 256 + (ch + 1) * 128], kde[:, ch, t * 128:(t + 1) * 128], self.ident_bf[:],
                            [kde, self.cbf], [Tb])
            self.CP(kdt[:].rearrange("p t f -> p (t f)"), Tb[:], [Tb], [kdt])
            self.LD(self.kdtok[gs, :].rearrange("(t p) f -> p t f", p=128), kdt[:], [kdt], [mb["kdtok"]], eng="gpsimd")
            for t in range(4):
                tp = Tp[t % 2]
                self.PROJ(tp[:], [(uT[:, kc, t * 128:(t + 1) * 128], w_in[:, kc, 928:1440]) for kc in range(8)], [w_in, uT], [tp])
                self.CP(gvs[:, t, :], tp[:], [tp], [gvs], eng=("scalar" if t % 2 else "vector"))
            self.LD(self.gv[gs, :].rearrange("(t p) f -> p t f", p=128), gvs[:], [gvs], [mb["gv"]], eng="gpsimd")
            for hh in range(4):
                a = A[hh % 2]
                self.PROJ(a[:], [(w_in[:, kc, 1456 + hh * 128:1456 + (hh + 1) * 128], uT[:, kc, :]) for kc in range(8)], [w_in, uT], [a])
                self.ACT(grt[:, hh, :], a[:], AF.Silu, [a], [grt])
            self.LD(self.grs.rearrange("(c p) t -> p c t", p=128)[:, :, gs], grt[:], [grt], [mb["grs"]], eng="gpsimd")
        self.LD(self.decd.rearrange("(c p) n -> p c n", p=128), dec[:], [dec], [mb["decd"]], eng="gpsimd")
    self.gdeps = []
    P.barrier()


KB.inproj0 = _inproj0


class U:
    __slots__ = ("A", "B", "C", "later")

    def __init__(self, A=None, B=None, C=None, later=None):
        self.A, self.B, self.C, self.later = A, B, C, later


def run_units(units, look):
    n = len(units)
    sched = {}
    for i in range(min(look, n)):
        if units[i].A:
            units[i].A()
    for i in range(n):
        if i + look < n and units[i + look].A:
            units[i + look].A()
        for fn in sched.pop(i, []):
            fn()
        if units[i].B:
            units[i].B()
        if units[i].C:
            units[i].C()
        for (dl, fn) in (units[i].later or []):
            sched.setdefault(i + dl, []).append(fn)
    for k in sorted(sched):
        for fn in sched[k]:
            fn()


def _attn_units(self, units, o, sp_list, pt_list, cnt, Kt, Qt, Vfn, qg, scale, ktiles, dk, pre=None):
    nk = len(ktiles)
    for i, kt in enumerate(ktiles):
        d = kt - 4 * qg
        sp = sp_list[cnt[0] % len(sp_list)]
        pt = pt_list[cnt[0] % len(pt_list)]
        cnt[0] += 1
        kc = slice(kt * 128, (kt + 1) * 128)
        q0 = qg * TG
        c0 = max(d, 0) * 128

        def A(sp=sp, kc=kc, d=d, c0=c0, q0=q0, pre=(pre if i == 0 else None)):
            if pre is not None:
                pre()
            if d < 0:
                self.MM(sp[:], Kt[0:dk, kc], Qt[0:dk, q0:q0 + TG], True, True, [Kt, Qt], [sp])
            else:
                self.MM(sp[:, c0:c0 + 128], Kt[0:dk, kc], Qt[0:dk, q0 + c0:q0 + c0 + 128], True, False, [Kt, Qt], [sp])
                self.MM(sp[:, c0:c0 + 128], self.ident_bf[:], self.cpen_bf[:], False, True, [self.cbf], [sp])
                if c0 + 128 < TG:
                    self.MM(sp[:, c0 + 128:TG], Kt[0:dk, kc], Qt[0:dk, q0 + c0 + 128:q0 + TG], True, True, [Kt, Qt], [sp])

        def B(sp=sp, pt=pt, c0=c0):
            self.ACT(pt[:, c0:TG], sp[:, c0:TG], AF.Exp, [sp], [pt], scale=scale)

        def C(pt=pt, c0=c0, kt=kt, i=i):
            self.MM(o[0:65, c0:TG], Vfn(kt), pt[:, c0:TG], i == 0, i == nk - 1, [pt, self.vdep], [o])
        units.append(U(A, B, C))


KB.attn_units = _attn_units


def _mla(self):
    P = self.P
    mb = self.m0b
    with ExitStack() as st:
        T = lambda shape, dt, n: self.tile(st, shape, dt, n)
        Vall = T([128, 32, 520], BF16, "Vall")
        for q4 in range(4):
            self.LD(Vall[:, q4 * 8:(q4 + 1) * 8, :], self.Vs[q4 * 1024:(q4 + 1) * 1024, :].rearrange("(n p) f -> p n f", p=128), [mb["Vs"]], [Vall])
        self.vdep = Vall
        KTh = [T([96, S], BF16, "KTh") for _ in range(2)]
        QTh = [T([96, S], BF16, "QTh") for _ in range(2)]
        PT = [T([128, TG], BF16, "PT") for _ in range(3)]
        rr2 = [T([65, TG], F32, "rr") for _ in range(2)]
        fb2 = [T([65, TG], BF16, "fb") for _ in range(2)]
        bcs2 = [T([64, TG], F32, "bcs") for _ in range(2)]
        ost = [T([64, TG], BF16, "ost") for _ in range(2)]
        Sp = [self.ptile(st, name="S") for _ in range(3)]
        Op = [self.ptile(st, name="O") for _ in range(3)]
        bc2 = [self.ptile(st, name="bc") for _ in range(2)]
        ones32 = self.cst("ones")
        cnt = [0]
        k = 0
        units = []

        def loader(h):
            def f():
                self.LD(KTh[h % 2][:], self.KT[h], [mb["KT"]], [KTh[h % 2]])
                self.LD(QTh[h % 2][:], self.QT[h], [mb["QT"]], [QTh[h % 2]])
            return f
        loader(0)()
        for h in range(8):
            kt_, qt_ = KTh[h % 2], QTh[h % 2]
            for qg in range(NG):
                o = Op[k % 3]
                pre = loader(h + 1) if (qg == 0 and h + 1 < 8) else None
                self.attn_units(units, o, Sp, PT, cnt, kt_, qt_, lambda kt, h=h: Vall[:, kt, h * 65:(h + 1) * 65], qg, 96 ** -0.5,
                                list(range(4 * qg + 4)), 96, pre=pre)

                rr_, fb_, bc_, bs_ = rr2[k % 2], fb2[k % 2], bc2[k % 2], bcs2[k % 2]

                def f0(o=o, rr_=rr_, fb_=fb_):
                    self.ACT(rr_[64:65, :], o[64:65, :], AF.Ln, [o], [rr_])
                    self.ACT(fb_[64:65, :], rr_[64:65, :], AF.Exp, [rr_], [fb_], scale=-1.0)

                def f1(fb_=fb_, bc_=bc_, bs_=bs_):
                    self.MM(bc_[0:64, :], self.ones_bf[64:65, 0:64], fb_[64:65, :], True, True, [fb_, self.cbf], [bc_])
                    self.CP(bs_[:], bc_[0:64, :], [bc_], [bs_], eng="scalar")

                def f2(o=o, h=h, qg=qg, os_=ost[k % 2], bs_=bs_):
                    self.TT(os_[:], o[0:64, :], bs_[:], ALU.mult, [o, bs_], [os_])
                    self.LD(self.OTm[h, :, qg * TG:(qg + 1) * TG], os_[:], [os_], [mb["OTm"]], eng="gpsimd")
                units.append(U(None, None, f0, later=[(2, f1), (3, f2)]))
                k += 1
        run_units(units, 2)
    P.barrier()


KB.mla = _mla


def _gla(self):
    P = self.P
    mb = self.m0b
    with ExitStack() as st:
        T = lambda shape, dt, n: self.tile(st, shape, dt, n)
        hv = lambda dr, gs: dr.rearrange("(h d) t -> d h t", d=64)[:, :, gs]
        qia = [T([64, 4, TG], BF16, "qia") for _ in range(2)]
        qie = [T([64, 4, TG], BF16, "qie") for _ in range(2)]
        kde = [T([64, 4, TG], BF16, "kde") for _ in range(2)]
        vv = [T([64, 8, 512], BF16, "vv") for _ in range(2)]
        kdt = [T([64, 8, 256], BF16, "kdt") for _ in range(2)]
        grs = [T([128, 4, TG], BF16, "grs") for _ in range(2)]
        dec = T([64, 4, 64], F32, "dec")
        self.LD(dec[:], self.decd.rearrange("(h d) n -> d h n", d=64), [mb["decd"]], [dec])
        onc = T([128, 1], F32, "onc")
        with self.nc.allow_non_contiguous_dma("tiny"):
            self.LD(onc[:], self.I["gla_out_norm"].rearrange("o p -> p o"), [], [onc], allow_slow_non_contiguous=True)
        St = T([64, 4, 128], F32, "St")
        Sbf = T([64, 4, 128], BF16, "Sbf")
        self.MS(St[:], 0.0, [St])
        self.MS(Sbf[:], 0.0, [Sbf])
        ats = [T([64, 256], BF16, "ats") for _ in range(2)]
        sq = [T([128, TG], BF16, "sq") for _ in range(2)]
        rstd = T([128, TG], F32, "rstd")
        on = [T([128, TG], F32, "on") for _ in range(2)]
        ost = [T([128, TG], BF16, "ost") for _ in range(2)]
        Op = [self.ptile(st, name="O") for _ in range(4)]
        at_t = self.ps(st, [64, 512], F32, "at")
        at = [Tl(at_t, "at0"), Tl(at_t, "at1")]
        kv = [self.ptile(st, [64, 512], F32, name="kv") for _ in range(2)]
        ss = self.ptile(st, name="ss")
        tri = self.cst("tri", rows=64)
        self.gdeps = [onc.b]
        for g in range(NG):
            gs = slice(g * TG, (g + 1) * TG)
            b = g % 2
            self.LD(qia[b][:], hv(self.qintra, gs), [mb["qintra"]], [qia[b]])
            self.LD(qie[b][:], hv(self.qinter, gs), [mb["qinter"]], [qie[b]])
            self.LD(kde[b][:], hv(self.kdec, gs), [mb["kdec"]], [kde[b]])
            self.LD(vv[b][:], self.gv[gs, :].rearrange("(c p) f -> p c f", p=64), [mb["gv"]], [vv[b]])
            self.LD(kdt[b][:], self.kdtok[gs, :].rearrange("(c p) f -> p c f", p=64), [mb["kdtok"]], [kdt[b]])
            self.LD(grs[b][:], self.grs.rearrange("(c p) t -> p c t", p=128)[:, :, gs], [mb["grs"]], [grs[b]])
            for c in range(8):
                n = g * 8 + c
                cs_ = slice(c * 64, (c + 1) * 64)
                a_ = at[c % 2]
                ao = (c % 2) * 256
                for h in range(4):
                    self.MM(a_[0:64, ao + h * 64:ao + (h + 1) * 64], kde[b][:, h, cs_], qia[b][:, h, cs_], True, True, [kde[b], qia[b]], [a_])
                as_ = ats[c % 2]
                self.TT(as_[:].rearrange("p (h i) -> p h i", i=64), a_[0:64, ao:ao + 256].rearrange("p (h i) -> p h i", i=64),
                        tri.unsqueeze(1).broadcast_to([64, 4, 64]), ALU.mult, [a_, self.cb], [as_])
                for h in range(4):
                    self.MM(Op[h][:, cs_], vv[b][:, c, h * 128:(h + 1) * 128], as_[:, h * 64:(h + 1) * 64], True, False, [vv[b], as_], [Op[h]])
                    self.MM(Op[h][:, cs_], Sbf[:, h, :], qie[b][:, h, cs_], False, True, [Sbf, qie[b]], [Op[h]])
                kv_ = kv[c % 2]
                for h in range(4):
                    self.MM(kv_[0:64, h * 128:(h + 1) * 128], kdt[b][:, c, h * 64:(h + 1) * 64], vv[b][:, c, h * 128:(h + 1) * 128], True, True,
                            [kdt[b], vv[b]], [kv_])
                self.TT(St[:], St[:], dec[:, :, n:n + 1].broadcast_to([64, 4, 128]), ALU.mult, [St, dec], [St])
                self.TT(St[:].rearrange("p h v -> p (h v)"), St[:].rearrange("p h v -> p (h v)"), kv_[0:64, :], ALU.add, [St, kv_], [St])
                self.CP(Sbf[:], St[:], [St], [Sbf], eng="scalar")
            for h in range(4):
                o = Op[h]
                self.norm_ps([(o[:], o)], [onc[:, 0:1]], 1.0 / 128, ss, sq, rstd, [on[h % 2][:]], on[h % 2])
                os_ = ost[h % 2]
                self.TT(os_[:], on[h % 2][:], grs[b][:, h, :], ALU.mult, [on[h % 2], grs[b]], [os_], eng="gpsimd")
                self.LD(self.OTg[h, :, gs], os_[:], [os_], [mb["OTg"]], eng="gpsimd")
    self.gdeps = []
    P.barrier()


KB.gla = _gla


def _outproj(self, src, dst, w_src, otm_d, otm_b, n_h64, otg_d=None, otg_b=None):
    P = self.P
    hin_v = self.hT[src].rearrange("(fc p) t -> p fc t", p=128)
    hout_v = self.hT[dst].rearrange("(fc p) t -> p fc t", p=128)
    with ExitStack() as st:
        T = lambda shape, dt, n: self.tile(st, shape, dt, n)
        wm = T([64, n_h64, D], BF16, "wm")
        half = n_h64 // 2
        for i in range(2):
            self.LD(wm[:, i * half:(i + 1) * half, :], w_src[i * half * 64:(i + 1) * half * 64, :].rearrange("(h r) n -> r h n", r=64), [], [wm], eng="gpsimd")
        ng = 0
        if otg_d is not None:
            ng = 4
            wg = T([128, 4, D], BF16, "wg")
            self.LD(wg[:], w_src[n_h64 * 64:, :].rearrange("(c p) n -> p c n", p=128), [], [wg], eng="gpsimd")
        hs = [T([128, 8, TG], F32, "hs") for _ in range(2)]
        om = [T([64, n_h64, TG], BF16, "om") for _ in range(2)]
        og = [T([128, 4, TG], BF16, "og") for _ in range(2)] if ng else None
        ho = [T([128, TG], F32, "ho") for _ in range(3)]
        Op = [self.ptile(st, name="O") for _ in range(2)]
        k = 0
        for g in range(NG):
            gs = slice(g * TG, (g + 1) * TG)
            b = g % 2
            self.LD(hs[b][:], hin_v[:, :, gs], [self.hbuf[src]], [hs[b]])
            self.LD(om[b][:], otm_d[:, :, gs].rearrange("h r t -> r h t"), [otm_b], [om[b]])
            if ng:
                self.LD(og[b][:], otg_d[:, :, gs].rearrange("h r t -> r h t"), [otg_b], [og[b]])
            for fc in range(8):
                o = Op[fc % 2]
                fcs = slice(fc * 128, (fc + 1) * 128)
                pairs = [(wm[:, h, fcs], om[b][:, h, :]) for h in range(n_h64)]
                deps = [wm, om[b]]
                if ng:
                    pairs += [(wg[:, c, fcs], og[b][:, c, :]) for c in range(4)]
                    deps += [wg, og[b]]
                self.PROJ(o[:], pairs, deps, [o])
                h_ = ho[k % 3]
                k += 1
                self.TT(h_[:], o[:], hs[b][:, fc, :], ALU.add, [o, hs[b]], [h_])
                self.LD(hout_v[:, fc, gs], h_[:], [h_], [self.hbuf[dst]], eng="gpsimd")
                self.pump(3)
    P.barrier()


KB.outproj = _outproj


def _declare_mix1(self):
    d = self.dram
    self.QN = d("QN", [1024, S], BF16)
    self.KSd = d("KSd", [256, S], BF16)
    self.KWd = d("KWd", [256, S], BF16)
    self.KCd = d("KCd", [256, S], BF16)
    self.VCd = d("VCd", [256, S], BF16)
    self.VSW = d("VSW", [S, 520], BF16)
    self.GT = d("GT", [48, S], F32)
    self.ONT = d("ONT", [16, 64, S], BF16)
    self.m1b = {n: Buf(n) for n in ["QN", "KSd", "KWd", "KCd", "VCd", "VSW", "GT", "ONT"]}


KB.declare_mix1 = _declare_mix1


def _inproj1(self, src):
    P, I = self.P, self.I
    hin_v = self.hT[src].rearrange("(fc p) t -> p fc t", p=128)
    hinb = self.hbuf[src]
    mb = self.m1b
    with ExitStack() as st:
        T = lambda shape, dt, n: self.tile(st, shape, dt, n)
        w_in = T([128, 8, NSA_IN], BF16, "w_in")
        w_sw = T([128, 8, 1536], BF16, "w_sw")
        insrc = I["nsa_w_in"].rearrange("(k p) n -> p k n", p=128)
        with self.nc.allow_non_contiguous_dma("swapped rope halves"):
            for kc in range(8):
                self.LD(w_in[:, kc, :], I["nsa_w_in"][kc * 128:(kc + 1) * 128, :], [], [w_in], eng="gpsimd")
                for (d0, s0, nb) in [(0, 0, 16), (1024, 1536, 4), (1280, 2048, 4)]:
                    dv = w_sw[:, kc, d0:d0 + nb * 64].rearrange("p (b t e) -> p b t e", t=2, e=32)
                    sv = insrc[:, kc, s0:s0 + nb * 64].rearrange("p (b t e) -> p b t e", t=2, e=32)
                    self.LD(dv[:, :, 0, :], sv[:, :, 1, :], [], [w_sw], eng="gpsimd")
                    self.LD(dv[:, :, 1, :], sv[:, :, 0, :], [], [w_sw], eng="gpsimd")
        C, Sg = self.rope_tables(st, 128, 0, 2, 3, "nsa")
        self.nsaC, self.nsaS = C, Sg
        hs = [T([128, 8, TG], F32, "hs") for _ in range(2)]
        uT = T([128, 8, TG], BF16, "uT")
        sq = [T([128, TG], BF16, "sq") for _ in range(2)]
        rstd = T([128, TG], F32, "rstd")
        t1 = [T([128, TG], F32, "t1") for _ in range(2)]
        t2 = [T([128, TG], F32, "t2") for _ in range(2)]
        qst = [T([128, TG], BF16, "qst") for _ in range(3)]
        vst = T([128, 4, 8, 65], BF16, "vst")
        self.MS(vst[:], 1.0, [vst])
        gts = T([48, TG], F32, "gts")
        ss = self.ptile(st, name="ss")
        A = [self.ptile(st, name="A") for _ in range(2)]
        Bp = [self.ptile(st, name="B") for _ in range(2)]
        Tp = [self.ptile(st, name="T") for _ in range(2)]
        self.gdeps = [self.gb]
        gm = [self.gcol[:, 5, fc:fc + 1] for fc in range(8)]
        k = 0
        for g in range(NG):
            gs = slice(g * TG, (g + 1) * TG)
            h_ = hs[g % 2]
            self.LD(h_[:], hin_v[:, :, gs], [hinb], [h_])
            self.norm_ps([(h_[:, fc, :], h_) for fc in range(8)], gm, 1.0 / D, ss, sq, rstd, [uT[:, fc, :] for fc in range(8)], uT)
            jobs = [(c * 128, c * 128, self.QN, c, "QN") for c in range(8)]
            jobs += [(1536 + c * 128, 1024 + c * 128, self.KSd, c, "KSd") for c in range(2)]
            jobs += [(2048 + c * 128, 1280 + c * 128, self.KWd, c, "KWd") for c in range(2)]
            for (ca, cb_, dst, c, nm) in jobs:
                a, b = A[k % 2], Bp[k % 2]
                x1, x2, q_ = t1[k % 2], t2[k % 2], qst[k % 3]
                k += 1
                self.PROJ(a[:], [(w_in[:, kc, ca:ca + 128], uT[:, kc, :]) for kc in range(8)], [w_in, uT], [a])
                self.PROJ(b[:], [(w_sw[:, kc, cb_:cb_ + 128], uT[:, kc, :]) for kc in range(8)], [w_sw, uT], [b])
                self.TT(x1[:], a[:], C[:, gs], ALU.mult, [a, C], [x1])
                self.TT(x2[:], b[:], Sg[:, gs], ALU.mult, [b, Sg], [x2])
                self.TT(q_[:], x1[:], x2[:], ALU.add, [x1, x2], [q_], eng="gpsimd")
                self.LD(dst[c * 128:(c + 1) * 128, gs], q_[:], [q_], [mb[nm]], eng="gpsimd")
            for (ca, dst, c, nm) in [(1024, self.KCd, 0, "KCd"), (1152, self.KCd, 1, "KCd"), (1280, self.VCd, 0, "VCd"), (1408, self.VCd, 1, "VCd")]:
                a = A[k % 2]
                q_ = qst[k % 3]
                k += 1
                self.PROJ(a[:], [(w_in[:, kc, ca:ca + 128], uT[:, kc, :]) for kc in range(8)], [w_in, uT], [a])
                self.CP(q_[:], a[:], [a], [q_], eng="scalar")
                self.LD(dst[c * 128:(c + 1) * 128, gs], q_[:], [q_], [mb[nm]], eng="gpsimd")
            for t in range(4):
                tp = Tp[t % 2]
                self.PROJ(tp[:, 0:256], [(uT[:, kc, t * 128:(t + 1) * 128], w_in[:, kc, 1792:2048]) for kc in range(8)], [w_in, uT], [tp])
                self.PROJ(tp[:, 256:512], [(uT[:, kc, t * 128:(t + 1) * 128], w_in[:, kc, 2304:2560]) for kc in range(8)], [w_in, uT], [tp])
                self.CP(vst[:, t, :, 0:64], tp[:].rearrange("p (h d) -> p h d", d=64), [tp], [vst], eng=("scalar" if t % 2 else "vector"))
            self.LD(self.VSW[gs, :].rearrange("(t p) f -> p t f", p=128), vst[:].rearrange("p t h d -> p t (h d)"), [vst], [mb["VSW"]], eng="gpsimd")
            self.PROJ(A[0][0:48, :], [(w_in[:, kc, 2560:2608], uT[:, kc, :]) for kc in range(8)], [w_in, uT], [A[0]])
            self.ACT(gts[:], A[0][0:48, :], AF.Sigmoid, [A[0]], [gts])
            self.LD(self.GT[:, gs], gts[:], [gts], [mb["GT"]], eng="gpsimd")
    self.gdeps = []
    P.barrier()


KB.inproj1 = _inproj1


def _nsa_attn(self):
    P, I = self.P, self.I
    mb = self.m1b
    SC = 64 ** -0.5
    with ExitStack() as st:
        T = lambda shape, dt, n: self.tile(st, shape, dt, n)
        VSW = T([128, 32, 520], BF16, "VSW")
        for q4 in range(4):
            self.LD(VSW[:, q4 * 8:(q4 + 1) * 8, :], self.VSW[q4 * 1024:(q4 + 1) * 1024, :].rearrange("(n p) f -> p n f", p=128), [mb["VSW"]], [VSW])
        self.vdep = VSW
        cmpm = T([128, 2, S], BF16, "cmpm")
        for i in range(2):
            self.LD(cmpm[:, i, :], I["cmpmask"][i * 128:(i + 1) * 128, :], [], [cmpm], eng="gpsimd")
        ovl = T([128, 2, 65], BF16, "ovl")
        self.LD(ovl[:], I["ovl"].rearrange("(i p) f -> p i f", p=128), [], [ovl], eng="gpsimd")
        eo = CB["E"][0]
        KCMP = T([64, 4, 256], BF16, "KCMP")
        VCMP = T([128, 4, 2, 65], BF16, "VCMP")
        self.MS(KCMP[:], 0.0, [KCMP])
        self.MS(VCMP[:], 0.0, [VCMP])
        Sp = [self.ptile(st, name="S") for _ in range(3)]
        Oc = self.ptile(st, name="Oc")
        Os = self.ptile(st, name="Os")
        Ow = self.ptile(st, name="Ow")
        imp = Os
        M1 = self.ptile(st, name="M1")
        M2 = self.ptile(st, name="M2")
        with ExitStack() as st2:
            T2 = lambda shape, dt, n: self.tile(st2, shape, dt, n)
            Cc, Sc = self.rope_tables(st2, 64, 0, 2, 3, "cmp", pos_ap=I["positions"][0:1, 31:S:16], S=255)
            w1 = [T2([64, 32, 128], BF16, "w1") for _ in range(2)]
            w2 = [T2([128, 64], BF16, "w2") for _ in range(2)]
            w2s = T2([128, 64], BF16, "w2s")
            posT = [T2([64, 32], BF16, "posT") for _ in range(2)]
            with self.nc.allow_non_contiguous_dma("small"):
                for i, (a, b_, pp) in enumerate([("nsa_ck_w1", "nsa_ck_w2", "nsa_pos_k"), ("nsa_cv_w1", "nsa_cv_w2", "nsa_pos_v")]):
                    self.LD(w1[i][:], I[a].rearrange("(l d) n -> d l n", d=64), [], [w1[i]], eng="gpsimd")
                    self.LD(w2[i][:], I[b_], [], [w2[i]], eng="gpsimd")
                    self.LD(posT[i][:], I[pp].rearrange("l d -> d l"), [], [posT[i]], eng="gpsimd", allow_slow_non_contiguous=True)
                self.LD(w2s[:, 0:32], I["nsa_ck_w2"][:, 32:64], [], [w2s], eng="gpsimd")
                self.LD(w2s[:, 32:64], I["nsa_ck_w2"][:, 0:32], [], [w2s], eng="gpsimd")
            cb_ = T2([128, 2], F32, "cbias")
            for i in range(2):
                for l in range(32):
                    self.MM(M1[:, i:i + 1], w1[i][:, l, :], posT[i][:, l:l + 1], l == 0, l == 31, [w1[i], posT[i]], [M1])
            self.CP(cb_[:], M1[:, 0:2], [M1], [cb_])
            src = [T2([64, S], BF16, "csrc") for _ in range(2)]
            hid = [T2([128, 256], BF16, "hid") for _ in range(2)]
            x1 = T2([64, 256], F32, "x1")
            x2 = T2([64, 256], F32, "x2")
            for i in range(2):
                self.MS(hid[i][:], 0.0, [hid[i]])
            k = 0
            for g in range(4):
                for i, (dsrc, nm) in enumerate([(self.KCd, "KCd"), (self.VCd, "VCd")]):
                    s_ = src[k % 2]
                    hd = hid[k % 2]
                    ps_ = Sp[k % 2]
                    k += 1
                    self.LD(s_[:], dsrc[g * 64:(g + 1) * 64, :], [mb[nm]], [s_])
                    for l in range(32):
                        self.MM(ps_[:, 0:255], w1[i][:, l, :], s_[:, l:l + 16 * 254 + 1:16], l == 0, l == 31, [w1[i], s_], [ps_])
                    self.ACT(hd[:, 0:255], ps_[:, 0:255], AF.Silu, [ps_, cb_], [hd], bias=cb_[:, i:i + 1], scale=1.0)
                    if i == 0:
                        self.MM(M1[0:64, 0:255], w2[0][:], hd[:, 0:255], True, True, [w2[0], hd], [M1])
                        self.MM(M2[0:64, 0:255], w2s[:], hd[:, 0:255], True, True, [w2s, hd], [M2])
                        self.TT(x1[:, 0:255], M1[0:64, 0:255], Cc[0:64, :], ALU.mult, [M1, Cc], [x1])
                        self.TT(x2[:, 0:255], M2[0:64, 0:255], Sc[0:64, :], ALU.mult, [M2, Sc], [x2])
                        self.TT(KCMP[:, g, 0:255], x1[:, 0:255], x2[:, 0:255], ALU.add, [x1, x2], [KCMP], eng="gpsimd")
                    else:
                        self.MM(M1[:, 0:64], hd[:, 0:128], w2[1][:], True, True, [w2[1], hd], [M1])
                        self.MM(M1[0:127, 64:128], hd[:, 128:255], w2[1][:], True, True, [w2[1], hd], [M1])
                        self.CP(VCMP[:, g, 0, 0:64], M1[:, 0:64], [M1], [VCMP])
                        self.CP(VCMP[0:127, g, 1, 0:64], M1[0:127, 64:128], [M1], [VCMP])
                        self.MS(VCMP[:, g, 0, 64:65], 1.0, [VCMP])
                        self.MS(VCMP[0:127, g, 1, 64:65], 1.0, [VCMP])
            P.barrier()
        KS = [T([128, S], BF16, "KS") for _ in range(2)]
        for i in range(2):
            self.LD(KS[i][64:128, :], I["consts"][0:64, eo:eo + 32 * 128], [], [KS[i]], eng="gpsimd")
        KW = [T([64, S], BF16, "KW") for _ in range(2)]
        PT = [T([128, TG], BF16, "PT") for _ in range(3)]
        PC = [T([128, 2, TG], BF16, "PC") for _ in range(4)]
        ost = [T([64, TG], BF16, "ost") for _ in range(2)]
        irec = T([128, 4], F32, "irec")
        itmp = T([128, 4, 64], F32, "itmp")
        iacc = T([128, 4, 64], F32, "iacc")
        score = T([128, 4, 64], F32, "score")
        sc2 = T([128, 64], F32, "sc2")
        m8 = T([128, 16], F32, "m8")
        ones32 = self.cst("ones")
        so = CB["sel48"][0]
        wo = CB["wide"][0]
        col0 = self.cst("col0")
        cnt = [0]
        hk = 0
        ak = 0

        A4 = [T([64, TG], F32, "A4") for _ in range(4)]
        Q4 = [T([128, S], BF16, "Q4") for _ in range(4)]
        Q4n = [Tl(q.t, "Q4n") for q in Q4]
        nself = T([128, 4, 128], F32, "nself")
        self.MS(nself[:], 0.0, [nself])
        ident32 = self.cst("ident")

        selbf = T([48, 48 * 64 + 1], BF16, "selbf")
        self.LD(selbf[:], I["consts"][0:48, so:so + 48 * 64 + 1], [], [selbf], eng="gpsimd")
        GTb = T([48, S], BF16, "GTb")
        self.LD(GTb[:], self.GT, [mb["GT"]], [GTb], eng="gpsimd")
        Mb = [M1, M2]
        Ocs = [Oc, Ow]
        rr2 = [T([65, TG], F32, "rr") for _ in range(2)]
        fb2 = [T([65, TG], BF16, "fb") for _ in range(2)]
        bcs2 = [T([64, TG], F32, "bcs") for _ in range(2)]
        tmp2 = [T([64, TG], F32, "tmp") for _ in range(2)]
        fi = [0]

        def finish_unit(o, row, a_, first, qs, pre=None, extra=None, delays=(1, 2)):
            k = fi[0]
            fi[0] += 1
            m, rr_, fb_, bs_, tmp_ = Mb[k % 2], rr2[k % 2], fb2[k % 2], bcs2[k % 2], tmp2[k % 2]

            def f0():
                if pre is not None:
                    pre()
                if first:
                    self.TS(rr_[64:65, :], o[64:65, :], 1e-18, None, ALU.max, None, [o], [rr_])
                    self.ACT(rr_[64:65, :], rr_[64:65, :], AF.Ln, [rr_], [rr_])
                else:
                    self.ACT(rr_[64:65, :], o[64:65, :], AF.Ln, [o], [rr_])
                self.ACT(rr_[64:65, :], rr_[64:65, :], AF.Exp, [rr_], [rr_], scale=-1.0)
                self.MM(m[0:65, :], selbf[0:48, row * 64:row * 64 + 65], GTb[0:48, qs], True, True, [GTb, selbf], [m])

            def f1():
                self.TT(fb_[64:65, :], m[64:65, :], rr_[64:65, :], ALU.mult, [m, rr_], [fb_])
                self.MM(m[0:64, :], self.ones_bf[64:65, 0:64], fb_[64:65, :], True, True, [fb_, self.cbf], [m])

            def f2():
                self.CP(bs_[:], m[0:64, :], [m], [bs_], eng="scalar")
                if first:
                    self.TT(a_[:], o[0:64, :], bs_[:], ALU.mult, [o, bs_], [a_])
                else:
                    self.TT(tmp_[:], o[0:64, :], bs_[:], ALU.mult, [o, bs_], [tmp_])
                    self.TT(a_[:], a_[:], tmp_[:], ALU.add, [a_, tmp_], [a_], eng="gpsimd")
                if extra is not None:
                    extra()
            return U(None, None, f0, later=[(delays[0], f1), (delays[1], f2)])

        for g in range(4):
            ks_, kw_ = KS[g % 2], KW[g % 2]
            self.LD(ks_[0:64, :], self.KSd[g * 64:(g + 1) * 64, :], [mb["KSd"]], [ks_])
            self.LD(kw_[:], self.KWd[g * 64:(g + 1) * 64, :], [mb["KWd"]], [kw_])
            for hh in range(4):
                h = g * 4 + hh
                self.LD(Q4[hh][0:64, :], self.QN[h * 64:(h + 1) * 64, :], [mb["QN"]], [Q4[hh]])
            for qg in range(NG):
                qs = slice(qg * TG, (qg + 1) * TG)
                q0 = qg * TG
                ntile = 2 if qg >= 4 else 1
                units = []
                accs = [Oc, Ow, Sp[1], Sp[2]]
                for hh in range(4):
                    h = g * 4 + hh
                    pc = PC[hh]
                    for i in range(ntile):
                        sp = Sp[0]

                        def A(sp=sp, i=i, hh=hh):
                            self.MM(sp[:], KCMP[:, g, i * 128:(i + 1) * 128], Q4[hh][0:64, qs], True, False, [KCMP, Q4[hh]], [sp])
                            self.MM(sp[:], self.ident_bf[:], cmpm[:, i, qs], False, True, [cmpm, self.cbf], [sp])

                        def B(sp=sp, i=i, pc=pc):
                            self.ACT(pc[:, i, :], sp[:], AF.Exp, [sp], [pc], scale=SC)

                        def C(i=i, pc=pc, oc=accs[hh]):
                            self.MM(oc[0:65, :], VCMP[:, g, i, :], pc[:, i, :], i == 0, i == ntile - 1, [VCMP, pc], [oc])
                        units.append(U(A, B, C))
                for hh in range(4):
                    h = g * 4 + hh
                    pc = PC[hh]

                    def pre(hh=hh, pc=pc):
                        for qt in range(4):
                            for i in range(ntile):
                                self.MM(imp[:, qt * 65:(qt + 1) * 65], pc[:, i, qt * 128:(qt + 1) * 128], ovl[:, i, :], i == 0, i == ntile - 1, [pc, ovl], [imp])
                        iv = imp[:, 0:260].rearrange("p (t f) -> p t f", f=65)
                        self.TS(irec[:], iv[:, :, 64], 1e-30, None, ALU.max, None, [imp], [irec])
                        self.P.dve(lambda e: e.reciprocal(out=irec[:], in_=irec[:]), reads=[irec.b], writes=[irec.b])
                        if hh == 0:
                            self.TT(iacc[:], iv[:, :, 0:64], irec[:].unsqueeze(2).broadcast_to([128, 4, 64]), ALU.mult, [imp, irec], [iacc])
                        else:
                            self.TT(itmp[:], iv[:, :, 0:64], irec[:].unsqueeze(2).broadcast_to([128, 4, 64]), ALU.mult, [imp, irec], [itmp])
                            self.TT(iacc[:], iacc[:], itmp[:], ALU.add, [iacc, itmp], [iacc], eng="gpsimd")
                    units.append(finish_unit(accs[hh], 3 * h + 0, A4[hh], True, qs, pre=pre))
                run_units(units, 0)
                for qt in range(4):
                    tix = qg * 4 + qt
                    self.TT(score[:, qt, :], iacc[:, qt, :], self.c32[:, wo + 64 - 2 * tix:wo + 128 - 2 * tix], ALU.add, [iacc, self.cb], [score])
                    self.TT(score[:, qt, :], score[:, qt, :], col0, ALU.max, [score, self.cb], [score])
                    self.P.dve(lambda e, qt=qt: e.max(out=m8[:, 0:8], in_=score[:, qt, :]), reads=[score.b], writes=[m8.b])
                    self.P.dve(lambda e, qt=qt: e.match_replace(out=sc2[:], in_to_replace=m8[:, 0:8], in_values=score[:, qt, :], imm_value=-3.0e38),
                               reads=[score.b, m8.b], writes=[sc2.b])
                    self.P.dve(lambda e: e.max(out=m8[:, 8:16], in_=sc2[:]), reads=[sc2.b], writes=[m8.b])
                    self.TS(nself[:, qt, 64:128], score[:, qt, :], m8[:, 15:16], -1.0, ALU.is_ge, ALU.add, [score, m8], [nself])
                    self.TR(M2[:, qt * 128:(qt + 1) * 128], nself[:, qt, :], ident32, [nself, self.cb], [M2])
                for hh in range(4):
                    self.CP(Q4[hh][64:128, qs], M2[64:128, :], [M2], [Q4n[hh]], eng="scalar")
                units = []
                for hh in range(4):
                    h = g * 4 + hh
                    qh_ = Q4[hh]
                    nk = 4 * qg + 4
                    for kt in range(nk):
                        d = kt - 4 * qg
                        sp = Sp[cnt[0] % 3]
                        pt = PT[cnt[0] % 3]
                        cnt[0] += 1
                        kc = slice(kt * 128, (kt + 1) * 128)
                        c0 = max(d, 0) * 128

                        def A(sp=sp, kc=kc, d=d, c0=c0, qh_=qh_, qn_=Q4n[hh]):
                            if d < 0:
                                self.MM(sp[:], ks_[:, kc], qh_[:, qs], True, True, [ks_, qh_, qn_], [sp])
                            else:
                                self.MM(sp[:, c0:c0 + 128], ks_[:, kc], qh_[:, q0 + c0:q0 + c0 + 128], True, False, [ks_, qh_, qn_], [sp])
                                self.MM(sp[:, c0:c0 + 128], self.ident_bf[:], self.cpen_bf[:], False, True, [self.cbf], [sp])
                                if c0 + 128 < TG:
                                    self.MM(sp[:, c0 + 128:TG], ks_[:, kc], qh_[:, q0 + c0 + 128:q0 + TG], True, True, [ks_, qh_, qn_], [sp])

                        def B(sp=sp, pt=pt, c0=c0):
                            self.ACT(pt[:, c0:TG], sp[:, c0:TG], AF.Exp, [sp], [pt], scale=SC)

                        def C(pt=pt, c0=c0, kt=kt):
                            self.MM(Os[0:65, c0:TG], VSW[:, kt, g * 65:(g + 1) * 65], pt[:, c0:TG], kt == 0, kt == nk - 1, [pt, VSW], [Os])
                        units.append(U(A, B, C))
                    units.append(finish_unit(Os, 3 * h + 1, A4[hh], False, qs, delays=((5, 7) if qg >= 1 else (2, 4))))
                    kts = [4 * qg] + [kt for kt in range(4 * qg - 4, 4 * qg + 4) if kt >= 0 and kt != 4 * qg]
                    for i, kt in enumerate(kts):
                        d = kt - 4 * qg
                        sp = Sp[cnt[0] % 3]
                        pt = PT[cnt[0] % 3]
                        cnt[0] += 1
                        kc = slice(kt * 128, (kt + 1) * 128)
                        lo = max(d, 0) * 128
                        hi = min(d + 5, 4) * 128
                        if d >= 0:
                            pb, pen = lo, self.cpen_bf
                            rest = (lo + 128, hi)
                        else:
                            pb, pen = hi - 128, self.bpen_bf
                            rest = (lo, hi - 128)

                        def A(sp=sp, kc=kc, pb=pb, pen=pen, rest=rest, qh_=qh_):
                            self.MM(sp[:, pb:pb + 128], kw_[:, kc], qh_[0:64, q0 + pb:q0 + pb + 128], True, False, [kw_, qh_], [sp])
                            self.MM(sp[:, pb:pb + 128], self.ident_bf[:], pen[:], False, True, [self.cbf], [sp])
                            if rest[1] > rest[0]:
                                self.MM(sp[:, rest[0]:rest[1]], kw_[:, kc], qh_[0:64, q0 + rest[0]:q0 + rest[1]], True, True, [kw_, qh_], [sp])

                        def B(sp=sp, pt=pt, lo=lo, hi=hi):
                            self.ACT(pt[:, lo:hi], sp[:, lo:hi], AF.Exp, [sp], [pt], scale=SC)

                        def C(pt=pt, lo=lo, hi=hi, kt=kt, i=i, nw=len(kts)):
                            self.MM(Ow[0:65, lo:hi], VSW[:, kt, (4 + g) * 65:(5 + g) * 65], pt[:, lo:hi], i == 0, i == nw - 1, [pt, VSW], [Ow])
                        units.append(U(A, B, C))

                    def store(h=h, hh=hh, qs=qs):
                        os_ = ost[h % 2]
                        self.CP(os_[:], A4[hh][:], [A4[hh]], [os_], eng="scalar")
                        self.LD(self.ONT[h, :, qs], os_[:], [os_], [mb["ONT"]], eng="gpsimd")
                    units.append(finish_unit(Ow, 3 * h + 2, A4[hh], False, qs, extra=store, delays=((5, 7) if qg >= 1 else (2, 4))))
                run_units(units, 2)
    P.barrier()


KB.nsa_attn = _nsa_attn
```

```python
import numpy as np
from contextlib import ExitStack
import concourse.bass as bass
import concourse.mybir as mybir
from concourse.bass_utils import run_bass_kernel_spmd

F32 = mybir.dt.float32
BF16 = mybir.dt.bfloat16
I32 = mybir.dt.int32
ALU = mybir.AluOpType
AF = mybir.ActivationFunctionType
AX = mybir.AxisListType

S = 4096
D = 1024
DFF = 2816
NFC = 22
NG = 8
TG = 512
EPS = 1e-6
HY_IN = 1968
NSA_IN = 2608
NEGB = -30000.0

ENGINES = ["tensor", "vector", "scalar", "gpsimd", "sync"]
class Buf:
    __slots__ = ("name", "last_w", "readers")

    def __init__(self, name=""):
        self.name = name
        self.last_w = None
        self.readers = []


class Op:
    __slots__ = ("eng", "fn", "raw", "oth", "is_dma", "sig", "sem", "semval", "prev_semval", "gidx")


class Prog:
    def __init__(self, nc, n_dma_sems=12):
        self.nc = nc
        self.ops = {e: [] for e in ENGINES}
        self.n = 0
        self.n_dma_sems = n_dma_sems
        self.pending_barrier = {}
        self.dma_last = {}
        self.dma_cnt = {}

    def op(self, eng, fn, reads=(), writes=(), dma=False):
        o = Op()
        o.eng = eng
        o.fn = fn
        o.is_dma = dma
        o.raw = set()
        o.oth = set()
        o.sig = False
        o.sem = None
        o.semval = 0
        o.prev_semval = 0
        o.gidx = self.n
        self.n += 1
        for b in reads:
            if b.last_w is not None:
                o.raw.add(b.last_w)
        for b in writes:
            if b.last_w is not None:
                o.oth.add(b.last_w)
            for r in b.readers:
                o.oth.add(r)
        for b in reads:
            b.readers.append(o)
        for b in writes:
            b.last_w = o
            b.readers = []
        o.raw.discard(o)
        o.oth.discard(o)
        if eng in self.pending_barrier:
            for d in self.pending_barrier.pop(eng):
                o.raw.add(d)
        self.ops[eng].append(o)
        if dma:
            k = self.dma_cnt.get(eng, 0)
            self.dma_cnt[eng] = k + 1
            self.dma_last[(eng, k % self.n_dma_sems)] = o
        return o

    def barrier(self):
        deps = []
        for e in ENGINES:
            comp = [x for x in self.ops[e] if not x.is_dma]
            if comp:
                deps.append(comp[-1])
        deps.extend(self.dma_last.values())
        for e in ENGINES:
            self.pending_barrier[e] = list(deps) + self.pending_barrier.get(e, [])

    def mm(self, fn, reads=(), writes=()):
        return self.op("tensor", fn, reads, writes)

    def dve(self, fn, reads=(), writes=()):
        return self.op("vector", fn, reads, writes)

    def act(self, fn, reads=(), writes=()):
        return self.op("scalar", fn, reads, writes)

    def pool(self, fn, reads=(), writes=()):
        return self.op("gpsimd", fn, reads, writes)

    def load(self, out, in_, reads=(), writes=(), eng="sync", **kw):
        return self.op(eng, lambda e: e.dma_start(out=out, in_=in_, **kw), reads, writes, dma=True)

    def needed_deps(self, o):
        res = []
        for d in o.raw:
            if d.eng == o.eng and not d.is_dma and not o.is_dma:
                if o.eng == "tensor":
                    continue
                res.append(d)
            else:
                res.append(d)
        for d in o.oth:
            if d.eng == o.eng and not d.is_dma and not o.is_dma:
                continue
            res.append(d)
        return res

    def finalize(self, stack):
        nc = self.nc
        for e in ENGINES:
            for o in self.ops[e]:
                if o.is_dma:
                    o.sig = True
                for d in self.needed_deps(o):
                    d.sig = True
        csem = {}
        for e in ["tensor", "vector", "scalar", "gpsimd"]:
            csem[e] = stack.enter_context(nc.semaphore("c_" + e))
        dpool = {}
        for e in ["sync", "gpsimd", "scalar"]:
            if any(o.is_dma for o in self.ops[e]):
                dpool[e] = [stack.enter_context(nc.semaphore("d_%s_%d" % (e, i))) for i in range(self.n_dma_sems)]
        for e in ENGINES:
            cnt = 0
            k = 0
            uses = {}
            for o in self.ops[e]:
                if o.is_dma:
                    s = dpool[e][k % self.n_dma_sems]
                    k += 1
                    o.sem = s
                    o.prev_semval = uses.get(id(s), 0)
                    o.semval = o.prev_semval + 16
                    uses[id(s)] = o.semval
                elif o.sig:
                    cnt += 1
                    o.sem = csem[e]
                    o.semval = cnt
        self.final_dma = []
        for e in dpool:
            last = {}
            for o in self.ops[e]:
                if o.is_dma:
                    last[id(o.sem)] = (o.sem, o.semval)
            self.final_dma.append((e, list(last.values())))
        block = stack.enter_context(nc.Block())
        prog = self

        def make(e):
            def body(eng):
                waited = {}
                for o in prog.ops[e]:
                    need = {}
                    for d in prog.needed_deps(o):
                        key = id(d.sem)
                        if key not in need or need[key][1] < d.semval:
                            need[key] = (d.sem, d.semval)
                    if o.is_dma and o.prev_semval > 0:
                        key = id(o.sem)
                        if key not in need or need[key][1] < o.prev_semval:
                            need[key] = (o.sem, o.prev_semval)
                    for key, (s, v) in need.items():
                        if waited.get(key, 0) >= v:
                            continue
                        eng.wait_ge(s, v)
                        waited[key] = v
                    ins = o.fn(eng)
                    if o.sig:
                        ins.then_inc(o.sem, 16 if o.is_dma else 1)
                for (ee, lst) in prog.final_dma:
                    if ee == e:
                        for (s, v) in lst:
                            if waited.get(id(s), 0) < v:
                                eng.wait_ge(s, v)
            return body

        for e in ENGINES:
            if not self.ops[e]:
                continue
            getattr(block, e)(make(e))


CB = {}
_off = 0
for _n, _w in [("ident", 128), ("ones", 128), ("cpen", 128), ("bpen", 128), ("tri", 64), ("scanmask", 512),
               ("freq", 4), ("wide", 128), ("col0", 64), ("sel48", 48 * 64 + 1), ("E", 32 * 128)]:
    CB[_n] = (_off, _w)
    _off += _w
CB_W = _off


def make_consts():
    c = np.zeros((128, CB_W), np.float32)
    p = np.arange(128)[:, None]

    def put(n, a):
        o, w = CB[n]
        c[: a.shape[0], o:o + a.shape[1]] = a
    put("ident", np.eye(128, dtype=np.float32))
    put("ones", np.ones((128, 128), np.float32))
    f = np.arange(128)[None, :]
    put("cpen", np.where(p <= f, 0.0, NEGB).astype(np.float32))
    put("bpen", np.where(p > f, 0.0, NEGB).astype(np.float32))
    j = np.arange(64)[:, None]
    i = np.arange(64)[None, :]
    put("tri", (j <= i).astype(np.float32))
    sm = np.ones((128, 512), np.float32)
    sm[:, ::64] = 0.0
    put("scanmask", sm)
    fr = np.zeros((128, 4), np.float32)
    r = np.arange(128)
    im = (r - 64) % 16
    fr[:, 0] = (10000.0 ** (-im * (2.0 / 32))) / (2 * np.pi)
    fr[:, 1] = np.where(((r - 64) % 32) < 16, -1.0, 1.0) * (2 * np.pi * (1 - 1e-6))
    i2 = r % 32
    fr[:, 2] = (10000.0 ** (-i2 * (2.0 / 64))) / (2 * np.pi)
    fr[:, 3] = np.where((r % 64) < 32, -1.0, 1.0) * (2 * np.pi * (1 - 1e-6))
    put("freq", fr)
    x = np.arange(128)[None, :]
    rel = x - 64 - (p >= 64)
    wide = np.where(rel > 0, -1e30, np.where(rel >= -1, 1e4, 0.0)).astype(np.float32)
    put("wide", wide)
    c0 = np.full((128, 64), -3e38, np.float32)
    c0[:, 0] = 1e4
    put("col0", c0)
    sel = np.zeros((128, 48 * 64 + 1), np.float32)
    for rr in range(48):
        sel[rr, 1 + rr * 64:1 + (rr + 1) * 64] = 1.0
    put("sel48", sel)
    E = np.zeros((128, 32 * 128), np.float32)
    for kt in range(32):
        for key in range(128):
            E[2 * kt + (key >= 64), kt * 128 + key] = -NEGB
    put("E", E)
    return c


def make_cmp_consts():
    n = np.arange(256)[:, None]
    t = np.arange(S)[None, :]
    cm = np.where((16 * n + 31 <= t) & (n < 255), 0.0, NEGB).astype(np.float32)
    ncmp = np.arange(255)
    cs_ = ncmp * 16
    bs_ = np.arange(64) * 64
    ov = np.minimum(cs_[:, None] + 32, bs_[None, :] + 64) - np.maximum(cs_[:, None], bs_[None, :])
    ovl = np.zeros((256, 65), np.float32)
    ovl[:255, :64] = np.clip(ov, 0, None) / 32.0
    ovl[:255, 64] = 1.0
    return cm, ovl


class KB:
    def __init__(self, debug_phases=None):
        self.nc = bass.Bass("TRN2", target_bir_lowering=False)
        self.P = Prog(self.nc)
        self.debug_phases = debug_phases
        self.uid = 0
        self.gdeps = []

    def name(self, p):
        self.uid += 1
        return "%s_%d" % (p, self.uid)

    def dram(self, name, shape, dt, kind="Internal"):
        return self.nc.dram_tensor(name, list(shape), dt, kind=kind).ap()

    def sb(self, st, shape, dt, name="t"):
        return st.enter_context(self.nc.sbuf_tensor(self.name(name), list(shape), dt))

    def ps(self, st, shape, dt=F32, name="p"):
        return st.enter_context(self.nc.psum_tensor(self.name(name), list(shape), dt))

    def declare(self):
        d = self.dram
        I = {}
        I["x"] = d("x", [S, D], F32, "ExternalInput")
        I["positions"] = d("positions", [1, S], I32, "ExternalInput")
        I["consts"] = d("consts", [128, CB_W], F32, "ExternalInput")
        I["cmpmask"] = d("cmpmask", [256, S], F32, "ExternalInput")
        I["ovl"] = d("ovl", [256, 65], F32, "ExternalInput")
        for n, shp in [("ffn_norm", [4, D]), ("ffn_w_gate", [4, D, DFF]), ("ffn_w_up", [4, D, DFF]),
                       ("ffn_w_down", [4, DFF, D]), ("mix_norm", [2, D]), ("hy_w_in", [D, HY_IN]),
                       ("mla_q_norm", [1, 256]), ("mla_w_uq", [256, 768]), ("mla_kv_norm", [1, 128]),
                       ("mla_w_ukv", [128, 1024]), ("gla_w_a2", [16, 256]), ("gla_b_a", [1, 256]),
                       ("gla_out_norm", [1, 128]), ("hy_w_out", [D, D]), ("nsa_w_in", [D, NSA_IN]),
                       ("nsa_pos_k", [32, 64]), ("nsa_pos_v", [32, 64]), ("nsa_ck_w1", [2048, 128]),
                       ("nsa_ck_w2", [128, 64]), ("nsa_cv_w1", [2048, 128]), ("nsa_cv_w2", [128, 64]),
                       ("nsa_w_out", [D, D]), ("final_norm", [1, D])]:
            I[n] = d(n, shp, F32, "ExternalInput")
        self.I = I
        self.out = d("out", [S, D], F32, "ExternalOutput")
        self.hT = [d("hT%d" % i, [D, S], F32) for i in range(2)]
        self.hbuf = [Buf("hT0"), Buf("hT1")]
        self.wgu = d("wgu", [4, NFC, 128, 2 * 8 * 128], BF16)
        self.wdn = d("wdn", [4, 8, 128, NFC * 128], BF16)
        self.wgu_b = {}
        self.wdn_b = {}
        self.prepq = []

    def prep_ffn(self, f):
        I = self.I
        items = []
        self.wgu_b[f] = {}
        self.wdn_b[f] = []
        for c in range(NFC):
            for which, src in enumerate([I["ffn_w_gate"], I["ffn_w_up"]]):
                bf_ = Buf("wgu")
                self.wgu_b[f][(which, c)] = bf_
                dst = self.wgu[f, c].rearrange("p (w k j) -> p w k j", w=2, k=8, j=128)[:, which, :, :]
                s_ = src[f, :, c * 128:(c + 1) * 128].rearrange("(k p) j -> p k j", p=128)
                items.append((dst, s_, bf_))
        for c in range(NFC):
            bf_ = Buf("wdn")
            self.wdn_b[f].append(bf_)
            dst = self.wdn[f].rearrange("fc p (c j) -> p fc c j", c=NFC, j=128)[:, :, c, :]
            s_ = I["ffn_w_down"][f, c * 128:(c + 1) * 128, :].rearrange("p (fc j) -> p fc j", j=128)
            items.append((dst, s_, bf_))
        self.prepq.extend(items)

    def pump(self, n):
        for _ in range(n):
            if not self.prepq:
                return
            dst, s_, bf_ = self.prepq.pop(0)
            self.P.load(dst, s_, writes=[bf_], eng="gpsimd")

    def load_consts(self, st):
        P = self.P
        self.c32 = self.sb(st, [128, CB["E"][0]], F32, "c32")
        self.cb = Buf("c32")
        P.load(self.c32[:], self.I["consts"][:, 0:CB["E"][0]], writes=[self.cb])
        self.ident_bf = self.sb(st, [128, 128], BF16, "identbf")
        self.ones_bf = self.sb(st, [128, 128], BF16, "onesbf")
        self.cpen_bf = self.sb(st, [128, 128], BF16, "cpenbf")
        self.bpen_bf = self.sb(st, [128, 128], BF16, "bpenbf")
        self.cbf = Buf("cbf")
        for t, n in [(self.ident_bf, "ident"), (self.ones_bf, "ones"), (self.cpen_bf, "cpen"), (self.bpen_bf, "bpen")]:
            o, w = CB[n]
            P.dve(lambda e, t=t, o=o, w=w: e.tensor_copy(out=t[:], in_=self.c32[:, o:o + w]), reads=[self.cb], writes=[self.cbf])
        self.gcol = self.sb(st, [128, 7, 8], F32, "gcol")
        self.gb = Buf("gcol")
        with self.nc.allow_non_contiguous_dma("tiny gain vectors"):
            for j, src in [(0, self.I["ffn_norm"][0:1, :]), (1, self.I["ffn_norm"][1:2, :]), (2, self.I["ffn_norm"][2:3, :]),
                           (3, self.I["ffn_norm"][3:4, :]), (4, self.I["mix_norm"][0:1, :]), (5, self.I["mix_norm"][1:2, :]),
                           (6, self.I["final_norm"][0:1, :])]:
                P.load(self.gcol[:, j, :], src.rearrange("o (fc p) -> p (o fc)", p=128), writes=[self.gb], allow_slow_non_contiguous=True)

    def cst(self, n, rows=128, c0=0, c1=None):
        o, w = CB[n]
        if c1 is None:
            c1 = w
        return self.c32[0:rows, o + c0:o + c1]

    def transpose_in(self):
        P = self.P
        with ExitStack() as st:
            xt = [self.sb(st, [128, D], F32, "xt") for _ in range(4)]
            xb = [Buf("xt%d" % i) for i in range(4)]
            tp = [self.ps(st, [128, 512], F32, "tp") for _ in range(2)]
            tb = [Buf("tp0"), Buf("tp1")]
            stg = [self.sb(st, [128, 8, 512], F32, "stg") for _ in range(2)]
            sgb = [Buf("stg0"), Buf("stg1")]
            ident = self.cst("ident")
            k = 0
            for g in range(NG):
                for t in range(4):
                    r0 = g * TG + t * 128
                    P.load(xt[t][:], self.I["x"][r0:r0 + 128, :], writes=[xb[t]])
                so = stg[g % 2]
                for fc in range(8):
                    pb = tp[k % 2]
                    for t in range(4):
                        P.mm(lambda e, pb=pb, t=t, fc=fc: e.transpose(out=pb[:, t * 128:(t + 1) * 128], in_=xt[t][:, fc * 128:(fc + 1) * 128], identity=ident),
                             reads=[xb[t], self.cb], writes=[tb[k % 2]])
                    if fc % 2 == 0:
                        P.act(lambda e, pb=pb, fc=fc, so=so: e.copy(out=so[:, fc, :], in_=pb[:]), reads=[tb[k % 2]], writes=[sgb[g % 2]])
                    else:
                        P.dve(lambda e, pb=pb, fc=fc, so=so: e.tensor_copy(out=so[:, fc, :], in_=pb[:]), reads=[tb[k % 2]], writes=[sgb[g % 2]])
                    k += 1
                P.load(self.hT[0].rearrange("(fc p) t -> p fc t", p=128)[:, :, g * TG:(g + 1) * TG], so[:],
                       reads=[sgb[g % 2]], writes=[self.hbuf[0]], eng="gpsimd")
        P.barrier()

    def norm_slab(self, hs, hsb, nfc, gcols, ss_ps, ssb, sq, sqb, rstd, rstdb, uT, uTb, inv_n, psum_src=False):
        P = self
        Pg = self.P
        for fc in range(nfc):
            s_ = sq[fc % len(sq)]
            sb_ = sqb[fc % len(sq)]
            Pg.act(lambda e, s_=s_, fc=fc: e.activation(out=s_[:], in_=hs[:, fc, :], func=AF.Square), reads=[hsb], writes=[sb_])
            Pg.mm(lambda e, s_=s_, fc=fc: e.matmul(ss_ps[:], lhsT=self.ones_bf[:], rhs=s_[:], start=(fc == 0), stop=(fc == nfc - 1)),
                  reads=[sb_, self.cbf], writes=[ssb])
        Pg.act(lambda e: e.activation(out=rstd[:], in_=ss_ps[:], func=AF.Ln, scale=float(inv_n), bias=float(EPS)), reads=[ssb], writes=[rstdb])
        Pg.act(lambda e: e.activation(out=rstd[:], in_=rstd[:], func=AF.Exp, scale=-0.5), reads=[rstdb], writes=[rstdb])
        for fc in range(nfc):
            Pg.dve(lambda e, fc=fc: e.scalar_tensor_tensor(out=uT[:, fc, :], in0=hs[:, fc, :], scalar=gcols[fc], in1=rstd[:],
                                                           op0=ALU.mult, op1=ALU.mult), reads=[hsb, rstdb, self.gb], writes=[uTb])

    def ffn(self, f, src, dst):
        P = self.P
        hin, hinb = self.hT[src], self.hbuf[src]
        hout, houtb = self.hT[dst], self.hbuf[dst]
        hin_v = hin.rearrange("(fc p) t -> p fc t", p=128)
        hout_v = hout.rearrange("(fc p) t -> p fc t", p=128)
        with ExitStack() as st:
            hs = [self.sb(st, [128, 8, TG], F32, "hs") for _ in range(2)]
            hsb = [Buf("hs0"), Buf("hs1")]
            uT = [self.sb(st, [128, 8, TG], BF16, "uT") for _ in range(2)]
            uTb = [Buf("uT0"), Buf("uT1")]
            sq = [self.sb(st, [128, TG], BF16, "sq") for _ in range(2)]
            sqb = [Buf("sq0"), Buf("sq1")]
            rstd = self.sb(st, [128, TG], F32, "rstd")
            rstdb = Buf("rstd")
            actT = [self.sb(st, [128, NFC, TG], BF16, "actT") for _ in range(2)]
            actb = [Buf("act0"), Buf("act1")]
            NW = 6
            wgu = [self.sb(st, [128, 2, 8, 128], BF16, "wgu") for _ in range(NW)]
            wgub = [Buf("wgu%d" % i) for i in range(NW)]
            NWD = 3
            wdn = [self.sb(st, [128, NFC, 128], BF16, "wdn") for _ in range(NWD)]
            wdnb = [Buf("wdn%d" % i) for i in range(NWD)]
            sg = [self.sb(st, [128, TG], BF16, "sg") for _ in range(2)]
            sgb = [Buf("sg0"), Buf("sg1")]
            ho = [self.sb(st, [128, TG], F32, "ho") for _ in range(3)]
            hob = [Buf("ho%d" % i) for i in range(3)]
            ss_ps = self.ps(st, [128, TG], F32, "ss")
            ssb = Buf("ss")
            g_ps = [self.ps(st, [128, TG], F32, "gps") for _ in range(2)]
            gpb = [Buf("g0"), Buf("g1")]
            u_ps = [self.ps(st, [128, TG], F32, "ups") for _ in range(2)]
            upb = [Buf("u0"), Buf("u1")]
            o_ps = [self.ps(st, [128, TG], F32, "ops") for _ in range(2)]
            opb = [Buf("o0"), Buf("o1")]
            gcols = [self.gcol[:, f, fc:fc + 1] for fc in range(8)]
            cnt = {"w": 0, "d": 0, "c": 0, "o": 0}

            def stage_load(g):
                P.load(hs[g % 2][:], hin_v[:, :, g * TG:(g + 1) * TG], reads=[hinb], writes=[hsb[g % 2]])

            def stage_norm(g):
                self.norm_slab(hs[g % 2], hsb[g % 2], 8, gcols, ss_ps, ssb, sq, sqb, rstd, rstdb, uT[g % 2], uTb[g % 2], 1.0 / D)

            def stage_gu(g):
                for c in range(NFC):
                    w = cnt["w"] % NW
                    cnt["w"] += 1
                    P.load(wgu[w][:], self.wgu[f, c].rearrange("p (w k j) -> p w k j", w=2, k=8), reads=[self.wgu_b[f][(0, c)], self.wgu_b[f][(1, c)]], writes=[wgub[w]])
                    b = cnt["c"] % 2
                    cnt["c"] += 1
                    for which, (pt, pbf) in enumerate([(g_ps[b], gpb[b]), (u_ps[b], upb[b])]):
                        for kc in range(8):
                            P.mm(lambda e, pt=pt, w=w, which=which, kc=kc: e.matmul(pt[:], lhsT=wgu[w][:, which, kc, :], rhs=uT[g % 2][:, kc, :],
                                                                                    start=(kc == 0), stop=(kc == 7)),
                                 reads=[wgub[w], uTb[g % 2]], writes=[pbf])
                    P.act(lambda e, b=b: e.activation(out=sg[b][:], in_=g_ps[b][:], func=AF.Silu), reads=[gpb[b]], writes=[sgb[b]])
                    P.dve(lambda e, b=b, c=c: e.tensor_tensor(out=actT[g % 2][:, c, :], in0=sg[b][:], in1=u_ps[b][:], op=ALU.mult),
                          reads=[sgb[b], upb[b]], writes=[actb[g % 2]])

            def stage_down(g):
                for fc in range(8):
                    w = cnt["d"] % NWD
                    cnt["d"] += 1
                    P.load(wdn[w][:], self.wdn[f, fc].rearrange("p (c j) -> p c j", j=128), reads=self.wdn_b[f], writes=[wdnb[w]])
                    b = fc % 2
                    for c in range(NFC):
                        P.mm(lambda e, b=b, w=w, c=c: e.matmul(o_ps[b][:], lhsT=wdn[w][:, c, :], rhs=actT[g % 2][:, c, :], start=(c == 0), stop=(c == NFC - 1)),
                             reads=[wdnb[w], actb[g % 2]], writes=[opb[b]])
                    k = cnt["o"] % 3
                    cnt["o"] += 1
                    P.dve(lambda e, b=b, k=k, fc=fc: e.scalar_tensor_tensor(out=ho[k][:], in0=o_ps[b][:], scalar=0.5, in1=hs[g % 2][:, fc, :],
                                                                            op0=ALU.mult, op1=ALU.add), reads=[opb[b], hsb[g % 2]], writes=[hob[k]])
                    P.load(hout_v[:, fc, g * TG:(g + 1) * TG], ho[k][:], reads=[hob[k]], writes=[houtb], eng="gpsimd")
                    self.pump(5)

            stage_load(0)
            stage_norm(0)
            for g in range(NG):
                if g + 1 < NG:
                    stage_load(g + 1)
                stage_gu(g)
                if g + 1 < NG:
                    stage_norm(g + 1)
                stage_down(g)
        P.barrier()

    def final(self, src, do_norm=True):
        P = self.P
        hin_v = self.hT[src].rearrange("(fc p) t -> p fc t", p=128)
        hinb = self.hbuf[src]
        outb = Buf("out")
        with ExitStack() as st:
            hs = [self.sb(st, [128, 8, TG], F32, "hs") for _ in range(2)]
            hsb = [Buf("hs0"), Buf("hs1")]
            yT = [self.sb(st, [128, 8, TG], F32, "yT") for _ in range(2)]
            yTb = [Buf("y0"), Buf("y1")]
            sq = [self.sb(st, [128, TG], BF16, "sq") for _ in range(2)]
            sqb = [Buf("sq0"), Buf("sq1")]
            rstd = self.sb(st, [128, TG], F32, "rstd")
            rstdb = Buf("rstd")
            ss_ps = self.ps(st, [128, TG], F32, "ss")
            ssb = Buf("ss")
            tp = [self.ps(st, [128, 512], F32, "tp") for _ in range(4)]
            tb = [Buf("tp%d" % i) for i in range(4)]
            ot = [self.sb(st, [128, D], F32, "ot") for _ in range(3)]
            otb = [Buf("ot%d" % i) for i in range(3)]
            gcols = [self.gcol[:, 6, fc:fc + 1] for fc in range(8)]
            ident = self.cst("ident")
            k = 0
            n = 0
            for g in range(NG):
                P.load(hs[g % 2][:], hin_v[:, :, g * TG:(g + 1) * TG], reads=[hinb], writes=[hsb[g % 2]])
                if do_norm:
                    self.norm_slab(hs[g % 2], hsb[g % 2], 8, gcols, ss_ps, ssb, sq, sqb, rstd, rstdb, yT[g % 2], yTb[g % 2], 1.0 / D)
                    y, yb = yT[g % 2], yTb[g % 2]
                else:
                    y, yb = hs[g % 2], hsb[g % 2]
                for t in range(4):
                    o_ = ot[n % 3]
                    ob_ = otb[n % 3]
                    n += 1
                    for half in range(2):
                        pb = tp[k % 4]
                        pbb = tb[k % 4]
                        k += 1
                        for j in range(4):
                            fc = half * 4 + j
                            P.mm(lambda e, pb=pb, j=j, fc=fc, t=t, y=y: e.transpose(out=pb[:, j * 128:(j + 1) * 128], in_=y[:, fc, t * 128:(t + 1) * 128], identity=ident),
                                 reads=[yb, self.cb], writes=[pbb])
                        if half == 0:
                            P.act(lambda e, pb=pb, o_=o_: e.copy(out=o_[:, 0:512], in_=pb[:]), reads=[pbb], writes=[ob_])
                        else:
                            P.dve(lambda e, pb=pb, o_=o_: e.tensor_copy(out=o_[:, 512:1024], in_=pb[:]), reads=[pbb], writes=[ob_])
                    r0 = g * TG + t * 128
                    P.load(self.out[r0:r0 + 128, :], o_[:], reads=[ob_], writes=[outb], eng="gpsimd")


def build(nphase=99, final_norm=True, only=None):
    import os
    kb = KB()
    kb.declare()
    P = kb.P
    with ExitStack() as st:
        kb.load_consts(st)
        cur = 0
        if only is None:
            kb.prep_ffn(0)
            kb.pump(10 ** 6)
        kb.transpose_in()
        if only == "mix1":
            kb.declare_mix1()
            kb.inproj1(cur)
            kb.nsa_attn()
            kb.outproj(cur, 1 - cur, kb.I["nsa_w_out"], kb.ONT, kb.m1b["ONT"], 16)
            cur = 1 - cur
        elif only is None:
            if nphase >= 1:
                kb.prep_ffn(1)
                kb.ffn(0, cur, 1 - cur)
                cur = 1 - cur
            if nphase >= 2:
                kb.declare_mix0()
                kb.inproj0(cur)
                kb.mla()
                kb.gla()
                kb.outproj(cur, 1 - cur, kb.I["hy_w_out"], kb.OTm, kb.m0b["OTm"], 8, kb.OTg, kb.m0b["OTg"])
                cur = 1 - cur
            if nphase >= 3:
                kb.prep_ffn(2)
                kb.ffn(1, cur, 1 - cur)
                cur = 1 - cur
                kb.prep_ffn(3)
                kb.ffn(2, cur, 1 - cur)
                cur = 1 - cur
            if nphase >= 4:
                kb.declare_mix1()
                kb.inproj1(cur)
                kb.nsa_attn()
                kb.outproj(cur, 1 - cur, kb.I["nsa_w_out"], kb.ONT, kb.m1b["ONT"], 16)
                cur = 1 - cur
            if nphase >= 5:
                kb.ffn(3, cur, 1 - cur)
                cur = 1 - cur
        kb.pump(10 ** 6)
        kb.final(cur, do_norm=final_norm)
        P.finalize(st)
    return kb.nc


WNAMES = ["ffn_norm", "ffn_w_gate", "ffn_w_up", "ffn_w_down", "mix_norm", "hy_w_in", "mla_q_norm", "mla_w_uq", "mla_kv_norm",
          "mla_w_ukv", "gla_w_a2", "gla_b_a", "gla_out_norm", "hy_w_out", "nsa_w_in", "nsa_pos_k", "nsa_pos_v", "nsa_ck_w1",
          "nsa_ck_w2", "nsa_cv_w1", "nsa_cv_w2", "nsa_w_out", "final_norm"]


def make_in_maps(inputs, ncores=8):
    consts = make_consts()
    cm, ovl = make_cmp_consts()
    shared = {"consts": consts, "cmpmask": cm, "ovl": ovl}
    f32 = lambda a: np.ascontiguousarray(np.asarray(a), dtype=np.float32)
    shared["ffn_norm"] = f32(inputs["ffn_norm"]).reshape(4, D)
    shared["ffn_w_gate"] = f32(inputs["ffn_w_gate"]).reshape(4, D, DFF)
    shared["ffn_w_up"] = f32(inputs["ffn_w_up"]).reshape(4, D, DFF)
    shared["ffn_w_down"] = f32(inputs["ffn_w_down"]).reshape(4, DFF, D)
    shared["mix_norm"] = f32(inputs["mix_norm"])
    for n in ["hy_w_in", "mla_w_uq", "mla_w_ukv", "gla_w_a2", "hy_w_out", "nsa_w_in", "nsa_pos_k", "nsa_pos_v", "nsa_ck_w1",
              "nsa_ck_w2", "nsa_cv_w1", "nsa_cv_w2", "nsa_w_out"]:
        shared[n] = f32(inputs[n])[0]
    for n in ["mla_q_norm", "mla_kv_norm", "gla_b_a", "gla_out_norm"]:
        shared[n] = f32(inputs[n]).reshape(1, -1)
    shared["final_norm"] = f32(inputs["final_norm"]).reshape(1, D)
    x = f32(inputs["x"])
    pos = np.ascontiguousarray(np.asarray(inputs["positions"]), dtype=np.int32)
    maps = []
    for c in range(ncores):
        m = dict(shared)
        m["x"] = x[c]
        m["positions"] = pos[c:c + 1]
        maps.append(m)
    return maps


def kernel(**inputs):
    nc = build()
    maps = make_in_maps(inputs)
    res = run_bass_kernel_spmd(nc, maps, core_ids=list(range(8)))
    return np.stack([r["out"] for r in res.results], axis=0)


class Tl:
    def __init__(self, t, name):
        self.t = t
        self.b = Buf(name)

    def __getitem__(self, k):
        return self.t[k]


def _kb_tile(self, st, shape, dt, name="t"):
    return Tl(self.sb(st, shape, dt, name), name)


def _kb_ptile(self, st, shape=(128, 512), dt=F32, name="p"):
    return Tl(self.ps(st, list(shape), dt, name), name)


def _bl(xs):
    return [x.b if isinstance(x, Tl) else x for x in xs]


def _MM(self, out, lhsT, rhs, start, stop, r, w):
    return self.P.mm(lambda e: e.matmul(out, lhsT=lhsT, rhs=rhs, start=start, stop=stop), reads=_bl(r), writes=_bl(w))


def _PROJ(self, out, pairs, r, w):
    n = len(pairs)
    for i, (l, rh) in enumerate(pairs):
        self.MM(out, l, rh, i == 0, i == n - 1, r, w)


def _TR(self, out, in_, ident, r, w):
    return self.P.mm(lambda e: e.transpose(out=out, in_=in_, identity=ident), reads=_bl(r), writes=_bl(w))


def _ACT(self, out, in_, func, r, w, **kw):
    return self.P.act(lambda e: e.activation(out=out, in_=in_, func=func, **kw), reads=_bl(r), writes=_bl(w))


def _TT(self, out, a, b, op, r, w, eng="vector"):
    return self.P.op(eng, lambda e: e.tensor_tensor(out=out, in0=a, in1=b, op=op), reads=_bl(r), writes=_bl(w))


def _STT(self, out, in0, scalar, in1, op0, op1, r, w):
    return self.P.dve(lambda e: e.scalar_tensor_tensor(out=out, in0=in0, scalar=scalar, in1=in1, op0=op0, op1=op1), reads=_bl(r), writes=_bl(w))


def _TS(self, out, in0, s1, s2, op0, op1, r, w, eng="vector"):
    if s2 is None:
        return self.P.op(eng, lambda e: e.tensor_scalar(out=out, in0=in0, scalar1=s1, scalar2=None, op0=op0), reads=_bl(r), writes=_bl(w))
    return self.P.op(eng, lambda e: e.tensor_scalar(out=out, in0=in0, scalar1=s1, scalar2=s2, op0=op0, op1=op1), reads=_bl(r), writes=_bl(w))


def _CP(self, out, in_, r, w, eng="vector"):
    if eng == "scalar":
        return self.P.act(lambda e: e.copy(out=out, in_=in_), reads=_bl(r), writes=_bl(w))
    return self.P.op(eng, lambda e: e.tensor_copy(out=out, in_=in_), reads=_bl(r), writes=_bl(w))


def _LD(self, out, in_, r, w, eng="sync", **kw):
    return self.P.load(out, in_, reads=_bl(r), writes=_bl(w), eng=eng, **kw)


def _MS(self, ap, val, w, eng="gpsimd"):
    return self.P.op(eng, lambda e: e.memset(ap, val), reads=[], writes=_bl(w))


for _n, _f in [("tile", _kb_tile), ("ptile", _kb_ptile), ("MM", _MM), ("PROJ", _PROJ), ("TR", _TR), ("ACT", _ACT), ("TT", _TT),
               ("STT", _STT), ("TS", _TS), ("CP", _CP), ("LD", _LD), ("MS", _MS)]:
    setattr(KB, _n, _f)


def _norm_ps(self, srcs, gcols, inv_n, ss, sq, rstd, outs, out_b):
    n = len(srcs)
    for i, (ap, tl) in enumerate(srcs):
        q = sq[i % len(sq)]
        self.ACT(q[:], ap, AF.Square, [tl], [q])
        self.MM(ss[:], self.ones_bf[:], q[:], i == 0, i == n - 1, [q, self.cbf], [ss])
    self.ACT(rstd[:], ss[:], AF.Ln, [ss], [rstd], scale=float(inv_n), bias=float(EPS))
    self.ACT(rstd[:], rstd[:], AF.Exp, [rstd], [rstd], scale=-0.5)
    for i, (ap, tl) in enumerate(srcs):
        self.STT(outs[i], ap, gcols[i], rstd[:], ALU.mult, ALU.mult, [tl, rstd] + self.gdeps, [out_b])


KB.norm_ps = _norm_ps


def _rope_tables(self, st, rows, r0, fcol, scol, name, pos_ap=None, S=S):
    C = self.tile(st, [rows, S], F32, name + "C")
    Sg = self.tile(st, [rows, S], F32, name + "S")
    with ExitStack() as st2:
        pi_ = self.tile(st2, [rows, S], I32, "posi")
        t = self.tile(st2, [rows, S], F32, "rt")
        u = self.tile(st2, [rows, S], F32, "ru")
        ti = self.tile(st2, [rows, S], I32, "rti")
        rs = slice(r0, rows)
        fo = CB["freq"][0]
        if pos_ap is None:
            pos_ap = self.I["positions"]
        with self.nc.allow_non_contiguous_dma("positions"):
            self.LD(pi_[rs, :], pos_ap.partition_broadcast(rows - r0).rearrange("p o s -> p (o s)"), [], [pi_], allow_slow_non_contiguous=True)
        self.CP(t[rs, :], pi_[rs, :], [pi_], [t])
        self.TS(t[rs, :], t[rs, :], self.c32[rs, fo + fcol:fo + fcol + 1], None, ALU.mult, None, [t, self.cb], [t])
        for tab, shift, scale in [(Sg, 0.0, self.c32[rs, fo + scol:fo + scol + 1]), (C, 0.25, float(2 * np.pi * (1 - 1e-6)))]:
            if shift:
                self.TS(u[rs, :], t[rs, :], shift, None, ALU.add, None, [t], [u])
            else:
                self.CP(u[rs, :], t[rs, :], [t], [u])
            self.CP(ti[rs, :], u[rs, :], [u], [ti])
            self.CP(tab[rs, :], ti[rs, :], [ti], [tab])
            self.TT(u[rs, :], u[rs, :], tab[rs, :], ALU.subtract, [u, tab], [u])
            self.ACT(tab[rs, :], u[rs, :], AF.Sin, [u, self.cb], [tab], scale=scale)
        self.P.barrier()
    return C, Sg


KB.rope_tables = _rope_tables


def _declare_mix0(self):
    d = self.dram
    self.QT = d("QT", [8, 96, S], BF16)
    self.KT = d("KT", [8, 96, S], BF16)
    self.Vs = d("Vs", [S, 520], BF16)
    self.qintra = d("qintra", [256, S], BF16)
    self.qinter = d("qinter", [256, S], BF16)
    self.kdec = d("kdec", [256, S], BF16)
    self.kdtok = d("kdtok", [S, 256], BF16)
    self.gv = d("gv", [S, 512], BF16)
    self.grs = d("grs", [512, S], BF16)
    self.decd = d("decd", [256, 64], F32)
    self.OTm = d("OTm", [8, 64, S], BF16)
    self.OTg = d("OTg", [4, 128, S], BF16)
    self.m0b = {n: Buf(n) for n in ["QT", "KT", "Vs", "qintra", "qinter", "kdec", "kdtok", "gv", "grs", "decd", "OTm", "OTg"]}


KB.declare_mix0 = _declare_mix0


def _inproj0(self, src):
    P, I = self.P, self.I
    hin_v = self.hT[src].rearrange("(fc p) t -> p fc t", p=128)
    hinb = self.hbuf[src]
    mb = self.m0b
    with ExitStack() as st:
        T = lambda shape, dt, n: self.tile(st, shape, dt, n)
        w_in = T([128, 8, HY_IN], BF16, "w_in")
        for kc in range(8):
            self.LD(w_in[:, kc, :], I["hy_w_in"][kc * 128:(kc + 1) * 128, :], [], [w_in], eng="gpsimd")
        w_uq = T([128, 2, 768], BF16, "w_uq")
        w_uqs = T([128, 2, 768], BF16, "w_uqs")
        uqsrc = I["mla_w_uq"].rearrange("(k p) n -> p k n", p=128)
        self.LD(w_uq[:], uqsrc, [], [w_uq], eng="gpsimd")
        self.LD(w_uqs[:], uqsrc, [], [w_uqs], eng="gpsimd")
        v4 = lambda ap: ap.rearrange("p k (h d) -> p k h d", d=96)
        with self.nc.allow_non_contiguous_dma("small swapped weight blocks"):
            for kc in range(2):
                self.LD(v4(w_uqs[:])[:, kc, :, 64:80], v4(uqsrc)[:, kc, :, 80:96], [], [w_uqs], eng="gpsimd")
                self.LD(v4(w_uqs[:])[:, kc, :, 80:96], v4(uqsrc)[:, kc, :, 64:80], [], [w_uqs], eng="gpsimd")
        w_ukv = T([128, 1024], BF16, "w_ukv")
        self.LD(w_ukv[:], I["mla_w_ukv"], [], [w_ukv], eng="gpsimd")
        wkrs = T([128, 8, 96], BF16, "wkrs")
        insrc = I["hy_w_in"].rearrange("(k p) n -> p k n", p=128)
        with self.nc.allow_non_contiguous_dma("small swapped weight blocks"):
            self.LD(wkrs[:, :, 0:64], insrc[:, :, 320:384], [], [wkrs], eng="gpsimd")
            self.LD(wkrs[:, :, 64:80], insrc[:, :, 400:416], [], [wkrs], eng="gpsimd")
            self.LD(wkrs[:, :, 80:96], insrc[:, :, 384:400], [], [wkrs], eng="gpsimd")
        w_a2 = T([16, 256], BF16, "w_a2")
        self.LD(w_a2[:], I["gla_w_a2"], [], [w_a2], eng="gpsimd")
        cols = T([128, 8], F32, "cols")
        with self.nc.allow_non_contiguous_dma("tiny vectors"):
            self.LD(cols[:, 0:2], I["mla_q_norm"].rearrange("o (k p) -> p (o k)", p=128), [], [cols], allow_slow_non_contiguous=True)
            self.LD(cols[:, 2:3], I["mla_kv_norm"].rearrange("o (k p) -> p (o k)", p=128), [], [cols], allow_slow_non_contiguous=True)
            self.LD(cols[:, 3:5], I["gla_b_a"].rearrange("o (k p) -> p (o k)", p=128), [], [cols], allow_slow_non_contiguous=True)
        self.TS(cols[:, 5:7], cols[:, 3:5], -1.0, None, ALU.mult, None, [cols], [cols])
        C, Sg = self.rope_tables(st, 96, 64, 0, 1, "mla")
        hs = [T([128, 8, TG], F32, "hs") for _ in range(2)]
        uT = T([128, 8, TG], BF16, "uT")
        sq = [T([128, TG], BF16, "sq") for _ in range(2)]
        rstd = T([128, TG], F32, "rstd")
        cqn = T([128, 2, TG], BF16, "cqn")
        ckvn = T([128, TG], BF16, "ckvn")
        qst = [T([96, TG], BF16, "qst") for _ in range(2)]
        t1 = [T([96, TG], F32, "t1") for _ in range(2)]
        t2 = [T([96, TG], F32, "t2") for _ in range(2)]
        kst = T([96, 8, TG], BF16, "kst")
        krot = T([96, TG], F32, "krot")
        vst = T([128, 4, 8, 65], BF16, "vst")
        self.MS(vst[:], 1.0, [vst])
        ga = T([16, TG], BF16, "ga")
        lt = T([128, TG], F32, "lt")
        cs = T([128, TG], F32, "cs")
        dd = T([128, TG], F32, "dd")
        E1 = T([128, TG], F32, "E1")
        E2 = T([128, TG], F32, "E2")
        E3 = T([128, TG], F32, "E3")
        qia = T([128, 2, TG], BF16, "qia")
        qie = T([128, 2, TG], BF16, "qie")
        kde = T([128, 2, TG], BF16, "kde")
        dec = T([128, 2, 64], F32, "dec")
        kdt = T([128, 4, 256], BF16, "kdt")
        gvs = T([128, 4, 512], BF16, "gvs")
        grt = T([128, 4, TG], BF16, "grt")
        ss = self.ptile(st, name="ss")
        A = [self.ptile(st, name="A") for _ in range(2)]
        Bp = [self.ptile(st, name="B") for _ in range(2)]
        Tp = [self.ptile(st, name="T") for _ in range(2)]
        Tb = self.ptile(st, [128, 1024], BF16, name="Tb")
        self.gdeps = [self.gb, cols.b]
        gm = [self.gcol[:, 4, fc:fc + 1] for fc in range(8)]
        scanmask = self.cst("scanmask")
        for g in range(NG):
            gs = slice(g * TG, (g + 1) * TG)
            h_ = hs[g % 2]
            self.LD(h_[:], hin_v[:, :, gs], [hinb], [h_])
            self.norm_ps([(h_[:, fc, :], h_) for fc in range(8)], gm, 1.0 / D, ss, sq, rstd, [uT[:, fc, :] for fc in range(8)], uT)
            for ch in range(2):
                self.PROJ(A[ch][:], [(w_in[:, kc, ch * 128:(ch + 1) * 128], uT[:, kc, :]) for kc in range(8)], [w_in, uT], [A[ch]])
            self.norm_ps([(A[ch][:], A[ch]) for ch in range(2)], [cols[:, ch:ch + 1] for ch in range(2)], 1.0 / 256, ss, sq, rstd,
                         [cqn[:, ch, :] for ch in range(2)], cqn)
            for h in range(8):
                a, b = A[h % 2], Bp[h % 2]
                q_, x1, x2 = qst[h % 2], t1[h % 2], t2[h % 2]
                self.PROJ(a[0:96, :], [(w_uq[:, kc, h * 96:(h + 1) * 96], cqn[:, kc, :]) for kc in range(2)], [w_uq, cqn], [a])
                self.PROJ(b[0:96, :], [(w_uqs[:, kc, h * 96:(h + 1) * 96], cqn[:, kc, :]) for kc in range(2)], [w_uqs, cqn], [b])
                self.CP(q_[0:64, :], a[0:64, :], [a], [q_], eng="scalar")
                self.TT(x1[64:96, :], a[64:96, :], C[64:96, gs], ALU.mult, [a, C], [x1])
                self.TT(x2[64:96, :], b[64:96, :], Sg[64:96, gs], ALU.mult, [b, Sg], [x2])
                self.TT(q_[64:96, :], x1[64:96, :], x2[64:96, :], ALU.add, [x1, x2], [q_], eng="gpsimd")
                self.LD(self.QT[h, :, gs], q_[:], [q_], [mb["QT"]], eng="gpsimd")
            self.PROJ(A[0][:], [(w_in[:, kc, 256:384], uT[:, kc, :]) for kc in range(8)], [w_in, uT], [A[0]])
            self.norm_ps([(A[0][:], A[0])], [cols[:, 2:3]], 1.0 / 128, ss, sq, rstd, [ckvn[:]], ckvn)
            for h in range(8):
                a = A[h % 2]
                self.MM(a[0:64, :], w_ukv[:, h * 128:h * 128 + 64], ckvn[:], True, True, [w_ukv, ckvn], [a])
                self.CP(kst[0:64, h, :], a[0:64, :], [a], [kst], eng=("scalar" if h % 2 else "vector"))
            self.PROJ(A[0][0:96, :], [(w_in[:, kc, 320:416], uT[:, kc, :]) for kc in range(8)], [w_in, uT], [A[0]])
            self.PROJ(Bp[0][0:96, :], [(wkrs[:, kc, :], uT[:, kc, :]) for kc in range(8)], [wkrs, uT], [Bp[0]])
            self.TT(t1[0][64:96, :], A[0][64:96, :], C[64:96, gs], ALU.mult, [A[0], C], [t1[0]])
            self.TT(t2[0][64:96, :], Bp[0][64:96, :], Sg[64:96, gs], ALU.mult, [Bp[0], Sg], [t2[0]])
            self.TT(krot[64:96, :], t1[0][64:96, :], t2[0][64:96, :], ALU.add, [t1[0], t2[0]], [krot], eng="gpsimd")
            self.CP(kst[64:96, :, :], krot[64:96, :].unsqueeze(1).broadcast_to([32, 8, TG]), [krot], [kst], eng="gpsimd")
            self.LD(self.KT[:, :, gs].rearrange("h r t -> r h t"), kst[:], [kst], [mb["KT"]], eng="gpsimd")
            wv = w_ukv[:].rearrange("p (h t d) -> p h t d", t=2, d=64)[:, :, 1, :]
            for t in range(4):
                tp = Tp[t % 2]
                self.MM(tp[:].rearrange("p (h d) -> p h d", d=64), ckvn[:, t * 128:(t + 1) * 128], wv, True, True, [ckvn, w_ukv], [tp])
                self.CP(vst[:, t, :, 0:64], tp[:].rearrange("p (h d) -> p h d", d=64), [tp], [vst], eng=("scalar" if t % 2 else "vector"))
            self.LD(self.Vs[gs, :].rearrange("(t p) f -> p t f", p=128), vst[:].rearrange("p t h d -> p t (h d)"), [vst], [mb["Vs"]], eng="gpsimd")
            for ch in range(2):
                self.PROJ(A[ch][:], [(w_in[:, kc, 416 + ch * 128:416 + (ch + 1) * 128], uT[:, kc, :]) for kc in range(8)], [w_in, uT], [A[ch]])
                self.PROJ(Bp[ch][:], [(w_in[:, kc, 672 + ch * 128:672 + (ch + 1) * 128], uT[:, kc, :]) for kc in range(8)], [w_in, uT], [Bp[ch]])
            self.PROJ(Tp[0][0:16, :], [(w_in[:, kc, 1440:1456], uT[:, kc, :]) for kc in range(8)], [w_in, uT], [Tp[0]])
            self.CP(ga[:], Tp[0][0:16, :], [Tp[0]], [ga])
            for ch in range(2):
                tp = Tp[1]
                self.MM(tp[:], w_a2[0:16, ch * 128:(ch + 1) * 128], ga[0:16, :], True, True, [w_a2, ga], [tp])
                self.ACT(lt[:], tp[:], AF.Exp, [tp, cols], [lt], scale=-1.0, bias=cols[:, 5 + ch:6 + ch])
                self.ACT(lt[:], lt[:], AF.Ln, [lt], [lt], scale=1.0, bias=1.0)
                self.P.dve(lambda e: e.tensor_tensor_scan(out=cs[:], data0=scanmask, data1=lt[:], initial=0.0, op0=ALU.mult, op1=ALU.add),
                           reads=[lt.b, self.cb], writes=[cs.b])
                cs3 = cs[:].rearrange("p (c k) -> p c k", k=64)
                self.TT(dd[:].rearrange("p (c k) -> p c k", k=64), cs3, cs3[:, :, 63:64].broadcast_to([128, 8, 64]), ALU.subtract, [cs], [dd])
                self.ACT(E1[:], dd[:], AF.Exp, [dd], [E1], scale=-1.0 / 16)
                self.ACT(E2[:], dd[:], AF.Exp, [dd], [E2], scale=1.0 / 16)
                self.ACT(E3[:], cs[:], AF.Exp, [cs], [E3], scale=-1.0 / 16)
                self.STT(qia[:, ch, :], A[ch][:], 0.125, E1[:], ALU.mult, ALU.mult, [A[ch], E1], [qia])
                self.STT(qie[:, ch, :], A[ch][:], 0.125, E3[:], ALU.mult, ALU.mult, [A[ch], E3], [qie])
                self.TT(kde[:, ch, :], Bp[ch][:], E2[:], ALU.mult, [Bp[ch], E2], [kde])
                self.CP(dec[:, ch, g * 8:(g + 1) * 8], E3[:].rearrange("p (c k) -> p c k", k=64)[:, :, 63], [E3], [dec], eng="gpsimd")
            fm = lambda dr: dr.rearrange("(c p) t -> p c t", p=128)[:, :, gs]
            self.LD(fm(self.qintra), qia[:], [qia], [mb["qintra"]], eng="gpsimd")
            self.LD(fm(self.qinter), qie[:], [qie], [mb["qinter"]], eng="gpsimd")
            self.LD(fm(self.kdec), kde[:], [kde], [mb["kdec"]], eng="gpsimd")
            for t in range(4):
                for ch in range(2):
                    self.TR(Tb[:, (t % 4) * 256 + ch * 128:(t % 4) * 256 + (ch + 1) * 128], kde[:, ch, t * 128:(t + 1) * 128], self.ident_bf[:],
                            [kde, self.cbf], [Tb])
            self.CP(kdt[:].rearrange("p t f -> p (t f)"), Tb[:], [Tb], [kdt])
            self.LD(self.kdtok[gs, :].rearrange("(t p) f -> p t f", p=128), kdt[:], [kdt], [mb["kdtok"]], eng="gpsimd")
            for t in range(4):
                tp = Tp[t % 2]
                self.PROJ(tp[:], [(uT[:, kc, t * 128:(t + 1) * 128], w_in[:, kc, 928:1440]) for kc in range(8)], [w_in, uT], [tp])
                self.CP(gvs[:, t, :], tp[:], [tp], [gvs], eng=("scalar" if t % 2 else "vector"))
            self.LD(self.gv[gs, :].rearrange("(t p) f -> p t f", p=128), gvs[:], [gvs], [mb["gv"]], eng="gpsimd")
            for hh in range(4):
                a = A[hh % 2]
                self.PROJ(a[:], [(w_in[:, kc, 1456 + hh * 128:1456 + (hh + 1) * 128], uT[:, kc, :]) for kc in range(8)], [w_in, uT], [a])
                self.ACT(grt[:, hh, :], a[:], AF.Silu, [a], [grt])
            self.LD(self.grs.rearrange("(c p) t -> p c t", p=128)[:, :, gs], grt[:], [grt], [mb["grs"]], eng="gpsimd")
        self.LD(self.decd.rearrange("(c p) n -> p c n", p=128), dec[:], [dec], [mb["decd"]], eng="gpsimd")
    self.gdeps = []
    P.barrier()


KB.inproj0 = _inproj0


class U:
    __slots__ = ("A", "B", "C", "later")

    def __init__(self, A=None, B=None, C=None, later=None):
        self.A, self.B, self.C, self.later = A, B, C, later


def run_units(units, look):
    n = len(units)
    sched = {}
    for i in range(min(look, n)):
        if units[i].A:
            units[i].A()
    for i in range(n):
        if i + look < n and units[i + look].A:
            units[i + look].A()
        if units[i].B:
            units[i].B()
        if units[i].C:
            units[i].C()
        for (dl, fn) in (units[i].later or []):
            sched.setdefault(i + dl, []).append(fn)
        for fn in sched.pop(i, []):
            fn()
    for k in sorted(sched):
        for fn in sched[k]:
            fn()


def _attn_units(self, units, o, sp_list, pt_list, cnt, Kt, Qt, Vfn, qg, scale, ktiles, dk, pre=None):
    nk = len(ktiles)
    for i, kt in enumerate(ktiles):
        d = kt - 4 * qg
        sp = sp_list[cnt[0] % len(sp_list)]
        pt = pt_list[cnt[0] % len(pt_list)]
        cnt[0] += 1
        kc = slice(kt * 128, (kt + 1) * 128)
        q0 = qg * TG
        c0 = max(d, 0) * 128

        def A(sp=sp, kc=kc, d=d, c0=c0, q0=q0, pre=(pre if i == 0 else None)):
            if pre is not None:
                pre()
            if d < 0:
                self.MM(sp[:], Kt[0:dk, kc], Qt[0:dk, q0:q0 + TG], True, True, [Kt, Qt], [sp])
            else:
                self.MM(sp[:, c0:c0 + 128], Kt[0:dk, kc], Qt[0:dk, q0 + c0:q0 + c0 + 128], True, False, [Kt, Qt], [sp])
                self.MM(sp[:, c0:c0 + 128], self.ident_bf[:], self.cpen_bf[:], False, True, [self.cbf], [sp])
                if c0 + 128 < TG:
                    self.MM(sp[:, c0 + 128:TG], Kt[0:dk, kc], Qt[0:dk, q0 + c0 + 128:q0 + TG], True, True, [Kt, Qt], [sp])

        def B(sp=sp, pt=pt, c0=c0):
            self.ACT(pt[:, c0:TG], sp[:, c0:TG], AF.Exp, [sp], [pt], scale=scale)

        def C(pt=pt, c0=c0, kt=kt, i=i):
            self.MM(o[0:65, c0:TG], Vfn(kt), pt[:, c0:TG], i == 0, i == nk - 1, [pt, self.vdep], [o])
        units.append(U(A, B, C))


KB.attn_units = _attn_units


def _mla(self):
    P = self.P
    mb = self.m0b
    with ExitStack() as st:
        T = lambda shape, dt, n: self.tile(st, shape, dt, n)
        Vall = T([128, 32, 520], BF16, "Vall")
        for q4 in range(4):
            self.LD(Vall[:, q4 * 8:(q4 + 1) * 8, :], self.Vs[q4 * 1024:(q4 + 1) * 1024, :].rearrange("(n p) f -> p n f", p=128), [mb["Vs"]], [Vall])
        self.vdep = Vall
        KTh = [T([96, S], BF16, "KTh") for _ in range(2)]
        QTh = [T([96, S], BF16, "QTh") for _ in range(2)]
        PT = [T([128, TG], BF16, "PT") for _ in range(3)]
        rr2 = [T([65, TG], F32, "rr") for _ in range(2)]
        fb2 = [T([65, TG], BF16, "fb") for _ in range(2)]
        bcs2 = [T([64, TG], F32, "bcs") for _ in range(2)]
        ost = [T([64, TG], BF16, "ost") for _ in range(2)]
        Sp = [self.ptile(st, name="S") for _ in range(3)]
        Op = [self.ptile(st, name="O") for _ in range(3)]
        bc2 = [self.ptile(st, name="bc") for _ in range(2)]
        ones32 = self.cst("ones")
        cnt = [0]
        k = 0
        units = []

        def loader(h):
            def f():
                self.LD(KTh[h % 2][:], self.KT[h], [mb["KT"]], [KTh[h % 2]])
                self.LD(QTh[h % 2][:], self.QT[h], [mb["QT"]], [QTh[h % 2]])
            return f
        loader(0)()
        for h in range(8):
            kt_, qt_ = KTh[h % 2], QTh[h % 2]
            for qg in range(NG):
                o = Op[k % 3]
                pre = loader(h + 1) if (qg == 0 and h + 1 < 8) else None
                self.attn_units(units, o, Sp, PT, cnt, kt_, qt_, lambda kt, h=h: Vall[:, kt, h * 65:(h + 1) * 65], qg, 96 ** -0.5,
                                list(range(4 * qg + 4)), 96, pre=pre)

                rr_, fb_, bc_, bs_ = rr2[k % 2], fb2[k % 2], bc2[k % 2], bcs2[k % 2]

                def f0(o=o, rr_=rr_, fb_=fb_):
                    self.ACT(rr_[64:65, :], o[64:65, :], AF.Ln, [o], [rr_])
                    self.ACT(fb_[64:65, :], rr_[64:65, :], AF.Exp, [rr_], [fb_], scale=-1.0)

                def f1(fb_=fb_, bc_=bc_, bs_=bs_):
                    self.MM(bc_[0:64, :], self.ones_bf[64:65, 0:64], fb_[64:65, :], True, True, [fb_, self.cbf], [bc_])
                    self.CP(bs_[:], bc_[0:64, :], [bc_], [bs_], eng="scalar")

                def f2(o=o, h=h, qg=qg, os_=ost[k % 2], bs_=bs_):
                    self.TT(os_[:], o[0:64, :], bs_[:], ALU.mult, [o, bs_], [os_])
                    self.LD(self.OTm[h, :, qg * TG:(qg + 1) * TG], os_[:], [os_], [mb["OTm"]], eng="gpsimd")
                units.append(U(None, None, None, later=[(1, f0), (3, f1), (4, f2)]))
                k += 1
        run_units(units, 2)
    P.barrier()


KB.mla = _mla


def _gla(self):
    P = self.P
    mb = self.m0b
    with ExitStack() as st:
        T = lambda shape, dt, n: self.tile(st, shape, dt, n)
        hv = lambda dr, gs: dr.rearrange("(h d) t -> d h t", d=64)[:, :, gs]
        qia = [T([64, 4, TG], BF16, "qia") for _ in range(2)]
        qie = [T([64, 4, TG], BF16, "qie") for _ in range(2)]
        kde = [T([64, 4, TG], BF16, "kde") for _ in range(2)]
        vv = [T([64, 8, 512], BF16, "vv") for _ in range(2)]
        kdt = [T([64, 8, 256], BF16, "kdt") for _ in range(2)]
        grs = [T([128, 4, TG], BF16, "grs") for _ in range(2)]
        dec = T([64, 4, 64], F32, "dec")
        self.LD(dec[:], self.decd.rearrange("(h d) n -> d h n", d=64), [mb["decd"]], [dec])
        onc = T([128, 1], F32, "onc")
        with self.nc.allow_non_contiguous_dma("tiny"):
            self.LD(onc[:], self.I["gla_out_norm"].rearrange("o p -> p o"), [], [onc], allow_slow_non_contiguous=True)
        St = T([64, 4, 128], F32, "St")
        Sbf = T([64, 4, 128], BF16, "Sbf")
        self.MS(St[:], 0.0, [St])
        self.MS(Sbf[:], 0.0, [Sbf])
        ats = [T([64, 256], BF16, "ats") for _ in range(2)]
        sq = [T([128, TG], BF16, "sq") for _ in range(2)]
        rstd = T([128, TG], F32, "rstd")
        on = [T([128, TG], F32, "on") for _ in range(2)]
        ost = [T([128, TG], BF16, "ost") for _ in range(2)]
        Op = [self.ptile(st, name="O") for _ in range(4)]
        at_t = self.ps(st, [64, 512], F32, "at")
        at = [Tl(at_t, "at0"), Tl(at_t, "at1")]
        kv = [self.ptile(st, [64, 512], F32, name="kv") for _ in range(2)]
        ss = self.ptile(st, name="ss")
        tri = self.cst("tri", rows=64)
        self.gdeps = [onc.b]
        for g in range(NG):
            gs = slice(g * TG, (g + 1) * TG)
            b = g % 2
            self.LD(qia[b][:], hv(self.qintra, gs), [mb["qintra"]], [qia[b]])
            self.LD(qie[b][:], hv(self.qinter, gs), [mb["qinter"]], [qie[b]])
            self.LD(kde[b][:], hv(self.kdec, gs), [mb["kdec"]], [kde[b]])
            self.LD(vv[b][:], self.gv[gs, :].rearrange("(c p) f -> p c f", p=64), [mb["gv"]], [vv[b]])
            self.LD(kdt[b][:], self.kdtok[gs, :].rearrange("(c p) f -> p c f", p=64), [mb["kdtok"]], [kdt[b]])
            self.LD(grs[b][:], self.grs.rearrange("(c p) t -> p c t", p=128)[:, :, gs], [mb["grs"]], [grs[b]])
            for c in range(8):
                n = g * 8 + c
                cs_ = slice(c * 64, (c + 1) * 64)
                a_ = at[c % 2]
                ao = (c % 2) * 256
                for h in range(4):
                    self.MM(a_[0:64, ao + h * 64:ao + (h + 1) * 64], kde[b][:, h, cs_], qia[b][:, h, cs_], True, True, [kde[b], qia[b]], [a_])
                as_ = ats[c % 2]
                self.TT(as_[:].rearrange("p (h i) -> p h i", i=64), a_[0:64, ao:ao + 256].rearrange("p (h i) -> p h i", i=64),
                        tri.unsqueeze(1).broadcast_to([64, 4, 64]), ALU.mult, [a_, self.cb], [as_])
                for h in range(4):
                    self.MM(Op[h][:, cs_], vv[b][:, c, h * 128:(h + 1) * 128], as_[:, h * 64:(h + 1) * 64], True, False, [vv[b], as_], [Op[h]])
                    self.MM(Op[h][:, cs_], Sbf[:, h, :], qie[b][:, h, cs_], False, True, [Sbf, qie[b]], [Op[h]])
                kv_ = kv[c % 2]
                for h in range(4):
                    self.MM(kv_[0:64, h * 128:(h + 1) * 128], kdt[b][:, c, h * 64:(h + 1) * 64], vv[b][:, c, h * 128:(h + 1) * 128], True, True,
                            [kdt[b], vv[b]], [kv_])
                self.TT(St[:], St[:], dec[:, :, n:n + 1].broadcast_to([64, 4, 128]), ALU.mult, [St, dec], [St])
                self.TT(St[:].rearrange("p h v -> p (h v)"), St[:].rearrange("p h v -> p (h v)"), kv_[0:64, :], ALU.add, [St, kv_], [St])
                self.CP(Sbf[:], St[:], [St], [Sbf], eng="scalar")
            for h in range(4):
                o = Op[h]
                self.norm_ps([(o[:], o)], [onc[:, 0:1]], 1.0 / 128, ss, sq, rstd, [on[h % 2][:]], on[h % 2])
                os_ = ost[h % 2]
                self.TT(os_[:], on[h % 2][:], grs[b][:, h, :], ALU.mult, [on[h % 2], grs[b]], [os_], eng="gpsimd")
                self.LD(self.OTg[h, :, gs], os_[:], [os_], [mb["OTg"]], eng="gpsimd")
    self.gdeps = []
    P.barrier()


KB.gla = _gla


def _outproj(self, src, dst, w_src, otm_d, otm_b, n_h64, otg_d=None, otg_b=None):
    P = self.P
    hin_v = self.hT[src].rearrange("(fc p) t -> p fc t", p=128)
    hout_v = self.hT[dst].rearrange("(fc p) t -> p fc t", p=128)
    with ExitStack() as st:
        T = lambda shape, dt, n: self.tile(st, shape, dt, n)
        wm = T([64, n_h64, D], BF16, "wm")
        half = n_h64 // 2
        for i in range(2):
            self.LD(wm[:, i * half:(i + 1) * half, :], w_src[i * half * 64:(i + 1) * half * 64, :].rearrange("(h r) n -> r h n", r=64), [], [wm], eng="gpsimd")
        ng = 0
        if otg_d is not None:
            ng = 4
            wg = T([128, 4, D], BF16, "wg")
            self.LD(wg[:], w_src[n_h64 * 64:, :].rearrange("(c p) n -> p c n", p=128), [], [wg], eng="gpsimd")
        hs = [T([128, 8, TG], F32, "hs") for _ in range(2)]
        om = [T([64, n_h64, TG], BF16, "om") for _ in range(2)]
        og = [T([128, 4, TG], BF16, "og") for _ in range(2)] if ng else None
        ho = [T([128, TG], F32, "ho") for _ in range(3)]
        Op = [self.ptile(st, name="O") for _ in range(2)]
        k = 0
        for g in range(NG):
            gs = slice(g * TG, (g + 1) * TG)
            b = g % 2
            self.LD(hs[b][:], hin_v[:, :, gs], [self.hbuf[src]], [hs[b]])
            self.LD(om[b][:], otm_d[:, :, gs].rearrange("h r t -> r h t"), [otm_b], [om[b]])
            if ng:
                self.LD(og[b][:], otg_d[:, :, gs].rearrange("h r t -> r h t"), [otg_b], [og[b]])
            for fc in range(8):
                o = Op[fc % 2]
                fcs = slice(fc * 128, (fc + 1) * 128)
                pairs = [(wm[:, h, fcs], om[b][:, h, :]) for h in range(n_h64)]
                deps = [wm, om[b]]
                if ng:
                    pairs += [(wg[:, c, fcs], og[b][:, c, :]) for c in range(4)]
                    deps += [wg, og[b]]
                self.PROJ(o[:], pairs, deps, [o])
                h_ = ho[k % 3]
                k += 1
                self.TT(h_[:], o[:], hs[b][:, fc, :], ALU.add, [o, hs[b]], [h_])
                self.LD(hout_v[:, fc, gs], h_[:], [h_], [self.hbuf[dst]], eng="gpsimd")
                self.pump(3)
    P.barrier()


KB.outproj = _outproj


def _declare_mix1(self):
    d = self.dram
    self.QN = d("QN", [1024, S], BF16)
    self.KSd = d("KSd", [256, S], BF16)
    self.KWd = d("KWd", [256, S], BF16)
    self.KCd = d("KCd", [256, S], BF16)
    self.VCd = d("VCd", [256, S], BF16)
    self.VSW = d("VSW", [S, 520], BF16)
    self.GT = d("GT", [48, S], F32)
    self.ONT = d("ONT", [16, 64, S], BF16)
    self.m1b = {n: Buf(n) for n in ["QN", "KSd", "KWd", "KCd", "VCd", "VSW", "GT", "ONT"]}


KB.declare_mix1 = _declare_mix1


def _inproj1(self, src):
    P, I = self.P, self.I
    hin_v = self.hT[src].rearrange("(fc p) t -> p fc t", p=128)
    hinb = self.hbuf[src]
    mb = self.m1b
    with ExitStack() as st:
        T = lambda shape, dt, n: self.tile(st, shape, dt, n)
        w_in = T([128, 8, NSA_IN], BF16, "w_in")
        w_sw = T([128, 8, 1536], BF16, "w_sw")
        insrc = I["nsa_w_in"].rearrange("(k p) n -> p k n", p=128)
        with self.nc.allow_non_contiguous_dma("swapped rope halves"):
            for kc in range(8):
                self.LD(w_in[:, kc, :], I["nsa_w_in"][kc * 128:(kc + 1) * 128, :], [], [w_in], eng="gpsimd")
                for (d0, s0, nb) in [(0, 0, 16), (1024, 1536, 4), (1280, 2048, 4)]:
                    dv = w_sw[:, kc, d0:d0 + nb * 64].rearrange("p (b t e) -> p b t e", t=2, e=32)
                    sv = insrc[:, kc, s0:s0 + nb * 64].rearrange("p (b t e) -> p b t e", t=2, e=32)
                    self.LD(dv[:, :, 0, :], sv[:, :, 1, :], [], [w_sw], eng="gpsimd")
                    self.LD(dv[:, :, 1, :], sv[:, :, 0, :], [], [w_sw], eng="gpsimd")
        C, Sg = self.rope_tables(st, 128, 0, 2, 3, "nsa")
        self.nsaC, self.nsaS = C, Sg
        hs = [T([128, 8, TG], F32, "hs") for _ in range(2)]
        uT = T([128, 8, TG], BF16, "uT")
        sq = [T([128, TG], BF16, "sq") for _ in range(2)]
        rstd = T([128, TG], F32, "rstd")
        t1 = [T([128, TG], F32, "t1") for _ in range(2)]
        t2 = [T([128, TG], F32, "t2") for _ in range(2)]
        qst = [T([128, TG], BF16, "qst") for _ in range(3)]
        vst = T([128, 4, 8, 65], BF16, "vst")
        self.MS(vst[:], 1.0, [vst])
        gts = T([48, TG], F32, "gts")
        ss = self.ptile(st, name="ss")
        A = [self.ptile(st, name="A") for _ in range(2)]
        Bp = [self.ptile(st, name="B") for _ in range(2)]
        Tp = [self.ptile(st, name="T") for _ in range(2)]
        self.gdeps = [self.gb]
        gm = [self.gcol[:, 5, fc:fc + 1] for fc in range(8)]
        k = 0
        for g in range(NG):
            gs = slice(g * TG, (g + 1) * TG)
            h_ = hs[g % 2]
            self.LD(h_[:], hin_v[:, :, gs], [hinb], [h_])
            self.norm_ps([(h_[:, fc, :], h_) for fc in range(8)], gm, 1.0 / D, ss, sq, rstd, [uT[:, fc, :] for fc in range(8)], uT)
            jobs = [(c * 128, c * 128, self.QN, c, "QN") for c in range(8)]
            jobs += [(1536 + c * 128, 1024 + c * 128, self.KSd, c, "KSd") for c in range(2)]
            jobs += [(2048 + c * 128, 1280 + c * 128, self.KWd, c, "KWd") for c in range(2)]
            for (ca, cb_, dst, c, nm) in jobs:
                a, b = A[k % 2], Bp[k % 2]
                x1, x2, q_ = t1[k % 2], t2[k % 2], qst[k % 3]
                k += 1
                self.PROJ(a[:], [(w_in[:, kc, ca:ca + 128], uT[:, kc, :]) for kc in range(8)], [w_in, uT], [a])
                self.PROJ(b[:], [(w_sw[:, kc, cb_:cb_ + 128], uT[:, kc, :]) for kc in range(8)], [w_sw, uT], [b])
                self.TT(x1[:], a[:], C[:, gs], ALU.mult, [a, C], [x1])
                self.TT(x2[:], b[:], Sg[:, gs], ALU.mult, [b, Sg], [x2])
                self.TT(q_[:], x1[:], x2[:], ALU.add, [x1, x2], [q_], eng="gpsimd")
                self.LD(dst[c * 128:(c + 1) * 128, gs], q_[:], [q_], [mb[nm]], eng="gpsimd")
            for (ca, dst, c, nm) in [(1024, self.KCd, 0, "KCd"), (1152, self.KCd, 1, "KCd"), (1280, self.VCd, 0, "VCd"), (1408, self.VCd, 1, "VCd")]:
                a = A[k % 2]
                q_ = qst[k % 3]
                k += 1
                self.PROJ(a[:], [(w_in[:, kc, ca:ca + 128], uT[:, kc, :]) for kc in range(8)], [w_in, uT], [a])
                self.CP(q_[:], a[:], [a], [q_], eng="scalar")
                self.LD(dst[c * 128:(c + 1) * 128, gs], q_[:], [q_], [mb[nm]], eng="gpsimd")
            for t in range(4):
                tp = Tp[t % 2]
                self.PROJ(tp[:, 0:256], [(uT[:, kc, t * 128:(t + 1) * 128], w_in[:, kc, 1792:2048]) for kc in range(8)], [w_in, uT], [tp])
                self.PROJ(tp[:, 256:512], [(uT[:, kc, t * 128:(t + 1) * 128], w_in[:, kc, 2304:2560]) for kc in range(8)], [w_in, uT], [tp])
                self.CP(vst[:, t, :, 0:64], tp[:].rearrange("p (h d) -> p h d", d=64), [tp], [vst], eng=("scalar" if t % 2 else "vector"))
            self.LD(self.VSW[gs, :].rearrange("(t p) f -> p t f", p=128), vst[:].rearrange("p t h d -> p t (h d)"), [vst], [mb["VSW"]], eng="gpsimd")
            self.PROJ(A[0][0:48, :], [(w_in[:, kc, 2560:2608], uT[:, kc, :]) for kc in range(8)], [w_in, uT], [A[0]])
            self.ACT(gts[:], A[0][0:48, :], AF.Sigmoid, [A[0]], [gts])
            self.LD(self.GT[:, gs], gts[:], [gts], [mb["GT"]], eng="gpsimd")
    self.gdeps = []
    P.barrier()


KB.inproj1 = _inproj1


def _nsa_attn(self):
    P, I = self.P, self.I
    mb = self.m1b
    SC = 64 ** -0.5
    with ExitStack() as st:
        T = lambda shape, dt, n: self.tile(st, shape, dt, n)
        VSW = T([128, 32, 520], BF16, "VSW")
        for q4 in range(4):
            self.LD(VSW[:, q4 * 8:(q4 + 1) * 8, :], self.VSW[q4 * 1024:(q4 + 1) * 1024, :].rearrange("(n p) f -> p n f", p=128), [mb["VSW"]], [VSW])
        self.vdep = VSW
        cmpm = T([128, 2, S], BF16, "cmpm")
        for i in range(2):
            self.LD(cmpm[:, i, :], I["cmpmask"][i * 128:(i + 1) * 128, :], [], [cmpm], eng="gpsimd")
        ovl = T([128, 2, 65], BF16, "ovl")
        self.LD(ovl[:], I["ovl"].rearrange("(i p) f -> p i f", p=128), [], [ovl], eng="gpsimd")
        eo = CB["E"][0]
        KCMP = T([64, 4, 256], BF16, "KCMP")
        VCMP = T([128, 4, 2, 65], BF16, "VCMP")
        self.MS(KCMP[:], 0.0, [KCMP])
        self.MS(VCMP[:], 0.0, [VCMP])
        Sp = [self.ptile(st, name="S") for _ in range(3)]
        Oc = self.ptile(st, name="Oc")
        Os = self.ptile(st, name="Os")
        Ow = self.ptile(st, name="Ow")
        imp = Os
        M1 = self.ptile(st, name="M1")
        M2 = self.ptile(st, name="M2")
        with ExitStack() as st2:
            T2 = lambda shape, dt, n: self.tile(st2, shape, dt, n)
            Cc, Sc = self.rope_tables(st2, 64, 0, 2, 3, "cmp", pos_ap=I["positions"][0:1, 31:S:16], S=255)
            w1 = [T2([64, 32, 128], BF16, "w1") for _ in range(2)]
            w2 = [T2([128, 64], BF16, "w2") for _ in range(2)]
            w2s = T2([128, 64], BF16, "w2s")
            posT = [T2([64, 32], BF16, "posT") for _ in range(2)]
            with self.nc.allow_non_contiguous_dma("small"):
                for i, (a, b_, pp) in enumerate([("nsa_ck_w1", "nsa_ck_w2", "nsa_pos_k"), ("nsa_cv_w1", "nsa_cv_w2", "nsa_pos_v")]):
                    self.LD(w1[i][:], I[a].rearrange("(l d) n -> d l n", d=64), [], [w1[i]], eng="gpsimd")
                    self.LD(w2[i][:], I[b_], [], [w2[i]], eng="gpsimd")
                    self.LD(posT[i][:], I[pp].rearrange("l d -> d l"), [], [posT[i]], eng="gpsimd", allow_slow_non_contiguous=True)
                self.LD(w2s[:, 0:32], I["nsa_ck_w2"][:, 32:64], [], [w2s], eng="gpsimd")
                self.LD(w2s[:, 32:64], I["nsa_ck_w2"][:, 0:32], [], [w2s], eng="gpsimd")
            cb_ = T2([128, 2], F32, "cbias")
            for i in range(2):
                for l in range(32):
                    self.MM(M1[:, i:i + 1], w1[i][:, l, :], posT[i][:, l:l + 1], l == 0, l == 31, [w1[i], posT[i]], [M1])
            self.CP(cb_[:], M1[:, 0:2], [M1], [cb_])
            src = [T2([64, S], BF16, "csrc") for _ in range(2)]
            hid = [T2([128, 256], BF16, "hid") for _ in range(2)]
            x1 = T2([64, 256], F32, "x1")
            x2 = T2([64, 256], F32, "x2")
            for i in range(2):
                self.MS(hid[i][:], 0.0, [hid[i]])
            k = 0
            for g in range(4):
                for i, (dsrc, nm) in enumerate([(self.KCd, "KCd"), (self.VCd, "VCd")]):
                    s_ = src[k % 2]
                    hd = hid[k % 2]
                    ps_ = Sp[k % 2]
                    k += 1
                    self.LD(s_[:], dsrc[g * 64:(g + 1) * 64, :], [mb[nm]], [s_])
                    for l in range(32):
                        self.MM(ps_[:, 0:255], w1[i][:, l, :], s_[:, l:l + 16 * 254 + 1:16], l == 0, l == 31, [w1[i], s_], [ps_])
                    self.ACT(hd[:, 0:255], ps_[:, 0:255], AF.Silu, [ps_, cb_], [hd], bias=cb_[:, i:i + 1], scale=1.0)
                    if i == 0:
                        self.MM(M1[0:64, 0:255], w2[0][:], hd[:, 0:255], True, True, [w2[0], hd], [M1])
                        self.MM(M2[0:64, 0:255], w2s[:], hd[:, 0:255], True, True, [w2s, hd], [M2])
                        self.TT(x1[:, 0:255], M1[0:64, 0:255], Cc[0:64, :], ALU.mult, [M1, Cc], [x1])
                        self.TT(x2[:, 0:255], M2[0:64, 0:255], Sc[0:64, :], ALU.mult, [M2, Sc], [x2])
                        self.TT(KCMP[:, g, 0:255], x1[:, 0:255], x2[:, 0:255], ALU.add, [x1, x2], [KCMP], eng="gpsimd")
                    else:
                        self.MM(M1[:, 0:64], hd[:, 0:128], w2[1][:], True, True, [w2[1], hd], [M1])
                        self.MM(M1[0:127, 64:128], hd[:, 128:255], w2[1][:], True, True, [w2[1], hd], [M1])
                        self.CP(VCMP[:, g, 0, 0:64], M1[:, 0:64], [M1], [VCMP])
                        self.CP(VCMP[0:127, g, 1, 0:64], M1[0:127, 64:128], [M1], [VCMP])
                        self.MS(VCMP[:, g, 0, 64:65], 1.0, [VCMP])
                        self.MS(VCMP[0:127, g, 1, 64:65], 1.0, [VCMP])
            P.barrier()
        KS = [T([128, S], BF16, "KS") for _ in range(2)]
        for i in range(2):
            self.LD(KS[i][64:128, :], I["consts"][0:64, eo:eo + 32 * 128], [], [KS[i]], eng="gpsimd")
        KW = [T([64, S], BF16, "KW") for _ in range(2)]
        PT = [T([128, TG], BF16, "PT") for _ in range(3)]
        PC = [T([128, 2, TG], BF16, "PC") for _ in range(2)]
        ost = [T([64, TG], BF16, "ost") for _ in range(2)]
        irec = T([128, 4], F32, "irec")
        itmp = T([128, 4, 64], F32, "itmp")
        iacc = T([128, 4, 64], F32, "iacc")
        score = T([128, 4, 64], F32, "score")
        sc2 = T([128, 64], F32, "sc2")
        m8 = T([128, 16], F32, "m8")
        ones32 = self.cst("ones")
        so = CB["sel48"][0]
        wo = CB["wide"][0]
        col0 = self.cst("col0")
        cnt = [0]
        hk = 0
        ak = 0

        A4 = [T([64, TG], F32, "A4") for _ in range(4)]
        Q4 = [T([128, S], BF16, "Q4") for _ in range(4)]
        Q4n = [Tl(q.t, "Q4n") for q in Q4]
        nself = T([128, 4, 128], F32, "nself")
        self.MS(nself[:], 0.0, [nself])
        ident32 = self.cst("ident")

        selbf = T([48, 48 * 64 + 1], BF16, "selbf")
        self.LD(selbf[:], I["consts"][0:48, so:so + 48 * 64 + 1], [], [selbf], eng="gpsimd")
        GTb = T([48, S], BF16, "GTb")
        self.LD(GTb[:], self.GT, [mb["GT"]], [GTb], eng="gpsimd")
        Mb = [M1, M2]
        Ocs = [Oc, Ow]
        rr2 = [T([65, TG], F32, "rr") for _ in range(2)]
        fb2 = [T([65, TG], BF16, "fb") for _ in range(2)]
        bcs2 = [T([64, TG], F32, "bcs") for _ in range(2)]
        tmp2 = [T([64, TG], F32, "tmp") for _ in range(2)]
        fi = [0]

        def finish_unit(o, row, a_, first, qs, pre=None, extra=None, delays=(1, 2)):
            k = fi[0]
            fi[0] += 1
            m, rr_, fb_, bs_, tmp_ = Mb[k % 2], rr2[k % 2], fb2[k % 2], bcs2[k % 2], tmp2[k % 2]

            def f0():
                if pre is not None:
                    pre()
                if first:
                    self.TS(rr_[64:65, :], o[64:65, :], 1e-18, None, ALU.max, None, [o], [rr_])
                    self.ACT(rr_[64:65, :], rr_[64:65, :], AF.Ln, [rr_], [rr_])
                else:
                    self.ACT(rr_[64:65, :], o[64:65, :], AF.Ln, [o], [rr_])
                self.ACT(rr_[64:65, :], rr_[64:65, :], AF.Exp, [rr_], [rr_], scale=-1.0)
                self.MM(m[0:65, :], selbf[0:48, row * 64:row * 64 + 65], GTb[0:48, qs], True, True, [GTb, selbf], [m])

            def f1():
                self.TT(fb_[64:65, :], m[64:65, :], rr_[64:65, :], ALU.mult, [m, rr_], [fb_])
                self.MM(m[0:64, :], self.ones_bf[64:65, 0:64], fb_[64:65, :], True, True, [fb_, self.cbf], [m])

            def f2():
                self.CP(bs_[:], m[0:64, :], [m], [bs_], eng="scalar")
                if first:
                    self.TT(a_[:], o[0:64, :], bs_[:], ALU.mult, [o, bs_], [a_])
                else:
                    self.TT(tmp_[:], o[0:64, :], bs_[:], ALU.mult, [o, bs_], [tmp_])
                    self.TT(a_[:], a_[:], tmp_[:], ALU.add, [a_, tmp_], [a_], eng="gpsimd")
                if extra is not None:
                    extra()
            if len(delays) == 3:
                return U(None, None, None, later=[(delays[0], f0), (delays[1], f1), (delays[2], f2)])
            return U(None, None, f0, later=[(delays[0], f1), (delays[1], f2)])

        for g in range(4):
            ks_, kw_ = KS[g % 2], KW[g % 2]
            self.LD(ks_[0:64, :], self.KSd[g * 64:(g + 1) * 64, :], [mb["KSd"]], [ks_])
            self.LD(kw_[:], self.KWd[g * 64:(g + 1) * 64, :], [mb["KWd"]], [kw_])
            for hh in range(4):
                h = g * 4 + hh
                self.LD(Q4[hh][0:64, :], self.QN[h * 64:(h + 1) * 64, :], [mb["QN"]], [Q4[hh]])
            for qg in range(NG):
                qs = slice(qg * TG, (qg + 1) * TG)
                q0 = qg * TG
                ntile = 2 if qg >= 4 else 1
                units = []
                for hh in range(4):
                    h = g * 4 + hh
                    pc = PC[hh % 2]
                    for i in range(ntile):
                        sp = Sp[cnt[0] % 3]
                        cnt[0] += 1

                        def A(sp=sp, i=i, hh=hh):
                            self.MM(sp[:], KCMP[:, g, i * 128:(i + 1) * 128], Q4[hh][0:64, qs], True, False, [KCMP, Q4[hh]], [sp])
                            self.MM(sp[:], self.ident_bf[:], cmpm[:, i, qs], False, True, [cmpm, self.cbf], [sp])

                        def B(sp=sp, i=i, pc=pc):
                            self.ACT(pc[:, i, :], sp[:], AF.Exp, [sp], [pc], scale=SC)

                        def C(i=i, pc=pc, oc=Ocs[hh % 2]):
                            self.MM(oc[0:65, :], VCMP[:, g, i, :], pc[:, i, :], i == 0, i == ntile - 1, [VCMP, pc], [oc])
                        units.append(U(A, B, C))

                    def pre(hh=hh, pc=pc):
                        for qt in range(4):
                            for i in range(ntile):
                                self.MM(imp[:, qt * 65:(qt + 1) * 65], pc[:, i, qt * 128:(qt + 1) * 128], ovl[:, i, :], i == 0, i == ntile - 1, [pc, ovl], [imp])
                        iv = imp[:, 0:260].rearrange("p (t f) -> p t f", f=65)
                        self.TS(irec[:], iv[:, :, 64], 1e-30, None, ALU.max, None, [imp], [irec])
                        self.P.dve(lambda e: e.reciprocal(out=irec[:], in_=irec[:]), reads=[irec.b], writes=[irec.b])
                        if hh == 0:
                            self.TT(iacc[:], iv[:, :, 0:64], irec[:].unsqueeze(2).broadcast_to([128, 4, 64]), ALU.mult, [imp, irec], [iacc])
                        else:
                            self.TT(itmp[:], iv[:, :, 0:64], irec[:].unsqueeze(2).broadcast_to([128, 4, 64]), ALU.mult, [imp, irec], [itmp])
                            self.TT(iacc[:], iacc[:], itmp[:], ALU.add, [iacc, itmp], [iacc], eng="gpsimd")
                    units.append(finish_unit(Ocs[hh % 2], 3 * h + 0, A4[hh], True, qs, pre=pre))
                run_units(units, 1)
                for qt in range(4):
                    tix = qg * 4 + qt
                    self.TT(score[:, qt, :], iacc[:, qt, :], self.c32[:, wo + 64 - 2 * tix:wo + 128 - 2 * tix], ALU.add, [iacc, self.cb], [score])
                    self.TT(score[:, qt, :], score[:, qt, :], col0, ALU.max, [score, self.cb], [score])
                    self.P.dve(lambda e, qt=qt: e.max(out=m8[:, 0:8], in_=score[:, qt, :]), reads=[score.b], writes=[m8.b])
                    self.P.dve(lambda e, qt=qt: e.match_replace(out=sc2[:], in_to_replace=m8[:, 0:8], in_values=score[:, qt, :], imm_value=-3.0e38),
                               reads=[score.b, m8.b], writes=[sc2.b])
                    self.P.dve(lambda e: e.max(out=m8[:, 8:16], in_=sc2[:]), reads=[sc2.b], writes=[m8.b])
                    self.TS(nself[:, qt, 64:128], score[:, qt, :], m8[:, 15:16], -1.0, ALU.is_ge, ALU.add, [score, m8], [nself])
                    self.TR(M2[:, qt * 128:(qt + 1) * 128], nself[:, qt, :], ident32, [nself, self.cb], [M2])
                for hh in range(4):
                    self.CP(Q4[hh][64:128, qs], M2[64:128, :], [M2], [Q4n[hh]], eng="scalar")
                units = []
                for hh in range(4):
                    h = g * 4 + hh
                    qh_ = Q4[hh]
                    nk = 4 * qg + 4
                    for kt in range(nk):
                        d = kt - 4 * qg
                        sp = Sp[cnt[0] % 3]
                        pt = PT[cnt[0] % 3]
                        cnt[0] += 1
                        kc = slice(kt * 128, (kt + 1) * 128)
                        c0 = max(d, 0) * 128

                        def A(sp=sp, kc=kc, d=d, c0=c0, qh_=qh_, qn_=Q4n[hh]):
                            if d < 0:
                                self.MM(sp[:], ks_[:, kc], qh_[:, qs], True, True, [ks_, qh_, qn_], [sp])
                            else:
                                self.MM(sp[:, c0:c0 + 128], ks_[:, kc], qh_[:, q0 + c0:q0 + c0 + 128], True, False, [ks_, qh_, qn_], [sp])
                                self.MM(sp[:, c0:c0 + 128], self.ident_bf[:], self.cpen_bf[:], False, True, [self.cbf], [sp])
                                if c0 + 128 < TG:
                                    self.MM(sp[:, c0 + 128:TG], ks_[:, kc], qh_[:, q0 + c0 + 128:q0 + TG], True, True, [ks_, qh_, qn_], [sp])

                        def B(sp=sp, pt=pt, c0=c0):
                            self.ACT(pt[:, c0:TG], sp[:, c0:TG], AF.Exp, [sp], [pt], scale=SC)

                        def C(pt=pt, c0=c0, kt=kt):
                            self.MM(Os[0:65, c0:TG], VSW[:, kt, g * 65:(g + 1) * 65], pt[:, c0:TG], kt == 0, kt == nk - 1, [pt, VSW], [Os])
                        units.append(U(A, B, C))
                    units.append(finish_unit(Os, 3 * h + 1, A4[hh], False, qs, delays=((2, 5, 7) if qg >= 1 else (1, 3, 4))))
                    kts = [4 * qg] + [kt for kt in range(4 * qg - 4, 4 * qg + 4) if kt >= 0 and kt != 4 * qg]
                    for i, kt in enumerate(kts):
                        d = kt - 4 * qg
                        sp = Sp[cnt[0] % 3]
                        pt = PT[cnt[0] % 3]
                        cnt[0] += 1
                        kc = slice(kt * 128, (kt + 1) * 128)
                        lo = max(d, 0) * 128
                        hi = min(d + 5, 4) * 128
                        if d >= 0:
                            pb, pen = lo, self.cpen_bf
                            rest = (lo + 128, hi)
                        else:
                            pb, pen = hi - 128, self.bpen_bf
                            rest = (lo, hi - 128)

                        def A(sp=sp, kc=kc, pb=pb, pen=pen, rest=rest, qh_=qh_):
                            self.MM(sp[:, pb:pb + 128], kw_[:, kc], qh_[0:64, q0 + pb:q0 + pb + 128], True, False, [kw_, qh_], [sp])
                            self.MM(sp[:, pb:pb + 128], self.ident_bf[:], pen[:], False, True, [self.cbf], [sp])
                            if rest[1] > rest[0]:
                                self.MM(sp[:, rest[0]:rest[1]], kw_[:, kc], qh_[0:64, q0 + rest[0]:q0 + rest[1]], True, True, [kw_, qh_], [sp])

                        def B(sp=sp, pt=pt, lo=lo, hi=hi):
                            self.ACT(pt[:, lo:hi], sp[:, lo:hi], AF.Exp, [sp], [pt], scale=SC)

                        def C(pt=pt, lo=lo, hi=hi, kt=kt, i=i, nw=len(kts)):
                            self.MM(Ow[0:65, lo:hi], VSW[:, kt, (4 + g) * 65:(5 + g) * 65], pt[:, lo:hi], i == 0, i == nw - 1, [pt, VSW], [Ow])
                        units.append(U(A, B, C))

                    def store(h=h, hh=hh, qs=qs):
                        os_ = ost[h % 2]
                        self.CP(os_[:], A4[hh][:], [A4[hh]], [os_], eng="scalar")
                        self.LD(self.ONT[h, :, qs], os_[:], [os_], [mb["ONT"]], eng="gpsimd")
                    units.append(finish_unit(Ow, 3 * h + 2, A4[hh], False, qs, extra=store, delays=((2, 5, 7) if qg >= 1 else (1, 3, 4))))
                run_units(units, 2)
    P.barrier()


KB.nsa_attn = _nsa_attn
```

```python
import numpy as np
from contextlib import ExitStack
import concourse.bass as bass
import concourse.mybir as mybir
from concourse.bass_utils import run_bass_kernel_spmd

F32 = mybir.dt.float32
BF16 = mybir.dt.bfloat16
I32 = mybir.dt.int32
ALU = mybir.AluOpType
AF = mybir.ActivationFunctionType
AX = mybir.AxisListType

S = 4096
D = 1024
DFF = 2816
NFC = 22
NG = 8
TG = 512
EPS = 1e-6
HY_IN = 1968
NSA_IN = 2608
NEGB = -30000.0

ENGINES = ["tensor", "vector", "scalar", "gpsimd", "sync"]
class Buf:
    __slots__ = ("name", "last_w", "readers")

    def __init__(self, name=""):
        self.name = name
        self.last_w = None
        self.readers = []


class Op:
    __slots__ = ("eng", "fn", "raw", "oth", "is_dma", "sig", "sem", "semval", "prev_semval", "gidx")


class Prog:
    def __init__(self, nc, n_dma_sems=12):
        self.nc = nc
        self.ops = {e: [] for e in ENGINES}
        self.n = 0
        self.n_dma_sems = n_dma_sems
        self.pending_barrier = {}
        self.dma_last = {}
        self.dma_cnt = {}

    def op(self, eng, fn, reads=(), writes=(), dma=False):
        o = Op()
        o.eng = eng
        o.fn = fn
        o.is_dma = dma
        o.raw = set()
        o.oth = set()
        o.sig = False
        o.sem = None
        o.semval = 0
        o.prev_semval = 0
        o.gidx = self.n
        self.n += 1
        for b in reads:
            if b.last_w is not None:
                o.raw.add(b.last_w)
        for b in writes:
            if b.last_w is not None:
                o.oth.add(b.last_w)
            for r in b.readers:
                o.oth.add(r)
        for b in reads:
            b.readers.append(o)
        for b in writes:
            b.last_w = o
            b.readers = []
        o.raw.discard(o)
        o.oth.discard(o)
        if eng in self.pending_barrier:
            for d in self.pending_barrier.pop(eng):
                o.raw.add(d)
        self.ops[eng].append(o)
        if dma:
            k = self.dma_cnt.get(eng, 0)
            self.dma_cnt[eng] = k + 1
            self.dma_last[(eng, k % self.n_dma_sems)] = o
        return o

    def barrier(self):
        deps = []
        for e in ENGINES:
            comp = [x for x in self.ops[e] if not x.is_dma]
            if comp:
                deps.append(comp[-1])
        deps.extend(self.dma_last.values())
        for e in ENGINES:
            self.pending_barrier[e] = list(deps) + self.pending_barrier.get(e, [])

    def mm(self, fn, reads=(), writes=()):
        return self.op("tensor", fn, reads, writes)

    def dve(self, fn, reads=(), writes=()):
        return self.op("vector", fn, reads, writes)

    def act(self, fn, reads=(), writes=()):
        return self.op("scalar", fn, reads, writes)

    def pool(self, fn, reads=(), writes=()):
        return self.op("gpsimd", fn, reads, writes)

    def load(self, out, in_, reads=(), writes=(), eng="sync", **kw):
        return self.op(eng, lambda e: e.dma_start(out=out, in_=in_, **kw), reads, writes, dma=True)

    def needed_deps(self, o):
        res = []
        for d in o.raw:
            if d.eng == o.eng and not d.is_dma and not o.is_dma:
                if o.eng == "tensor":
                    continue
                res.append(d)
            else:
                res.append(d)
        for d in o.oth:
            if d.eng == o.eng and not d.is_dma and not o.is_dma:
                continue
            res.append(d)
        return res

    def finalize(self, stack):
        nc = self.nc
        for e in ENGINES:
            for o in self.ops[e]:
                if o.is_dma:
                    o.sig = True
                for d in self.needed_deps(o):
                    d.sig = True
        csem = {}
        for e in ["tensor", "vector", "scalar", "gpsimd"]:
            csem[e] = stack.enter_context(nc.semaphore("c_" + e))
        dpool = {}
        for e in ["sync", "gpsimd", "scalar"]:
            if any(o.is_dma for o in self.ops[e]):
                dpool[e] = [stack.enter_context(nc.semaphore("d_%s_%d" % (e, i))) for i in range(self.n_dma_sems)]
        for e in ENGINES:
            cnt = 0
            k = 0
            uses = {}
            for o in self.ops[e]:
                if o.is_dma:
                    s = dpool[e][k % self.n_dma_sems]
                    k += 1
                    o.sem = s
                    o.prev_semval = uses.get(id(s), 0)
                    o.semval = o.prev_semval + 16
                    uses[id(s)] = o.semval
                elif o.sig:
                    cnt += 1
                    o.sem = csem[e]
                    o.semval = cnt
        self.final_dma = []
        for e in dpool:
            last = {}
            for o in self.ops[e]:
                if o.is_dma:
                    last[id(o.sem)] = (o.sem, o.semval)
            self.final_dma.append((e, list(last.values())))
        block = stack.enter_context(nc.Block())
        prog = self

        def make(e):
            def body(eng):
                waited = {}
                for o in prog.ops[e]:
                    need = {}
                    for d in prog.needed_deps(o):
                        key = id(d.sem)
                        if key not in need or need[key][1] < d.semval:
                            need[key] = (d.sem, d.semval)
                    if o.is_dma and o.prev_semval > 0:
                        key = id(o.sem)
                        if key not in need or need[key][1] < o.prev_semval:
                            need[key] = (o.sem, o.prev_semval)
                    for key, (s, v) in need.items():
                        if waited.get(key, 0) >= v:
                            continue
                        eng.wait_ge(s, v)
                        waited[key] = v
                    ins = o.fn(eng)
                    if o.sig:
                        ins.then_inc(o.sem, 16 if o.is_dma else 1)
                for (ee, lst) in prog.final_dma:
                    if ee == e:
                        for (s, v) in lst:
                            if waited.get(id(s), 0) < v:
                                eng.wait_ge(s, v)
            return body

        for e in ENGINES:
            if not self.ops[e]:
                continue
            getattr(block, e)(make(e))


CB = {}
_off = 0
for _n, _w in [("ident", 128), ("ones", 128), ("cpen", 128), ("bpen", 128), ("tri", 64), ("scanmask", 512),
               ("freq", 4), ("wide", 128), ("col0", 64), ("sel48", 48 * 64 + 1), ("E", 32 * 128)]:
    CB[_n] = (_off, _w)
    _off += _w
CB_W = _off


def make_consts():
    c = np.zeros((128, CB_W), np.float32)
    p = np.arange(128)[:, None]

    def put(n, a):
        o, w = CB[n]
        c[: a.shape[0], o:o + a.shape[1]] = a
    put("ident", np.eye(128, dtype=np.float32))
    put("ones", np.ones((128, 128), np.float32))
    f = np.arange(128)[None, :]
    put("cpen", np.where(p <= f, 0.0, NEGB).astype(np.float32))
    put("bpen", np.where(p > f, 0.0, NEGB).astype(np.float32))
    j = np.arange(64)[:, None]
    i = np.arange(64)[None, :]
    put("tri", (j <= i).astype(np.float32))
    sm = np.ones((128, 512), np.float32)
    sm[:, ::64] = 0.0
    put("scanmask", sm)
    fr = np.zeros((128, 4), np.float32)
    r = np.arange(128)
    im = (r - 64) % 16
    fr[:, 0] = (10000.0 ** (-im * (2.0 / 32))) / (2 * np.pi)
    fr[:, 1] = np.where(((r - 64) % 32) < 16, -1.0, 1.0) * (2 * np.pi * (1 - 1e-6))
    i2 = r % 32
    fr[:, 2] = (10000.0 ** (-i2 * (2.0 / 64))) / (2 * np.pi)
    fr[:, 3] = np.where((r % 64) < 32, -1.0, 1.0) * (2 * np.pi * (1 - 1e-6))
    put("freq", fr)
    x = np.arange(128)[None, :]
    rel = x - 64 - (p >= 64)
    wide = np.where(rel > 0, -1e30, np.where(rel >= -1, 1e4, 0.0)).astype(np.float32)
    put("wide", wide)
    c0 = np.full((128, 64), -3e38, np.float32)
    c0[:, 0] = 1e4
    put("col0", c0)
    sel = np.zeros((128, 48 * 64 + 1), np.float32)
    for rr in range(48):
        sel[rr, 1 + rr * 64:1 + (rr + 1) * 64] = 1.0
    put("sel48", sel)
    E = np.zeros((128, 32 * 128), np.float32)
    for kt in range(32):
        for key in range(128):
            E[2 * kt + (key >= 64), kt * 128 + key] = -NEGB
    put("E", E)
    return c


def make_cmp_consts():
    n = np.arange(256)[:, None]
    t = np.arange(S)[None, :]
    cm = np.where((16 * n + 31 <= t) & (n < 255), 0.0, NEGB).astype(np.float32)
    ncmp = np.arange(255)
    cs_ = ncmp * 16
    bs_ = np.arange(64) * 64
    ov = np.minimum(cs_[:, None] + 32, bs_[None, :] + 64) - np.maximum(cs_[:, None], bs_[None, :])
    ovl = np.zeros((256, 65), np.float32)
    ovl[:255, :64] = np.clip(ov, 0, None) / 32.0
    ovl[:255, 64] = 1.0
    return cm, ovl


class KB:
    def __init__(self, debug_phases=None):
        self.nc = bass.Bass("TRN2", target_bir_lowering=False)
        self.P = Prog(self.nc)
        self.debug_phases = debug_phases
        self.uid = 0
        self.gdeps = []

    def name(self, p):
        self.uid += 1
        return "%s_%d" % (p, self.uid)

    def dram(self, name, shape, dt, kind="Internal"):
        return self.nc.dram_tensor(name, list(shape), dt, kind=kind).ap()

    def sb(self, st, shape, dt, name="t"):
        return st.enter_context(self.nc.sbuf_tensor(self.name(name), list(shape), dt))

    def ps(self, st, shape, dt=F32, name="p"):
        return st.enter_context(self.nc.psum_tensor(self.name(name), list(shape), dt))

    def declare(self):
        d = self.dram
        I = {}
        I["x"] = d("x", [S, D], F32, "ExternalInput")
        I["positions"] = d("positions", [1, S], I32, "ExternalInput")
        I["consts"] = d("consts", [128, CB_W], F32, "ExternalInput")
        I["cmpmask"] = d("cmpmask", [256, S], F32, "ExternalInput")
        I["ovl"] = d("ovl", [256, 65], F32, "ExternalInput")
        for n, shp in [("ffn_norm", [4, D]), ("ffn_w_gate", [4, D, DFF]), ("ffn_w_up", [4, D, DFF]),
                       ("ffn_w_down", [4, DFF, D]), ("mix_norm", [2, D]), ("hy_w_in", [D, HY_IN]),
                       ("mla_q_norm", [1, 256]), ("mla_w_uq", [256, 768]), ("mla_kv_norm", [1, 128]),
                       ("mla_w_ukv", [128, 1024]), ("gla_w_a2", [16, 256]), ("gla_b_a", [1, 256]),
                       ("gla_out_norm", [1, 128]), ("hy_w_out", [D, D]), ("nsa_w_in", [D, NSA_IN]),
                       ("nsa_pos_k", [32, 64]), ("nsa_pos_v", [32, 64]), ("nsa_ck_w1", [2048, 128]),
                       ("nsa_ck_w2", [128, 64]), ("nsa_cv_w1", [2048, 128]), ("nsa_cv_w2", [128, 64]),
                       ("nsa_w_out", [D, D]), ("final_norm", [1, D])]:
            I[n] = d(n, shp, F32, "ExternalInput")
        self.I = I
        self.out = d("out", [S, D], F32, "ExternalOutput")
        self.hT = [d("hT%d" % i, [D, S], F32) for i in range(2)]
        self.hbuf = [Buf("hT0"), Buf("hT1")]
        self.wgu = d("wgu", [4, NFC, 128, 2 * 8 * 128], BF16)
        self.wdn = d("wdn", [4, 8, 128, NFC * 128], BF16)
        self.wgu_b = {}
        self.wdn_b = {}
        self.prepq = []

    def prep_ffn(self, f):
        I = self.I
        items = []
        self.wgu_b[f] = {}
        self.wdn_b[f] = []
        for c in range(NFC):
            for which, src in enumerate([I["ffn_w_gate"], I["ffn_w_up"]]):
                bf_ = Buf("wgu")
                self.wgu_b[f][(which, c)] = bf_
                dst = self.wgu[f, c].rearrange("p (w k j) -> p w k j", w=2, k=8, j=128)[:, which, :, :]
                s_ = src[f, :, c * 128:(c + 1) * 128].rearrange("(k p) j -> p k j", p=128)
                items.append((dst, s_, bf_))
        for c in range(NFC):
            bf_ = Buf("wdn")
            self.wdn_b[f].append(bf_)
            dst = self.wdn[f].rearrange("fc p (c j) -> p fc c j", c=NFC, j=128)[:, :, c, :]
            s_ = I["ffn_w_down"][f, c * 128:(c + 1) * 128, :].rearrange("p (fc j) -> p fc j", j=128)
            items.append((dst, s_, bf_))
        self.prepq.extend(items)

    def pump(self, n):
        for _ in range(n):
            if not self.prepq:
                return
            dst, s_, bf_ = self.prepq.pop(0)
            self.P.load(dst, s_, writes=[bf_], eng="gpsimd")

    def load_consts(self, st):
        P = self.P
        self.c32 = self.sb(st, [128, CB["E"][0]], F32, "c32")
        self.cb = Buf("c32")
        P.load(self.c32[:], self.I["consts"][:, 0:CB["E"][0]], writes=[self.cb])
        self.ident_bf = self.sb(st, [128, 128], BF16, "identbf")
        self.ones_bf = self.sb(st, [128, 128], BF16, "onesbf")
        self.cpen_bf = self.sb(st, [128, 128], BF16, "cpenbf")
        self.bpen_bf = self.sb(st, [128, 128], BF16, "bpenbf")
        self.cbf = Buf("cbf")
        for t, n in [(self.ident_bf, "ident"), (self.ones_bf, "ones"), (self.cpen_bf, "cpen"), (self.bpen_bf, "bpen")]:
            o, w = CB[n]
            P.dve(lambda e, t=t, o=o, w=w: e.tensor_copy(out=t[:], in_=self.c32[:, o:o + w]), reads=[self.cb], writes=[self.cbf])
        self.gcol = self.sb(st, [128, 7, 8], F32, "gcol")
        self.gb = Buf("gcol")
        with self.nc.allow_non_contiguous_dma("tiny gain vectors"):
            for j, src in [(0, self.I["ffn_norm"][0:1, :]), (1, self.I["ffn_norm"][1:2, :]), (2, self.I["ffn_norm"][2:3, :]),
                           (3, self.I["ffn_norm"][3:4, :]), (4, self.I["mix_norm"][0:1, :]), (5, self.I["mix_norm"][1:2, :]),
                           (6, self.I["final_norm"][0:1, :])]:
                P.load(self.gcol[:, j, :], src.rearrange("o (fc p) -> p (o fc)", p=128), writes=[self.gb], allow_slow_non_contiguous=True)

    def cst(self, n, rows=128, c0=0, c1=None):
        o, w = CB[n]
        if c1 is None:
            c1 = w
        return self.c32[0:rows, o + c0:o + c1]

    def transpose_in(self):
        P = self.P
        with ExitStack() as st:
            xt = [self.sb(st, [128, D], F32, "xt") for _ in range(4)]
            xb = [Buf("xt%d" % i) for i in range(4)]
            tp = [self.ps(st, [128, 512], F32, "tp") for _ in range(2)]
            tb = [Buf("tp0"), Buf("tp1")]
            stg = [self.sb(st, [128, 8, 512], F32, "stg") for _ in range(2)]
            sgb = [Buf("stg0"), Buf("stg1")]
            ident = self.cst("ident")
            k = 0
            for g in range(NG):
                for t in range(4):
                    r0 = g * TG + t * 128
                    P.load(xt[t][:], self.I["x"][r0:r0 + 128, :], writes=[xb[t]])
                so = stg[g % 2]
                for fc in range(8):
                    pb = tp[k % 2]
                    for t in range(4):
                        P.mm(lambda e, pb=pb, t=t, fc=fc: e.transpose(out=pb[:, t * 128:(t + 1) * 128], in_=xt[t][:, fc * 128:(fc + 1) * 128], identity=ident),
                             reads=[xb[t], self.cb], writes=[tb[k % 2]])
                    if fc % 2 == 0:
                        P.act(lambda e, pb=pb, fc=fc, so=so: e.copy(out=so[:, fc, :], in_=pb[:]), reads=[tb[k % 2]], writes=[sgb[g % 2]])
                    else:
                        P.dve(lambda e, pb=pb, fc=fc, so=so: e.tensor_copy(out=so[:, fc, :], in_=pb[:]), reads=[tb[k % 2]], writes=[sgb[g % 2]])
                    k += 1
                P.load(self.hT[0].rearrange("(fc p) t -> p fc t", p=128)[:, :, g * TG:(g + 1) * TG], so[:],
                       reads=[sgb[g % 2]], writes=[self.hbuf[0]], eng="gpsimd")
        P.barrier()

    def norm_slab(self, hs, hsb, nfc, gcols, ss_ps, ssb, sq, sqb, rstd, rstdb, uT, uTb, inv_n, psum_src=False):
        P = self
        Pg = self.P
        for fc in range(nfc):
            s_ = sq[fc % len(sq)]
            sb_ = sqb[fc % len(sq)]
            Pg.act(lambda e, s_=s_, fc=fc: e.activation(out=s_[:], in_=hs[:, fc, :], func=AF.Square), reads=[hsb], writes=[sb_])
            Pg.mm(lambda e, s_=s_, fc=fc: e.matmul(ss_ps[:], lhsT=self.ones_bf[:], rhs=s_[:], start=(fc == 0), stop=(fc == nfc - 1)),
                  reads=[sb_, self.cbf], writes=[ssb])
        Pg.act(lambda e: e.activation(out=rstd[:], in_=ss_ps[:], func=AF.Ln, scale=float(inv_n), bias=float(EPS)), reads=[ssb], writes=[rstdb])
        Pg.act(lambda e: e.activation(out=rstd[:], in_=rstd[:], func=AF.Exp, scale=-0.5), reads=[rstdb], writes=[rstdb])
        for fc in range(nfc):
            Pg.dve(lambda e, fc=fc: e.scalar_tensor_tensor(out=uT[:, fc, :], in0=hs[:, fc, :], scalar=gcols[fc], in1=rstd[:],
                                                           op0=ALU.mult, op1=ALU.mult), reads=[hsb, rstdb, self.gb], writes=[uTb])

    def ffn(self, f, src, dst):
        P = self.P
        hin, hinb = self.hT[src], self.hbuf[src]
        hout, houtb = self.hT[dst], self.hbuf[dst]
        hin_v = hin.rearrange("(fc p) t -> p fc t", p=128)
        hout_v = hout.rearrange("(fc p) t -> p fc t", p=128)
        with ExitStack() as st:
            hs = [self.sb(st, [128, 8, TG], F32, "hs") for _ in range(2)]
            hsb = [Buf("hs0"), Buf("hs1")]
            uT = [self.sb(st, [128, 8, TG], BF16, "uT") for _ in range(2)]
            uTb = [Buf("uT0"), Buf("uT1")]
            sq = [self.sb(st, [128, TG], BF16, "sq") for _ in range(2)]
            sqb = [Buf("sq0"), Buf("sq1")]
            rstd = self.sb(st, [128, TG], F32, "rstd")
            rstdb = Buf("rstd")
            actT = [self.sb(st, [128, NFC, TG], BF16, "actT") for _ in range(2)]
            actb = [Buf("act0"), Buf("act1")]
            NW = 6
            wgu = [self.sb(st, [128, 2, 8, 128], BF16, "wgu") for _ in range(NW)]
            wgub = [Buf("wgu%d" % i) for i in range(NW)]
            NWD = 3
            wdn = [self.sb(st, [128, NFC, 128], BF16, "wdn") for _ in range(NWD)]
            wdnb = [Buf("wdn%d" % i) for i in range(NWD)]
            sg = [self.sb(st, [128, TG], BF16, "sg") for _ in range(2)]
            sgb = [Buf("sg0"), Buf("sg1")]
            ho = [self.sb(st, [128, TG], F32, "ho") for _ in range(3)]
            hob = [Buf("ho%d" % i) for i in range(3)]
            ss_ps = self.ps(st, [128, TG], F32, "ss")
            ssb = Buf("ss")
            g_ps = [self.ps(st, [128, TG], F32, "gps") for _ in range(2)]
            gpb = [Buf("g0"), Buf("g1")]
            u_ps = [self.ps(st, [128, TG], F32, "ups") for _ in range(2)]
            upb = [Buf("u0"), Buf("u1")]
            o_ps = [self.ps(st, [128, TG], F32, "ops") for _ in range(2)]
            opb = [Buf("o0"), Buf("o1")]
            gcols = [self.gcol[:, f, fc:fc + 1] for fc in range(8)]
            cnt = {"w": 0, "d": 0, "c": 0, "o": 0}

            def stage_load(g):
                P.load(hs[g % 2][:], hin_v[:, :, g * TG:(g + 1) * TG], reads=[hinb], writes=[hsb[g % 2]])

            def stage_norm(g):
                self.norm_slab(hs[g % 2], hsb[g % 2], 8, gcols, ss_ps, ssb, sq, sqb, rstd, rstdb, uT[g % 2], uTb[g % 2], 1.0 / D)

            def stage_gu(g):
                for c in range(NFC):
                    w = cnt["w"] % NW
                    cnt["w"] += 1
                    P.load(wgu[w][:], self.wgu[f, c].rearrange("p (w k j) -> p w k j", w=2, k=8), reads=[self.wgu_b[f][(0, c)], self.wgu_b[f][(1, c)]], writes=[wgub[w]])
                    b = cnt["c"] % 2
                    cnt["c"] += 1
                    for which, (pt, pbf) in enumerate([(g_ps[b], gpb[b]), (u_ps[b], upb[b])]):
                        for kc in range(8):
                            P.mm(lambda e, pt=pt, w=w, which=which, kc=kc: e.matmul(pt[:], lhsT=wgu[w][:, which, kc, :], rhs=uT[g % 2][:, kc, :],
                                                                                    start=(kc == 0), stop=(kc == 7)),
                                 reads=[wgub[w], uTb[g % 2]], writes=[pbf])
                    P.act(lambda e, b=b: e.activation(out=sg[b][:], in_=g_ps[b][:], func=AF.Silu), reads=[gpb[b]], writes=[sgb[b]])
                    P.dve(lambda e, b=b, c=c: e.tensor_tensor(out=actT[g % 2][:, c, :], in0=sg[b][:], in1=u_ps[b][:], op=ALU.mult),
                          reads=[sgb[b], upb[b]], writes=[actb[g % 2]])

            def stage_down(g):
                for fc in range(8):
                    w = cnt["d"] % NWD
                    cnt["d"] += 1
                    P.load(wdn[w][:], self.wdn[f, fc].rearrange("p (c j) -> p c j", j=128), reads=self.wdn_b[f], writes=[wdnb[w]])
                    b = fc % 2
                    for c in range(NFC):
                        P.mm(lambda e, b=b, w=w, c=c: e.matmul(o_ps[b][:], lhsT=wdn[w][:, c, :], rhs=actT[g % 2][:, c, :], start=(c == 0), stop=(c == NFC - 1)),
                             reads=[wdnb[w], actb[g % 2]], writes=[opb[b]])
                    k = cnt["o"] % 3
                    cnt["o"] += 1
                    P.dve(lambda e, b=b, k=k, fc=fc: e.scalar_tensor_tensor(out=ho[k][:], in0=o_ps[b][:], scalar=0.5, in1=hs[g % 2][:, fc, :],
                                                                            op0=ALU.mult, op1=ALU.add), reads=[opb[b], hsb[g % 2]], writes=[hob[k]])
                    P.load(hout_v[:, fc, g * TG:(g + 1) * TG], ho[k][:], reads=[hob[k]], writes=[houtb], eng="gpsimd")
                    self.pump(5)

            stage_load(0)
            stage_norm(0)
            for g in range(NG):
                if g + 1 < NG:
                    stage_load(g + 1)
                stage_gu(g)
                if g + 1 < NG:
                    stage_norm(g + 1)
                stage_down(g)
        P.barrier()

    def final(self, src, do_norm=True):
        P = self.P
        hin_v = self.hT[src].rearrange("(fc p) t -> p fc t", p=128)
        hinb = self.hbuf[src]
        outb = Buf("out")
        with ExitStack() as st:
            hs = [self.sb(st, [128, 8, TG], F32, "hs") for _ in range(2)]
            hsb = [Buf("hs0"), Buf("hs1")]
            yT = [self.sb(st, [128, 8, TG], F32, "yT") for _ in range(2)]
            yTb = [Buf("y0"), Buf("y1")]
            sq = [self.sb(st, [128, TG], BF16, "sq") for _ in range(2)]
            sqb = [Buf("sq0"), Buf("sq1")]
            rstd = self.sb(st, [128, TG], F32, "rstd")
            rstdb = Buf("rstd")
            ss_ps = self.ps(st, [128, TG], F32, "ss")
            ssb = Buf("ss")
            tp = [self.ps(st, [128, 512], F32, "tp") for _ in range(4)]
            tb = [Buf("tp%d" % i) for i in range(4)]
            ot = [self.sb(st, [128, D], F32, "ot") for _ in range(3)]
            otb = [Buf("ot%d" % i) for i in range(3)]
            gcols = [self.gcol[:, 6, fc:fc + 1] for fc in range(8)]
            ident = self.cst("ident")
            k = 0
            n = 0
            for g in range(NG):
                P.load(hs[g % 2][:], hin_v[:, :, g * TG:(g + 1) * TG], reads=[hinb], writes=[hsb[g % 2]])
                if do_norm:
                    self.norm_slab(hs[g % 2], hsb[g % 2], 8, gcols, ss_ps, ssb, sq, sqb, rstd, rstdb, yT[g % 2], yTb[g % 2], 1.0 / D)
                    y, yb = yT[g % 2], yTb[g % 2]
                else:
                    y, yb = hs[g % 2], hsb[g % 2]
                for t in range(4):
                    o_ = ot[n % 3]
                    ob_ = otb[n % 3]
                    n += 1
                    for half in range(2):
                        pb = tp[k % 4]
                        pbb = tb[k % 4]
                        k += 1
                        for j in range(4):
                            fc = half * 4 + j
                            P.mm(lambda e, pb=pb, j=j, fc=fc, t=t, y=y: e.transpose(out=pb[:, j * 128:(j + 1) * 128], in_=y[:, fc, t * 128:(t + 1) * 128], identity=ident),
                                 reads=[yb, self.cb], writes=[pbb])
                        if half == 0:
                            P.act(lambda e, pb=pb, o_=o_: e.copy(out=o_[:, 0:512], in_=pb[:]), reads=[pbb], writes=[ob_])
                        else:
                            P.dve(lambda e, pb=pb, o_=o_: e.tensor_copy(out=o_[:, 512:1024], in_=pb[:]), reads=[pbb], writes=[ob_])
                    r0 = g * TG + t * 128
                    P.load(self.out[r0:r0 + 128, :], o_[:], reads=[ob_], writes=[outb], eng="gpsimd")


def build(nphase=99, final_norm=True, only=None):
    import os
    kb = KB()
    kb.declare()
    P = kb.P
    with ExitStack() as st:
        kb.load_consts(st)
        cur = 0
        if only is None:
            kb.prep_ffn(0)
            kb.pump(10 ** 6)
        kb.transpose_in()
        if only == "mix1":
            kb.declare_mix1()
            kb.inproj1(cur)
            kb.nsa_attn()
            kb.outproj(cur, 1 - cur, kb.I["nsa_w_out"], kb.ONT, kb.m1b["ONT"], 16)
            cur = 1 - cur
        elif only is None:
            if nphase >= 1:
                kb.prep_ffn(1)
                kb.ffn(0, cur, 1 - cur)
                cur = 1 - cur
            if nphase >= 2:
                kb.declare_mix0()
                kb.inproj0(cur)
                kb.mla()
                kb.gla()
                kb.outproj(cur, 1 - cur, kb.I["hy_w_out"], kb.OTm, kb.m0b["OTm"], 8, kb.OTg, kb.m0b["OTg"])
                cur = 1 - cur
            if nphase >= 3:
                kb.prep_ffn(2)
                kb.ffn(1, cur, 1 - cur)
                cur = 1 - cur
                kb.prep_ffn(3)
                kb.ffn(2, cur, 1 - cur)
                cur = 1 - cur
            if nphase >= 4:
                kb.declare_mix1()
                kb.inproj1(cur)
                kb.nsa_attn()
                kb.outproj(cur, 1 - cur, kb.I["nsa_w_out"], kb.ONT, kb.m1b["ONT"], 16)
                cur = 1 - cur
            if nphase >= 5:
                kb.ffn(3, cur, 1 - cur)
                cur = 1 - cur
        kb.pump(10 ** 6)
        kb.final(cur, do_norm=final_norm)
        P.finalize(st)
    return kb.nc


WNAMES = ["ffn_norm", "ffn_w_gate", "ffn_w_up", "ffn_w_down", "mix_norm", "hy_w_in", "mla_q_norm", "mla_w_uq", "mla_kv_norm",
          "mla_w_ukv", "gla_w_a2", "gla_b_a", "gla_out_norm", "hy_w_out", "nsa_w_in", "nsa_pos_k", "nsa_pos_v", "nsa_ck_w1",
          "nsa_ck_w2", "nsa_cv_w1", "nsa_cv_w2", "nsa_w_out", "final_norm"]


def make_in_maps(inputs, ncores=8):
    consts = make_consts()
    cm, ovl = make_cmp_consts()
    shared = {"consts": consts, "cmpmask": cm, "ovl": ovl}
    f32 = lambda a: np.ascontiguousarray(np.asarray(a), dtype=np.float32)
    shared["ffn_norm"] = f32(inputs["ffn_norm"]).reshape(4, D)
    shared["ffn_w_gate"] = f32(inputs["ffn_w_gate"]).reshape(4, D, DFF)
    shared["ffn_w_up"] = f32(inputs["ffn_w_up"]).reshape(4, D, DFF)
    shared["ffn_w_down"] = f32(inputs["ffn_w_down"]).reshape(4, DFF, D)
    shared["mix_norm"] = f32(inputs["mix_norm"])
    for n in ["hy_w_in", "mla_w_uq", "mla_w_ukv", "gla_w_a2", "hy_w_out", "nsa_w_in", "nsa_pos_k", "nsa_pos_v", "nsa_ck_w1",
              "nsa_ck_w2", "nsa_cv_w1", "nsa_cv_w2", "nsa_w_out"]:
        shared[n] = f32(inputs[n])[0]
    for n in ["mla_q_norm", "mla_kv_norm", "gla_b_a", "gla_out_norm"]:
        shared[n] = f32(inputs[n]).reshape(1, -1)
    shared["final_norm"] = f32(inputs["final_norm"]).reshape(1, D)
    x = f32(inputs["x"])
    pos = np.ascontiguousarray(np.asarray(inputs["positions"]), dtype=np.int32)
    maps = []
    for c in range(ncores):
        m = dict(shared)
        m["x"] = x[c]
        m["positions"] = pos[c:c + 1]
        maps.append(m)
    return maps


def kernel(**inputs):
    nc = build()
    maps = make_in_maps(inputs)
    res = run_bass_kernel_spmd(nc, maps, core_ids=list(range(8)))
    return np.stack([r["out"] for r in res.results], axis=0)


class Tl:
    def __init__(self, t, name):
        self.t = t
        self.b = Buf(name)

    def __getitem__(self, k):
        return self.t[k]


def _kb_tile(self, st, shape, dt, name="t"):
    return Tl(self.sb(st, shape, dt, name), name)


def _kb_ptile(self, st, shape=(128, 512), dt=F32, name="p"):
    return Tl(self.ps(st, list(shape), dt, name), name)


def _bl(xs):
    return [x.b if isinstance(x, Tl) else x for x in xs]


def _MM(self, out, lhsT, rhs, start, stop, r, w):
    return self.P.mm(lambda e: e.matmul(out, lhsT=lhsT, rhs=rhs, start=start, stop=stop), reads=_bl(r), writes=_bl(w))


def _PROJ(self, out, pairs, r, w):
    n = len(pairs)
    for i, (l, rh) in enumerate(pairs):
        self.MM(out, l, rh, i == 0, i == n - 1, r, w)


def _TR(self, out, in_, ident, r, w):
    return self.P.mm(lambda e: e.transpose(out=out, in_=in_, identity=ident), reads=_bl(r), writes=_bl(w))


def _ACT(self, out, in_, func, r, w, **kw):
    return self.P.act(lambda e: e.activation(out=out, in_=in_, func=func, **kw), reads=_bl(r), writes=_bl(w))


def _TT(self, out, a, b, op, r, w, eng="vector"):
    return self.P.op(eng, lambda e: e.tensor_tensor(out=out, in0=a, in1=b, op=op), reads=_bl(r), writes=_bl(w))


def _STT(self, out, in0, scalar, in1, op0, op1, r, w):
    return self.P.dve(lambda e: e.scalar_tensor_tensor(out=out, in0=in0, scalar=scalar, in1=in1, op0=op0, op1=op1), reads=_bl(r), writes=_bl(w))


def _TS(self, out, in0, s1, s2, op0, op1, r, w, eng="vector"):
    if s2 is None:
        return self.P.op(eng, lambda e: e.tensor_scalar(out=out, in0=in0, scalar1=s1, scalar2=None, op0=op0), reads=_bl(r), writes=_bl(w))
    return self.P.op(eng, lambda e: e.tensor_scalar(out=out, in0=in0, scalar1=s1, scalar2=s2, op0=op0, op1=op1), reads=_bl(r), writes=_bl(w))


def _CP(self, out, in_, r, w, eng="vector"):
    if eng == "scalar":
        return self.P.act(lambda e: e.copy(out=out, in_=in_), reads=_bl(r), writes=_bl(w))
    return self.P.op(eng, lambda e: e.tensor_copy(out=out, in_=in_), reads=_bl(r), writes=_bl(w))


def _LD(self, out, in_, r, w, eng="sync", **kw):
    return self.P.load(out, in_, reads=_bl(r), writes=_bl(w), eng=eng, **kw)


def _MS(self, ap, val, w, eng="gpsimd"):
    return self.P.op(eng, lambda e: e.memset(ap, val), reads=[], writes=_bl(w))


for _n, _f in [("tile", _kb_tile), ("ptile", _kb_ptile), ("MM", _MM), ("PROJ", _PROJ), ("TR", _TR), ("ACT", _ACT), ("TT", _TT),
               ("STT", _STT), ("TS", _TS), ("CP", _CP), ("LD", _LD), ("MS", _MS)]:
    setattr(KB, _n, _f)


def _norm_ps(self, srcs, gcols, inv_n, ss, sq, rstd, outs, out_b):
    n = len(srcs)
    for i, (ap, tl) in enumerate(srcs):
        q = sq[i % len(sq)]
        self.ACT(q[:], ap, AF.Square, [tl], [q])
        self.MM(ss[:], self.ones_bf[:], q[:], i == 0, i == n - 1, [q, self.cbf], [ss])
    self.ACT(rstd[:], ss[:], AF.Ln, [ss], [rstd], scale=float(inv_n), bias=float(EPS))
    self.ACT(rstd[:], rstd[:], AF.Exp, [rstd], [rstd], scale=-0.5)
    for i, (ap, tl) in enumerate(srcs):
        self.STT(outs[i], ap, gcols[i], rstd[:], ALU.mult, ALU.mult, [tl, rstd] + self.gdeps, [out_b])


KB.norm_ps = _norm_ps


def _rope_tables(self, st, rows, r0, fcol, scol, name, pos_ap=None, S=S):
    C = self.tile(st, [rows, S], F32, name + "C")
    Sg = self.tile(st, [rows, S], F32, name + "S")
    with ExitStack() as st2:
        pi_ = self.tile(st2, [rows, S], I32, "posi")
        t = self.tile(st2, [rows, S], F32, "rt")
        u = self.tile(st2, [rows, S], F32, "ru")
        ti = self.tile(st2, [rows, S], I32, "rti")
        rs = slice(r0, rows)
        fo = CB["freq"][0]
        if pos_ap is None:
            pos_ap = self.I["positions"]
        with self.nc.allow_non_contiguous_dma("positions"):
            self.LD(pi_[rs, :], pos_ap.partition_broadcast(rows - r0).rearrange("p o s -> p (o s)"), [], [pi_], allow_slow_non_contiguous=True)
        self.CP(t[rs, :], pi_[rs, :], [pi_], [t])
        self.TS(t[rs, :], t[rs, :], self.c32[rs, fo + fcol:fo + fcol + 1], None, ALU.mult, None, [t, self.cb], [t])
        for tab, shift, scale in [(Sg, 0.0, self.c32[rs, fo + scol:fo + scol + 1]), (C, 0.25, float(2 * np.pi * (1 - 1e-6)))]:
            if shift:
                self.TS(u[rs, :], t[rs, :], shift, None, ALU.add, None, [t], [u])
            else:
                self.CP(u[rs, :], t[rs, :], [t], [u])
            self.CP(ti[rs, :], u[rs, :], [u], [ti])
            self.CP(tab[rs, :], ti[rs, :], [ti], [tab])
            self.TT(u[rs, :], u[rs, :], tab[rs, :], ALU.subtract, [u, tab], [u])
            self.ACT(tab[rs, :], u[rs, :], AF.Sin, [u, self.cb], [tab], scale=scale)
        self.P.barrier()
    return C, Sg


KB.rope_tables = _rope_tables


def _declare_mix0(self):
    d = self.dram
    self.QT = d("QT", [8, 96, S], BF16)
    self.KT = d("KT", [8, 96, S], BF16)
    self.Vs = d("Vs", [S, 520], BF16)
    self.qintra = d("qintra", [256, S], BF16)
    self.qinter = d("qinter", [256, S], BF16)
    self.kdec = d("kdec", [256, S], BF16)
    self.kdtok = d("kdtok", [S, 256], BF16)
    self.gv = d("gv", [S, 512], BF16)
    self.grs = d("grs", [512, S], BF16)
    self.decd = d("decd", [256, 64], F32)
    self.OTm = d("OTm", [8, 64, S], BF16)
    self.OTg = d("OTg", [4, 128, S], BF16)
    self.m0b = {n: Buf(n) for n in ["QT", "KT", "Vs", "qintra", "qinter", "kdec", "kdtok", "gv", "grs", "decd", "OTm", "OTg"]}


KB.declare_mix0 = _declare_mix0


def _inproj0(self, src):
    P, I = self.P, self.I
    hin_v = self.hT[src].rearrange("(fc p) t -> p fc t", p=128)
    hinb = self.hbuf[src]
    mb = self.m0b
    with ExitStack() as st:
        T = lambda shape, dt, n: self.tile(st, shape, dt, n)
        w_in = T([128, 8, HY_IN], BF16, "w_in")
        for kc in range(8):
            self.LD(w_in[:, kc, :], I["hy_w_in"][kc * 128:(kc + 1) * 128, :], [], [w_in], eng="gpsimd")
        w_uq = T([128, 2, 768], BF16, "w_uq")
        w_uqs = T([128, 2, 768], BF16, "w_uqs")
        uqsrc = I["mla_w_uq"].rearrange("(k p) n -> p k n", p=128)
        self.LD(w_uq[:], uqsrc, [], [w_uq], eng="gpsimd")
        self.LD(w_uqs[:], uqsrc, [], [w_uqs], eng="gpsimd")
        v4 = lambda ap: ap.rearrange("p k (h d) -> p k h d", d=96)
        with self.nc.allow_non_contiguous_dma("small swapped weight blocks"):
            for kc in range(2):
                self.LD(v4(w_uqs[:])[:, kc, :, 64:80], v4(uqsrc)[:, kc, :, 80:96], [], [w_uqs], eng="gpsimd")
                self.LD(v4(w_uqs[:])[:, kc, :, 80:96], v4(uqsrc)[:, kc, :, 64:80], [], [w_uqs], eng="gpsimd")
        w_ukv = T([128, 1024], BF16, "w_ukv")
        self.LD(w_ukv[:], I["mla_w_ukv"], [], [w_ukv], eng="gpsimd")
        wkrs = T([128, 8, 96], BF16, "wkrs")
        insrc = I["hy_w_in"].rearrange("(k p) n -> p k n", p=128)
        with self.nc.allow_non_contiguous_dma("small swapped weight blocks"):
            self.LD(wkrs[:, :, 0:64], insrc[:, :, 320:384], [], [wkrs], eng="gpsimd")
            self.LD(wkrs[:, :, 64:80], insrc[:, :, 400:416], [], [wkrs], eng="gpsimd")
            self.LD(wkrs[:, :, 80:96], insrc[:, :, 384:400], [], [wkrs], eng="gpsimd")
        w_a2 = T([16, 256], BF16, "w_a2")
        self.LD(w_a2[:], I["gla_w_a2"], [], [w_a2], eng="gpsimd")
        cols = T([128, 8], F32, "cols")
        with self.nc.allow_non_contiguous_dma("tiny vectors"):
            self.LD(cols[:, 0:2], I["mla_q_norm"].rearrange("o (k p) -> p (o k)", p=128), [], [cols], allow_slow_non_contiguous=True)
            self.LD(cols[:, 2:3], I["mla_kv_norm"].rearrange("o (k p) -> p (o k)", p=128), [], [cols], allow_slow_non_contiguous=True)
            self.LD(cols[:, 3:5], I["gla_b_a"].rearrange("o (k p) -> p (o k)", p=128), [], [cols], allow_slow_non_contiguous=True)
        self.TS(cols[:, 5:7], cols[:, 3:5], -1.0, None, ALU.mult, None, [cols], [cols])
        C, Sg = self.rope_tables(st, 96, 64, 0, 1, "mla")
        hs = [T([128, 8, TG], F32, "hs") for _ in range(2)]
        uT = T([128, 8, TG], BF16, "uT")
        sq = [T([128, TG], BF16, "sq") for _ in range(2)]
        rstd = T([128, TG], F32, "rstd")
        cqn = T([128, 2, TG], BF16, "cqn")
        ckvn = T([128, TG], BF16, "ckvn")
        qst = [T([96, TG], BF16, "qst") for _ in range(2)]
        t1 = [T([96, TG], F32, "t1") for _ in range(2)]
        t2 = [T([96, TG], F32, "t2") for _ in range(2)]
        kst = T([96, 8, TG], BF16, "kst")
        krot = T([96, TG], F32, "krot")
        vst = T([128, 4, 8, 65], BF16, "vst")
        self.MS(vst[:], 1.0, [vst])
        ga = T([16, TG], BF16, "ga")
        lt = T([128, TG], F32, "lt")
        cs = T([128, TG], F32, "cs")
        dd = T([128, TG], F32, "dd")
        E1 = T([128, TG], F32, "E1")
        E2 = T([128, TG], F32, "E2")
        E3 = T([128, TG], F32, "E3")
        qia = T([128, 2, TG], BF16, "qia")
        qie = T([128, 2, TG], BF16, "qie")
        kde = T([128, 2, TG], BF16, "kde")
        dec = T([128, 2, 64], F32, "dec")
        kdt = T([128, 4, 256], BF16, "kdt")
        gvs = T([128, 4, 512], BF16, "gvs")
        grt = T([128, 4, TG], BF16, "grt")
        ss = self.ptile(st, name="ss")
        A = [self.ptile(st, name="A") for _ in range(2)]
        Bp = [self.ptile(st, name="B") for _ in range(2)]
        Tp = [self.ptile(st, name="T") for _ in range(2)]
        Tb = self.ptile(st, [128, 1024], BF16, name="Tb")
        self.gdeps = [self.gb, cols.b]
        gm = [self.gcol[:, 4, fc:fc + 1] for fc in range(8)]
        scanmask = self.cst("scanmask")
        for g in range(NG):
            gs = slice(g * TG, (g + 1) * TG)
            h_ = hs[g % 2]
            self.LD(h_[:], hin_v[:, :, gs], [hinb], [h_])
            self.norm_ps([(h_[:, fc, :], h_) for fc in range(8)], gm, 1.0 / D, ss, sq, rstd, [uT[:, fc, :] for fc in range(8)], uT)
            for ch in range(2):
                self.PROJ(A[ch][:], [(w_in[:, kc, ch * 128:(ch + 1) * 128], uT[:, kc, :]) for kc in range(8)], [w_in, uT], [A[ch]])
            self.norm_ps([(A[ch][:], A[ch]) for ch in range(2)], [cols[:, ch:ch + 1] for ch in range(2)], 1.0 / 256, ss, sq, rstd,
                         [cqn[:, ch, :] for ch in range(2)], cqn)
            for h in range(8):
                a, b = A[h % 2], Bp[h % 2]
                q_, x1, x2 = qst[h % 2], t1[h % 2], t2[h % 2]
                self.PROJ(a[0:96, :], [(w_uq[:, kc, h * 96:(h + 1) * 96], cqn[:, kc, :]) for kc in range(2)], [w_uq, cqn], [a])
                self.PROJ(b[0:96, :], [(w_uqs[:, kc, h * 96:(h + 1) * 96], cqn[:, kc, :]) for kc in range(2)], [w_uqs, cqn], [b])
                self.CP(q_[0:64, :], a[0:64, :], [a], [q_], eng="scalar")
                self.TT(x1[64:96, :], a[64:96, :], C[64:96, gs], ALU.mult, [a, C], [x1])
                self.TT(x2[64:96, :], b[64:96, :], Sg[64:96, gs], ALU.mult, [b, Sg], [x2])
                self.TT(q_[64:96, :], x1[64:96, :], x2[64:96, :], ALU.add, [x1, x2], [q_], eng="gpsimd")
                self.LD(self.QT[h, :, gs], q_[:], [q_], [mb["QT"]], eng="gpsimd")
            self.PROJ(A[0][:], [(w_in[:, kc, 256:384], uT[:, kc, :]) for kc in range(8)], [w_in, uT], [A[0]])
            self.norm_ps([(A[0][:], A[0])], [cols[:, 2:3]], 1.0 / 128, ss, sq, rstd, [ckvn[:]], ckvn)
            for h in range(8):
                a = A[h % 2]
                self.MM(a[0:64, :], w_ukv[:, h * 128:h * 128 + 64], ckvn[:], True, True, [w_ukv, ckvn], [a])
                self.CP(kst[0:64, h, :], a[0:64, :], [a], [kst], eng=("scalar" if h % 2 else "vector"))
            self.PROJ(A[0][0:96, :], [(w_in[:, kc, 320:416], uT[:, kc, :]) for kc in range(8)], [w_in, uT], [A[0]])
            self.PROJ(Bp[0][0:96, :], [(wkrs[:, kc, :], uT[:, kc, :]) for kc in range(8)], [wkrs, uT], [Bp[0]])
            self.TT(t1[0][64:96, :], A[0][64:96, :], C[64:96, gs], ALU.mult, [A[0], C], [t1[0]])
            self.TT(t2[0][64:96, :], Bp[0][64:96, :], Sg[64:96, gs], ALU.mult, [Bp[0], Sg], [t2[0]])
            self.TT(krot[64:96, :], t1[0][64:96, :], t2[0][64:96, :], ALU.add, [t1[0], t2[0]], [krot], eng="gpsimd")
            self.CP(kst[64:96, :, :], krot[64:96, :].unsqueeze(1).broadcast_to([32, 8, TG]), [krot], [kst], eng="gpsimd")
            self.LD(self.KT[:, :, gs].rearrange("h r t -> r h t"), kst[:], [kst], [mb["KT"]], eng="gpsimd")
            wv = w_ukv[:].rearrange("p (h t d) -> p h t d", t=2, d=64)[:, :, 1, :]
            for t in range(4):
                tp = Tp[t % 2]
                self.MM(tp[:].rearrange("p (h d) -> p h d", d=64), ckvn[:, t * 128:(t + 1) * 128], wv, True, True, [ckvn, w_ukv], [tp])
                self.CP(vst[:, t, :, 0:64], tp[:].rearrange("p (h d) -> p h d", d=64), [tp], [vst], eng=("scalar" if t % 2 else "vector"))
            self.LD(self.Vs[gs, :].rearrange("(t p) f -> p t f", p=128), vst[:].rearrange("p t h d -> p t (h d)"), [vst], [mb["Vs"]], eng="gpsimd")
            for ch in range(2):
                self.PROJ(A[ch][:], [(w_in[:, kc, 416 + ch * 128:416 + (ch + 1) * 128], uT[:, kc, :]) for kc in range(8)], [w_in, uT], [A[ch]])
                self.PROJ(Bp[ch][:], [(w_in[:, kc, 672 + ch * 128:672 + (ch + 1) * 128], uT[:, kc, :]) for kc in range(8)], [w_in, uT], [Bp[ch]])
            self.PROJ(Tp[0][0:16, :], [(w_in[:, kc, 1440:1456], uT[:, kc, :]) for kc in range(8)], [w_in, uT], [Tp[0]])
            self.CP(ga[:], Tp[0][0:16, :], [Tp[0]], [ga])
            for ch in range(2):
                tp = Tp[1]
                self.MM(tp[:], w_a2[0:16, ch * 128:(ch + 1) * 128], ga[0:16, :], True, True, [w_a2, ga], [tp])
                self.ACT(lt[:], tp[:], AF.Exp, [tp, cols], [lt], scale=-1.0, bias=cols[:, 5 + ch:6 + ch])
                self.ACT(lt[:], lt[:], AF.Ln, [lt], [lt], scale=1.0, bias=1.0)
                self.P.dve(lambda e: e.tensor_tensor_scan(out=cs[:], data0=scanmask, data1=lt[:], initial=0.0, op0=ALU.mult, op1=ALU.add),
                           reads=[lt.b, self.cb], writes=[cs.b])
                cs3 = cs[:].rearrange("p (c k) -> p c k", k=64)
                self.TT(dd[:].rearrange("p (c k) -> p c k", k=64), cs3, cs3[:, :, 63:64].broadcast_to([128, 8, 64]), ALU.subtract, [cs], [dd])
                self.ACT(E1[:], dd[:], AF.Exp, [dd], [E1], scale=-1.0 / 16)
                self.ACT(E2[:], dd[:], AF.Exp, [dd], [E2], scale=1.0 / 16)
                self.ACT(E3[:], cs[:], AF.Exp, [cs], [E3], scale=-1.0 / 16)
                self.STT(qia[:, ch, :], A[ch][:], 0.125, E1[:], ALU.mult, ALU.mult, [A[ch], E1], [qia])
                self.STT(qie[:, ch, :], A[ch][:], 0.125, E3[:], ALU.mult, ALU.mult, [A[ch], E3], [qie])
                self.TT(kde[:, ch, :], Bp[ch][:], E2[:], ALU.mult, [Bp[ch], E2], [kde])
                self.CP(dec[:, ch, g * 8:(g + 1) * 8], E3[:].rearrange("p (c k) -> p c k", k=64)[:, :, 63], [E3], [dec], eng="gpsimd")
            fm = lambda dr: dr.rearrange("(c p) t -> p c t", p=128)[:, :, gs]
            self.LD(fm(self.qintra), qia[:], [qia], [mb["qintra"]], eng="gpsimd")
            self.LD(fm(self.qinter), qie[:], [qie], [mb["qinter"]], eng="gpsimd")
            self.LD(fm(self.kdec), kde[:], [kde], [mb["kdec"]], eng="gpsimd")
            for t in range(4):
                for ch in range(2):
                    self.TR(Tb[:, (t % 4) * 256 + ch * 128:(t % 4) * 256 + (ch + 1) * 128], kde[:, ch, t * 128:(t + 1) * 128], self.ident_bf[:],
                            [kde, self.cbf], [Tb])
            self.CP(kdt[:].rearrange("p t f -> p (t f)"), Tb[:], [Tb], [kdt])
            self.LD(self.kdtok[gs, :].rearrange("(t p) f -> p t f", p=128), kdt[:], [kdt], [mb["kdtok"]], eng="gpsimd")
            for t in range(4):
                tp = Tp[t % 2]
                self.PROJ(tp[:], [(uT[:, kc, t * 128:(t + 1) * 128], w_in[:, kc, 928:1440]) for kc in range(8)], [w_in, uT], [tp])
                self.CP(gvs[:, t, :], tp[:], [tp], [gvs], eng=("scalar" if t % 2 else "vector"))
            self.LD(self.gv[gs, :].rearrange("(t p) f -> p t f", p=128), gvs[:], [gvs], [mb["gv"]], eng="gpsimd")
            for hh in range(4):
                a = A[hh % 2]
                self.PROJ(a[:], [(w_in[:, kc, 1456 + hh * 128:1456 + (hh + 1) * 128], uT[:, kc, :]) for kc in range(8)], [w_in, uT], [a])
                self.ACT(grt[:, hh, :], a[:], AF.Silu, [a], [grt])
            self.LD(self.grs.rearrange("(c p) t -> p c t", p=128)[:, :, gs], grt[:], [grt], [mb["grs"]], eng="gpsimd")
        self.LD(self.decd.rearrange("(c p) n -> p c n", p=128), dec[:], [dec], [mb["decd"]], eng="gpsimd")
    self.gdeps = []
    P.barrier()


KB.inproj0 = _inproj0


class U:
    __slots__ = ("A", "B", "C", "later")

    def __init__(self, A=None, B=None, C=None, later=None):
        self.A, self.B, self.C, self.later = A, B, C, later


def run_units(units, look):
    n = len(units)
    sched = {}
    for i in range(min(look, n)):
        if units[i].A:
            units[i].A()
    for i in range(n):
        if i + look < n and units[i + look].A:
            units[i + look].A()
        if units[i].B:
            units[i].B()
        if units[i].C:
            units[i].C()
        for (dl, fn) in (units[i].later or []):
            sched.setdefault(i + dl, []).append(fn)
        for fn in sched.pop(i, []):
            fn()
    for k in sorted(sched):
        for fn in sched[k]:
            fn()


def _attn_units(self, units, o, sp_list, pt_list, cnt, Kt, Qt, Vfn, qg, scale, ktiles, dk, pre=None):
    nk = len(ktiles)
    for i, kt in enumerate(ktiles):
        d = kt - 4 * qg
        sp = sp_list[cnt[0] % len(sp_list)]
        pt = pt_list[cnt[0] % len(pt_list)]
        cnt[0] += 1
        kc = slice(kt * 128, (kt + 1) * 128)
        q0 = qg * TG
        c0 = max(d, 0) * 128

        def A(sp=sp, kc=kc, d=d, c0=c0, q0=q0, pre=(pre if i == 0 else None)):
            if pre is not None:
                pre()
            if d < 0:
                self.MM(sp[:], Kt[0:dk, kc], Qt[0:dk, q0:q0 + TG], True, True, [Kt, Qt], [sp])
            else:
                self.MM(sp[:, c0:c0 + 128], Kt[0:dk, kc], Qt[0:dk, q0 + c0:q0 + c0 + 128], True, False, [Kt, Qt], [sp])
                self.MM(sp[:, c0:c0 + 128], self.ident_bf[:], self.cpen_bf[:], False, True, [self.cbf], [sp])
                if c0 + 128 < TG:
                    self.MM(sp[:, c0 + 128:TG], Kt[0:dk, kc], Qt[0:dk, q0 + c0 + 128:q0 + TG], True, True, [Kt, Qt], [sp])

        def B(sp=sp, pt=pt, c0=c0):
            self.ACT(pt[:, c0:TG], sp[:, c0:TG], AF.Exp, [sp], [pt], scale=scale)

        def C(pt=pt, c0=c0, kt=kt, i=i):
            self.MM(o[0:65, c0:TG], Vfn(kt), pt[:, c0:TG], i == 0, i == nk - 1, [pt, self.vdep], [o])
        units.append(U(A, B, C))


KB.attn_units = _attn_units


def _mla(self):
    P = self.P
    mb = self.m0b
    with ExitStack() as st:
        T = lambda shape, dt, n: self.tile(st, shape, dt, n)
        Vall = T([128, 32, 520], BF16, "Vall")
        for q4 in range(4):
            self.LD(Vall[:, q4 * 8:(q4 + 1) * 8, :], self.Vs[q4 * 1024:(q4 + 1) * 1024, :].rearrange("(n p) f -> p n f", p=128), [mb["Vs"]], [Vall])
        self.vdep = Vall
        KTh = [T([96, S], BF16, "KTh") for _ in range(2)]
        QTh = [T([96, S], BF16, "QTh") for _ in range(2)]
        PT = [T([128, TG], BF16, "PT") for _ in range(3)]
        rr2 = [T([65, TG], F32, "rr") for _ in range(2)]
        fb2 = [T([65, TG], BF16, "fb") for _ in range(2)]
        bcs2 = [T([64, TG], F32, "bcs") for _ in range(2)]
        ost = [T([64, TG], BF16, "ost") for _ in range(2)]
        Sp = [self.ptile(st, name="S") for _ in range(3)]
        Op = [self.ptile(st, name="O") for _ in range(3)]
        bc2 = [self.ptile(st, name="bc") for _ in range(2)]
        ones32 = self.cst("ones")
        cnt = [0]
        k = 0
        units = []

        def loader(h):
            def f():
                self.LD(KTh[h % 2][:], self.KT[h], [mb["KT"]], [KTh[h % 2]])
                self.LD(QTh[h % 2][:], self.QT[h], [mb["QT"]], [QTh[h % 2]])
            return f
        loader(0)()
        for h in range(8):
            kt_, qt_ = KTh[h % 2], QTh[h % 2]
            for qg in range(NG):
                o = Op[k % 3]
                pre = loader(h + 1) if (qg == 0 and h + 1 < 8) else None
                self.attn_units(units, o, Sp, PT, cnt, kt_, qt_, lambda kt, h=h: Vall[:, kt, h * 65:(h + 1) * 65], qg, 96 ** -0.5,
                                list(range(4 * qg + 4)), 96, pre=pre)

                rr_, fb_, bc_, bs_ = rr2[k % 2], fb2[k % 2], bc2[k % 2], bcs2[k % 2]

                def f0(o=o, rr_=rr_, fb_=fb_):
                    self.ACT(rr_[64:65, :], o[64:65, :], AF.Ln, [o], [rr_])
                    self.ACT(fb_[64:65, :], rr_[64:65, :], AF.Exp, [rr_], [fb_], scale=-1.0)

                def f1(fb_=fb_, bc_=bc_, bs_=bs_):
                    self.MM(bc_[0:64, :], self.ones_bf[64:65, 0:64], fb_[64:65, :], True, True, [fb_, self.cbf], [bc_])
                    self.CP(bs_[:], bc_[0:64, :], [bc_], [bs_], eng="vector")

                def f2(o=o, h=h, qg=qg, os_=ost[k % 2], bs_=bs_):
                    self.TT(os_[:], o[0:64, :], bs_[:], ALU.mult, [o, bs_], [os_])
                    self.LD(self.OTm[h, :, qg * TG:(qg + 1) * TG], os_[:], [os_], [mb["OTm"]], eng="gpsimd")
                units.append(U(None, None, None, later=[(1, f0), (3, f1), (4, f2)]))
                k += 1
        run_units(units, 2)
    P.barrier()


KB.mla = _mla


def _gla(self):
    P = self.P
    mb = self.m0b
    with ExitStack() as st:
        T = lambda shape, dt, n: self.tile(st, shape, dt, n)
        hv = lambda dr, gs: dr.rearrange("(h d) t -> d h t", d=64)[:, :, gs]
        qia = [T([64, 4, TG], BF16, "qia") for _ in range(2)]
        qie = [T([64, 4, TG], BF16, "qie") for _ in range(2)]
        kde = [T([64, 4, TG], BF16, "kde") for _ in range(2)]
        vv = [T([64, 8, 512], BF16, "vv") for _ in range(2)]
        kdt = [T([64, 8, 256], BF16, "kdt") for _ in range(2)]
        grs = [T([128, 4, TG], BF16, "grs") for _ in range(2)]
        dec = T([64, 4, 64], F32, "dec")
        self.LD(dec[:], self.decd.rearrange("(h d) n -> d h n", d=64), [mb["decd"]], [dec])
        onc = T([128, 1], F32, "onc")
        with self.nc.allow_non_contiguous_dma("tiny"):
            self.LD(onc[:], self.I["gla_out_norm"].rearrange("o p -> p o"), [], [onc], allow_slow_non_contiguous=True)
        St = T([64, 4, 128], F32, "St")
        Sbf = T([64, 4, 128], BF16, "Sbf")
        self.MS(St[:], 0.0, [St])
        self.MS(Sbf[:], 0.0, [Sbf])
        ats = [T([64, 256], BF16, "ats") for _ in range(2)]
        sq = [T([128, TG], BF16, "sq") for _ in range(2)]
        rstd = T([128, TG], F32, "rstd")
        on = [T([128, TG], F32, "on") for _ in range(2)]
        ost = [T([128, TG], BF16, "ost") for _ in range(2)]
        Op = [self.ptile(st, name="O") for _ in range(4)]
        at_t = self.ps(st, [64, 512], F32, "at")
        at = [Tl(at_t, "at0"), Tl(at_t, "at1")]
        kv = [self.ptile(st, [64, 512], F32, name="kv") for _ in range(2)]
        ss = self.ptile(st, name="ss")
        tri = self.cst("tri", rows=64)
        self.gdeps = [onc.b]
        for g in range(NG):
            gs = slice(g * TG, (g + 1) * TG)
            b = g % 2
            self.LD(qia[b][:], hv(self.qintra, gs), [mb["qintra"]], [qia[b]])
            self.LD(qie[b][:], hv(self.qinter, gs), [mb["qinter"]], [qie[b]])
            self.LD(kde[b][:], hv(self.kdec, gs), [mb["kdec"]], [kde[b]])
            self.LD(vv[b][:], self.gv[gs, :].rearrange("(c p) f -> p c f", p=64), [mb["gv"]], [vv[b]])
            self.LD(kdt[b][:], self.kdtok[gs, :].rearrange("(c p) f -> p c f", p=64), [mb["kdtok"]], [kdt[b]])
            self.LD(grs[b][:], self.grs.rearrange("(c p) t -> p c t", p=128)[:, :, gs], [mb["grs"]], [grs[b]])
            for c in range(8):
                n = g * 8 + c
                cs_ = slice(c * 64, (c + 1) * 64)
                a_ = at[c % 2]
                ao = (c % 2) * 256
                for h in range(4):
                    self.MM(a_[0:64, ao + h * 64:ao + (h + 1) * 64], kde[b][:, h, cs_], qia[b][:, h, cs_], True, True, [kde[b], qia[b]], [a_])
                as_ = ats[c % 2]
                self.TT(as_[:].rearrange("p (h i) -> p h i", i=64), a_[0:64, ao:ao + 256].rearrange("p (h i) -> p h i", i=64),
                        tri.unsqueeze(1).broadcast_to([64, 4, 64]), ALU.mult, [a_, self.cb], [as_])
                for h in range(4):
                    self.MM(Op[h][:, cs_], vv[b][:, c, h * 128:(h + 1) * 128], as_[:, h * 64:(h + 1) * 64], True, False, [vv[b], as_], [Op[h]])
                    self.MM(Op[h][:, cs_], Sbf[:, h, :], qie[b][:, h, cs_], False, True, [Sbf, qie[b]], [Op[h]])
                kv_ = kv[c % 2]
                for h in range(4):
                    self.MM(kv_[0:64, h * 128:(h + 1) * 128], kdt[b][:, c, h * 64:(h + 1) * 64], vv[b][:, c, h * 128:(h + 1) * 128], True, True,
                            [kdt[b], vv[b]], [kv_])
                self.TT(St[:], St[:], dec[:, :, n:n + 1].broadcast_to([64, 4, 128]), ALU.mult, [St, dec], [St])
                self.TT(St[:].rearrange("p h v -> p (h v)"), St[:].rearrange("p h v -> p (h v)"), kv_[0:64, :], ALU.add, [St, kv_], [St])
                self.CP(Sbf[:], St[:], [St], [Sbf], eng="scalar")
            for h in range(4):
                o = Op[h]
                self.norm_ps([(o[:], o)], [onc[:, 0:1]], 1.0 / 128, ss, sq, rstd, [on[h % 2][:]], on[h % 2])
                os_ = ost[h % 2]
                self.TT(os_[:], on[h % 2][:], grs[b][:, h, :], ALU.mult, [on[h % 2], grs[b]], [os_], eng="gpsimd")
                self.LD(self.OTg[h, :, gs], os_[:], [os_], [mb["OTg"]], eng="gpsimd")
    self.gdeps = []
    P.barrier()


KB.gla = _gla


def _outproj(self, src, dst, w_src, otm_d, otm_b, n_h64, otg_d=None, otg_b=None):
    P = self.P
    hin_v = self.hT[src].rearrange("(fc p) t -> p fc t", p=128)
    hout_v = self.hT[dst].rearrange("(fc p) t -> p fc t", p=128)
    with ExitStack() as st:
        T = lambda shape, dt, n: self.tile(st, shape, dt, n)
        wm = T([64, n_h64, D], BF16, "wm")
        half = n_h64 // 2
        for i in range(2):
            self.LD(wm[:, i * half:(i + 1) * half, :], w_src[i * half * 64:(i + 1) * half * 64, :].rearrange("(h r) n -> r h n", r=64), [], [wm], eng="gpsimd")
        ng = 0
        if otg_d is not None:
            ng = 4
            wg = T([128, 4, D], BF16, "wg")
            self.LD(wg[:], w_src[n_h64 * 64:, :].rearrange("(c p) n -> p c n", p=128), [], [wg], eng="gpsimd")
        hs = [T([128, 8, TG], F32, "hs") for _ in range(2)]
        om = [T([64, n_h64, TG], BF16, "om") for _ in range(2)]
        og = [T([128, 4, TG], BF16, "og") for _ in range(2)] if ng else None
        ho = [T([128, TG], F32, "ho") for _ in range(3)]
        Op = [self.ptile(st, name="O") for _ in range(2)]
        k = 0
        for g in range(NG):
            gs = slice(g * TG, (g + 1) * TG)
            b = g % 2
            self.LD(hs[b][:], hin_v[:, :, gs], [self.hbuf[src]], [hs[b]])
            self.LD(om[b][:], otm_d[:, :, gs].rearrange("h r t -> r h t"), [otm_b], [om[b]])
            if ng:
                self.LD(og[b][:], otg_d[:, :, gs].rearrange("h r t -> r h t"), [otg_b], [og[b]])
            for fc in range(8):
                o = Op[fc % 2]
                fcs = slice(fc * 128, (fc + 1) * 128)
                pairs = [(wm[:, h, fcs], om[b][:, h, :]) for h in range(n_h64)]
                deps = [wm, om[b]]
                if ng:
                    pairs += [(wg[:, c, fcs], og[b][:, c, :]) for c in range(4)]
                    deps += [wg, og[b]]
                self.PROJ(o[:], pairs, deps, [o])
                h_ = ho[k % 3]
                k += 1
                self.TT(h_[:], o[:], hs[b][:, fc, :], ALU.add, [o, hs[b]], [h_])
                self.LD(hout_v[:, fc, gs], h_[:], [h_], [self.hbuf[dst]], eng="gpsimd")
                self.pump(3)
    P.barrier()


KB.outproj = _outproj


def _declare_mix1(self):
    d = self.dram
    self.QN = d("QN", [1024, S], BF16)
    self.KSd = d("KSd", [256, S], BF16)
    self.KWd = d("KWd", [256, S], BF16)
    self.KCd = d("KCd", [256, S], BF16)
    self.VCd = d("VCd", [256, S], BF16)
    self.VSW = d("VSW", [S, 520], BF16)
    self.GT = d("GT", [48, S], F32)
    self.ONT = d("ONT", [16, 64, S], BF16)
    self.m1b = {n: Buf(n) for n in ["QN", "KSd", "KWd", "KCd", "VCd", "VSW", "GT", "ONT"]}


KB.declare_mix1 = _declare_mix1


def _inproj1(self, src):
    P, I = self.P, self.I
    hin_v = self.hT[src].rearrange("(fc p) t -> p fc t", p=128)
    hinb = self.hbuf[src]
    mb = self.m1b
    with ExitStack() as st:
        T = lambda shape, dt, n: self.tile(st, shape, dt, n)
        w_in = T([128, 8, NSA_IN], BF16, "w_in")
        w_sw = T([128, 8, 1536], BF16, "w_sw")
        insrc = I["nsa_w_in"].rearrange("(k p) n -> p k n", p=128)
        with self.nc.allow_non_contiguous_dma("swapped rope halves"):
            for kc in range(8):
                self.LD(w_in[:, kc, :], I["nsa_w_in"][kc * 128:(kc + 1) * 128, :], [], [w_in], eng="gpsimd")
                for (d0, s0, nb) in [(0, 0, 16), (1024, 1536, 4), (1280, 2048, 4)]:
                    dv = w_sw[:, kc, d0:d0 + nb * 64].rearrange("p (b t e) -> p b t e", t=2, e=32)
                    sv = insrc[:, kc, s0:s0 + nb * 64].rearrange("p (b t e) -> p b t e", t=2, e=32)
                    self.LD(dv[:, :, 0, :], sv[:, :, 1, :], [], [w_sw], eng="gpsimd")
                    self.LD(dv[:, :, 1, :], sv[:, :, 0, :], [], [w_sw], eng="gpsimd")
        C, Sg = self.rope_tables(st, 128, 0, 2, 3, "nsa")
        self.nsaC, self.nsaS = C, Sg
        hs = [T([128, 8, TG], F32, "hs") for _ in range(2)]
        uT = T([128, 8, TG], BF16, "uT")
        sq = [T([128, TG], BF16, "sq") for _ in range(2)]
        rstd = T([128, TG], F32, "rstd")
        t1 = [T([128, TG], F32, "t1") for _ in range(2)]
        t2 = [T([128, TG], F32, "t2") for _ in range(2)]
        qst = [T([128, TG], BF16, "qst") for _ in range(3)]
        vst = T([128, 4, 8, 65], BF16, "vst")
        self.MS(vst[:], 1.0, [vst])
        gts = T([48, TG], F32, "gts")
        ss = self.ptile(st, name="ss")
        A = [self.ptile(st, name="A") for _ in range(2)]
        Bp = [self.ptile(st, name="B") for _ in range(2)]
        Tp = [self.ptile(st, name="T") for _ in range(2)]
        self.gdeps = [self.gb]
        gm = [self.gcol[:, 5, fc:fc + 1] for fc in range(8)]
        k = 0
        for g in range(NG):
            gs = slice(g * TG, (g + 1) * TG)
            h_ = hs[g % 2]
            self.LD(h_[:], hin_v[:, :, gs], [hinb], [h_])
            self.norm_ps([(h_[:, fc, :], h_) for fc in range(8)], gm, 1.0 / D, ss, sq, rstd, [uT[:, fc, :] for fc in range(8)], uT)
            jobs = [(c * 128, c * 128, self.QN, c, "QN") for c in range(8)]
            jobs += [(1536 + c * 128, 1024 + c * 128, self.KSd, c, "KSd") for c in range(2)]
            jobs += [(2048 + c * 128, 1280 + c * 128, self.KWd, c, "KWd") for c in range(2)]
            for (ca, cb_, dst, c, nm) in jobs:
                a, b = A[k % 2], Bp[k % 2]
                x1, x2, q_ = t1[k % 2], t2[k % 2], qst[k % 3]
                k += 1
                self.PROJ(a[:], [(w_in[:, kc, ca:ca + 128], uT[:, kc, :]) for kc in range(8)], [w_in, uT], [a])
                self.PROJ(b[:], [(w_sw[:, kc, cb_:cb_ + 128], uT[:, kc, :]) for kc in range(8)], [w_sw, uT], [b])
                self.TT(x1[:], a[:], C[:, gs], ALU.mult, [a, C], [x1])
                self.TT(x2[:], b[:], Sg[:, gs], ALU.mult, [b, Sg], [x2])
                self.TT(q_[:], x1[:], x2[:], ALU.add, [x1, x2], [q_], eng="gpsimd")
                self.LD(dst[c * 128:(c + 1) * 128, gs], q_[:], [q_], [mb[nm]], eng="gpsimd")
            for (ca, dst, c, nm) in [(1024, self.KCd, 0, "KCd"), (1152, self.KCd, 1, "KCd"), (1280, self.VCd, 0, "VCd"), (1408, self.VCd, 1, "VCd")]:
                a = A[k % 2]
                q_ = qst[k % 3]
                k += 1
                self.PROJ(a[:], [(w_in[:, kc, ca:ca + 128], uT[:, kc, :]) for kc in range(8)], [w_in, uT], [a])
                self.CP(q_[:], a[:], [a], [q_], eng="scalar")
                self.LD(dst[c * 128:(c + 1) * 128, gs], q_[:], [q_], [mb[nm]], eng="gpsimd")
            for t in range(4):
                tp = Tp[t % 2]
                self.PROJ(tp[:, 0:256], [(uT[:, kc, t * 128:(t + 1) * 128], w_in[:, kc, 1792:2048]) for kc in range(8)], [w_in, uT], [tp])
                self.PROJ(tp[:, 256:512], [(uT[:, kc, t * 128:(t + 1) * 128], w_in[:, kc, 2304:2560]) for kc in range(8)], [w_in, uT], [tp])
                self.CP(vst[:, t, :, 0:64], tp[:].rearrange("p (h d) -> p h d", d=64), [tp], [vst], eng=("scalar" if t % 2 else "vector"))
            self.LD(self.VSW[gs, :].rearrange("(t p) f -> p t f", p=128), vst[:].rearrange("p t h d -> p t (h d)"), [vst], [mb["VSW"]], eng="gpsimd")
            self.PROJ(A[0][0:48, :], [(w_in[:, kc, 2560:2608], uT[:, kc, :]) for kc in range(8)], [w_in, uT], [A[0]])
            self.ACT(gts[:], A[0][0:48, :], AF.Sigmoid, [A[0]], [gts])
            self.LD(self.GT[:, gs], gts[:], [gts], [mb["GT"]], eng="gpsimd")
    self.gdeps = []
    P.barrier()


KB.inproj1 = _inproj1


def _nsa_attn(self):
    P, I = self.P, self.I
    mb = self.m1b
    SC = 64 ** -0.5
    with ExitStack() as st:
        T = lambda shape, dt, n: self.tile(st, shape, dt, n)
        VSW = T([128, 32, 520], BF16, "VSW")
        for q4 in range(4):
            self.LD(VSW[:, q4 * 8:(q4 + 1) * 8, :], self.VSW[q4 * 1024:(q4 + 1) * 1024, :].rearrange("(n p) f -> p n f", p=128), [mb["VSW"]], [VSW])
        self.vdep = VSW
        cmpm = T([128, 2, S], BF16, "cmpm")
        for i in range(2):
            self.LD(cmpm[:, i, :], I["cmpmask"][i * 128:(i + 1) * 128, :], [], [cmpm], eng="gpsimd")
        ovl = T([128, 2, 65], BF16, "ovl")
        self.LD(ovl[:], I["ovl"].rearrange("(i p) f -> p i f", p=128), [], [ovl], eng="gpsimd")
        eo = CB["E"][0]
        KCMP = T([64, 4, 256], BF16, "KCMP")
        VCMP = T([128, 4, 2, 65], BF16, "VCMP")
        self.MS(KCMP[:], 0.0, [KCMP])
        self.MS(VCMP[:], 0.0, [VCMP])
        Sp = [self.ptile(st, name="S") for _ in range(3)]
        Oc = self.ptile(st, name="Oc")
        Os = self.ptile(st, name="Os")
        Ow = self.ptile(st, name="Ow")
        imp = Os
        M1 = self.ptile(st, name="M1")
        M2 = self.ptile(st, name="M2")
        with ExitStack() as st2:
            T2 = lambda shape, dt, n: self.tile(st2, shape, dt, n)
            Cc, Sc = self.rope_tables(st2, 64, 0, 2, 3, "cmp", pos_ap=I["positions"][0:1, 31:S:16], S=255)
            w1 = [T2([64, 32, 128], BF16, "w1") for _ in range(2)]
            w2 = [T2([128, 64], BF16, "w2") for _ in range(2)]
            w2s = T2([128, 64], BF16, "w2s")
            posT = [T2([64, 32], BF16, "posT") for _ in range(2)]
            with self.nc.allow_non_contiguous_dma("small"):
                for i, (a, b_, pp) in enumerate([("nsa_ck_w1", "nsa_ck_w2", "nsa_pos_k"), ("nsa_cv_w1", "nsa_cv_w2", "nsa_pos_v")]):
                    self.LD(w1[i][:], I[a].rearrange("(l d) n -> d l n", d=64), [], [w1[i]], eng="gpsimd")
                    self.LD(w2[i][:], I[b_], [], [w2[i]], eng="gpsimd")
                    self.LD(posT[i][:], I[pp].rearrange("l d -> d l"), [], [posT[i]], eng="gpsimd", allow_slow_non_contiguous=True)
                self.LD(w2s[:, 0:32], I["nsa_ck_w2"][:, 32:64], [], [w2s], eng="gpsimd")
                self.LD(w2s[:, 32:64], I["nsa_ck_w2"][:, 0:32], [], [w2s], eng="gpsimd")
            cb_ = T2([128, 2], F32, "cbias")
            for i in range(2):
                for l in range(32):
                    self.MM(M1[:, i:i + 1], w1[i][:, l, :], posT[i][:, l:l + 1], l == 0, l == 31, [w1[i], posT[i]], [M1])
            self.CP(cb_[:], M1[:, 0:2], [M1], [cb_])
            src = [T2([64, S], BF16, "csrc") for _ in range(2)]
            hid = [T2([128, 256], BF16, "hid") for _ in range(2)]
            x1 = T2([64, 256], F32, "x1")
            x2 = T2([64, 256], F32, "x2")
            for i in range(2):
                self.MS(hid[i][:], 0.0, [hid[i]])
            k = 0
            for g in range(4):
                for i, (dsrc, nm) in enumerate([(self.KCd, "KCd"), (self.VCd, "VCd")]):
                    s_ = src[k % 2]
                    hd = hid[k % 2]
                    ps_ = Sp[k % 2]
                    k += 1
                    self.LD(s_[:], dsrc[g * 64:(g + 1) * 64, :], [mb[nm]], [s_])
                    for l in range(32):
                        self.MM(ps_[:, 0:255], w1[i][:, l, :], s_[:, l:l + 16 * 254 + 1:16], l == 0, l == 31, [w1[i], s_], [ps_])
                    self.ACT(hd[:, 0:255], ps_[:, 0:255], AF.Silu, [ps_, cb_], [hd], bias=cb_[:, i:i + 1], scale=1.0)
                    if i == 0:
                        self.MM(M1[0:64, 0:255], w2[0][:], hd[:, 0:255], True, True, [w2[0], hd], [M1])
                        self.MM(M2[0:64, 0:255], w2s[:], hd[:, 0:255], True, True, [w2s, hd], [M2])
                        self.TT(x1[:, 0:255], M1[0:64, 0:255], Cc[0:64, :], ALU.mult, [M1, Cc], [x1])
                        self.TT(x2[:, 0:255], M2[0:64, 0:255], Sc[0:64, :], ALU.mult, [M2, Sc], [x2])
                        self.TT(KCMP[:, g, 0:255], x1[:, 0:255], x2[:, 0:255], ALU.add, [x1, x2], [KCMP], eng="gpsimd")
                    else:
                        self.MM(M1[:, 0:64], hd[:, 0:128], w2[1][:], True, True, [w2[1], hd], [M1])
                        self.MM(M1[0:127, 64:128], hd[:, 128:255], w2[1][:], True, True, [w2[1], hd], [M1])
                        self.CP(VCMP[:, g, 0, 0:64], M1[:, 0:64], [M1], [VCMP])
                        self.CP(VCMP[0:127, g, 1, 0:64], M1[0:127, 64:128], [M1], [VCMP])
                        self.MS(VCMP[:, g, 0, 64:65], 1.0, [VCMP])
                        self.MS(VCMP[0:127, g, 1, 64:65], 1.0, [VCMP])
            P.barrier()
        KS = [T([128, S], BF16, "KS") for _ in range(2)]
        for i in range(2):
            self.LD(KS[i][64:128, :], I["consts"][0:64, eo:eo + 32 * 128], [], [KS[i]], eng="gpsimd")
        KW = [T([64, S], BF16, "KW") for _ in range(2)]
        PT = [T([128, TG], BF16, "PT") for _ in range(3)]
        PC = [T([128, 2, TG], BF16, "PC") for _ in range(2)]
        ost = [T([64, TG], BF16, "ost") for _ in range(2)]
        irec = T([128, 4], F32, "irec")
        itmp = T([128, 4, 64], F32, "itmp")
        iacc = T([128, 4, 64], F32, "iacc")
        score = T([128, 4, 64], F32, "score")
        sc2 = T([128, 64], F32, "sc2")
        m8 = T([128, 16], F32, "m8")
        ones32 = self.cst("ones")
        so = CB["sel48"][0]
        wo = CB["wide"][0]
        col0 = self.cst("col0")
        cnt = [0]
        hk = 0
        ak = 0

        A4 = [T([64, TG], F32, "A4") for _ in range(4)]
        Q4 = [T([128, S], BF16, "Q4") for _ in range(4)]
        Q4n = [Tl(q.t, "Q4n") for q in Q4]
        nself = T([128, 4, 128], F32, "nself")
        self.MS(nself[:], 0.0, [nself])
        ident32 = self.cst("ident")

        selbf = T([48, 48 * 64 + 1], BF16, "selbf")
        self.LD(selbf[:], I["consts"][0:48, so:so + 48 * 64 + 1], [], [selbf], eng="gpsimd")
        GTb = T([48, S], BF16, "GTb")
        self.LD(GTb[:], self.GT, [mb["GT"]], [GTb], eng="gpsimd")
        Mb = [M1, M2]
        Ocs = [Oc, Ow]
        rr2 = [T([65, TG], F32, "rr") for _ in range(2)]
        fb2 = [T([65, TG], BF16, "fb") for _ in range(2)]
        bcs2 = [T([64, TG], F32, "bcs") for _ in range(2)]
        tmp2 = [T([64, TG], F32, "tmp") for _ in range(2)]
        fi = [0]

        def finish_unit(o, row, a_, first, qs, pre=None, extra=None, delays=(1, 2)):
            k = fi[0]
            fi[0] += 1
            m, rr_, fb_, bs_, tmp_ = Mb[k % 2], rr2[k % 2], fb2[k % 2], bcs2[k % 2], tmp2[k % 2]

            def f0():
                if pre is not None:
                    pre()
                if first:
                    self.TS(rr_[64:65, :], o[64:65, :], 1e-18, None, ALU.max, None, [o], [rr_])
                    self.ACT(rr_[64:65, :], rr_[64:65, :], AF.Ln, [rr_], [rr_])
                else:
                    self.ACT(rr_[64:65, :], o[64:65, :], AF.Ln, [o], [rr_])
                self.ACT(rr_[64:65, :], rr_[64:65, :], AF.Exp, [rr_], [rr_], scale=-1.0)
                self.MM(m[0:65, :], selbf[0:48, row * 64:row * 64 + 65], GTb[0:48, qs], True, True, [GTb, selbf], [m])

            def f1():
                self.TT(fb_[64:65, :], m[64:65, :], rr_[64:65, :], ALU.mult, [m, rr_], [fb_])
                self.MM(m[0:64, :], self.ones_bf[64:65, 0:64], fb_[64:65, :], True, True, [fb_, self.cbf], [m])

            def f2():
                self.CP(bs_[:], m[0:64, :], [m], [bs_], eng="vector")
                if first:
                    self.TT(a_[:], o[0:64, :], bs_[:], ALU.mult, [o, bs_], [a_])
                else:
                    self.TT(tmp_[:], o[0:64, :], bs_[:], ALU.mult, [o, bs_], [tmp_])
                    self.TT(a_[:], a_[:], tmp_[:], ALU.add, [a_, tmp_], [a_], eng="gpsimd")
                if extra is not None:
                    extra()
            if len(delays) == 3:
                return U(None, None, None, later=[(delays[0], f0), (delays[1], f1), (delays[2], f2)])
            return U(None, None, f0, later=[(delays[0], f1), (delays[1], f2)])

        for g in range(4):
            ks_, kw_ = KS[g % 2], KW[g % 2]
            self.LD(ks_[0:64, :], self.KSd[g * 64:(g + 1) * 64, :], [mb["KSd"]], [ks_])
            self.LD(kw_[:], self.KWd[g * 64:(g + 1) * 64, :], [mb["KWd"]], [kw_])
            for hh in range(4):
                h = g * 4 + hh
                self.LD(Q4[hh][0:64, :], self.QN[h * 64:(h + 1) * 64, :], [mb["QN"]], [Q4[hh]])
            for qg in range(NG):
                qs = slice(qg * TG, (qg + 1) * TG)
                q0 = qg * TG
                ntile = 2 if qg >= 4 else 1
                units = []
                for hh in range(4):
                    h = g * 4 + hh
                    pc = PC[hh % 2]
                    for i in range(ntile):
                        sp = Sp[cnt[0] % 3]
                        cnt[0] += 1

                        def A(sp=sp, i=i, hh=hh):
                            self.MM(sp[:], KCMP[:, g, i * 128:(i + 1) * 128], Q4[hh][0:64, qs], True, False, [KCMP, Q4[hh]], [sp])
                            self.MM(sp[:], self.ident_bf[:], cmpm[:, i, qs], False, True, [cmpm, self.cbf], [sp])

                        def B(sp=sp, i=i, pc=pc):
                            self.ACT(pc[:, i, :], sp[:], AF.Exp, [sp], [pc], scale=SC)

                        def C(i=i, pc=pc, oc=Ocs[hh % 2]):
                            self.MM(oc[0:65, :], VCMP[:, g, i, :], pc[:, i, :], i == 0, i == ntile - 1, [VCMP, pc], [oc])
                        units.append(U(A, B, C))

                    def pre(hh=hh, pc=pc):
                        for qt in range(4):
                            for i in range(ntile):
                                self.MM(imp[:, qt * 65:(qt + 1) * 65], pc[:, i, qt * 128:(qt + 1) * 128], ovl[:, i, :], i == 0, i == ntile - 1, [pc, ovl], [imp])
                        iv = imp[:, 0:260].rearrange("p (t f) -> p t f", f=65)
                        self.TS(irec[:], iv[:, :, 64], 1e-30, None, ALU.max, None, [imp], [irec])
                        self.P.dve(lambda e: e.reciprocal(out=irec[:], in_=irec[:]), reads=[irec.b], writes=[irec.b])
                        if hh == 0:
                            self.TT(iacc[:], iv[:, :, 0:64], irec[:].unsqueeze(2).broadcast_to([128, 4, 64]), ALU.mult, [imp, irec], [iacc])
                        else:
                            self.TT(itmp[:], iv[:, :, 0:64], irec[:].unsqueeze(2).broadcast_to([128, 4, 64]), ALU.mult, [imp, irec], [itmp])
                            self.TT(iacc[:], iacc[:], itmp[:], ALU.add, [iacc, itmp], [iacc], eng="gpsimd")
                    units.append(finish_unit(Ocs[hh % 2], 3 * h + 0, A4[hh], True, qs, pre=pre))
                run_units(units, 1)
                for qt in range(4):
                    tix = qg * 4 + qt
                    self.TT(score[:, qt, :], iacc[:, qt, :], self.c32[:, wo + 64 - 2 * tix:wo + 128 - 2 * tix], ALU.add, [iacc, self.cb], [score])
                    self.TT(score[:, qt, :], score[:, qt, :], col0, ALU.max, [score, self.cb], [score])
                    self.P.dve(lambda e, qt=qt: e.max(out=m8[:, 0:8], in_=score[:, qt, :]), reads=[score.b], writes=[m8.b])
                    self.P.dve(lambda e, qt=qt: e.match_replace(out=sc2[:], in_to_replace=m8[:, 0:8], in_values=score[:, qt, :], imm_value=-3.0e38),
                               reads=[score.b, m8.b], writes=[sc2.b])
                    self.P.dve(lambda e: e.max(out=m8[:, 8:16], in_=sc2[:]), reads=[sc2.b], writes=[m8.b])
                    self.TS(nself[:, qt, 64:128], score[:, qt, :], m8[:, 15:16], -1.0, ALU.is_ge, ALU.add, [score, m8], [nself])
                    self.TR(M2[:, qt * 128:(qt + 1) * 128], nself[:, qt, :], ident32, [nself, self.cb], [M2])
                for hh in range(4):
                    self.CP(Q4[hh][64:128, qs], M2[64:128, :], [M2], [Q4n[hh]], eng="scalar")
                units = []
                for hh in range(4):
                    h = g * 4 + hh
                    qh_ = Q4[hh]
                    nk = 4 * qg + 4
                    for kt in range(nk):
                        d = kt - 4 * qg
                        sp = Sp[cnt[0] % 3]
                        pt = PT[cnt[0] % 3]
                        cnt[0] += 1
                        kc = slice(kt * 128, (kt + 1) * 128)
                        c0 = max(d, 0) * 128

                        def A(sp=sp, kc=kc, d=d, c0=c0, qh_=qh_, qn_=Q4n[hh]):
                            if d < 0:
                                self.MM(sp[:], ks_[:, kc], qh_[:, qs], True, True, [ks_, qh_, qn_], [sp])
                            else:
                                self.MM(sp[:, c0:c0 + 128], ks_[:, kc], qh_[:, q0 + c0:q0 + c0 + 128], True, False, [ks_, qh_, qn_], [sp])
                                self.MM(sp[:, c0:c0 + 128], self.ident_bf[:], self.cpen_bf[:], False, True, [self.cbf], [sp])
                                if c0 + 128 < TG:
                                    self.MM(sp[:, c0 + 128:TG], ks_[:, kc], qh_[:, q0 + c0 + 128:q0 + TG], True, True, [ks_, qh_, qn_], [sp])

                        def B(sp=sp, pt=pt, c0=c0):
                            self.ACT(pt[:, c0:TG], sp[:, c0:TG], AF.Exp, [sp], [pt], scale=SC)

                        def C(pt=pt, c0=c0, kt=kt):
                            self.MM(Os[0:65, c0:TG], VSW[:, kt, g * 65:(g + 1) * 65], pt[:, c0:TG], kt == 0, kt == nk - 1, [pt, VSW], [Os])
                        units.append(U(A, B, C))
                    units.append(finish_unit(Os, 3 * h + 1, A4[hh], False, qs, delays=((2, 5, 7) if qg >= 1 else (1, 3, 4))))
                    kts = [4 * qg] + [kt for kt in range(4 * qg - 4, 4 * qg + 4) if kt >= 0 and kt != 4 * qg]
                    for i, kt in enumerate(kts):
                        d = kt - 4 * qg
                        sp = Sp[cnt[0] % 3]
                        pt = PT[cnt[0] % 3]
                        cnt[0] += 1
                        kc = slice(kt * 128, (kt + 1) * 128)
                        lo = max(d, 0) * 128
                        hi = min(d + 5, 4) * 128
                        if d >= 0:
                            pb, pen = lo, self.cpen_bf
                            rest = (lo + 128, hi)
                        else:
                            pb, pen = hi - 128, self.bpen_bf
                            rest = (lo, hi - 128)

                        def A(sp=sp, kc=kc, pb=pb, pen=pen, rest=rest, qh_=qh_):
                            self.MM(sp[:, pb:pb + 128], kw_[:, kc], qh_[0:64, q0 + pb:q0 + pb + 128], True, False, [kw_, qh_], [sp])
                            self.MM(sp[:, pb:pb + 128], self.ident_bf[:], pen[:], False, True, [self.cbf], [sp])
                            if rest[1] > rest[0]:
                                self.MM(sp[:, rest[0]:rest[1]], kw_[:, kc], qh_[0:64, q0 + rest[0]:q0 + rest[1]], True, True, [kw_, qh_], [sp])

                        def B(sp=sp, pt=pt, lo=lo, hi=hi):
                            self.ACT(pt[:, lo:hi], sp[:, lo:hi], AF.Exp, [sp], [pt], scale=SC)

                        def C(pt=pt, lo=lo, hi=hi, kt=kt, i=i, nw=len(kts)):
                            self.MM(Ow[0:65, lo:hi], VSW[:, kt, (4 + g) * 65:(5 + g) * 65], pt[:, lo:hi], i == 0, i == nw - 1, [pt, VSW], [Ow])
                        units.append(U(A, B, C))

                    def store(h=h, hh=hh, qs=qs):
                        os_ = ost[h % 2]
                        self.CP(os_[:], A4[hh][:], [A4[hh]], [os_], eng="gpsimd")
                        self.LD(self.ONT[h, :, qs], os_[:], [os_], [mb["ONT"]], eng="gpsimd")
                    units.append(finish_unit(Ow, 3 * h + 2, A4[hh], False, qs, extra=store, delays=((2, 5, 7) if qg >= 1 else (1, 3, 4))))
                run_units(units, 2)
    P.barrier()


KB.nsa_attn = _nsa_attn
```

```python
import numpy as np
from contextlib import ExitStack
import concourse.bass as bass
import concourse.mybir as mybir
from concourse.bass_utils import run_bass_kernel_spmd

F32 = mybir.dt.float32
BF16 = mybir.dt.bfloat16
I32 = mybir.dt.int32
ALU = mybir.AluOpType
AF = mybir.ActivationFunctionType
AX = mybir.AxisListType

S = 4096
D = 1024
DFF = 2816
NFC = 22
NG = 8
TG = 512
EPS = 1e-6
HY_IN = 1968
NSA_IN = 2608
NEGB = -30000.0

ENGINES = ["tensor", "vector", "scalar", "gpsimd", "sync"]
class Buf:
    __slots__ = ("name", "last_w", "readers")

    def __init__(self, name=""):
        self.name = name
        self.last_w = None
        self.readers = []


class Op:
    __slots__ = ("eng", "fn", "raw", "oth", "is_dma", "sig", "sem", "semval", "prev_semval", "gidx")


class Prog:
    def __init__(self, nc, n_dma_sems=12):
        self.nc = nc
        self.ops = {e: [] for e in ENGINES}
        self.n = 0
        self.n_dma_sems = n_dma_sems
        self.pending_barrier = {}
        self.dma_last = {}
        self.dma_cnt = {}

    def op(self, eng, fn, reads=(), writes=(), dma=False):
        o = Op()
        o.eng = eng
        o.fn = fn
        o.is_dma = dma
        o.raw = set()
        o.oth = set()
        o.sig = False
        o.sem = None
        o.semval = 0
        o.prev_semval = 0
        o.gidx = self.n
        self.n += 1
        for b in reads:
            if b.last_w is not None:
                o.raw.add(b.last_w)
        for b in writes:
            if b.last_w is not None:
                o.oth.add(b.last_w)
            for r in b.readers:
                o.oth.add(r)
        for b in reads:
            b.readers.append(o)
        for b in writes:
            b.last_w = o
            b.readers = []
        o.raw.discard(o)
        o.oth.discard(o)
        if eng in self.pending_barrier:
            for d in self.pending_barrier.pop(eng):
                o.raw.add(d)
        self.ops[eng].append(o)
        if dma:
            k = self.dma_cnt.get(eng, 0)
            self.dma_cnt[eng] = k + 1
            self.dma_last[(eng, k % self.n_dma_sems)] = o
        return o

    def barrier(self):
        deps = []
        for e in ENGINES:
            comp = [x for x in self.ops[e] if not x.is_dma]
            if comp:
                deps.append(comp[-1])
        deps.extend(self.dma_last.values())
        for e in ENGINES:
            self.pending_barrier[e] = list(deps) + self.pending_barrier.get(e, [])

    def mm(self, fn, reads=(), writes=()):
        return self.op("tensor", fn, reads, writes)

    def dve(self, fn, reads=(), writes=()):
        return self.op("vector", fn, reads, writes)

    def act(self, fn, reads=(), writes=()):
        return self.op("scalar", fn, reads, writes)

    def pool(self, fn, reads=(), writes=()):
        return self.op("gpsimd", fn, reads, writes)

    def load(self, out, in_, reads=(), writes=(), eng="sync", **kw):
        return self.op(eng, lambda e: e.dma_start(out=out, in_=in_, **kw), reads, writes, dma=True)

    def needed_deps(self, o):
        res = []
        for d in o.raw:
            if d.eng == o.eng and not d.is_dma and not o.is_dma:
                if o.eng == "tensor":
                    continue
                res.append(d)
            else:
                res.append(d)
        for d in o.oth:
            if d.eng == o.eng and not d.is_dma and not o.is_dma:
                continue
            res.append(d)
        return res

    def finalize(self, stack):
        nc = self.nc
        for e in ENGINES:
            for o in self.ops[e]:
                if o.is_dma:
                    o.sig = True
                for d in self.needed_deps(o):
                    d.sig = True
        csem = {}
        for e in ["tensor", "vector", "scalar", "gpsimd"]:
            csem[e] = stack.enter_context(nc.semaphore("c_" + e))
        dpool = {}
        for e in ["sync", "gpsimd", "scalar"]:
            if any(o.is_dma for o in self.ops[e]):
                dpool[e] = [stack.enter_context(nc.semaphore("d_%s_%d" % (e, i))) for i in range(self.n_dma_sems)]
        for e in ENGINES:
            cnt = 0
            k = 0
            uses = {}
            for o in self.ops[e]:
                if o.is_dma:
                    s = dpool[e][k % self.n_dma_sems]
                    k += 1
                    o.sem = s
                    o.prev_semval = uses.get(id(s), 0)
                    o.semval = o.prev_semval + 16
                    uses[id(s)] = o.semval
                elif o.sig:
                    cnt += 1
                    o.sem = csem[e]
                    o.semval = cnt
        self.final_dma = []
        for e in dpool:
            last = {}
            for o in self.ops[e]:
                if o.is_dma:
                    last[id(o.sem)] = (o.sem, o.semval)
            self.final_dma.append((e, list(last.values())))
        block = stack.enter_context(nc.Block())
        prog = self

        def make(e):
            def body(eng):
                waited = {}
                for o in prog.ops[e]:
                    need = {}
                    for d in prog.needed_deps(o):
                        key = id(d.sem)
                        if key not in need or need[key][1] < d.semval:
                            need[key] = (d.sem, d.semval)
                    if o.is_dma and o.prev_semval > 0:
                        key = id(o.sem)
                        if key not in need or need[key][1] < o.prev_semval:
                            need[key] = (o.sem, o.prev_semval)
                    for key, (s, v) in need.items():
                        if waited.get(key, 0) >= v:
                            continue
                        eng.wait_ge(s, v)
                        waited[key] = v
                    ins = o.fn(eng)
                    if o.sig:
                        ins.then_inc(o.sem, 16 if o.is_dma else 1)
                for (ee, lst) in prog.final_dma:
                    if ee == e:
                        for (s, v) in lst:
                            if waited.get(id(s), 0) < v:
                                eng.wait_ge(s, v)
            return body

        for e in ENGINES:
            if not self.ops[e]:
                continue
            getattr(block, e)(make(e))


CB = {}
_off = 0
for _n, _w in [("ident", 128), ("ones", 128), ("cpen", 128), ("bpen", 128), ("tri", 64), ("scanmask", 512),
               ("freq", 4), ("wide", 128), ("col0", 64), ("sel48", 48 * 64 + 1), ("E", 32 * 128)]:
    CB[_n] = (_off, _w)
    _off += _w
CB_W = _off


def make_consts():
    c = np.zeros((128, CB_W), np.float32)
    p = np.arange(128)[:, None]

    def put(n, a):
        o, w = CB[n]
        c[: a.shape[0], o:o + a.shape[1]] = a
    put("ident", np.eye(128, dtype=np.float32))
    put("ones", np.ones((128, 128), np.float32))
    f = np.arange(128)[None, :]
    put("cpen", np.where(p <= f, 0.0, NEGB).astype(np.float32))
    put("bpen", np.where(p > f, 0.0, NEGB).astype(np.float32))
    j = np.arange(64)[:, None]
    i = np.arange(64)[None, :]
    put("tri", (j <= i).astype(np.float32))
    sm = np.ones((128, 512), np.float32)
    sm[:, ::64] = 0.0
    put("scanmask", sm)
    fr = np.zeros((128, 4), np.float32)
    r = np.arange(128)
    im = (r - 64) % 16
    fr[:, 0] = (10000.0 ** (-im * (2.0 / 32))) / (2 * np.pi)
    fr[:, 1] = np.where(((r - 64) % 32) < 16, -1.0, 1.0) * (2 * np.pi * (1 - 1e-6))
    i2 = r % 32
    fr[:, 2] = (10000.0 ** (-i2 * (2.0 / 64))) / (2 * np.pi)
    fr[:, 3] = np.where((r % 64) < 32, -1.0, 1.0) * (2 * np.pi * (1 - 1e-6))
    put("freq", fr)
    x = np.arange(128)[None, :]
    rel = x - 64 - (p >= 64)
    wide = np.where(rel > 0, -1e30, np.where(rel >= -1, 1e4, 0.0)).astype(np.float32)
    put("wide", wide)
    c0 = np.full((128, 64), -3e38, np.float32)
    c0[:, 0] = 1e4
    put("col0", c0)
    sel = np.zeros((128, 48 * 64 + 1), np.float32)
    for rr in range(48):
        sel[rr, 1 + rr * 64:1 + (rr + 1) * 64] = 1.0
    put("sel48", sel)
    E = np.zeros((128, 32 * 128), np.float32)
    for kt in range(32):
        for key in range(128):
            E[2 * kt + (key >= 64), kt * 128 + key] = -NEGB
    put("E", E)
    return c


def make_cmp_consts():
    n = np.arange(256)[:, None]
    t = np.arange(S)[None, :]
    cm = np.where((16 * n + 31 <= t) & (n < 255), 0.0, NEGB).astype(np.float32)
    ncmp = np.arange(255)
    cs_ = ncmp * 16
    bs_ = np.arange(64) * 64
    ov = np.minimum(cs_[:, None] + 32, bs_[None, :] + 64) - np.maximum(cs_[:, None], bs_[None, :])
    ovl = np.zeros((256, 65), np.float32)
    ovl[:255, :64] = np.clip(ov, 0, None) / 32.0
    ovl[:255, 64] = 1.0
    return cm, ovl


class KB:
    def __init__(self, debug_phases=None):
        self.nc = bass.Bass("TRN2", target_bir_lowering=False)
        self.P = Prog(self.nc)
        self.debug_phases = debug_phases
        self.uid = 0
        self.gdeps = []

    def name(self, p):
        self.uid += 1
        return "%s_%d" % (p, self.uid)

    def dram(self, name, shape, dt, kind="Internal"):
        return self.nc.dram_tensor(name, list(shape), dt, kind=kind).ap()

    def sb(self, st, shape, dt, name="t"):
        return st.enter_context(self.nc.sbuf_tensor(self.name(name), list(shape), dt))

    def ps(self, st, shape, dt=F32, name="p"):
        return st.enter_context(self.nc.psum_tensor(self.name(name), list(shape), dt))

    def declare(self):
        d = self.dram
        I = {}
        I["x"] = d("x", [S, D], F32, "ExternalInput")
        I["positions"] = d("positions", [1, S], I32, "ExternalInput")
        I["consts"] = d("consts", [128, CB_W], F32, "ExternalInput")
        I["cmpmask"] = d("cmpmask", [256, S], F32, "ExternalInput")
        I["ovl"] = d("ovl", [256, 65], F32, "ExternalInput")
        for n, shp in [("ffn_norm", [4, D]), ("ffn_w_gate", [4, D, DFF]), ("ffn_w_up", [4, D, DFF]),
                       ("ffn_w_down", [4, DFF, D]), ("mix_norm", [2, D]), ("hy_w_in", [D, HY_IN]),
                       ("mla_q_norm", [1, 256]), ("mla_w_uq", [256, 768]), ("mla_kv_norm", [1, 128]),
                       ("mla_w_ukv", [128, 1024]), ("gla_w_a2", [16, 256]), ("gla_b_a", [1, 256]),
                       ("gla_out_norm", [1, 128]), ("hy_w_out", [D, D]), ("nsa_w_in", [D, NSA_IN]),
                       ("nsa_pos_k", [32, 64]), ("nsa_pos_v", [32, 64]), ("nsa_ck_w1", [2048, 128]),
                       ("nsa_ck_w2", [128, 64]), ("nsa_cv_w1", [2048, 128]), ("nsa_cv_w2", [128, 64]),
                       ("nsa_w_out", [D, D]), ("final_norm", [1, D])]:
            I[n] = d(n, shp, F32, "ExternalInput")
        self.I = I
        self.out = d("out", [S, D], F32, "ExternalOutput")
        self.hT = [d("hT%d" % i, [D, S], F32) for i in range(2)]
        self.hbuf = [Buf("hT0"), Buf("hT1")]
        self.wgu = d("wgu", [4, NFC, 128, 2 * 8 * 128], BF16)
        self.wdn = d("wdn", [4, 8, 128, NFC * 128], BF16)
        self.wgu_b = {}
        self.wdn_b = {}
        self.prepq = []

    def prep_ffn(self, f):
        I = self.I
        items = []
        self.wgu_b[f] = {}
        self.wdn_b[f] = []
        for c in range(NFC):
            for which, src in enumerate([I["ffn_w_gate"], I["ffn_w_up"]]):
                bf_ = Buf("wgu")
                self.wgu_b[f][(which, c)] = bf_
                dst = self.wgu[f, c].rearrange("p (w k j) -> p w k j", w=2, k=8, j=128)[:, which, :, :]
                s_ = src[f, :, c * 128:(c + 1) * 128].rearrange("(k p) j -> p k j", p=128)
                items.append((dst, s_, bf_))
        for c in range(NFC):
            bf_ = Buf("wdn")
            self.wdn_b[f].append(bf_)
            dst = self.wdn[f].rearrange("fc p (c j) -> p fc c j", c=NFC, j=128)[:, :, c, :]
            s_ = I["ffn_w_down"][f, c * 128:(c + 1) * 128, :].rearrange("p (fc j) -> p fc j", j=128)
            items.append((dst, s_, bf_))
        self.prepq.extend(items)

    def pump(self, n):
        for _ in range(n):
            if not self.prepq:
                return
            dst, s_, bf_ = self.prepq.pop(0)
            self.P.load(dst, s_, writes=[bf_], eng="gpsimd")

    def load_consts(self, st):
        P = self.P
        self.c32 = self.sb(st, [128, CB["E"][0]], F32, "c32")
        self.cb = Buf("c32")
        P.load(self.c32[:], self.I["consts"][:, 0:CB["E"][0]], writes=[self.cb])
        self.ident_bf = self.sb(st, [128, 128], BF16, "identbf")
        self.ones_bf = self.sb(st, [128, 128], BF16, "onesbf")
        self.cpen_bf = self.sb(st, [128, 128], BF16, "cpenbf")
        self.bpen_bf = self.sb(st, [128, 128], BF16, "bpenbf")
        self.cbf = Buf("cbf")
        for t, n in [(self.ident_bf, "ident"), (self.ones_bf, "ones"), (self.cpen_bf, "cpen"), (self.bpen_bf, "bpen")]:
            o, w = CB[n]
            P.dve(lambda e, t=t, o=o, w=w: e.tensor_copy(out=t[:], in_=self.c32[:, o:o + w]), reads=[self.cb], writes=[self.cbf])
        self.gcol = self.sb(st, [128, 7, 8], F32, "gcol")
        self.gb = Buf("gcol")
        with self.nc.allow_non_contiguous_dma("tiny gain vectors"):
            for j, src in [(0, self.I["ffn_norm"][0:1, :]), (1, self.I["ffn_norm"][1:2, :]), (2, self.I["ffn_norm"][2:3, :]),
                           (3, self.I["ffn_norm"][3:4, :]), (4, self.I["mix_norm"][0:1, :]), (5, self.I["mix_norm"][1:2, :]),
                           (6, self.I["final_norm"][0:1, :])]:
                P.load(self.gcol[:, j, :], src.rearrange("o (fc p) -> p (o fc)", p=128), writes=[self.gb], allow_slow_non_contiguous=True)

    def cst(self, n, rows=128, c0=0, c1=None):
        o, w = CB[n]
        if c1 is None:
            c1 = w
        return self.c32[0:rows, o + c0:o + c1]

    def transpose_in(self):
        P = self.P
        with ExitStack() as st:
            xt = [self.sb(st, [128, D], F32, "xt") for _ in range(4)]
            xb = [Buf("xt%d" % i) for i in range(4)]
            tp = [self.ps(st, [128, 512], F32, "tp") for _ in range(2)]
            tb = [Buf("tp0"), Buf("tp1")]
            stg = [self.sb(st, [128, 8, 512], F32, "stg") for _ in range(2)]
            sgb = [Buf("stg0"), Buf("stg1")]
            ident = self.cst("ident")
            k = 0
            for g in range(NG):
                for t in range(4):
                    r0 = g * TG + t * 128
                    P.load(xt[t][:], self.I["x"][r0:r0 + 128, :], writes=[xb[t]])
                so = stg[g % 2]
                for fc in range(8):
                    pb = tp[k % 2]
                    for t in range(4):
                        P.mm(lambda e, pb=pb, t=t, fc=fc: e.transpose(out=pb[:, t * 128:(t + 1) * 128], in_=xt[t][:, fc * 128:(fc + 1) * 128], identity=ident),
                             reads=[xb[t], self.cb], writes=[tb[k % 2]])
                    if fc % 2 == 0:
                        P.act(lambda e, pb=pb, fc=fc, so=so: e.copy(out=so[:, fc, :], in_=pb[:]), reads=[tb[k % 2]], writes=[sgb[g % 2]])
                    else:
                        P.dve(lambda e, pb=pb, fc=fc, so=so: e.tensor_copy(out=so[:, fc, :], in_=pb[:]), reads=[tb[k % 2]], writes=[sgb[g % 2]])
                    k += 1
                P.load(self.hT[0].rearrange("(fc p) t -> p fc t", p=128)[:, :, g * TG:(g + 1) * TG], so[:],
                       reads=[sgb[g % 2]], writes=[self.hbuf[0]], eng="gpsimd")
        P.barrier()

    def norm_slab(self, hs, hsb, nfc, gcols, ss_ps, ssb, sq, sqb, rstd, rstdb, uT, uTb, inv_n, psum_src=False):
        P = self
        Pg = self.P
        for fc in range(nfc):
            s_ = sq[fc % len(sq)]
            sb_ = sqb[fc % len(sq)]
            Pg.act(lambda e, s_=s_, fc=fc: e.activation(out=s_[:], in_=hs[:, fc, :], func=AF.Square), reads=[hsb], writes=[sb_])
            Pg.mm(lambda e, s_=s_, fc=fc: e.matmul(ss_ps[:], lhsT=self.ones_bf[:], rhs=s_[:], start=(fc == 0), stop=(fc == nfc - 1)),
                  reads=[sb_, self.cbf], writes=[ssb])
        Pg.act(lambda e: e.activation(out=rstd[:], in_=ss_ps[:], func=AF.Ln, scale=float(inv_n), bias=float(EPS)), reads=[ssb], writes=[rstdb])
        Pg.act(lambda e: e.activation(out=rstd[:], in_=rstd[:], func=AF.Exp, scale=-0.5), reads=[rstdb], writes=[rstdb])
        for fc in range(nfc):
            Pg.dve(lambda e, fc=fc: e.scalar_tensor_tensor(out=uT[:, fc, :], in0=hs[:, fc, :], scalar=gcols[fc], in1=rstd[:],
                                                           op0=ALU.mult, op1=ALU.mult), reads=[hsb, rstdb, self.gb], writes=[uTb])

    def ffn(self, f, src, dst):
        P = self.P
        hin, hinb = self.hT[src], self.hbuf[src]
        hout, houtb = self.hT[dst], self.hbuf[dst]
        hin_v = hin.rearrange("(fc p) t -> p fc t", p=128)
        hout_v = hout.rearrange("(fc p) t -> p fc t", p=128)
        with ExitStack() as st:
            hs = [self.sb(st, [128, 8, TG], F32, "hs") for _ in range(2)]
            hsb = [Buf("hs0"), Buf("hs1")]
            uT = [self.sb(st, [128, 8, TG], BF16, "uT") for _ in range(2)]
            uTb = [Buf("uT0"), Buf("uT1")]
            sq = [self.sb(st, [128, TG], BF16, "sq") for _ in range(2)]
            sqb = [Buf("sq0"), Buf("sq1")]
            rstd = self.sb(st, [128, TG], F32, "rstd")
            rstdb = Buf("rstd")
            actT = [self.sb(st, [128, NFC, TG], BF16, "actT") for _ in range(2)]
            actb = [Buf("act0"), Buf("act1")]
            NW = 6
            wgu = [self.sb(st, [128, 2, 8, 128], BF16, "wgu") for _ in range(NW)]
            wgub = [Buf("wgu%d" % i) for i in range(NW)]
            NWD = 3
            wdn = [self.sb(st, [128, NFC, 128], BF16, "wdn") for _ in range(NWD)]
            wdnb = [Buf("wdn%d" % i) for i in range(NWD)]
            sg = [self.sb(st, [128, TG], BF16, "sg") for _ in range(2)]
            sgb = [Buf("sg0"), Buf("sg1")]
            ho = [self.sb(st, [128, TG], F32, "ho") for _ in range(3)]
            hob = [Buf("ho%d" % i) for i in range(3)]
            ss_ps = self.ps(st, [128, TG], F32, "ss")
            ssb = Buf("ss")
            g_ps = [self.ps(st, [128, TG], F32, "gps") for _ in range(2)]
            gpb = [Buf("g0"), Buf("g1")]
            u_ps = [self.ps(st, [128, TG], F32, "ups") for _ in range(2)]
            upb = [Buf("u0"), Buf("u1")]
            o_ps = [self.ps(st, [128, TG], F32, "ops") for _ in range(2)]
            opb = [Buf("o0"), Buf("o1")]
            gcols = [self.gcol[:, f, fc:fc + 1] for fc in range(8)]
            cnt = {"w": 0, "d": 0, "c": 0, "o": 0}

            def stage_load(g):
                P.load(hs[g % 2][:], hin_v[:, :, g * TG:(g + 1) * TG], reads=[hinb], writes=[hsb[g % 2]])

            def stage_norm(g):
                self.norm_slab(hs[g % 2], hsb[g % 2], 8, gcols, ss_ps, ssb, sq, sqb, rstd, rstdb, uT[g % 2], uTb[g % 2], 1.0 / D)

            def stage_gu(g):
                for c in range(NFC):
                    w = cnt["w"] % NW
                    cnt["w"] += 1
                    P.load(wgu[w][:], self.wgu[f, c].rearrange("p (w k j) -> p w k j", w=2, k=8), reads=[self.wgu_b[f][(0, c)], self.wgu_b[f][(1, c)]], writes=[wgub[w]])
                    b = cnt["c"] % 2
                    cnt["c"] += 1
                    for which, (pt, pbf) in enumerate([(g_ps[b], gpb[b]), (u_ps[b], upb[b])]):
                        for kc in range(8):
                            P.mm(lambda e, pt=pt, w=w, which=which, kc=kc: e.matmul(pt[:], lhsT=wgu[w][:, which, kc, :], rhs=uT[g % 2][:, kc, :],
                                                                                    start=(kc == 0), stop=(kc == 7)),
                                 reads=[wgub[w], uTb[g % 2]], writes=[pbf])
                    P.act(lambda e, b=b: e.activation(out=sg[b][:], in_=g_ps[b][:], func=AF.Silu), reads=[gpb[b]], writes=[sgb[b]])
                    P.dve(lambda e, b=b, c=c: e.tensor_tensor(out=actT[g % 2][:, c, :], in0=sg[b][:], in1=u_ps[b][:], op=ALU.mult),
                          reads=[sgb[b], upb[b]], writes=[actb[g % 2]])

            def stage_down(g):
                for fc in range(8):
                    w = cnt["d"] % NWD
                    cnt["d"] += 1
                    P.load(wdn[w][:], self.wdn[f, fc].rearrange("p (c j) -> p c j", j=128), reads=self.wdn_b[f], writes=[wdnb[w]])
                    b = fc % 2
                    for c in range(NFC):
                        P.mm(lambda e, b=b, w=w, c=c: e.matmul(o_ps[b][:], lhsT=wdn[w][:, c, :], rhs=actT[g % 2][:, c, :], start=(c == 0), stop=(c == NFC - 1)),
                             reads=[wdnb[w], actb[g % 2]], writes=[opb[b]])
                    k = cnt["o"] % 3
                    cnt["o"] += 1
                    P.dve(lambda e, b=b, k=k, fc=fc: e.scalar_tensor_tensor(out=ho[k][:], in0=o_ps[b][:], scalar=0.5, in1=hs[g % 2][:, fc, :],
                                                                            op0=ALU.mult, op1=ALU.add), reads=[opb[b], hsb[g % 2]], writes=[hob[k]])
                    P.load(hout_v[:, fc, g * TG:(g + 1) * TG], ho[k][:], reads=[hob[k]], writes=[houtb], eng="gpsimd")
                    self.pump(5)

            stage_load(0)
            stage_norm(0)
            for g in range(NG):
                if g + 1 < NG:
                    stage_load(g + 1)
                stage_gu(g)
                if g + 1 < NG:
                    stage_norm(g + 1)
                stage_down(g)
        P.barrier()

    def final(self, src, do_norm=True):
        P = self.P
        hin_v = self.hT[src].rearrange("(fc p) t -> p fc t", p=128)
        hinb = self.hbuf[src]
        outb = Buf("out")
        with ExitStack() as st:
            hs = [self.sb(st, [128, 8, TG], F32, "hs") for _ in range(2)]
            hsb = [Buf("hs0"), Buf("hs1")]
            yT = [self.sb(st, [128, 8, TG], F32, "yT") for _ in range(2)]
            yTb = [Buf("y0"), Buf("y1")]
            sq = [self.sb(st, [128, TG], BF16, "sq") for _ in range(2)]
            sqb = [Buf("sq0"), Buf("sq1")]
            rstd = self.sb(st, [128, TG], F32, "rstd")
            rstdb = Buf("rstd")
            ss_ps = self.ps(st, [128, TG], F32, "ss")
            ssb = Buf("ss")
            tp = [self.ps(st, [128, 512], F32, "tp") for _ in range(4)]
            tb = [Buf("tp%d" % i) for i in range(4)]
            ot = [self.sb(st, [128, D], F32, "ot") for _ in range(3)]
            otb = [Buf("ot%d" % i) for i in range(3)]
            gcols = [self.gcol[:, 6, fc:fc + 1] for fc in range(8)]
            ident = self.cst("ident")
            k = 0
            n = 0
            for g in range(NG):
                P.load(hs[g % 2][:], hin_v[:, :, g * TG:(g + 1) * TG], reads=[hinb], writes=[hsb[g % 2]])
                if do_norm:
                    self.norm_slab(hs[g % 2], hsb[g % 2], 8, gcols, ss_ps, ssb, sq, sqb, rstd, rstdb, yT[g % 2], yTb[g % 2], 1.0 / D)
                    y, yb = yT[g % 2], yTb[g % 2]
                else:
                    y, yb = hs[g % 2], hsb[g % 2]
                for t in range(4):
                    o_ = ot[n % 3]
                    ob_ = otb[n % 3]
                    n += 1
                    for half in range(2):
                        pb = tp[k % 4]
                        pbb = tb[k % 4]
                        k += 1
                        for j in range(4):
                            fc = half * 4 + j
                            P.mm(lambda e, pb=pb, j=j, fc=fc, t=t, y=y: e.transpose(out=pb[:, j * 128:(j + 1) * 128], in_=y[:, fc, t * 128:(t + 1) * 128], identity=ident),
                                 reads=[yb, self.cb], writes=[pbb])
                        if half == 0:
                            P.act(lambda e, pb=pb, o_=o_: e.copy(out=o_[:, 0:512], in_=pb[:]), reads=[pbb], writes=[ob_])
                        else:
                            P.dve(lambda e, pb=pb, o_=o_: e.tensor_copy(out=o_[:, 512:1024], in_=pb[:]), reads=[pbb], writes=[ob_])
                    r0 = g * TG + t * 128
                    P.load(self.out[r0:r0 + 128, :], o_[:], reads=[ob_], writes=[outb], eng="gpsimd")


def build(nphase=99, final_norm=True, only=None):
    import os
    kb = KB()
    kb.declare()
    P = kb.P
    with ExitStack() as st:
        kb.load_consts(st)
        cur = 0
        if only is None:
            kb.prep_ffn(0)
            kb.pump(10 ** 6)
        kb.transpose_in()
        if only == "mix1":
            kb.declare_mix1()
            kb.inproj1(cur)
            kb.nsa_attn()
            kb.outproj(cur, 1 - cur, kb.I["nsa_w_out"], kb.ONT, kb.m1b["ONT"], 16)
            cur = 1 - cur
        elif only is None:
            if nphase >= 1:
                kb.prep_ffn(1)
                kb.ffn(0, cur, 1 - cur)
                cur = 1 - cur
            if nphase >= 2:
                kb.declare_mix0()
                kb.inproj0(cur)
                kb.mla()
                kb.gla()
                kb.outproj(cur, 1 - cur, kb.I["hy_w_out"], kb.OTm, kb.m0b["OTm"], 8, kb.OTg, kb.m0b["OTg"])
                cur = 1 - cur
            if nphase >= 3:
                kb.prep_ffn(2)
                kb.ffn(1, cur, 1 - cur)
                cur = 1 - cur
                kb.prep_ffn(3)
                kb.ffn(2, cur, 1 - cur)
                cur = 1 - cur
            if nphase >= 4:
                kb.declare_mix1()
                kb.inproj1(cur)
                kb.nsa_attn()
                kb.outproj(cur, 1 - cur, kb.I["nsa_w_out"], kb.ONT, kb.m1b["ONT"], 16)
                cur = 1 - cur
            if nphase >= 5:
                kb.ffn(3, cur, 1 - cur)
                cur = 1 - cur
        kb.pump(10 ** 6)
        kb.final(cur, do_norm=final_norm)
        P.finalize(st)
    return kb.nc


WNAMES = ["ffn_norm", "ffn_w_gate", "ffn_w_up", "ffn_w_down", "mix_norm", "hy_w_in", "mla_q_norm", "mla_w_uq", "mla_kv_norm",
          "mla_w_ukv", "gla_w_a2", "gla_b_a", "gla_out_norm", "hy_w_out", "nsa_w_in", "nsa_pos_k", "nsa_pos_v", "nsa_ck_w1",
          "nsa_ck_w2", "nsa_cv_w1", "nsa_cv_w2", "nsa_w_out", "final_norm"]


def make_in_maps(inputs, ncores=8):
    consts = make_consts()
    cm, ovl = make_cmp_consts()
    shared = {"consts": consts, "cmpmask": cm, "ovl": ovl}
    f32 = lambda a: np.ascontiguousarray(np.asarray(a), dtype=np.float32)
    shared["ffn_norm"] = f32(inputs["ffn_norm"]).reshape(4, D)
    shared["ffn_w_gate"] = f32(inputs["ffn_w_gate"]).reshape(4, D, DFF)
    shared["ffn_w_up"] = f32(inputs["ffn_w_up"]).reshape(4, D, DFF)
    shared["ffn_w_down"] = f32(inputs["ffn_w_down"]).reshape(4, DFF, D)
    shared["mix_norm"] = f32(inputs["mix_norm"])
    for n in ["hy_w_in", "mla_w_uq", "mla_w_ukv", "gla_w_a2", "hy_w_out", "nsa_w_in", "nsa_pos_k", "nsa_pos_v", "nsa_ck_w1",
              "nsa_ck_w2", "nsa_cv_w1", "nsa_cv_w2", "nsa_w_out"]:
        shared[n] = f32(inputs[n])[0]
    for n in ["mla_q_norm", "mla_kv_norm", "gla_b_a", "gla_out_norm"]:
        shared[n] = f32(inputs[n]).reshape(1, -1)
    shared["final_norm"] = f32(inputs["final_norm"]).reshape(1, D)
    x = f32(inputs["x"])
    pos = np.ascontiguousarray(np.asarray(inputs["positions"]), dtype=np.int32)
    maps = []
    for c in range(ncores):
        m = dict(shared)
        m["x"] = x[c]
        m["positions"] = pos[c:c + 1]
        maps.append(m)
    return maps


def kernel(**inputs):
    nc = build()
    maps = make_in_maps(inputs)
    res = run_bass_kernel_spmd(nc, maps, core_ids=list(range(8)))
    return np.stack([r["out"] for r in res.results], axis=0)


class Tl:
    def __init__(self, t, name):
        self.t = t
        self.b = Buf(name)

    def __getitem__(self, k):
        return self.t[k]


def _kb_tile(self, st, shape, dt, name="t"):
    return Tl(self.sb(st, shape, dt, name), name)


def _kb_ptile(self, st, shape=(128, 512), dt=F32, name="p"):
    return Tl(self.ps(st, list(shape), dt, name), name)


def _bl(xs):
    return [x.b if isinstance(x, Tl) else x for x in xs]


def _MM(self, out, lhsT, rhs, start, stop, r, w):
    return self.P.mm(lambda e: e.matmul(out, lhsT=lhsT, rhs=rhs, start=start, stop=stop), reads=_bl(r), writes=_bl(w))


def _PROJ(self, out, pairs, r, w):
    n = len(pairs)
    for i, (l, rh) in enumerate(pairs):
        self.MM(out, l, rh, i == 0, i == n - 1, r, w)


def _TR(self, out, in_, ident, r, w):
    return self.P.mm(lambda e: e.transpose(out=out, in_=in_, identity=ident), reads=_bl(r), writes=_bl(w))


def _ACT(self, out, in_, func, r, w, **kw):
    return self.P.act(lambda e: e.activation(out=out, in_=in_, func=func, **kw), reads=_bl(r), writes=_bl(w))


def _TT(self, out, a, b, op, r, w, eng="vector"):
    return self.P.op(eng, lambda e: e.tensor_tensor(out=out, in0=a, in1=b, op=op), reads=_bl(r), writes=_bl(w))


def _STT(self, out, in0, scalar, in1, op0, op1, r, w):
    return self.P.dve(lambda e: e.scalar_tensor_tensor(out=out, in0=in0, scalar=scalar, in1=in1, op0=op0, op1=op1), reads=_bl(r), writes=_bl(w))


def _TS(self, out, in0, s1, s2, op0, op1, r, w, eng="vector"):
    if s2 is None:
        return self.P.op(eng, lambda e: e.tensor_scalar(out=out, in0=in0, scalar1=s1, scalar2=None, op0=op0), reads=_bl(r), writes=_bl(w))
    return self.P.op(eng, lambda e: e.tensor_scalar(out=out, in0=in0, scalar1=s1, scalar2=s2, op0=op0, op1=op1), reads=_bl(r), writes=_bl(w))


def _CP(self, out, in_, r, w, eng="vector"):
    if eng == "scalar":
        return self.P.act(lambda e: e.copy(out=out, in_=in_), reads=_bl(r), writes=_bl(w))
    return self.P.op(eng, lambda e: e.tensor_copy(out=out, in_=in_), reads=_bl(r), writes=_bl(w))


def _LD(self, out, in_, r, w, eng="sync", **kw):
    return self.P.load(out, in_, reads=_bl(r), writes=_bl(w), eng=eng, **kw)


def _MS(self, ap, val, w, eng="gpsimd"):
    return self.P.op(eng, lambda e: e.memset(ap, val), reads=[], writes=_bl(w))


for _n, _f in [("tile", _kb_tile), ("ptile", _kb_ptile), ("MM", _MM), ("PROJ", _PROJ), ("TR", _TR), ("ACT", _ACT), ("TT", _TT),
               ("STT", _STT), ("TS", _TS), ("CP", _CP), ("LD", _LD), ("MS", _MS)]:
    setattr(KB, _n, _f)


def _norm_ps(self, srcs, gcols, inv_n, ss, sq, rstd, outs, out_b):
    n = len(srcs)
    for i, (ap, tl) in enumerate(srcs):
        q = sq[i % len(sq)]
        self.ACT(q[:], ap, AF.Square, [tl], [q])
        self.MM(ss[:], self.ones_bf[:], q[:], i == 0, i == n - 1, [q, self.cbf], [ss])
    self.ACT(rstd[:], ss[:], AF.Ln, [ss], [rstd], scale=float(inv_n), bias=float(EPS))
    self.ACT(rstd[:], rstd[:], AF.Exp, [rstd], [rstd], scale=-0.5)
    for i, (ap, tl) in enumerate(srcs):
        self.STT(outs[i], ap, gcols[i], rstd[:], ALU.mult, ALU.mult, [tl, rstd] + self.gdeps, [out_b])


KB.norm_ps = _norm_ps


def _rope_tables(self, st, rows, r0, fcol, scol, name, pos_ap=None, S=S):
    C = self.tile(st, [rows, S], F32, name + "C")
    Sg = self.tile(st, [rows, S], F32, name + "S")
    with ExitStack() as st2:
        pi_ = self.tile(st2, [rows, S], I32, "posi")
        t = self.tile(st2, [rows, S], F32, "rt")
        u = self.tile(st2, [rows, S], F32, "ru")
        ti = self.tile(st2, [rows, S], I32, "rti")
        rs = slice(r0, rows)
        fo = CB["freq"][0]
        if pos_ap is None:
            pos_ap = self.I["positions"]
        with self.nc.allow_non_contiguous_dma("positions"):
            self.LD(pi_[rs, :], pos_ap.partition_broadcast(rows - r0).rearrange("p o s -> p (o s)"), [], [pi_], allow_slow_non_contiguous=True)
        self.CP(t[rs, :], pi_[rs, :], [pi_], [t])
        self.TS(t[rs, :], t[rs, :], self.c32[rs, fo + fcol:fo + fcol + 1], None, ALU.mult, None, [t, self.cb], [t])
        for tab, shift, scale in [(Sg, 0.0, self.c32[rs, fo + scol:fo + scol + 1]), (C, 0.25, float(2 * np.pi * (1 - 1e-6)))]:
            if shift:
                self.TS(u[rs, :], t[rs, :], shift, None, ALU.add, None, [t], [u])
            else:
                self.CP(u[rs, :], t[rs, :], [t], [u])
            self.CP(ti[rs, :], u[rs, :], [u], [ti])
            self.CP(tab[rs, :], ti[rs, :], [ti], [tab])
            self.TT(u[rs, :], u[rs, :], tab[rs, :], ALU.subtract, [u, tab], [u])
            self.ACT(tab[rs, :], u[rs, :], AF.Sin, [u, self.cb], [tab], scale=scale)
        self.P.barrier()
    return C, Sg


KB.rope_tables = _rope_tables


def _declare_mix0(self):
    d = self.dram
    self.QT = d("QT", [8, 96, S], BF16)
    self.KT = d("KT", [8, 96, S], BF16)
    self.Vs = d("Vs", [S, 520], BF16)
    self.qintra = d("qintra", [256, S], BF16)
    self.qinter = d("qinter", [256, S], BF16)
    self.kdec = d("kdec", [256, S], BF16)
    self.kdtok = d("kdtok", [S, 256], BF16)
    self.gv = d("gv", [S, 512], BF16)
    self.grs = d("grs", [512, S], BF16)
    self.decd = d("decd", [256, 64], F32)
    self.OTm = d("OTm", [8, 64, S], BF16)
    self.OTg = d("OTg", [4, 128, S], BF16)
    self.m0b = {n: Buf(n) for n in ["QT", "KT", "Vs", "qintra", "qinter", "kdec", "kdtok", "gv", "grs", "decd", "OTm", "OTg"]}


KB.declare_mix0 = _declare_mix0


def _inproj0(self, src):
    P, I = self.P, self.I
    hin_v = self.hT[src].rearrange("(fc p) t -> p fc t", p=128)
    hinb = self.hbuf[src]
    mb = self.m0b
    with ExitStack() as st:
        T = lambda shape, dt, n: self.tile(st, shape, dt, n)
        w_in = T([128, 8, HY_IN], BF16, "w_in")
        for kc in range(8):
            self.LD(w_in[:, kc, :], I["hy_w_in"][kc * 128:(kc + 1) * 128, :], [], [w_in], eng="gpsimd")
        w_uq = T([128, 2, 768], BF16, "w_uq")
        w_uqs = T([128, 2, 768], BF16, "w_uqs")
        uqsrc = I["mla_w_uq"].rearrange("(k p) n -> p k n", p=128)
        self.LD(w_uq[:], uqsrc, [], [w_uq], eng="gpsimd")
        self.LD(w_uqs[:], uqsrc, [], [w_uqs], eng="gpsimd")
        v4 = lambda ap: ap.rearrange("p k (h d) -> p k h d", d=96)
        with self.nc.allow_non_contiguous_dma("small swapped weight blocks"):
            for kc in range(2):
                self.LD(v4(w_uqs[:])[:, kc, :, 64:80], v4(uqsrc)[:, kc, :, 80:96], [], [w_uqs], eng="gpsimd")
                self.LD(v4(w_uqs[:])[:, kc, :, 80:96], v4(uqsrc)[:, kc, :, 64:80], [], [w_uqs], eng="gpsimd")
        w_ukv = T([128, 1024], BF16, "w_ukv")
        self.LD(w_ukv[:], I["mla_w_ukv"], [], [w_ukv], eng="gpsimd")
        wkrs = T([128, 8, 96], BF16, "wkrs")
        insrc = I["hy_w_in"].rearrange("(k p) n -> p k n", p=128)
        with self.nc.allow_non_contiguous_dma("small swapped weight blocks"):
            self.LD(wkrs[:, :, 0:64], insrc[:, :, 320:384], [], [wkrs], eng="gpsimd")
            self.LD(wkrs[:, :, 64:80], insrc[:, :, 400:416], [], [wkrs], eng="gpsimd")
            self.LD(wkrs[:, :, 80:96], insrc[:, :, 384:400], [], [wkrs], eng="gpsimd")
        w_a2 = T([16, 256], BF16, "w_a2")
        self.LD(w_a2[:], I["gla_w_a2"], [], [w_a2], eng="gpsimd")
        cols = T([128, 8], F32, "cols")
        with self.nc.allow_non_contiguous_dma("tiny vectors"):
            self.LD(cols[:, 0:2], I["mla_q_norm"].rearrange("o (k p) -> p (o k)", p=128), [], [cols], allow_slow_non_contiguous=True)
            self.LD(cols[:, 2:3], I["mla_kv_norm"].rearrange("o (k p) -> p (o k)", p=128), [], [cols], allow_slow_non_contiguous=True)
            self.LD(cols[:, 3:5], I["gla_b_a"].rearrange("o (k p) -> p (o k)", p=128), [], [cols], allow_slow_non_contiguous=True)
        self.TS(cols[:, 5:7], cols[:, 3:5], -1.0, None, ALU.mult, None, [cols], [cols])
        C, Sg = self.rope_tables(st, 96, 64, 0, 1, "mla")
        hs = [T([128, 8, TG], F32, "hs") for _ in range(2)]
        uT = T([128, 8, TG], BF16, "uT")
        sq = [T([128, TG], BF16, "sq") for _ in range(2)]
        rstd = T([128, TG], F32, "rstd")
        cqn = T([128, 2, TG], BF16, "cqn")
        ckvn = T([128, TG], BF16, "ckvn")
        qst = [T([96, TG], BF16, "qst") for _ in range(2)]
        t1 = [T([96, TG], F32, "t1") for _ in range(2)]
        t2 = [T([96, TG], F32, "t2") for _ in range(2)]
        kst = T([96, 8, TG], BF16, "kst")
        krot = T([96, TG], F32, "krot")
        vst = T([128, 4, 8, 65], BF16, "vst")
        self.MS(vst[:], 1.0, [vst])
        ga = T([16, TG], BF16, "ga")
        lt = T([128, TG], F32, "lt")
        cs = T([128, TG], F32, "cs")
        dd = T([128, TG], F32, "dd")
        E1 = T([128, TG], F32, "E1")
        E2 = T([128, TG], F32, "E2")
        E3 = T([128, TG], F32, "E3")
        qia = T([128, 2, TG], BF16, "qia")
        qie = T([128, 2, TG], BF16, "qie")
        kde = T([128, 2, TG], BF16, "kde")
        dec = T([128, 2, 64], F32, "dec")
        kdt = T([128, 4, 256], BF16, "kdt")
        gvs = T([128, 4, 512], BF16, "gvs")
        grt = T([128, 4, TG], BF16, "grt")
        ss = self.ptile(st, name="ss")
        A = [self.ptile(st, name="A") for _ in range(2)]
        Bp = [self.ptile(st, name="B") for _ in range(2)]
        Tp = [self.ptile(st, name="T") for _ in range(2)]
        Tb = self.ptile(st, [128, 1024], BF16, name="Tb")
        self.gdeps = [self.gb, cols.b]
        gm = [self.gcol[:, 4, fc:fc + 1] for fc in range(8)]
        scanmask = self.cst("scanmask")
        for g in range(NG):
            gs = slice(g * TG, (g + 1) * TG)
            h_ = hs[g % 2]
            self.LD(h_[:], hin_v[:, :, gs], [hinb], [h_])
            self.norm_ps([(h_[:, fc, :], h_) for fc in range(8)], gm, 1.0 / D, ss, sq, rstd, [uT[:, fc, :] for fc in range(8)], uT)
            for ch in range(2):
                self.PROJ(A[ch][:], [(w_in[:, kc, ch * 128:(ch + 1) * 128], uT[:, kc, :]) for kc in range(8)], [w_in, uT], [A[ch]])
            self.norm_ps([(A[ch][:], A[ch]) for ch in range(2)], [cols[:, ch:ch + 1] for ch in range(2)], 1.0 / 256, ss, sq, rstd,
                         [cqn[:, ch, :] for ch in range(2)], cqn)
            for h in range(8):
                a, b = A[h % 2], Bp[h % 2]
                q_, x1, x2 = qst[h % 2], t1[h % 2], t2[h % 2]
                self.PROJ(a[0:96, :], [(w_uq[:, kc, h * 96:(h + 1) * 96], cqn[:, kc, :]) for kc in range(2)], [w_uq, cqn], [a])
                self.PROJ(b[0:96, :], [(w_uqs[:, kc, h * 96:(h + 1) * 96], cqn[:, kc, :]) for kc in range(2)], [w_uqs, cqn], [b])
                self.CP(q_[0:64, :], a[0:64, :], [a], [q_], eng="scalar")
                self.TT(x1[64:96, :], a[64:96, :], C[64:96, gs], ALU.mult, [a, C], [x1])
                self.TT(x2[64:96, :], b[64:96, :], Sg[64:96, gs], ALU.mult, [b, Sg], [x2])
                self.TT(q_[64:96, :], x1[64:96, :], x2[64:96, :], ALU.add, [x1, x2], [q_], eng="gpsimd")
                self.LD(self.QT[h, :, gs], q_[:], [q_], [mb["QT"]], eng="gpsimd")
            self.PROJ(A[0][:], [(w_in[:, kc, 256:384], uT[:, kc, :]) for kc in range(8)], [w_in, uT], [A[0]])
            self.norm_ps([(A[0][:], A[0])], [cols[:, 2:3]], 1.0 / 128, ss, sq, rstd, [ckvn[:]], ckvn)
            for h in range(8):
                a = A[h % 2]
                self.MM(a[0:64, :], w_ukv[:, h * 128:h * 128 + 64], ckvn[:], True, True, [w_ukv, ckvn], [a])
                self.CP(kst[0:64, h, :], a[0:64, :], [a], [kst], eng=("scalar" if h % 2 else "vector"))
            self.PROJ(A[0][0:96, :], [(w_in[:, kc, 320:416], uT[:, kc, :]) for kc in range(8)], [w_in, uT], [A[0]])
            self.PROJ(Bp[0][0:96, :], [(wkrs[:, kc, :], uT[:, kc, :]) for kc in range(8)], [wkrs, uT], [Bp[0]])
            self.TT(t1[0][64:96, :], A[0][64:96, :], C[64:96, gs], ALU.mult, [A[0], C], [t1[0]])
            self.TT(t2[0][64:96, :], Bp[0][64:96, :], Sg[64:96, gs], ALU.mult, [Bp[0], Sg], [t2[0]])
            self.TT(krot[64:96, :], t1[0][64:96, :], t2[0][64:96, :], ALU.add, [t1[0], t2[0]], [krot], eng="gpsimd")
            self.CP(kst[64:96, :, :], krot[64:96, :].unsqueeze(1).broadcast_to([32, 8, TG]), [krot], [kst], eng="gpsimd")
            self.LD(self.KT[:, :, gs].rearrange("h r t -> r h t"), kst[:], [kst], [mb["KT"]], eng="gpsimd")
            wv = w_ukv[:].rearrange("p (h t d) -> p h t d", t=2, d=64)[:, :, 1, :]
            for t in range(4):
                tp = Tp[t % 2]
                self.MM(tp[:].rearrange("p (h d) -> p h d", d=64), ckvn[:, t * 128:(t + 1) * 128], wv, True, True, [ckvn, w_ukv], [tp])
                self.CP(vst[:, t, :, 0:64], tp[:].rearrange("p (h d) -> p h d", d=64), [tp], [vst], eng=("scalar" if t % 2 else "vector"))
            self.LD(self.Vs[gs, :].rearrange("(t p) f -> p t f", p=128), vst[:].rearrange("p t h d -> p t (h d)"), [vst], [mb["Vs"]], eng="gpsimd")
            for ch in range(2):
                self.PROJ(A[ch][:], [(w_in[:, kc, 416 + ch * 128:416 + (ch + 1) * 128], uT[:, kc, :]) for kc in range(8)], [w_in, uT], [A[ch]])
                self.PROJ(Bp[ch][:], [(w_in[:, kc, 672 + ch * 128:672 + (ch + 1) * 128], uT[:, kc, :]) for kc in range(8)], [w_in, uT], [Bp[ch]])
            self.PROJ(Tp[0][0:16, :], [(w_in[:, kc, 1440:1456], uT[:, kc, :]) for kc in range(8)], [w_in, uT], [Tp[0]])
            self.CP(ga[:], Tp[0][0:16, :], [Tp[0]], [ga])
            for ch in range(2):
                tp = Tp[1]
                self.MM(tp[:], w_a2[0:16, ch * 128:(ch + 1) * 128], ga[0:16, :], True, True, [w_a2, ga], [tp])
                self.ACT(lt[:], tp[:], AF.Exp, [tp, cols], [lt], scale=-1.0, bias=cols[:, 5 + ch:6 + ch])
                self.ACT(lt[:], lt[:], AF.Ln, [lt], [lt], scale=1.0, bias=1.0)
                self.P.dve(lambda e: e.tensor_tensor_scan(out=cs[:], data0=scanmask, data1=lt[:], initial=0.0, op0=ALU.mult, op1=ALU.add),
                           reads=[lt.b, self.cb], writes=[cs.b])
                cs3 = cs[:].rearrange("p (c k) -> p c k", k=64)
                self.TT(dd[:].rearrange("p (c k) -> p c k", k=64), cs3, cs3[:, :, 63:64].broadcast_to([128, 8, 64]), ALU.subtract, [cs], [dd])
                self.ACT(E1[:], dd[:], AF.Exp, [dd], [E1], scale=-1.0 / 16)
                self.ACT(E2[:], dd[:], AF.Exp, [dd], [E2], scale=1.0 / 16)
                self.ACT(E3[:], cs[:], AF.Exp, [cs], [E3], scale=-1.0 / 16)
                self.STT(qia[:, ch, :], A[ch][:], 0.125, E1[:], ALU.mult, ALU.mult, [A[ch], E1], [qia])
                self.STT(qie[:, ch, :], A[ch][:], 0.125, E3[:], ALU.mult, ALU.mult, [A[ch], E3], [qie])
                self.TT(kde[:, ch, :], Bp[ch][:], E2[:], ALU.mult, [Bp[ch], E2], [kde])
                self.CP(dec[:, ch, g * 8:(g + 1) * 8], E3[:].rearrange("p (c k) -> p c k", k=64)[:, :, 63], [E3], [dec], eng="gpsimd")
            fm = lambda dr: dr.rearrange("(c p) t -> p c t", p=128)[:, :, gs]
            self.LD(fm(self.qintra), qia[:], [qia], [mb["qintra"]], eng="gpsimd")
            self.LD(fm(self.qinter), qie[:], [qie], [mb["qinter"]], eng="gpsimd")
            self.LD(fm(self.kdec), kde[:], [kde], [mb["kdec"]], eng="gpsimd")
            for t in range(4):
                for ch in range(2):
                    self.TR(Tb[:, (t % 4) * 256 + ch * 128:(t % 4) * 256 + (ch + 1) * 128], kde[:, ch, t * 128:(t + 1) * 128], self.ident_bf[:],
                            [kde, self.cbf], [Tb])
            self.CP(kdt[:].rearrange("p t f -> p (t f)"), Tb[:], [Tb], [kdt])
            self.LD(self.kdtok[gs, :].rearrange("(t p) f -> p t f", p=128), kdt[:], [kdt], [mb["kdtok"]], eng="gpsimd")
            for t in range(4):
                tp = Tp[t % 2]
                self.PROJ(tp[:], [(uT[:, kc, t * 128:(t + 1) * 128], w_in[:, kc, 928:1440]) for kc in range(8)], [w_in, uT], [tp])
                self.CP(gvs[:, t, :], tp[:], [tp], [gvs], eng=("scalar" if t % 2 else "vector"))
            self.LD(self.gv[gs, :].rearrange("(t p) f -> p t f", p=128), gvs[:], [gvs], [mb["gv"]], eng="gpsimd")
            for hh in range(4):
                a = A[hh % 2]
                self.PROJ(a[:], [(w_in[:, kc, 1456 + hh * 128:1456 + (hh + 1) * 128], uT[:, kc, :]) for kc in range(8)], [w_in, uT], [a])
                self.ACT(grt[:, hh, :], a[:], AF.Silu, [a], [grt])
            self.LD(self.grs.rearrange("(c p) t -> p c t", p=128)[:, :, gs], grt[:], [grt], [mb["grs"]], eng="gpsimd")
        self.LD(self.decd.rearrange("(c p) n -> p c n", p=128), dec[:], [dec], [mb["decd"]], eng="gpsimd")
    self.gdeps = []
    P.barrier()


KB.inproj0 = _inproj0


class U:
    __slots__ = ("A", "B", "C", "later")

    def __init__(self, A=None, B=None, C=None, later=None):
        self.A, self.B, self.C, self.later = A, B, C, later


def run_units(units, look):
    n = len(units)
    sched = {}
    for i in range(min(look, n)):
        if units[i].A:
            units[i].A()
    for i in range(n):
        if i + look < n and units[i + look].A:
            units[i + look].A()
        if units[i].B:
            units[i].B()
        if units[i].C:
            units[i].C()
        for (dl, fn) in (units[i].later or []):
            sched.setdefault(i + dl, []).append(fn)
        for fn in sched.pop(i, []):
            fn()
    for k in sorted(sched):
        for fn in sched[k]:
            fn()


def _attn_units(self, units, o, sp_list, pt_list, cnt, Kt, Qt, Vfn, qg, scale, ktiles, dk, pre=None):
    nk = len(ktiles)
    for i, kt in enumerate(ktiles):
        d = kt - 4 * qg
        sp = sp_list[cnt[0] % len(sp_list)]
        pt = pt_list[cnt[0] % len(pt_list)]
        cnt[0] += 1
        kc = slice(kt * 128, (kt + 1) * 128)
        q0 = qg * TG
        c0 = max(d, 0) * 128

        def A(sp=sp, kc=kc, d=d, c0=c0, q0=q0, pre=(pre if i == 0 else None)):
            if pre is not None:
                pre()
            if d < 0:
                self.MM(sp[:], Kt[0:dk, kc], Qt[0:dk, q0:q0 + TG], True, True, [Kt, Qt], [sp])
            else:
                self.MM(sp[:, c0:c0 + 128], Kt[0:dk, kc], Qt[0:dk, q0 + c0:q0 + c0 + 128], True, False, [Kt, Qt], [sp])
                self.MM(sp[:, c0:c0 + 128], self.ident_bf[:], self.cpen_bf[:], False, True, [self.cbf], [sp])
                if c0 + 128 < TG:
                    self.MM(sp[:, c0 + 128:TG], Kt[0:dk, kc], Qt[0:dk, q0 + c0 + 128:q0 + TG], True, True, [Kt, Qt], [sp])

        def B(sp=sp, pt=pt, c0=c0):
            self.ACT(pt[:, c0:TG], sp[:, c0:TG], AF.Exp, [sp], [pt], scale=scale)

        def C(pt=pt, c0=c0, kt=kt, i=i):
            self.MM(o[0:65, c0:TG], Vfn(kt), pt[:, c0:TG], i == 0, i == nk - 1, [pt, self.vdep], [o])
        units.append(U(A, B, C))


KB.attn_units = _attn_units


def _mla(self):
    P = self.P
    mb = self.m0b
    with ExitStack() as st:
        T = lambda shape, dt, n: self.tile(st, shape, dt, n)
        Vall = T([128, 32, 520], BF16, "Vall")
        for q4 in range(4):
            self.LD(Vall[:, q4 * 8:(q4 + 1) * 8, :], self.Vs[q4 * 1024:(q4 + 1) * 1024, :].rearrange("(n p) f -> p n f", p=128), [mb["Vs"]], [Vall])
        self.vdep = Vall
        KTh = [T([96, S], BF16, "KTh") for _ in range(2)]
        QTh = [T([96, S], BF16, "QTh") for _ in range(2)]
        PT = [T([128, TG], BF16, "PT") for _ in range(3)]
        rr2 = [T([65, TG], F32, "rr") for _ in range(2)]
        fb2 = [T([65, TG], BF16, "fb") for _ in range(2)]
        bcs2 = [T([64, TG], F32, "bcs") for _ in range(2)]
        ost = [T([64, TG], BF16, "ost") for _ in range(2)]
        Sp = [self.ptile(st, name="S") for _ in range(3)]
        Op = [self.ptile(st, name="O") for _ in range(3)]
        bc2 = [self.ptile(st, name="bc") for _ in range(2)]
        ones32 = self.cst("ones")
        cnt = [0]
        k = 0
        units = []

        def loader(h):
            def f():
                self.LD(KTh[h % 2][:], self.KT[h], [mb["KT"]], [KTh[h % 2]])
                self.LD(QTh[h % 2][:], self.QT[h], [mb["QT"]], [QTh[h % 2]])
            return f
        loader(0)()
        for h in range(8):
            kt_, qt_ = KTh[h % 2], QTh[h % 2]
            for qg in range(NG):
                o = Op[k % 3]
                pre = loader(h + 1) if (qg == 0 and h + 1 < 8) else None
                self.attn_units(units, o, Sp, PT, cnt, kt_, qt_, lambda kt, h=h: Vall[:, kt, h * 65:(h + 1) * 65], qg, 96 ** -0.5,
                                list(range(4 * qg + 4)), 96, pre=pre)

                rr_, fb_, bc_, bs_ = rr2[k % 2], fb2[k % 2], bc2[k % 2], bcs2[k % 2]

                def f0(o=o, rr_=rr_, fb_=fb_):
                    self.ACT(rr_[64:65, :], o[64:65, :], AF.Ln, [o], [rr_])
                    self.ACT(fb_[64:65, :], rr_[64:65, :], AF.Exp, [rr_], [fb_], scale=-1.0)

                def f1(fb_=fb_, bc_=bc_, bs_=bs_):
                    self.MM(bc_[0:64, :], self.ones_bf[64:65, 0:64], fb_[64:65, :], True, True, [fb_, self.cbf], [bc_])
                    self.CP(bs_[:], bc_[0:64, :], [bc_], [bs_], eng="vector")

                def f2(o=o, h=h, qg=qg, os_=ost[k % 2], bs_=bs_):
                    self.TT(os_[:], o[0:64, :], bs_[:], ALU.mult, [o, bs_], [os_])
                    self.LD(self.OTm[h, :, qg * TG:(qg + 1) * TG], os_[:], [os_], [mb["OTm"]], eng="gpsimd")
                units.append(U(None, None, None, later=[(1, f0), (3, f1), (4, f2)]))
                k += 1
        run_units(units, 2)
    P.barrier()


KB.mla = _mla


def _gla(self):
    P = self.P
    mb = self.m0b
    with ExitStack() as st:
        T = lambda shape, dt, n: self.tile(st, shape, dt, n)
        hv = lambda dr, gs: dr.rearrange("(h d) t -> d h t", d=64)[:, :, gs]
        qia = [T([64, 4, TG], BF16, "qia") for _ in range(2)]
        qie = [T([64, 4, TG], BF16, "qie") for _ in range(2)]
        kde = [T([64, 4, TG], BF16, "kde") for _ in range(2)]
        vv = [T([64, 8, 512], BF16, "vv") for _ in range(2)]
        kdt = [T([64, 8, 256], BF16, "kdt") for _ in range(2)]
        grs = [T([128, 4, TG], BF16, "grs") for _ in range(2)]
        dec = T([64, 4, 64], F32, "dec")
        self.LD(dec[:], self.decd.rearrange("(h d) n -> d h n", d=64), [mb["decd"]], [dec])
        onc = T([128, 1], F32, "onc")
        with self.nc.allow_non_contiguous_dma("tiny"):
            self.LD(onc[:], self.I["gla_out_norm"].rearrange("o p -> p o"), [], [onc], allow_slow_non_contiguous=True)
        St = T([64, 4, 128], F32, "St")
        Sbf = T([64, 4, 128], BF16, "Sbf")
        self.MS(St[:], 0.0, [St])
        self.MS(Sbf[:], 0.0, [Sbf])
        ats = [T([64, 256], BF16, "ats") for _ in range(2)]
        sq = [T([128, TG], BF16, "sq") for _ in range(2)]
        rstd = T([128, TG], F32, "rstd")
        on = [T([128, TG], F32, "on") for _ in range(2)]
        ost = [T([128, TG], BF16, "ost") for _ in range(2)]
        Op = [self.ptile(st, name="O") for _ in range(4)]
        at_t = self.ps(st, [64, 512], F32, "at")
        at = [Tl(at_t, "at0"), Tl(at_t, "at1")]
        kv = [self.ptile(st, [64, 512], F32, name="kv") for _ in range(2)]
        ss = self.ptile(st, name="ss")
        tri = self.cst("tri", rows=64)
        self.gdeps = [onc.b]
        for g in range(NG):
            gs = slice(g * TG, (g + 1) * TG)
            b = g % 2
            self.LD(qia[b][:], hv(self.qintra, gs), [mb["qintra"]], [qia[b]])
            self.LD(qie[b][:], hv(self.qinter, gs), [mb["qinter"]], [qie[b]])
            self.LD(kde[b][:], hv(self.kdec, gs), [mb["kdec"]], [kde[b]])
            self.LD(vv[b][:], self.gv[gs, :].rearrange("(c p) f -> p c f", p=64), [mb["gv"]], [vv[b]])
            self.LD(kdt[b][:], self.kdtok[gs, :].rearrange("(c p) f -> p c f", p=64), [mb["kdtok"]], [kdt[b]])
            self.LD(grs[b][:], self.grs.rearrange("(c p) t -> p c t", p=128)[:, :, gs], [mb["grs"]], [grs[b]])
            for c in range(8):
                n = g * 8 + c
                cs_ = slice(c * 64, (c + 1) * 64)
                a_ = at[c % 2]
                ao = (c % 2) * 256
                for h in range(4):
                    self.MM(a_[0:64, ao + h * 64:ao + (h + 1) * 64], kde[b][:, h, cs_], qia[b][:, h, cs_], True, True, [kde[b], qia[b]], [a_])
                as_ = ats[c % 2]
                self.TT(as_[:].rearrange("p (h i) -> p h i", i=64), a_[0:64, ao:ao + 256].rearrange("p (h i) -> p h i", i=64),
                        tri.unsqueeze(1).broadcast_to([64, 4, 64]), ALU.mult, [a_, self.cb], [as_])
                for h in range(4):
                    self.MM(Op[h][:, cs_], vv[b][:, c, h * 128:(h + 1) * 128], as_[:, h * 64:(h + 1) * 64], True, False, [vv[b], as_], [Op[h]])
                    self.MM(Op[h][:, cs_], Sbf[:, h, :], qie[b][:, h, cs_], False, True, [Sbf, qie[b]], [Op[h]])
                kv_ = kv[c % 2]
                for h in range(4):
                    self.MM(kv_[0:64, h * 128:(h + 1) * 128], kdt[b][:, c, h * 64:(h + 1) * 64], vv[b][:, c, h * 128:(h + 1) * 128], True, True,
                            [kdt[b], vv[b]], [kv_])
                self.TT(St[:], St[:], dec[:, :, n:n + 1].broadcast_to([64, 4, 128]), ALU.mult, [St, dec], [St])
                self.TT(St[:].rearrange("p h v -> p (h v)"), St[:].rearrange("p h v -> p (h v)"), kv_[0:64, :], ALU.add, [St, kv_], [St])
                self.CP(Sbf[:], St[:], [St], [Sbf], eng="scalar")
            for h in range(4):
                o = Op[h]
                self.norm_ps([(o[:], o)], [onc[:, 0:1]], 1.0 / 128, ss, sq, rstd, [on[h % 2][:]], on[h % 2])
                os_ = ost[h % 2]
                self.TT(os_[:], on[h % 2][:], grs[b][:, h, :], ALU.mult, [on[h % 2], grs[b]], [os_], eng="gpsimd")
                self.LD(self.OTg[h, :, gs], os_[:], [os_], [mb["OTg"]], eng="gpsimd")
    self.gdeps = []
    P.barrier()


KB.gla = _gla


def _outproj(self, src, dst, w_src, otm_d, otm_b, n_h64, otg_d=None, otg_b=None):
    P = self.P
    hin_v = self.hT[src].rearrange("(fc p) t -> p fc t", p=128)
    hout_v = self.hT[dst].rearrange("(fc p) t -> p fc t", p=128)
    with ExitStack() as st:
        T = lambda shape, dt, n: self.tile(st, shape, dt, n)
        wm = T([64, n_h64, D], BF16, "wm")
        half = n_h64 // 2
        for i in range(2):
            self.LD(wm[:, i * half:(i + 1) * half, :], w_src[i * half * 64:(i + 1) * half * 64, :].rearrange("(h r) n -> r h n", r=64), [], [wm], eng="gpsimd")
        ng = 0
        if otg_d is not None:
            ng = 4
            wg = T([128, 4, D], BF16, "wg")
            self.LD(wg[:], w_src[n_h64 * 64:, :].rearrange("(c p) n -> p c n", p=128), [], [wg], eng="gpsimd")
        hs = [T([128, 8, TG], F32, "hs") for _ in range(2)]
        om = [T([64, n_h64, TG], BF16, "om") for _ in range(2)]
        og = [T([128, 4, TG], BF16, "og") for _ in range(2)] if ng else None
        ho = [T([128, TG], F32, "ho") for _ in range(3)]
        Op = [self.ptile(st, name="O") for _ in range(2)]
        k = 0
        for g in range(NG):
            gs = slice(g * TG, (g + 1) * TG)
            b = g % 2
            self.LD(hs[b][:], hin_v[:, :, gs], [self.hbuf[src]], [hs[b]])
            self.LD(om[b][:], otm_d[:, :, gs].rearrange("h r t -> r h t"), [otm_b], [om[b]])
            if ng:
                self.LD(og[b][:], otg_d[:, :, gs].rearrange("h r t -> r h t"), [otg_b], [og[b]])
            for fc in range(8):
                o = Op[fc % 2]
                fcs = slice(fc * 128, (fc + 1) * 128)
                pairs = [(wm[:, h, fcs], om[b][:, h, :]) for h in range(n_h64)]
                deps = [wm, om[b]]
                if ng:
                    pairs += [(wg[:, c, fcs], og[b][:, c, :]) for c in range(4)]
                    deps += [wg, og[b]]
                self.PROJ(o[:], pairs, deps, [o])
                h_ = ho[k % 3]
                k += 1
                self.TT(h_[:], o[:], hs[b][:, fc, :], ALU.add, [o, hs[b]], [h_])
                self.LD(hout_v[:, fc, gs], h_[:], [h_], [self.hbuf[dst]], eng="gpsimd")
                self.pump(3)
    P.barrier()


KB.outproj = _outproj


def _declare_mix1(self):
    d = self.dram
    self.QN = d("QN", [1024, S], BF16)
    self.KSd = d("KSd", [256, S], BF16)
    self.KWd = d("KWd", [256, S], BF16)
    self.KCd = d("KCd", [256, S], BF16)
    self.VCd = d("VCd", [256, S], BF16)
    self.VSW = d("VSW", [S, 520], BF16)
    self.GT = d("GT", [48, S], F32)
    self.ONT = d("ONT", [16, 64, S], BF16)
    self.m1b = {n: Buf(n) for n in ["QN", "KSd", "KWd", "KCd", "VCd", "VSW", "GT", "ONT"]}


KB.declare_mix1 = _declare_mix1


def _inproj1(self, src):
    P, I = self.P, self.I
    hin_v = self.hT[src].rearrange("(fc p) t -> p fc t", p=128)
    hinb = self.hbuf[src]
    mb = self.m1b
    with ExitStack() as st:
        T = lambda shape, dt, n: self.tile(st, shape, dt, n)
        w_in = T([128, 8, NSA_IN], BF16, "w_in")
        w_sw = T([128, 8, 1536], BF16, "w_sw")
        insrc = I["nsa_w_in"].rearrange("(k p) n -> p k n", p=128)
        with self.nc.allow_non_contiguous_dma("swapped rope halves"):
            for kc in range(8):
                self.LD(w_in[:, kc, :], I["nsa_w_in"][kc * 128:(kc + 1) * 128, :], [], [w_in], eng="gpsimd")
                for (d0, s0, nb) in [(0, 0, 16), (1024, 1536, 4), (1280, 2048, 4)]:
                    dv = w_sw[:, kc, d0:d0 + nb * 64].rearrange("p (b t e) -> p b t e", t=2, e=32)
                    sv = insrc[:, kc, s0:s0 + nb * 64].rearrange("p (b t e) -> p b t e", t=2, e=32)
                    self.LD(dv[:, :, 0, :], sv[:, :, 1, :], [], [w_sw], eng="gpsimd")
                    self.LD(dv[:, :, 1, :], sv[:, :, 0, :], [], [w_sw], eng="gpsimd")
        C, Sg = self.rope_tables(st, 128, 0, 2, 3, "nsa")
        self.nsaC, self.nsaS = C, Sg
        hs = [T([128, 8, TG], F32, "hs") for _ in range(2)]
        uT = T([128, 8, TG], BF16, "uT")
        sq = [T([128, TG], BF16, "sq") for _ in range(2)]
        rstd = T([128, TG], F32, "rstd")
        t1 = [T([128, TG], F32, "t1") for _ in range(2)]
        t2 = [T([128, TG], F32, "t2") for _ in range(2)]
        qst = [T([128, TG], BF16, "qst") for _ in range(3)]
        vst = T([128, 4, 8, 65], BF16, "vst")
        self.MS(vst[:], 1.0, [vst])
        gts = T([48, TG], F32, "gts")
        ss = self.ptile(st, name="ss")
        A = [self.ptile(st, name="A") for _ in range(2)]
        Bp = [self.ptile(st, name="B") for _ in range(2)]
        Tp = [self.ptile(st, name="T") for _ in range(2)]
        self.gdeps = [self.gb]
        gm = [self.gcol[:, 5, fc:fc + 1] for fc in range(8)]
        k = 0
        for g in range(NG):
            gs = slice(g * TG, (g + 1) * TG)
            h_ = hs[g % 2]
            self.LD(h_[:], hin_v[:, :, gs], [hinb], [h_])
            self.norm_ps([(h_[:, fc, :], h_) for fc in range(8)], gm, 1.0 / D, ss, sq, rstd, [uT[:, fc, :] for fc in range(8)], uT)
            jobs = [(c * 128, c * 128, self.QN, c, "QN") for c in range(8)]
            jobs += [(1536 + c * 128, 1024 + c * 128, self.KSd, c, "KSd") for c in range(2)]
            jobs += [(2048 + c * 128, 1280 + c * 128, self.KWd, c, "KWd") for c in range(2)]
            for (ca, cb_, dst, c, nm) in jobs:
                a, b = A[k % 2], Bp[k % 2]
                x1, x2, q_ = t1[k % 2], t2[k % 2], qst[k % 3]
                k += 1
                self.PROJ(a[:], [(w_in[:, kc, ca:ca + 128], uT[:, kc, :]) for kc in range(8)], [w_in, uT], [a])
                self.PROJ(b[:], [(w_sw[:, kc, cb_:cb_ + 128], uT[:, kc, :]) for kc in range(8)], [w_sw, uT], [b])
                self.TT(x1[:], a[:], C[:, gs], ALU.mult, [a, C], [x1])
                self.TT(x2[:], b[:], Sg[:, gs], ALU.mult, [b, Sg], [x2])
                self.TT(q_[:], x1[:], x2[:], ALU.add, [x1, x2], [q_], eng="gpsimd")
                self.LD(dst[c * 128:(c + 1) * 128, gs], q_[:], [q_], [mb[nm]], eng="gpsimd")
            for (ca, dst, c, nm) in [(1024, self.KCd, 0, "KCd"), (1152, self.KCd, 1, "KCd"), (1280, self.VCd, 0, "VCd"), (1408, self.VCd, 1, "VCd")]:
                a = A[k % 2]
                q_ = qst[k % 3]
                k += 1
                self.PROJ(a[:], [(w_in[:, kc, ca:ca + 128], uT[:, kc, :]) for kc in range(8)], [w_in, uT], [a])
                self.CP(q_[:], a[:], [a], [q_], eng="scalar")
                self.LD(dst[c * 128:(c + 1) * 128, gs], q_[:], [q_], [mb[nm]], eng="gpsimd")
            for t in range(4):
                tp = Tp[t % 2]
                self.PROJ(tp[:, 0:256], [(uT[:, kc, t * 128:(t + 1) * 128], w_in[:, kc, 1792:2048]) for kc in range(8)], [w_in, uT], [tp])
                self.PROJ(tp[:, 256:512], [(uT[:, kc, t * 128:(t + 1) * 128], w_in[:, kc, 2304:2560]) for kc in range(8)], [w_in, uT], [tp])
                self.CP(vst[:, t, :, 0:64], tp[:].rearrange("p (h d) -> p h d", d=64), [tp], [vst], eng=("scalar" if t % 2 else "vector"))
            self.LD(self.VSW[gs, :].rearrange("(t p) f -> p t f", p=128), vst[:].rearrange("p t h d -> p t (h d)"), [vst], [mb["VSW"]], eng="gpsimd")
            self.PROJ(A[0][0:48, :], [(w_in[:, kc, 2560:2608], uT[:, kc, :]) for kc in range(8)], [w_in, uT], [A[0]])
            self.ACT(gts[:], A[0][0:48, :], AF.Sigmoid, [A[0]], [gts])
            self.LD(self.GT[:, gs], gts[:], [gts], [mb["GT"]], eng="gpsimd")
    self.gdeps = []
    P.barrier()


KB.inproj1 = _inproj1


def _nsa_attn(self):
    P, I = self.P, self.I
    mb = self.m1b
    SC = 64 ** -0.5
    with ExitStack() as st:
        T = lambda shape, dt, n: self.tile(st, shape, dt, n)
        VSW = T([128, 32, 520], BF16, "VSW")
        for q4 in range(4):
            self.LD(VSW[:, q4 * 8:(q4 + 1) * 8, :], self.VSW[q4 * 1024:(q4 + 1) * 1024, :].rearrange("(n p) f -> p n f", p=128), [mb["VSW"]], [VSW])
        self.vdep = VSW
        cmpm = T([128, 2, S], BF16, "cmpm")
        for i in range(2):
            self.LD(cmpm[:, i, :], I["cmpmask"][i * 128:(i + 1) * 128, :], [], [cmpm], eng="gpsimd")
        ovl = T([128, 2, 65], BF16, "ovl")
        self.LD(ovl[:], I["ovl"].rearrange("(i p) f -> p i f", p=128), [], [ovl], eng="gpsimd")
        eo = CB["E"][0]
        KCMP = T([64, 4, 256], BF16, "KCMP")
        VCMP = T([128, 4, 2, 65], BF16, "VCMP")
        self.MS(KCMP[:], 0.0, [KCMP])
        self.MS(VCMP[:], 0.0, [VCMP])
        Sp = [self.ptile(st, name="S") for _ in range(3)]
        Oc = self.ptile(st, name="Oc")
        Os = self.ptile(st, name="Os")
        Ow = self.ptile(st, name="Ow")
        imp = Os
        M1 = self.ptile(st, name="M1")
        M2 = self.ptile(st, name="M2")
        with ExitStack() as st2:
            T2 = lambda shape, dt, n: self.tile(st2, shape, dt, n)
            Cc, Sc = self.rope_tables(st2, 64, 0, 2, 3, "cmp", pos_ap=I["positions"][0:1, 31:S:16], S=255)
            w1 = [T2([64, 32, 128], BF16, "w1") for _ in range(2)]
            w2 = [T2([128, 64], BF16, "w2") for _ in range(2)]
            w2s = T2([128, 64], BF16, "w2s")
            posT = [T2([64, 32], BF16, "posT") for _ in range(2)]
            with self.nc.allow_non_contiguous_dma("small"):
                for i, (a, b_, pp) in enumerate([("nsa_ck_w1", "nsa_ck_w2", "nsa_pos_k"), ("nsa_cv_w1", "nsa_cv_w2", "nsa_pos_v")]):
                    self.LD(w1[i][:], I[a].rearrange("(l d) n -> d l n", d=64), [], [w1[i]], eng="gpsimd")
                    self.LD(w2[i][:], I[b_], [], [w2[i]], eng="gpsimd")
                    self.LD(posT[i][:], I[pp].rearrange("l d -> d l"), [], [posT[i]], eng="gpsimd", allow_slow_non_contiguous=True)
                self.LD(w2s[:, 0:32], I["nsa_ck_w2"][:, 32:64], [], [w2s], eng="gpsimd")
                self.LD(w2s[:, 32:64], I["nsa_ck_w2"][:, 0:32], [], [w2s], eng="gpsimd")
            cb_ = T2([128, 2], F32, "cbias")
            for i in range(2):
                for l in range(32):
                    self.MM(M1[:, i:i + 1], w1[i][:, l, :], posT[i][:, l:l + 1], l == 0, l == 31, [w1[i], posT[i]], [M1])
            self.CP(cb_[:], M1[:, 0:2], [M1], [cb_])
            src = [T2([64, S], BF16, "csrc") for _ in range(2)]
            hid = [T2([128, 256], BF16, "hid") for _ in range(2)]
            x1 = T2([64, 256], F32, "x1")
            x2 = T2([64, 256], F32, "x2")
            for i in range(2):
                self.MS(hid[i][:], 0.0, [hid[i]])
            k = 0
            for g in range(4):
                for i, (dsrc, nm) in enumerate([(self.KCd, "KCd"), (self.VCd, "VCd")]):
                    s_ = src[k % 2]
                    hd = hid[k % 2]
                    ps_ = Sp[k % 2]
                    k += 1
                    self.LD(s_[:], dsrc[g * 64:(g + 1) * 64, :], [mb[nm]], [s_])
                    for l in range(32):
                        self.MM(ps_[:, 0:255], w1[i][:, l, :], s_[:, l:l + 16 * 254 + 1:16], l == 0, l == 31, [w1[i], s_], [ps_])
                    self.ACT(hd[:, 0:255], ps_[:, 0:255], AF.Silu, [ps_, cb_], [hd], bias=cb_[:, i:i + 1], scale=1.0)
                    if i == 0:
                        self.MM(M1[0:64, 0:255], w2[0][:], hd[:, 0:255], True, True, [w2[0], hd], [M1])
                        self.MM(M2[0:64, 0:255], w2s[:], hd[:, 0:255], True, True, [w2s, hd], [M2])
                        self.TT(x1[:, 0:255], M1[0:64, 0:255], Cc[0:64, :], ALU.mult, [M1, Cc], [x1])
                        self.TT(x2[:, 0:255], M2[0:64, 0:255], Sc[0:64, :], ALU.mult, [M2, Sc], [x2])
                        self.TT(KCMP[:, g, 0:255], x1[:, 0:255], x2[:, 0:255], ALU.add, [x1, x2], [KCMP], eng="gpsimd")
                    else:
                        self.MM(M1[:, 0:64], hd[:, 0:128], w2[1][:], True, True, [w2[1], hd], [M1])
                        self.MM(M1[0:127, 64:128], hd[:, 128:255], w2[1][:], True, True, [w2[1], hd], [M1])
                        self.CP(VCMP[:, g, 0, 0:64], M1[:, 0:64], [M1], [VCMP])
                        self.CP(VCMP[0:127, g, 1, 0:64], M1[0:127, 64:128], [M1], [VCMP])
                        self.MS(VCMP[:, g, 0, 64:65], 1.0, [VCMP])
                        self.MS(VCMP[0:127, g, 1, 64:65], 1.0, [VCMP])
            P.barrier()
        KS = [T([128, S], BF16, "KS") for _ in range(2)]
        for i in range(2):
            self.LD(KS[i][64:128, :], I["consts"][0:64, eo:eo + 32 * 128], [], [KS[i]], eng="gpsimd")
        KW = [T([64, S], BF16, "KW") for _ in range(2)]
        PT = [T([128, TG], BF16, "PT") for _ in range(3)]
        PC = [T([128, 2, TG], BF16, "PC") for _ in range(4)]
        ost = [T([64, TG], BF16, "ost") for _ in range(2)]
        irec = T([128, 4], F32, "irec")
        itmp = T([128, 4, 64], F32, "itmp")
        iacc = T([128, 4, 64], F32, "iacc")
        score = T([128, 4, 64], F32, "score")
        sc2 = T([128, 64], F32, "sc2")
        m8 = T([128, 16], F32, "m8")
        ones32 = self.cst("ones")
        so = CB["sel48"][0]
        wo = CB["wide"][0]
        col0 = self.cst("col0")
        cnt = [0]
        hk = 0
        ak = 0

        A4 = [T([64, TG], F32, "A4") for _ in range(4)]
        Q4 = [T([128, S], BF16, "Q4") for _ in range(4)]
        Q4n = [Tl(q.t, "Q4n") for q in Q4]
        nself = T([128, 4, 128], F32, "nself")
        self.MS(nself[:], 0.0, [nself])
        ident32 = self.cst("ident")

        selbf = T([48, 48 * 64 + 1], BF16, "selbf")
        self.LD(selbf[:], I["consts"][0:48, so:so + 48 * 64 + 1], [], [selbf], eng="gpsimd")
        GTb = T([48, S], BF16, "GTb")
        self.LD(GTb[:], self.GT, [mb["GT"]], [GTb], eng="gpsimd")
        Mb = [M1, M2]
        Ocs = [Oc, Ow]
        rr2 = [T([65, TG], F32, "rr") for _ in range(2)]
        fb2 = [T([65, TG], BF16, "fb") for _ in range(2)]
        bcs2 = [T([64, TG], F32, "bcs") for _ in range(2)]
        tmp2 = [T([64, TG], F32, "tmp") for _ in range(2)]
        fi = [0]

        def finish_unit(o, row, a_, first, qs, pre=None, extra=None, delays=(1, 2)):
            k = fi[0]
            fi[0] += 1
            m, rr_, fb_, bs_, tmp_ = Mb[k % 2], rr2[k % 2], fb2[k % 2], bcs2[k % 2], tmp2[k % 2]

            def f0():
                if pre is not None:
                    pre()
                if first:
                    self.TS(rr_[64:65, :], o[64:65, :], 1e-18, None, ALU.max, None, [o], [rr_])
                    self.ACT(rr_[64:65, :], rr_[64:65, :], AF.Ln, [rr_], [rr_])
                else:
                    self.ACT(rr_[64:65, :], o[64:65, :], AF.Ln, [o], [rr_])
                self.ACT(rr_[64:65, :], rr_[64:65, :], AF.Exp, [rr_], [rr_], scale=-1.0)
                self.MM(m[0:65, :], selbf[0:48, row * 64:row * 64 + 65], GTb[0:48, qs], True, True, [GTb, selbf], [m])

            def f1():
                self.TT(fb_[64:65, :], m[64:65, :], rr_[64:65, :], ALU.mult, [m, rr_], [fb_])
                self.MM(m[0:64, :], self.ones_bf[64:65, 0:64], fb_[64:65, :], True, True, [fb_, self.cbf], [m])

            def f2():
                self.CP(bs_[:], m[0:64, :], [m], [bs_], eng="vector")
                if first:
                    self.TT(a_[:], o[0:64, :], bs_[:], ALU.mult, [o, bs_], [a_])
                else:
                    self.TT(tmp_[:], o[0:64, :], bs_[:], ALU.mult, [o, bs_], [tmp_])
                    self.TT(a_[:], a_[:], tmp_[:], ALU.add, [a_, tmp_], [a_], eng="gpsimd")
                if extra is not None:
                    extra()
            if len(delays) == 3:
                return U(None, None, None, later=[(delays[0], f0), (delays[1], f1), (delays[2], f2)])
            return U(None, None, f0, later=[(delays[0], f1), (delays[1], f2)])

        for g in range(4):
            ks_, kw_ = KS[g % 2], KW[g % 2]
            self.LD(ks_[0:64, :], self.KSd[g * 64:(g + 1) * 64, :], [mb["KSd"]], [ks_])
            self.LD(kw_[:], self.KWd[g * 64:(g + 1) * 64, :], [mb["KWd"]], [kw_])
            for hh in range(4):
                h = g * 4 + hh
                self.LD(Q4[hh][0:64, :], self.QN[h * 64:(h + 1) * 64, :], [mb["QN"]], [Q4[hh]])
            for qg in range(NG):
                qs = slice(qg * TG, (qg + 1) * TG)
                q0 = qg * TG
                ntile = 2 if qg >= 4 else 1
                units = []
                accs = [Oc, Ow, Sp[1], Sp[2]]
                XS = [Sp[0], M1, M2]
                xi = 0
                for hh in range(4):
                    h = g * 4 + hh
                    pc = PC[hh]
                    for i in range(ntile):
                        sp = XS[xi % 3]
                        xi += 1

                        def A(sp=sp, i=i, hh=hh):
                            self.MM(sp[:], KCMP[:, g, i * 128:(i + 1) * 128], Q4[hh][0:64, qs], True, False, [KCMP, Q4[hh]], [sp])
                            self.MM(sp[:], self.ident_bf[:], cmpm[:, i, qs], False, True, [cmpm, self.cbf], [sp])

                        def B(sp=sp, i=i, pc=pc):
                            self.ACT(pc[:, i, :], sp[:], AF.Exp, [sp], [pc], scale=SC)

                        def C(i=i, pc=pc, oc=accs[hh]):
                            self.MM(oc[0:65, :], VCMP[:, g, i, :], pc[:, i, :], i == 0, i == ntile - 1, [VCMP, pc], [oc])
                        units.append(U(A, B, C))
                for hh in range(4):
                    h = g * 4 + hh
                    pc = PC[hh]

                    def pre(hh=hh, pc=pc):
                        for qt in range(4):
                            for i in range(ntile):
                                self.MM(imp[:, qt * 65:(qt + 1) * 65], pc[:, i, qt * 128:(qt + 1) * 128], ovl[:, i, :], i == 0, i == ntile - 1, [pc, ovl], [imp])
                        iv = imp[:, 0:260].rearrange("p (t f) -> p t f", f=65)
                        self.TS(irec[:], iv[:, :, 64], 1e-30, None, ALU.max, None, [imp], [irec])
                        self.P.dve(lambda e: e.reciprocal(out=irec[:], in_=irec[:]), reads=[irec.b], writes=[irec.b])
                        if hh == 0:
                            self.TT(iacc[:], iv[:, :, 0:64], irec[:].unsqueeze(2).broadcast_to([128, 4, 64]), ALU.mult, [imp, irec], [iacc])
                        else:
                            self.TT(itmp[:], iv[:, :, 0:64], irec[:].unsqueeze(2).broadcast_to([128, 4, 64]), ALU.mult, [imp, irec], [itmp])
                            self.TT(iacc[:], iacc[:], itmp[:], ALU.add, [iacc, itmp], [iacc], eng="gpsimd")
                    units.append(finish_unit(accs[hh], 3 * h + 0, A4[hh], True, qs, pre=pre, delays=(1, 1)))
                run_units(units, 2)
                for qt in range(4):
                    tix = qg * 4 + qt
                    self.TT(score[:, qt, :], iacc[:, qt, :], self.c32[:, wo + 64 - 2 * tix:wo + 128 - 2 * tix], ALU.add, [iacc, self.cb], [score])
                    self.TT(score[:, qt, :], score[:, qt, :], col0, ALU.max, [score, self.cb], [score])
                    self.P.dve(lambda e, qt=qt: e.max(out=m8[:, 0:8], in_=score[:, qt, :]), reads=[score.b], writes=[m8.b])
                    self.P.dve(lambda e, qt=qt: e.match_replace(out=sc2[:], in_to_replace=m8[:, 0:8], in_values=score[:, qt, :], imm_value=-3.0e38),
                               reads=[score.b, m8.b], writes=[sc2.b])
                    self.P.dve(lambda e: e.max(out=m8[:, 8:16], in_=sc2[:]), reads=[sc2.b], writes=[m8.b])
                    self.TS(nself[:, qt, 64:128], score[:, qt, :], m8[:, 15:16], -1.0, ALU.is_ge, ALU.add, [score, m8], [nself])
                    self.TR(M2[:, qt * 128:(qt + 1) * 128], nself[:, qt, :], ident32, [nself, self.cb], [M2])
                for hh in range(4):
                    self.CP(Q4[hh][64:128, qs], M2[64:128, :], [M2], [Q4n[hh]], eng="scalar")
                units = []
                for hh in range(4):
                    h = g * 4 + hh
                    qh_ = Q4[hh]
                    nk = 4 * qg + 4
                    for kt in range(nk):
                        d = kt - 4 * qg
                        sp = Sp[cnt[0] % 3]
                        pt = PT[cnt[0] % 3]
                        cnt[0] += 1
                        kc = slice(kt * 128, (kt + 1) * 128)
                        c0 = max(d, 0) * 128

                        def A(sp=sp, kc=kc, d=d, c0=c0, qh_=qh_, qn_=Q4n[hh]):
                            if d < 0:
                                self.MM(sp[:], ks_[:, kc], qh_[:, qs], True, True, [ks_, qh_, qn_], [sp])
                            else:
                                self.MM(sp[:, c0:c0 + 128], ks_[:, kc], qh_[:, q0 + c0:q0 + c0 + 128], True, False, [ks_, qh_, qn_], [sp])
                                self.MM(sp[:, c0:c0 + 128], self.ident_bf[:], self.cpen_bf[:], False, True, [self.cbf], [sp])
                                if c0 + 128 < TG:
                                    self.MM(sp[:, c0 + 128:TG], ks_[:, kc], qh_[:, q0 + c0 + 128:q0 + TG], True, True, [ks_, qh_, qn_], [sp])

                        def B(sp=sp, pt=pt, c0=c0):
                            self.ACT(pt[:, c0:TG], sp[:, c0:TG], AF.Exp, [sp], [pt], scale=SC)

                        def C(pt=pt, c0=c0, kt=kt):
                            self.MM(Os[0:65, c0:TG], VSW[:, kt, g * 65:(g + 1) * 65], pt[:, c0:TG], kt == 0, kt == nk - 1, [pt, VSW], [Os])
                        units.append(U(A, B, C))
                    units.append(finish_unit(Os, 3 * h + 1, A4[hh], False, qs, delays=((2, 5, 7) if qg >= 1 else (1, 3, 4))))
                    kts = [4 * qg] + [kt for kt in range(4 * qg - 4, 4 * qg + 4) if kt >= 0 and kt != 4 * qg]
                    for i, kt in enumerate(kts):
                        d = kt - 4 * qg
                        sp = Sp[cnt[0] % 3]
                        pt = PT[cnt[0] % 3]
                        cnt[0] += 1
                        kc = slice(kt * 128, (kt + 1) * 128)
                        lo = max(d, 0) * 128
                        hi = min(d + 5, 4) * 128
                        if d >= 0:
                            pb, pen = lo, self.cpen_bf
                            rest = (lo + 128, hi)
                        else:
                            pb, pen = hi - 128, self.bpen_bf
                            rest = (lo, hi - 128)

                        def A(sp=sp, kc=kc, pb=pb, pen=pen, rest=rest, qh_=qh_):
                            self.MM(sp[:, pb:pb + 128], kw_[:, kc], qh_[0:64, q0 + pb:q0 + pb + 128], True, False, [kw_, qh_], [sp])
                            self.MM(sp[:, pb:pb + 128], self.ident_bf[:], pen[:], False, True, [self.cbf], [sp])
                            if rest[1] > rest[0]:
                                self.MM(sp[:, rest[0]:rest[1]], kw_[:, kc], qh_[0:64, q0 + rest[0]:q0 + rest[1]], True, True, [kw_, qh_], [sp])

                        def B(sp=sp, pt=pt, lo=lo, hi=hi):
                            self.ACT(pt[:, lo:hi], sp[:, lo:hi], AF.Exp, [sp], [pt], scale=SC)

                        def C(pt=pt, lo=lo, hi=hi, kt=kt, i=i, nw=len(kts)):
                            self.MM(Ow[0:65, lo:hi], VSW[:, kt, (4 + g) * 65:(5 + g) * 65], pt[:, lo:hi], i == 0, i == nw - 1, [pt, VSW], [Ow])
                        units.append(U(A, B, C))

                    def store(h=h, hh=hh, qs=qs):
                        os_ = ost[h % 2]
                        self.CP(os_[:], A4[hh][:], [A4[hh]], [os_], eng="gpsimd")
                        self.LD(self.ONT[h, :, qs], os_[:], [os_], [mb["ONT"]], eng="gpsimd")
                    units.append(finish_unit(Ow, 3 * h + 2, A4[hh], False, qs, extra=store, delays=((2, 5, 7) if qg >= 1 else (1, 3, 4))))
                run_units(units, 2)
    P.barrier()


KB.nsa_attn = _nsa_attn
```

```python
import numpy as np
from contextlib import ExitStack
import concourse.bass as bass
import concourse.mybir as mybir
from concourse.bass_utils import run_bass_kernel_spmd

F32 = mybir.dt.float32
BF16 = mybir.dt.bfloat16
I32 = mybir.dt.int32
ALU = mybir.AluOpType
AF = mybir.ActivationFunctionType
AX = mybir.AxisListType

S = 4096
D = 1024
DFF = 2816
NFC = 22
NG = 8
TG = 512
EPS = 1e-6
HY_IN = 1968
NSA_IN = 2608
NEGB = -30000.0

ENGINES = ["tensor", "vector", "scalar", "gpsimd", "sync"]
class Buf:
    __slots__ = ("name", "last_w", "readers")

    def __init__(self, name=""):
        self.name = name
        self.last_w = None
        self.readers = []


class Op:
    __slots__ = ("eng", "fn", "raw", "oth", "is_dma", "sig", "sem", "semval", "prev_semval", "gidx")


class Prog:
    def __init__(self, nc, n_dma_sems=12):
        self.nc = nc
        self.ops = {e: [] for e in ENGINES}
        self.n = 0
        self.n_dma_sems = n_dma_sems
        self.pending_barrier = {}
        self.dma_last = {}
        self.dma_cnt = {}

    def op(self, eng, fn, reads=(), writes=(), dma=False):
        o = Op()
        o.eng = eng
        o.fn = fn
        o.is_dma = dma
        o.raw = set()
        o.oth = set()
        o.sig = False
        o.sem = None
        o.semval = 0
        o.prev_semval = 0
        o.gidx = self.n
        self.n += 1
        for b in reads:
            if b.last_w is not None:
                o.raw.add(b.last_w)
        for b in writes:
            if b.last_w is not None:
                o.oth.add(b.last_w)
            for r in b.readers:
                o.oth.add(r)
        for b in reads:
            b.readers.append(o)
        for b in writes:
            b.last_w = o
            b.readers = []
        o.raw.discard(o)
        o.oth.discard(o)
        if eng in self.pending_barrier:
            for d in self.pending_barrier.pop(eng):
                o.raw.add(d)
        self.ops[eng].append(o)
        if dma:
            k = self.dma_cnt.get(eng, 0)
            self.dma_cnt[eng] = k + 1
            self.dma_last[(eng, k % self.n_dma_sems)] = o
        return o

    def barrier(self):
        deps = []
        for e in ENGINES:
            comp = [x for x in self.ops[e] if not x.is_dma]
            if comp:
                deps.append(comp[-1])
        deps.extend(self.dma_last.values())
        for e in ENGINES:
            self.pending_barrier[e] = list(deps) + self.pending_barrier.get(e, [])

    def mm(self, fn, reads=(), writes=()):
        return self.op("tensor", fn, reads, writes)

    def dve(self, fn, reads=(), writes=()):
        return self.op("vector", fn, reads, writes)

    def act(self, fn, reads=(), writes=()):
        return self.op("scalar", fn, reads, writes)

    def pool(self, fn, reads=(), writes=()):
        return self.op("gpsimd", fn, reads, writes)

    def load(self, out, in_, reads=(), writes=(), eng="sync", **kw):
        return self.op(eng, lambda e: e.dma_start(out=out, in_=in_, **kw), reads, writes, dma=True)

    def needed_deps(self, o):
        res = []
        for d in o.raw:
            if d.eng == o.eng and not d.is_dma and not o.is_dma:
                if o.eng == "tensor":
                    continue
                res.append(d)
            else:
                res.append(d)
        for d in o.oth:
            if d.eng == o.eng and not d.is_dma and not o.is_dma:
                continue
            res.append(d)
        return res

    def finalize(self, stack):
        nc = self.nc
        for e in ENGINES:
            for o in self.ops[e]:
                if o.is_dma:
                    o.sig = True
                for d in self.needed_deps(o):
                    d.sig = True
        csem = {}
        for e in ["tensor", "vector", "scalar", "gpsimd"]:
            csem[e] = stack.enter_context(nc.semaphore("c_" + e))
        dpool = {}
        for e in ["sync", "gpsimd", "scalar"]:
            if any(o.is_dma for o in self.ops[e]):
                dpool[e] = [stack.enter_context(nc.semaphore("d_%s_%d" % (e, i))) for i in range(self.n_dma_sems)]
        for e in ENGINES:
            cnt = 0
            k = 0
            uses = {}
            for o in self.ops[e]:
                if o.is_dma:
                    s = dpool[e][k % self.n_dma_sems]
                    k += 1
                    o.sem = s
                    o.prev_semval = uses.get(id(s), 0)
                    o.semval = o.prev_semval + 16
                    uses[id(s)] = o.semval
                elif o.sig:
                    cnt += 1
                    o.sem = csem[e]
                    o.semval = cnt
        self.final_dma = []
        for e in dpool:
            last = {}
            for o in self.ops[e]:
                if o.is_dma:
                    last[id(o.sem)] = (o.sem, o.semval)
            self.final_dma.append((e, list(last.values())))
        block = stack.enter_context(nc.Block())
        prog = self

        def make(e):
            def body(eng):
                waited = {}
                for o in prog.ops[e]:
                    need = {}
                    for d in prog.needed_deps(o):
                        key = id(d.sem)
                        if key not in need or need[key][1] < d.semval:
                            need[key] = (d.sem, d.semval)
                    if o.is_dma and o.prev_semval > 0:
                        key = id(o.sem)
                        if key not in need or need[key][1] < o.prev_semval:
                            need[key] = (o.sem, o.prev_semval)
                    for key, (s, v) in need.items():
                        if waited.get(key, 0) >= v:
                            continue
                        eng.wait_ge(s, v)
                        waited[key] = v
                    ins = o.fn(eng)
                    if o.sig:
                        ins.then_inc(o.sem, 16 if o.is_dma else 1)
                for (ee, lst) in prog.final_dma:
                    if ee == e:
                        for (s, v) in lst:
                            if waited.get(id(s), 0) < v:
                                eng.wait_ge(s, v)
            return body

        for e in ENGINES:
            if not self.ops[e]:
                continue
            getattr(block, e)(make(e))


CB = {}
_off = 0
for _n, _w in [("ident", 128), ("ones", 128), ("cpen", 128), ("bpen", 128), ("tri", 64), ("scanmask", 512),
               ("freq", 4), ("wide", 128), ("col0", 64), ("sel48", 48 * 64 + 1), ("E", 32 * 128)]:
    CB[_n] = (_off, _w)
    _off += _w
CB_W = _off


def make_consts():
    c = np.zeros((128, CB_W), np.float32)
    p = np.arange(128)[:, None]

    def put(n, a):
        o, w = CB[n]
        c[: a.shape[0], o:o + a.shape[1]] = a
    put("ident", np.eye(128, dtype=np.float32))
    put("ones", np.ones((128, 128), np.float32))
    f = np.arange(128)[None, :]
    put("cpen", np.where(p <= f, 0.0, NEGB).astype(np.float32))
    put("bpen", np.where(p > f, 0.0, NEGB).astype(np.float32))
    j = np.arange(64)[:, None]
    i = np.arange(64)[None, :]
    put("tri", (j <= i).astype(np.float32))
    sm = np.ones((128, 512), np.float32)
    sm[:, ::64] = 0.0
    put("scanmask", sm)
    fr = np.zeros((128, 4), np.float32)
    r = np.arange(128)
    im = (r - 64) % 16
    fr[:, 0] = (10000.0 ** (-im * (2.0 / 32))) / (2 * np.pi)
    fr[:, 1] = np.where(((r - 64) % 32) < 16, -1.0, 1.0) * (2 * np.pi * (1 - 1e-6))
    i2 = r % 32
    fr[:, 2] = (10000.0 ** (-i2 * (2.0 / 64))) / (2 * np.pi)
    fr[:, 3] = np.where((r % 64) < 32, -1.0, 1.0) * (2 * np.pi * (1 - 1e-6))
    put("freq", fr)
    x = np.arange(128)[None, :]
    rel = x - 64 - (p >= 64)
    wide = np.where(rel > 0, -1e30, np.where(rel >= -1, 1e4, 0.0)).astype(np.float32)
    put("wide", wide)
    c0 = np.full((128, 64), -3e38, np.float32)
    c0[:, 0] = 1e4
    put("col0", c0)
    sel = np.zeros((128, 48 * 64 + 1), np.float32)
    for rr in range(48):
        sel[rr, 1 + rr * 64:1 + (rr + 1) * 64] = 1.0
    put("sel48", sel)
    E = np.zeros((128, 32 * 128), np.float32)
    for kt in range(32):
        for key in range(128):
            E[2 * kt + (key >= 64), kt * 128 + key] = -NEGB
    put("E", E)
    return c


def make_cmp_consts():
    n = np.arange(256)[:, None]
    t = np.arange(S)[None, :]
    cm = np.where((16 * n + 31 <= t) & (n < 255), 0.0, NEGB).astype(np.float32)
    ncmp = np.arange(255)
    cs_ = ncmp * 16
    bs_ = np.arange(64) * 64
    ov = np.minimum(cs_[:, None] + 32, bs_[None, :] + 64) - np.maximum(cs_[:, None], bs_[None, :])
    ovl = np.zeros((256, 65), np.float32)
    ovl[:255, :64] = np.clip(ov, 0, None) / 32.0
    ovl[:255, 64] = 1.0
    return cm, ovl


class KB:
    def __init__(self, debug_phases=None):
        self.nc = bass.Bass("TRN2", target_bir_lowering=False)
        self.P = Prog(self.nc)
        self.debug_phases = debug_phases
        self.uid = 0
        self.gdeps = []

    def name(self, p):
        self.uid += 1
        return "%s_%d" % (p, self.uid)

    def dram(self, name, shape, dt, kind="Internal"):
        return self.nc.dram_tensor(name, list(shape), dt, kind=kind).ap()

    def sb(self, st, shape, dt, name="t"):
        return st.enter_context(self.nc.sbuf_tensor(self.name(name), list(shape), dt))

    def ps(self, st, shape, dt=F32, name="p"):
        return st.enter_context(self.nc.psum_tensor(self.name(name), list(shape), dt))

    def declare(self):
        d = self.dram
        I = {}
        I["x"] = d("x", [S, D], F32, "ExternalInput")
        I["positions"] = d("positions", [1, S], I32, "ExternalInput")
        I["consts"] = d("consts", [128, CB_W], F32, "ExternalInput")
        I["cmpmask"] = d("cmpmask", [256, S], F32, "ExternalInput")
        I["ovl"] = d("ovl", [256, 65], F32, "ExternalInput")
        for n, shp in [("ffn_norm", [4, D]), ("ffn_w_gate", [4, D, DFF]), ("ffn_w_up", [4, D, DFF]),
                       ("ffn_w_down", [4, DFF, D]), ("mix_norm", [2, D]), ("hy_w_in", [D, HY_IN]),
                       ("mla_q_norm", [1, 256]), ("mla_w_uq", [256, 768]), ("mla_kv_norm", [1, 128]),
                       ("mla_w_ukv", [128, 1024]), ("gla_w_a2", [16, 256]), ("gla_b_a", [1, 256]),
                       ("gla_out_norm", [1, 128]), ("hy_w_out", [D, D]), ("nsa_w_in", [D, NSA_IN]),
                       ("nsa_pos_k", [32, 64]), ("nsa_pos_v", [32, 64]), ("nsa_ck_w1", [2048, 128]),
                       ("nsa_ck_w2", [128, 64]), ("nsa_cv_w1", [2048, 128]), ("nsa_cv_w2", [128, 64]),
                       ("nsa_w_out", [D, D]), ("final_norm", [1, D])]:
            I[n] = d(n, shp, F32, "ExternalInput")
        self.I = I
        self.out = d("out", [S, D], F32, "ExternalOutput")
        self.hT = [d("hT%d" % i, [D, S], F32) for i in range(2)]
        self.hbuf = [Buf("hT0"), Buf("hT1")]
        self.wgu = d("wgu", [4, NFC, 128, 2 * 8 * 128], BF16)
        self.wdn = d("wdn", [4, 8, 128, NFC * 128], BF16)
        self.wgu_b = {}
        self.wdn_b = {}
        self.prepq = []

    def prep_ffn(self, f):
        I = self.I
        items = []
        self.wgu_b[f] = {}
        self.wdn_b[f] = []
        for c in range(NFC):
            for which, src in enumerate([I["ffn_w_gate"], I["ffn_w_up"]]):
                bf_ = Buf("wgu")
                self.wgu_b[f][(which, c)] = bf_
                dst = self.wgu[f, c].rearrange("p (w k j) -> p w k j", w=2, k=8, j=128)[:, which, :, :]
                s_ = src[f, :, c * 128:(c + 1) * 128].rearrange("(k p) j -> p k j", p=128)
                items.append((dst, s_, bf_))
        for c in range(NFC):
            bf_ = Buf("wdn")
            self.wdn_b[f].append(bf_)
            dst = self.wdn[f].rearrange("fc p (c j) -> p fc c j", c=NFC, j=128)[:, :, c, :]
            s_ = I["ffn_w_down"][f, c * 128:(c + 1) * 128, :].rearrange("p (fc j) -> p fc j", j=128)
            items.append((dst, s_, bf_))
        self.prepq.extend(items)

    def pump(self, n):
        for _ in range(n):
            if not self.prepq:
                return
            dst, s_, bf_ = self.prepq.pop(0)
            self.P.load(dst, s_, writes=[bf_], eng="gpsimd")

    def load_consts(self, st):
        P = self.P
        self.c32 = self.sb(st, [128, CB["E"][0]], F32, "c32")
        self.cb = Buf("c32")
        P.load(self.c32[:], self.I["consts"][:, 0:CB["E"][0]], writes=[self.cb])
        self.ident_bf = self.sb(st, [128, 128], BF16, "identbf")
        self.ones_bf = self.sb(st, [128, 128], BF16, "onesbf")
        self.cpen_bf = self.sb(st, [128, 128], BF16, "cpenbf")
        self.bpen_bf = self.sb(st, [128, 128], BF16, "bpenbf")
        self.cbf = Buf("cbf")
        for t, n in [(self.ident_bf, "ident"), (self.ones_bf, "ones"), (self.cpen_bf, "cpen"), (self.bpen_bf, "bpen")]:
            o, w = CB[n]
            P.dve(lambda e, t=t, o=o, w=w: e.tensor_copy(out=t[:], in_=self.c32[:, o:o + w]), reads=[self.cb], writes=[self.cbf])
        self.gcol = self.sb(st, [128, 7, 8], F32, "gcol")
        self.gb = Buf("gcol")
        with self.nc.allow_non_contiguous_dma("tiny gain vectors"):
            for j, src in [(0, self.I["ffn_norm"][0:1, :]), (1, self.I["ffn_norm"][1:2, :]), (2, self.I["ffn_norm"][2:3, :]),
                           (3, self.I["ffn_norm"][3:4, :]), (4, self.I["mix_norm"][0:1, :]), (5, self.I["mix_norm"][1:2, :]),
                           (6, self.I["final_norm"][0:1, :])]:
                P.load(self.gcol[:, j, :], src.rearrange("o (fc p) -> p (o fc)", p=128), writes=[self.gb], allow_slow_non_contiguous=True)

    def cst(self, n, rows=128, c0=0, c1=None):
        o, w = CB[n]
        if c1 is None:
            c1 = w
        return self.c32[0:rows, o + c0:o + c1]

    def transpose_in(self):
        P = self.P
        with ExitStack() as st:
            xt = [self.sb(st, [128, D], F32, "xt") for _ in range(4)]
            xb = [Buf("xt%d" % i) for i in range(4)]
            tp = [self.ps(st, [128, 512], F32, "tp") for _ in range(2)]
            tb = [Buf("tp0"), Buf("tp1")]
            stg = [self.sb(st, [128, 8, 512], F32, "stg") for _ in range(2)]
            sgb = [Buf("stg0"), Buf("stg1")]
            ident = self.cst("ident")
            k = 0
            for g in range(NG):
                for t in range(4):
                    r0 = g * TG + t * 128
                    P.load(xt[t][:], self.I["x"][r0:r0 + 128, :], writes=[xb[t]])
                so = stg[g % 2]
                for fc in range(8):
                    pb = tp[k % 2]
                    for t in range(4):
                        P.mm(lambda e, pb=pb, t=t, fc=fc: e.transpose(out=pb[:, t * 128:(t + 1) * 128], in_=xt[t][:, fc * 128:(fc + 1) * 128], identity=ident),
                             reads=[xb[t], self.cb], writes=[tb[k % 2]])
                    if fc % 2 == 0:
                        P.act(lambda e, pb=pb, fc=fc, so=so: e.copy(out=so[:, fc, :], in_=pb[:]), reads=[tb[k % 2]], writes=[sgb[g % 2]])
                    else:
                        P.dve(lambda e, pb=pb, fc=fc, so=so: e.tensor_copy(out=so[:, fc, :], in_=pb[:]), reads=[tb[k % 2]], writes=[sgb[g % 2]])
                    k += 1
                P.load(self.hT[0].rearrange("(fc p) t -> p fc t", p=128)[:, :, g * TG:(g + 1) * TG], so[:],
                       reads=[sgb[g % 2]], writes=[self.hbuf[0]], eng="gpsimd")
        P.barrier()

    def norm_slab(self, hs, hsb, nfc, gcols, ss_ps, ssb, sq, sqb, rstd, rstdb, uT, uTb, inv_n, psum_src=False):
        P = self
        Pg = self.P
        for fc in range(nfc):
            s_ = sq[fc % len(sq)]
            sb_ = sqb[fc % len(sq)]
            Pg.act(lambda e, s_=s_, fc=fc: e.activation(out=s_[:], in_=hs[:, fc, :], func=AF.Square), reads=[hsb], writes=[sb_])
            Pg.mm(lambda e, s_=s_, fc=fc: e.matmul(ss_ps[:], lhsT=self.ones_bf[:], rhs=s_[:], start=(fc == 0), stop=(fc == nfc - 1)),
                  reads=[sb_, self.cbf], writes=[ssb])
        Pg.act(lambda e: e.activation(out=rstd[:], in_=ss_ps[:], func=AF.Ln, scale=float(inv_n), bias=float(EPS)), reads=[ssb], writes=[rstdb])
        Pg.act(lambda e: e.activation(out=rstd[:], in_=rstd[:], func=AF.Exp, scale=-0.5), reads=[rstdb], writes=[rstdb])
        for fc in range(nfc):
            Pg.dve(lambda e, fc=fc: e.scalar_tensor_tensor(out=uT[:, fc, :], in0=hs[:, fc, :], scalar=gcols[fc], in1=rstd[:],
                                                           op0=ALU.mult, op1=ALU.mult), reads=[hsb, rstdb, self.gb], writes=[uTb])

    def ffn(self, f, src, dst):
        P = self.P
        hin, hinb = self.hT[src], self.hbuf[src]
        hout, houtb = self.hT[dst], self.hbuf[dst]
        hin_v = hin.rearrange("(fc p) t -> p fc t", p=128)
        hout_v = hout.rearrange("(fc p) t -> p fc t", p=128)
        with ExitStack() as st:
            hs = [self.sb(st, [128, 8, TG], F32, "hs") for _ in range(2)]
            hsb = [Buf("hs0"), Buf("hs1")]
            uT = [self.sb(st, [128, 8, TG], BF16, "uT") for _ in range(2)]
            uTb = [Buf("uT0"), Buf("uT1")]
            sq = [self.sb(st, [128, TG], BF16, "sq") for _ in range(2)]
            sqb = [Buf("sq0"), Buf("sq1")]
            rstd = self.sb(st, [128, TG], F32, "rstd")
            rstdb = Buf("rstd")
            actT = [self.sb(st, [128, NFC, TG], BF16, "actT") for _ in range(2)]
            actb = [Buf("act0"), Buf("act1")]
            NW = 6
            wgu = [self.sb(st, [128, 2, 8, 128], BF16, "wgu") for _ in range(NW)]
            wgub = [Buf("wgu%d" % i) for i in range(NW)]
            NWD = 3
            wdn = [self.sb(st, [128, NFC, 128], BF16, "wdn") for _ in range(NWD)]
            wdnb = [Buf("wdn%d" % i) for i in range(NWD)]
            sg = [self.sb(st, [128, TG], BF16, "sg") for _ in range(2)]
            sgb = [Buf("sg0"), Buf("sg1")]
            ho = [self.sb(st, [128, TG], F32, "ho") for _ in range(3)]
            hob = [Buf("ho%d" % i) for i in range(3)]
            ss_ps = self.ps(st, [128, TG], F32, "ss")
            ssb = Buf("ss")
            g_ps = [self.ps(st, [128, TG], F32, "gps") for _ in range(2)]
            gpb = [Buf("g0"), Buf("g1")]
            u_ps = [self.ps(st, [128, TG], F32, "ups") for _ in range(2)]
            upb = [Buf("u0"), Buf("u1")]
            o_ps = [self.ps(st, [128, TG], F32, "ops") for _ in range(2)]
            opb = [Buf("o0"), Buf("o1")]
            gcols = [self.gcol[:, f, fc:fc + 1] for fc in range(8)]
            cnt = {"w": 0, "d": 0, "c": 0, "o": 0}

            def stage_load(g):
                P.load(hs[g % 2][:], hin_v[:, :, g * TG:(g + 1) * TG], reads=[hinb], writes=[hsb[g % 2]])

            def stage_norm(g):
                self.norm_slab(hs[g % 2], hsb[g % 2], 8, gcols, ss_ps, ssb, sq, sqb, rstd, rstdb, uT[g % 2], uTb[g % 2], 1.0 / D)

            def stage_gu(g):
                for c in range(NFC):
                    w = cnt["w"] % NW
                    cnt["w"] += 1
                    P.load(wgu[w][:], self.wgu[f, c].rearrange("p (w k j) -> p w k j", w=2, k=8), reads=[self.wgu_b[f][(0, c)], self.wgu_b[f][(1, c)]], writes=[wgub[w]])
                    b = cnt["c"] % 2
                    cnt["c"] += 1
                    for which, (pt, pbf) in enumerate([(g_ps[b], gpb[b]), (u_ps[b], upb[b])]):
                        for kc in range(8):
                            P.mm(lambda e, pt=pt, w=w, which=which, kc=kc: e.matmul(pt[:], lhsT=wgu[w][:, which, kc, :], rhs=uT[g % 2][:, kc, :],
                                                                                    start=(kc == 0), stop=(kc == 7)),
                                 reads=[wgub[w], uTb[g % 2]], writes=[pbf])
                    P.act(lambda e, b=b: e.activation(out=sg[b][:], in_=g_ps[b][:], func=AF.Silu), reads=[gpb[b]], writes=[sgb[b]])
                    P.dve(lambda e, b=b, c=c: e.tensor_tensor(out=actT[g % 2][:, c, :], in0=sg[b][:], in1=u_ps[b][:], op=ALU.mult),
                          reads=[sgb[b], upb[b]], writes=[actb[g % 2]])

            def stage_down(g):
                for fc in range(8):
                    w = cnt["d"] % NWD
                    cnt["d"] += 1
                    P.load(wdn[w][:], self.wdn[f, fc].rearrange("p (c j) -> p c j", j=128), reads=self.wdn_b[f], writes=[wdnb[w]])
                    b = fc % 2
                    for c in range(NFC):
                        P.mm(lambda e, b=b, w=w, c=c: e.matmul(o_ps[b][:], lhsT=wdn[w][:, c, :], rhs=actT[g % 2][:, c, :], start=(c == 0), stop=(c == NFC - 1)),
                             reads=[wdnb[w], actb[g % 2]], writes=[opb[b]])
                    k = cnt["o"] % 3
                    cnt["o"] += 1
                    P.dve(lambda e, b=b, k=k, fc=fc: e.scalar_tensor_tensor(out=ho[k][:], in0=o_ps[b][:], scalar=0.5, in1=hs[g % 2][:, fc, :],
                                                                            op0=ALU.mult, op1=ALU.add), reads=[opb[b], hsb[g % 2]], writes=[hob[k]])
                    P.load(hout_v[:, fc, g * TG:(g + 1) * TG], ho[k][:], reads=[hob[k]], writes=[houtb], eng="gpsimd")
                    self.pump(5)

            stage_load(0)
            stage_norm(0)
            for g in range(NG):
                if g + 1 < NG:
                    stage_load(g + 1)
                stage_gu(g)
                if g + 1 < NG:
                    stage_norm(g + 1)
                stage_down(g)
        P.barrier()

    def final(self, src, do_norm=True):
        P = self.P
        hin_v = self.hT[src].rearrange("(fc p) t -> p fc t", p=128)
        hinb = self.hbuf[src]
        outb = Buf("out")
        with ExitStack() as st:
            hs = [self.sb(st, [128, 8, TG], F32, "hs") for _ in range(2)]
            hsb = [Buf("hs0"), Buf("hs1")]
            yT = [self.sb(st, [128, 8, TG], F32, "yT") for _ in range(2)]
            yTb = [Buf("y0"), Buf("y1")]
            sq = [self.sb(st, [128, TG], BF16, "sq") for _ in range(2)]
            sqb = [Buf("sq0"), Buf("sq1")]
            rstd = self.sb(st, [128, TG], F32, "rstd")
            rstdb = Buf("rstd")
            ss_ps = self.ps(st, [128, TG], F32, "ss")
            ssb = Buf("ss")
            tp = [self.ps(st, [128, 512], F32, "tp") for _ in range(4)]
            tb = [Buf("tp%d" % i) for i in range(4)]
            ot = [self.sb(st, [128, D], F32, "ot") for _ in range(3)]
            otb = [Buf("ot%d" % i) for i in range(3)]
            gcols = [self.gcol[:, 6, fc:fc + 1] for fc in range(8)]
            ident = self.cst("ident")
            k = 0
            n = 0
            for g in range(NG):
                P.load(hs[g % 2][:], hin_v[:, :, g * TG:(g + 1) * TG], reads=[hinb], writes=[hsb[g % 2]])
                if do_norm:
                    self.norm_slab(hs[g % 2], hsb[g % 2], 8, gcols, ss_ps, ssb, sq, sqb, rstd, rstdb, yT[g % 2], yTb[g % 2], 1.0 / D)
                    y, yb = yT[g % 2], yTb[g % 2]
                else:
                    y, yb = hs[g % 2], hsb[g % 2]
                for t in range(4):
                    o_ = ot[n % 3]
                    ob_ = otb[n % 3]
                    n += 1
                    for half in range(2):
                        pb = tp[k % 4]
                        pbb = tb[k % 4]
                        k += 1
                        for j in range(4):
                            fc = half * 4 + j
                            P.mm(lambda e, pb=pb, j=j, fc=fc, t=t, y=y: e.transpose(out=pb[:, j * 128:(j + 1) * 128], in_=y[:, fc, t * 128:(t + 1) * 128], identity=ident),
                                 reads=[yb, self.cb], writes=[pbb])
                        if half == 0:
                            P.act(lambda e, pb=pb, o_=o_: e.copy(out=o_[:, 0:512], in_=pb[:]), reads=[pbb], writes=[ob_])
                        else:
                            P.dve(lambda e, pb=pb, o_=o_: e.tensor_copy(out=o_[:, 512:1024], in_=pb[:]), reads=[pbb], writes=[ob_])
                    r0 = g * TG + t * 128
                    P.load(self.out[r0:r0 + 128, :], o_[:], reads=[ob_], writes=[outb], eng="gpsimd")


def build(nphase=99, final_norm=True, only=None):
    import os
    kb = KB()
    kb.declare()
    P = kb.P
    with ExitStack() as st:
        kb.load_consts(st)
        cur = 0
        if only is None:
            kb.prep_ffn(0)
            kb.pump(10 ** 6)
        kb.transpose_in()
        if only == "mix1":
            kb.declare_mix1()
            kb.inproj1(cur)
            kb.nsa_attn()
            kb.outproj(cur, 1 - cur, kb.I["nsa_w_out"], kb.ONT, kb.m1b["ONT"], 16)
            cur = 1 - cur
        elif only is None:
            if nphase >= 1:
                kb.prep_ffn(1)
                kb.ffn(0, cur, 1 - cur)
                cur = 1 - cur
            if nphase >= 2:
                kb.declare_mix0()
                kb.inproj0(cur)
                kb.mla()
                kb.gla()
                kb.outproj(cur, 1 - cur, kb.I["hy_w_out"], kb.OTm, kb.m0b["OTm"], 8, kb.OTg, kb.m0b["OTg"])
                cur = 1 - cur
            if nphase >= 3:
                kb.prep_ffn(2)
                kb.ffn(1, cur, 1 - cur)
                cur = 1 - cur
                kb.prep_ffn(3)
                kb.ffn(2, cur, 1 - cur)
                cur = 1 - cur
            if nphase >= 4:
                kb.declare_mix1()
                kb.inproj1(cur)
                kb.nsa_attn()
                kb.outproj(cur, 1 - cur, kb.I["nsa_w_out"], kb.ONT, kb.m1b["ONT"], 16)
                cur = 1 - cur
            if nphase >= 5:
                kb.ffn(3, cur, 1 - cur)
                cur = 1 - cur
        kb.pump(10 ** 6)
        kb.final(cur, do_norm=final_norm)
        P.finalize(st)
    return kb.nc


WNAMES = ["ffn_norm", "ffn_w_gate", "ffn_w_up", "ffn_w_down", "mix_norm", "hy_w_in", "mla_q_norm", "mla_w_uq", "mla_kv_norm",
          "mla_w_ukv", "gla_w_a2", "gla_b_a", "gla_out_norm", "hy_w_out", "nsa_w_in", "nsa_pos_k", "nsa_pos_v", "nsa_ck_w1",
          "nsa_ck_w2", "nsa_cv_w1", "nsa_cv_w2", "nsa_w_out", "final_norm"]


def make_in_maps(inputs, ncores=8):
    consts = make_consts()
    cm, ovl = make_cmp_consts()
    shared = {"consts": consts, "cmpmask": cm, "ovl": ovl}
    f32 = lambda a: np.ascontiguousarray(np.asarray(a), dtype=np.float32)
    shared["ffn_norm"] = f32(inputs["ffn_norm"]).reshape(4, D)
    shared["ffn_w_gate"] = f32(inputs["ffn_w_gate"]).reshape(4, D, DFF)
    shared["ffn_w_up"] = f32(inputs["ffn_w_up"]).reshape(4, D, DFF)
    shared["ffn_w_down"] = f32(inputs["ffn_w_down"]).reshape(4, DFF, D)
    shared["mix_norm"] = f32(inputs["mix_norm"])
    for n in ["hy_w_in", "mla_w_uq", "mla_w_ukv", "gla_w_a2", "hy_w_out", "nsa_w_in", "nsa_pos_k", "nsa_pos_v", "nsa_ck_w1",
              "nsa_ck_w2", "nsa_cv_w1", "nsa_cv_w2", "nsa_w_out"]:
        shared[n] = f32(inputs[n])[0]
    for n in ["mla_q_norm", "mla_kv_norm", "gla_b_a", "gla_out_norm"]:
        shared[n] = f32(inputs[n]).reshape(1, -1)
    shared["final_norm"] = f32(inputs["final_norm"]).reshape(1, D)
    x = f32(inputs["x"])
    pos = np.ascontiguousarray(np.asarray(inputs["positions"]), dtype=np.int32)
    maps = []
    for c in range(ncores):
        m = dict(shared)
        m["x"] = x[c]
        m["positions"] = pos[c:c + 1]
        maps.append(m)
    return maps


def kernel(**inputs):
    nc = build()
    maps = make_in_maps(inputs)
    res = run_bass_kernel_spmd(nc, maps, core_ids=list(range(8)))
    return np.stack([r["out"] for r in res.results], axis=0)


class Tl:
    def __init__(self, t, name):
        self.t = t
        self.b = Buf(name)

    def __getitem__(self, k):
        return self.t[k]


def _kb_tile(self, st, shape, dt, name="t"):
    return Tl(self.sb(st, shape, dt, name), name)


def _kb_ptile(self, st, shape=(128, 512), dt=F32, name="p"):
    return Tl(self.ps(st, list(shape), dt, name), name)


def _bl(xs):
    return [x.b if isinstance(x, Tl) else x for x in xs]


def _MM(self, out, lhsT, rhs, start, stop, r, w):
    return self.P.mm(lambda e: e.matmul(out, lhsT=lhsT, rhs=rhs, start=start, stop=stop), reads=_bl(r), writes=_bl(w))


def _PROJ(self, out, pairs, r, w):
    n = len(pairs)
    for i, (l, rh) in enumerate(pairs):
        self.MM(out, l, rh, i == 0, i == n - 1, r, w)


def _TR(self, out, in_, ident, r, w):
    return self.P.mm(lambda e: e.transpose(out=out, in_=in_, identity=ident), reads=_bl(r), writes=_bl(w))


def _ACT(self, out, in_, func, r, w, **kw):
    return self.P.act(lambda e: e.activation(out=out, in_=in_, func=func, **kw), reads=_bl(r), writes=_bl(w))


def _TT(self, out, a, b, op, r, w, eng="vector"):
    return self.P.op(eng, lambda e: e.tensor_tensor(out=out, in0=a, in1=b, op=op), reads=_bl(r), writes=_bl(w))


def _STT(self, out, in0, scalar, in1, op0, op1, r, w):
    return self.P.dve(lambda e: e.scalar_tensor_tensor(out=out, in0=in0, scalar=scalar, in1=in1, op0=op0, op1=op1), reads=_bl(r), writes=_bl(w))


def _TS(self, out, in0, s1, s2, op0, op1, r, w, eng="vector"):
    if s2 is None:
        return self.P.op(eng, lambda e: e.tensor_scalar(out=out, in0=in0, scalar1=s1, scalar2=None, op0=op0), reads=_bl(r), writes=_bl(w))
    return self.P.op(eng, lambda e: e.tensor_scalar(out=out, in0=in0, scalar1=s1, scalar2=s2, op0=op0, op1=op1), reads=_bl(r), writes=_bl(w))


def _CP(self, out, in_, r, w, eng="vector"):
    if eng == "scalar":
        return self.P.act(lambda e: e.copy(out=out, in_=in_), reads=_bl(r), writes=_bl(w))
    return self.P.op(eng, lambda e: e.tensor_copy(out=out, in_=in_), reads=_bl(r), writes=_bl(w))


def _LD(self, out, in_, r, w, eng="sync", **kw):
    return self.P.load(out, in_, reads=_bl(r), writes=_bl(w), eng=eng, **kw)


def _MS(self, ap, val, w, eng="gpsimd"):
    return self.P.op(eng, lambda e: e.memset(ap, val), reads=[], writes=_bl(w))


for _n, _f in [("tile", _kb_tile), ("ptile", _kb_ptile), ("MM", _MM), ("PROJ", _PROJ), ("TR", _TR), ("ACT", _ACT), ("TT", _TT),
               ("STT", _STT), ("TS", _TS), ("CP", _CP), ("LD", _LD), ("MS", _MS)]:
    setattr(KB, _n, _f)


def _norm_ps(self, srcs, gcols, inv_n, ss, sq, rstd, outs, out_b):
    n = len(srcs)
    for i, (ap, tl) in enumerate(srcs):
        q = sq[i % len(sq)]
        self.ACT(q[:], ap, AF.Square, [tl], [q])
        self.MM(ss[:], self.ones_bf[:], q[:], i == 0, i == n - 1, [q, self.cbf], [ss])
    self.ACT(rstd[:], ss[:], AF.Ln, [ss], [rstd], scale=float(inv_n), bias=float(EPS))
    self.ACT(rstd[:], rstd[:], AF.Exp, [rstd], [rstd], scale=-0.5)
    for i, (ap, tl) in enumerate(srcs):
        self.STT(outs[i], ap, gcols[i], rstd[:], ALU.mult, ALU.mult, [tl, rstd] + self.gdeps, [out_b])


KB.norm_ps = _norm_ps


def _rope_tables(self, st, rows, r0, fcol, scol, name, pos_ap=None, S=S):
    C = self.tile(st, [rows, S], F32, name + "C")
    Sg = self.tile(st, [rows, S], F32, name + "S")
    with ExitStack() as st2:
        pi_ = self.tile(st2, [rows, S], I32, "posi")
        t = self.tile(st2, [rows, S], F32, "rt")
        u = self.tile(st2, [rows, S], F32, "ru")
        ti = self.tile(st2, [rows, S], I32, "rti")
        rs = slice(r0, rows)
        fo = CB["freq"][0]
        if pos_ap is None:
            pos_ap = self.I["positions"]
        with self.nc.allow_non_contiguous_dma("positions"):
            self.LD(pi_[rs, :], pos_ap.partition_broadcast(rows - r0).rearrange("p o s -> p (o s)"), [], [pi_], allow_slow_non_contiguous=True)
        self.CP(t[rs, :], pi_[rs, :], [pi_], [t])
        self.TS(t[rs, :], t[rs, :], self.c32[rs, fo + fcol:fo + fcol + 1], None, ALU.mult, None, [t, self.cb], [t])
        for tab, shift, scale in [(Sg, 0.0, self.c32[rs, fo + scol:fo + scol + 1]), (C, 0.25, float(2 * np.pi * (1 - 1e-6)))]:
            if shift:
                self.TS(u[rs, :], t[rs, :], shift, None, ALU.add, None, [t], [u])
            else:
                self.CP(u[rs, :], t[rs, :], [t], [u])
            self.CP(ti[rs, :], u[rs, :], [u], [ti])
            self.CP(tab[rs, :], ti[rs, :], [ti], [tab])
            self.TT(u[rs, :], u[rs, :], tab[rs, :], ALU.subtract, [u, tab], [u])
            self.ACT(tab[rs, :], u[rs, :], AF.Sin, [u, self.cb], [tab], scale=scale)
        self.P.barrier()
    return C, Sg


KB.rope_tables = _rope_tables


def _declare_mix0(self):
    d = self.dram
    self.QT = d("QT", [8, 96, S], BF16)
    self.KT = d("KT", [8, 96, S], BF16)
    self.Vs = d("Vs", [S, 520], BF16)
    self.qintra = d("qintra", [256, S], BF16)
    self.qinter = d("qinter", [256, S], BF16)
    self.kdec = d("kdec", [256, S], BF16)
    self.kdtok = d("kdtok", [S, 256], BF16)
    self.gv = d("gv", [S, 512], BF16)
    self.grs = d("grs", [512, S], BF16)
    self.decd = d("decd", [256, 64], F32)
    self.OTm = d("OTm", [8, 64, S], BF16)
    self.OTg = d("OTg", [4, 128, S], BF16)
    self.m0b = {n: Buf(n) for n in ["QT", "KT", "Vs", "qintra", "qinter", "kdec", "kdtok", "gv", "grs", "decd", "OTm", "OTg"]}


KB.declare_mix0 = _declare_mix0


def _inproj0(self, src):
    P, I = self.P, self.I
    hin_v = self.hT[src].rearrange("(fc p) t -> p fc t", p=128)
    hinb = self.hbuf[src]
    mb = self.m0b
    with ExitStack() as st:
        T = lambda shape, dt, n: self.tile(st, shape, dt, n)
        w_in = T([128, 8, HY_IN], BF16, "w_in")
        for kc in range(8):
            self.LD(w_in[:, kc, :], I["hy_w_in"][kc * 128:(kc + 1) * 128, :], [], [w_in], eng="gpsimd")
        w_uq = T([128, 2, 768], BF16, "w_uq")
        w_uqs = T([128, 2, 768], BF16, "w_uqs")
        uqsrc = I["mla_w_uq"].rearrange("(k p) n -> p k n", p=128)
        self.LD(w_uq[:], uqsrc, [], [w_uq], eng="gpsimd")
        self.LD(w_uqs[:], uqsrc, [], [w_uqs], eng="gpsimd")
        v4 = lambda ap: ap.rearrange("p k (h d) -> p k h d", d=96)
        with self.nc.allow_non_contiguous_dma("small swapped weight blocks"):
            for kc in range(2):
                self.LD(v4(w_uqs[:])[:, kc, :, 64:80], v4(uqsrc)[:, kc, :, 80:96], [], [w_uqs], eng="gpsimd")
                self.LD(v4(w_uqs[:])[:, kc, :, 80:96], v4(uqsrc)[:, kc, :, 64:80], [], [w_uqs], eng="gpsimd")
        w_ukv = T([128, 1024], BF16, "w_ukv")
        self.LD(w_ukv[:], I["mla_w_ukv"], [], [w_ukv], eng="gpsimd")
        wkrs = T([128, 8, 96], BF16, "wkrs")
        insrc = I["hy_w_in"].rearrange("(k p) n -> p k n", p=128)
        with self.nc.allow_non_contiguous_dma("small swapped weight blocks"):
            self.LD(wkrs[:, :, 0:64], insrc[:, :, 320:384], [], [wkrs], eng="gpsimd")
            self.LD(wkrs[:, :, 64:80], insrc[:, :, 400:416], [], [wkrs], eng="gpsimd")
            self.LD(wkrs[:, :, 80:96], insrc[:, :, 384:400], [], [wkrs], eng="gpsimd")
        w_a2 = T([16, 256], BF16, "w_a2")
        self.LD(w_a2[:], I["gla_w_a2"], [], [w_a2], eng="gpsimd")
        cols = T([128, 8], F32, "cols")
        with self.nc.allow_non_contiguous_dma("tiny vectors"):
            self.LD(cols[:, 0:2], I["mla_q_norm"].rearrange("o (k p) -> p (o k)", p=128), [], [cols], allow_slow_non_contiguous=True)
            self.LD(cols[:, 2:3], I["mla_kv_norm"].rearrange("o (k p) -> p (o k)", p=128), [], [cols], allow_slow_non_contiguous=True)
            self.LD(cols[:, 3:5], I["gla_b_a"].rearrange("o (k p) -> p (o k)", p=128), [], [cols], allow_slow_non_contiguous=True)
        self.TS(cols[:, 5:7], cols[:, 3:5], -1.0, None, ALU.mult, None, [cols], [cols])
        C, Sg = self.rope_tables(st, 96, 64, 0, 1, "mla")
        hs = [T([128, 8, TG], F32, "hs") for _ in range(2)]
        uT = T([128, 8, TG], BF16, "uT")
        sq = [T([128, TG], BF16, "sq") for _ in range(2)]
        rstd = T([128, TG], F32, "rstd")
        cqn = T([128, 2, TG], BF16, "cqn")
        ckvn = T([128, TG], BF16, "ckvn")
        qst = [T([96, TG], BF16, "qst") for _ in range(2)]
        t1 = [T([96, TG], F32, "t1") for _ in range(2)]
        t2 = [T([96, TG], F32, "t2") for _ in range(2)]
        kst = T([96, 8, TG], BF16, "kst")
        krot = T([96, TG], F32, "krot")
        vst = T([128, 4, 8, 65], BF16, "vst")
        self.MS(vst[:], 1.0, [vst])
        ga = T([16, TG], BF16, "ga")
        lt = T([128, TG], F32, "lt")
        cs = T([128, TG], F32, "cs")
        dd = T([128, TG], F32, "dd")
        E1 = T([128, TG], F32, "E1")
        E2 = T([128, TG], F32, "E2")
        E3 = T([128, TG], F32, "E3")
        qia = T([128, 2, TG], BF16, "qia")
        qie = T([128, 2, TG], BF16, "qie")
        kde = T([128, 2, TG], BF16, "kde")
        dec = T([128, 2, 64], F32, "dec")
        kdt = T([128, 4, 256], BF16, "kdt")
        gvs = T([128, 4, 512], BF16, "gvs")
        grt = T([128, 4, TG], BF16, "grt")
        ss = self.ptile(st, name="ss")
        A = [self.ptile(st, name="A") for _ in range(2)]
        Bp = [self.ptile(st, name="B") for _ in range(2)]
        Tp = [self.ptile(st, name="T") for _ in range(2)]
        Tb = self.ptile(st, [128, 1024], BF16, name="Tb")
        self.gdeps = [self.gb, cols.b]
        gm = [self.gcol[:, 4, fc:fc + 1] for fc in range(8)]
        scanmask = self.cst("scanmask")
        for g in range(NG):
            gs = slice(g * TG, (g + 1) * TG)
            h_ = hs[g % 2]
            self.LD(h_[:], hin_v[:, :, gs], [hinb], [h_])
            self.norm_ps([(h_[:, fc, :], h_) for fc in range(8)], gm, 1.0 / D, ss, sq, rstd, [uT[:, fc, :] for fc in range(8)], uT)
            for ch in range(2):
                self.PROJ(A[ch][:], [(w_in[:, kc, ch * 128:(ch + 1) * 128], uT[:, kc, :]) for kc in range(8)], [w_in, uT], [A[ch]])
            self.norm_ps([(A[ch][:], A[ch]) for ch in range(2)], [cols[:, ch:ch + 1] for ch in range(2)], 1.0 / 256, ss, sq, rstd,
                         [cqn[:, ch, :] for ch in range(2)], cqn)
            for h in range(8):
                a, b = A[h % 2], Bp[h % 2]
                q_, x1, x2 = qst[h % 2], t1[h % 2], t2[h % 2]
                self.PROJ(a[0:96, :], [(w_uq[:, kc, h * 96:(h + 1) * 96], cqn[:, kc, :]) for kc in range(2)], [w_uq, cqn], [a])
                self.PROJ(b[0:96, :], [(w_uqs[:, kc, h * 96:(h + 1) * 96], cqn[:, kc, :]) for kc in range(2)], [w_uqs, cqn], [b])
                self.CP(q_[0:64, :], a[0:64, :], [a], [q_], eng="scalar")
                self.TT(x1[64:96, :], a[64:96, :], C[64:96, gs], ALU.mult, [a, C], [x1])
                self.TT(x2[64:96, :], b[64:96, :], Sg[64:96, gs], ALU.mult, [b, Sg], [x2])
                self.TT(q_[64:96, :], x1[64:96, :], x2[64:96, :], ALU.add, [x1, x2], [q_], eng="gpsimd")
                self.LD(self.QT[h, :, gs], q_[:], [q_], [mb["QT"]], eng="gpsimd")
            self.PROJ(A[0][:], [(w_in[:, kc, 256:384], uT[:, kc, :]) for kc in range(8)], [w_in, uT], [A[0]])
            self.norm_ps([(A[0][:], A[0])], [cols[:, 2:3]], 1.0 / 128, ss, sq, rstd, [ckvn[:]], ckvn)
            for h in range(8):
                a = A[h % 2]
                self.MM(a[0:64, :], w_ukv[:, h * 128:h * 128 + 64], ckvn[:], True, True, [w_ukv, ckvn], [a])
                self.CP(kst[0:64, h, :], a[0:64, :], [a], [kst], eng=("scalar" if h % 2 else "vector"))
            self.PROJ(A[0][0:96, :], [(w_in[:, kc, 320:416], uT[:, kc, :]) for kc in range(8)], [w_in, uT], [A[0]])
            self.PROJ(Bp[0][0:96, :], [(wkrs[:, kc, :], uT[:, kc, :]) for kc in range(8)], [wkrs, uT], [Bp[0]])
            self.TT(t1[0][64:96, :], A[0][64:96, :], C[64:96, gs], ALU.mult, [A[0], C], [t1[0]])
            self.TT(t2[0][64:96, :], Bp[0][64:96, :], Sg[64:96, gs], ALU.mult, [Bp[0], Sg], [t2[0]])
            self.TT(krot[64:96, :], t1[0][64:96, :], t2[0][64:96, :], ALU.add, [t1[0], t2[0]], [krot], eng="gpsimd")
            self.CP(kst[64:96, :, :], krot[64:96, :].unsqueeze(1).broadcast_to([32, 8, TG]), [krot], [kst], eng="gpsimd")
            self.LD(self.KT[:, :, gs].rearrange("h r t -> r h t"), kst[:], [kst], [mb["KT"]], eng="gpsimd")
            wv = w_ukv[:].rearrange("p (h t d) -> p h t d", t=2, d=64)[:, :, 1, :]
            for t in range(4):
                tp = Tp[t % 2]
                self.MM(tp[:].rearrange("p (h d) -> p h d", d=64), ckvn[:, t * 128:(t + 1) * 128], wv, True, True, [ckvn, w_ukv], [tp])
                self.CP(vst[:, t, :, 0:64], tp[:].rearrange("p (h d) -> p h d", d=64), [tp], [vst], eng=("scalar" if t % 2 else "vector"))
            self.LD(self.Vs[gs, :].rearrange("(t p) f -> p t f", p=128), vst[:].rearrange("p t h d -> p t (h d)"), [vst], [mb["Vs"]], eng="gpsimd")
            for ch in range(2):
                self.PROJ(A[ch][:], [(w_in[:, kc, 416 + ch * 128:416 + (ch + 1) * 128], uT[:, kc, :]) for kc in range(8)], [w_in, uT], [A[ch]])
                self.PROJ(Bp[ch][:], [(w_in[:, kc, 672 + ch * 128:672 + (ch + 1) * 128], uT[:, kc, :]) for kc in range(8)], [w_in, uT], [Bp[ch]])
            self.PROJ(Tp[0][0:16, :], [(w_in[:, kc, 1440:1456], uT[:, kc, :]) for kc in range(8)], [w_in, uT], [Tp[0]])
            self.CP(ga[:], Tp[0][0:16, :], [Tp[0]], [ga])
            for ch in range(2):
                tp = Tp[1]
                self.MM(tp[:], w_a2[0:16, ch * 128:(ch + 1) * 128], ga[0:16, :], True, True, [w_a2, ga], [tp])
                self.ACT(lt[:], tp[:], AF.Exp, [tp, cols], [lt], scale=-1.0, bias=cols[:, 5 + ch:6 + ch])
                self.ACT(lt[:], lt[:], AF.Ln, [lt], [lt], scale=1.0, bias=1.0)
                self.P.dve(lambda e: e.tensor_tensor_scan(out=cs[:], data0=scanmask, data1=lt[:], initial=0.0, op0=ALU.mult, op1=ALU.add),
                           reads=[lt.b, self.cb], writes=[cs.b])
                cs3 = cs[:].rearrange("p (c k) -> p c k", k=64)
                self.TT(dd[:].rearrange("p (c k) -> p c k", k=64), cs3, cs3[:, :, 63:64].broadcast_to([128, 8, 64]), ALU.subtract, [cs], [dd])
                self.ACT(E1[:], dd[:], AF.Exp, [dd], [E1], scale=-1.0 / 16)
                self.ACT(E2[:], dd[:], AF.Exp, [dd], [E2], scale=1.0 / 16)
                self.ACT(E3[:], cs[:], AF.Exp, [cs], [E3], scale=-1.0 / 16)
                self.STT(qia[:, ch, :], A[ch][:], 0.125, E1[:], ALU.mult, ALU.mult, [A[ch], E1], [qia])
                self.STT(qie[:, ch, :], A[ch][:], 0.125, E3[:], ALU.mult, ALU.mult, [A[ch], E3], [qie])
                self.TT(kde[:, ch, :], Bp[ch][:], E2[:], ALU.mult, [Bp[ch], E2], [kde])
                self.CP(dec[:, ch, g * 8:(g + 1) * 8], E3[:].rearrange("p (c k) -> p c k", k=64)[:, :, 63], [E3], [dec], eng="gpsimd")
            fm = lambda dr: dr.rearrange("(c p) t -> p c t", p=128)[:, :, gs]
            self.LD(fm(self.qintra), qia[:], [qia], [mb["qintra"]], eng="gpsimd")
            self.LD(fm(self.qinter), qie[:], [qie], [mb["qinter"]], eng="gpsimd")
            self.LD(fm(self.kdec), kde[:], [kde], [mb["kdec"]], eng="gpsimd")
            for t in range(4):
                for ch in range(2):
                    self.TR(Tb[:, (t % 4) * 256 + ch * 128:(t % 4) * 256 + (ch + 1) * 128], kde[:, ch, t * 128:(t + 1) * 128], self.ident_bf[:],
                            [kde, self.cbf], [Tb])
            self.CP(kdt[:].rearrange("p t f -> p (t f)"), Tb[:], [Tb], [kdt])
            self.LD(self.kdtok[gs, :].rearrange("(t p) f -> p t f", p=128), kdt[:], [kdt], [mb["kdtok"]], eng="gpsimd")
            for t in range(4):
                tp = Tp[t % 2]
                self.PROJ(tp[:], [(uT[:, kc, t * 128:(t + 1) * 128], w_in[:, kc, 928:1440]) for kc in range(8)], [w_in, uT], [tp])
                self.CP(gvs[:, t, :], tp[:], [tp], [gvs], eng=("scalar" if t % 2 else "vector"))
            self.LD(self.gv[gs, :].rearrange("(t p) f -> p t f", p=128), gvs[:], [gvs], [mb["gv"]], eng="gpsimd")
            for hh in range(4):
                a = A[hh % 2]
                self.PROJ(a[:], [(w_in[:, kc, 1456 + hh * 128:1456 + (hh + 1) * 128], uT[:, kc, :]) for kc in range(8)], [w_in, uT], [a])
                self.ACT(grt[:, hh, :], a[:], AF.Silu, [a], [grt])
            self.LD(self.grs.rearrange("(c p) t -> p c t", p=128)[:, :, gs], grt[:], [grt], [mb["grs"]], eng="gpsimd")
        self.LD(self.decd.rearrange("(c p) n -> p c n", p=128), dec[:], [dec], [mb["decd"]], eng="gpsimd")
    self.gdeps = []
    P.barrier()


KB.inproj0 = _inproj0


class U:
    __slots__ = ("A", "B", "C", "later")

    def __init__(self, A=None, B=None, C=None, later=None):
        self.A, self.B, self.C, self.later = A, B, C, later


def run_units(units, look):
    n = len(units)
    sched = {}
    for i in range(min(look, n)):
        if units[i].A:
            units[i].A()
    for i in range(n):
        if i + look < n and units[i + look].A:
            units[i + look].A()
        if units[i].B:
            units[i].B()
        if units[i].C:
            units[i].C()
        for (dl, fn) in (units[i].later or []):
            sched.setdefault(i + dl, []).append(fn)
        for fn in sched.pop(i, []):
            fn()
    for k in sorted(sched):
        for fn in sched[k]:
            fn()


def _attn_units(self, units, o, sp_list, pt_list, cnt, Kt, Qt, Vfn, qg, scale, ktiles, dk, pre=None):
    nk = len(ktiles)
    for i, kt in enumerate(ktiles):
        d = kt - 4 * qg
        sp = sp_list[cnt[0] % len(sp_list)]
        pt = pt_list[cnt[0] % len(pt_list)]
        cnt[0] += 1
        kc = slice(kt * 128, (kt + 1) * 128)
        q0 = qg * TG
        c0 = max(d, 0) * 128

        def A(sp=sp, kc=kc, d=d, c0=c0, q0=q0, pre=(pre if i == 0 else None)):
            if pre is not None:
                pre()
            if d < 0:
                self.MM(sp[:], Kt[0:dk, kc], Qt[0:dk, q0:q0 + TG], True, True, [Kt, Qt], [sp])
            else:
                self.MM(sp[:, c0:c0 + 128], Kt[0:dk, kc], Qt[0:dk, q0 + c0:q0 + c0 + 128], True, False, [Kt, Qt], [sp])
                self.MM(sp[:, c0:c0 + 128], self.ident_bf[:], self.cpen_bf[:], False, True, [self.cbf], [sp])
                if c0 + 128 < TG:
                    self.MM(sp[:, c0 + 128:TG], Kt[0:dk, kc], Qt[0:dk, q0 + c0 + 128:q0 + TG], True, True, [Kt, Qt], [sp])

        def B(sp=sp, pt=pt, c0=c0):
            self.ACT(pt[:, c0:TG], sp[:, c0:TG], AF.Exp, [sp], [pt], scale=scale)

        def C(pt=pt, c0=c0, kt=kt, i=i):
            self.MM(o[0:128, c0:TG], Vfn(kt), pt[:, c0:TG], i == 0, i == nk - 1, [pt, self.vdep], [o])
        units.append(U(A, B, C))


KB.attn_units = _attn_units


def _mla(self):
    P = self.P
    mb = self.m0b
    with ExitStack() as st:
        T = lambda shape, dt, n: self.tile(st, shape, dt, n)
        Vall = T([128, 32, 584], BF16, "Vall")
        self.MS(Vall[:, :, 520:584], 0.0, [Vall])
        for q4 in range(4):
            self.LD(Vall[:, q4 * 8:(q4 + 1) * 8, 0:520], self.Vs[q4 * 1024:(q4 + 1) * 1024, :].rearrange("(n p) f -> p n f", p=128), [mb["Vs"]], [Vall])
        self.vdep = Vall
        KTh = [T([96, S], BF16, "KTh") for _ in range(2)]
        QTh = [T([96, S], BF16, "QTh") for _ in range(2)]
        PT = [T([128, TG], BF16, "PT") for _ in range(3)]
        rr2 = [T([65, TG], F32, "rr") for _ in range(2)]
        fb2 = [T([65, TG], BF16, "fb") for _ in range(2)]
        bcs2 = [T([64, TG], F32, "bcs") for _ in range(2)]
        ost = [T([64, TG], BF16, "ost") for _ in range(2)]
        Sp = [self.ptile(st, name="S") for _ in range(3)]
        Op = [self.ptile(st, name="O") for _ in range(3)]
        bc2 = [self.ptile(st, name="bc") for _ in range(2)]
        ones32 = self.cst("ones")
        cnt = [0]
        k = 0
        units = []

        def loader(h):
            def f():
                self.LD(KTh[h % 2][:], self.KT[h], [mb["KT"]], [KTh[h % 2]])
                self.LD(QTh[h % 2][:], self.QT[h], [mb["QT"]], [QTh[h % 2]])
            return f
        loader(0)()
        for h in range(8):
            kt_, qt_ = KTh[h % 2], QTh[h % 2]
            for qg in range(NG):
                o = Op[k % 3]
                pre = loader(h + 1) if (qg == 0 and h + 1 < 8) else None
                self.attn_units(units, o, Sp, PT, cnt, kt_, qt_, lambda kt, h=h: Vall[:, kt, h * 65:h * 65 + 128], qg, 96 ** -0.5,
                                list(range(4 * qg + 4)), 96, pre=pre)

                rr_, fb_, bc_, bs_ = rr2[k % 2], fb2[k % 2], bc2[k % 2], bcs2[k % 2]

                def f0(o=o, rr_=rr_, fb_=fb_):
                    self.ACT(rr_[64:65, :], o[64:65, :], AF.Ln, [o], [rr_])
                    self.ACT(fb_[64:65, :], rr_[64:65, :], AF.Exp, [rr_], [fb_], scale=-1.0)

                def f1(fb_=fb_, bc_=bc_, bs_=bs_):
                    self.MM(bc_[0:64, :], self.ones_bf[64:65, 0:64], fb_[64:65, :], True, True, [fb_, self.cbf], [bc_])
                    self.CP(bs_[:], bc_[0:64, :], [bc_], [bs_], eng="vector")

                def f2(o=o, h=h, qg=qg, os_=ost[k % 2], bs_=bs_):
                    self.TT(os_[:], o[0:64, :], bs_[:], ALU.mult, [o, bs_], [os_])
                    self.LD(self.OTm[h, :, qg * TG:(qg + 1) * TG], os_[:], [os_], [mb["OTm"]], eng="gpsimd")
                units.append(U(None, None, None, later=[(1, f0), (3, f1), (4, f2)]))
                k += 1
        run_units(units, 2)
    P.barrier()


KB.mla = _mla


def _gla(self):
    P = self.P
    mb = self.m0b
    with ExitStack() as st:
        T = lambda shape, dt, n: self.tile(st, shape, dt, n)
        hv = lambda dr, gs: dr.rearrange("(h d) t -> d h t", d=64)[:, :, gs]
        qia = [T([64, 4, TG], BF16, "qia") for _ in range(2)]
        qie = [T([64, 4, TG], BF16, "qie") for _ in range(2)]
        kde = [T([64, 4, TG], BF16, "kde") for _ in range(2)]
        vv = [T([64, 8, 512], BF16, "vv") for _ in range(2)]
        kdt = [T([64, 8, 256], BF16, "kdt") for _ in range(2)]
        grs = [T([128, 4, TG], BF16, "grs") for _ in range(2)]
        dec = T([64, 4, 64], F32, "dec")
        self.LD(dec[:], self.decd.rearrange("(h d) n -> d h n", d=64), [mb["decd"]], [dec])
        onc = T([128, 1], F32, "onc")
        with self.nc.allow_non_contiguous_dma("tiny"):
            self.LD(onc[:], self.I["gla_out_norm"].rearrange("o p -> p o"), [], [onc], allow_slow_non_contiguous=True)
        St = T([64, 4, 128], F32, "St")
        Sbf = T([64, 4, 128], BF16, "Sbf")
        self.MS(St[:], 0.0, [St])
        self.MS(Sbf[:], 0.0, [Sbf])
        ats = [T([64, 256], BF16, "ats") for _ in range(2)]
        sq = [T([128, TG], BF16, "sq") for _ in range(2)]
        rstd = T([128, TG], F32, "rstd")
        on = [T([128, TG], F32, "on") for _ in range(2)]
        ost = [T([128, TG], BF16, "ost") for _ in range(2)]
        Op = [self.ptile(st, name="O") for _ in range(4)]
        at_t = self.ps(st, [64, 512], F32, "at")
        at = [Tl(at_t, "at0"), Tl(at_t, "at1")]
        kv = [self.ptile(st, [64, 512], F32, name="kv") for _ in range(2)]
        ss = self.ptile(st, name="ss")
        tri = self.cst("tri", rows=64)
        self.gdeps = [onc.b]
        for g in range(NG):
            gs = slice(g * TG, (g + 1) * TG)
            b = g % 2
            self.LD(qia[b][:], hv(self.qintra, gs), [mb["qintra"]], [qia[b]])
            self.LD(qie[b][:], hv(self.qinter, gs), [mb["qinter"]], [qie[b]])
            self.LD(kde[b][:], hv(self.kdec, gs), [mb["kdec"]], [kde[b]])
            self.LD(vv[b][:], self.gv[gs, :].rearrange("(c p) f -> p c f", p=64), [mb["gv"]], [vv[b]])
            self.LD(kdt[b][:], self.kdtok[gs, :].rearrange("(c p) f -> p c f", p=64), [mb["kdtok"]], [kdt[b]])
            self.LD(grs[b][:], self.grs.rearrange("(c p) t -> p c t", p=128)[:, :, gs], [mb["grs"]], [grs[b]])
            for c in range(8):
                n = g * 8 + c
                cs_ = slice(c * 64, (c + 1) * 64)
                a_ = at[c % 2]
                ao = (c % 2) * 256
                for h in range(4):
                    self.MM(a_[0:64, ao + h * 64:ao + (h + 1) * 64], kde[b][:, h, cs_], qia[b][:, h, cs_], True, True, [kde[b], qia[b]], [a_])
                as_ = ats[c % 2]
                self.TT(as_[:].rearrange("p (h i) -> p h i", i=64), a_[0:64, ao:ao + 256].rearrange("p (h i) -> p h i", i=64),
                        tri.unsqueeze(1).broadcast_to([64, 4, 64]), ALU.mult, [a_, self.cb], [as_])
                for h in range(4):
                    self.MM(Op[h][:, cs_], vv[b][:, c, h * 128:(h + 1) * 128], as_[:, h * 64:(h + 1) * 64], True, False, [vv[b], as_], [Op[h]])
                    self.MM(Op[h][:, cs_], Sbf[:, h, :], qie[b][:, h, cs_], False, True, [Sbf, qie[b]], [Op[h]])
                kv_ = kv[c % 2]
                for h in range(4):
                    self.MM(kv_[0:64, h * 128:(h + 1) * 128], kdt[b][:, c, h * 64:(h + 1) * 64], vv[b][:, c, h * 128:(h + 1) * 128], True, True,
                            [kdt[b], vv[b]], [kv_])
                self.TT(St[:], St[:], dec[:, :, n:n + 1].broadcast_to([64, 4, 128]), ALU.mult, [St, dec], [St])
                self.TT(St[:].rearrange("p h v -> p (h v)"), St[:].rearrange("p h v -> p (h v)"), kv_[0:64, :], ALU.add, [St, kv_], [St])
                self.CP(Sbf[:], St[:], [St], [Sbf], eng="scalar")
            for h in range(4):
                o = Op[h]
                self.norm_ps([(o[:], o)], [onc[:, 0:1]], 1.0 / 128, ss, sq, rstd, [on[h % 2][:]], on[h % 2])
                os_ = ost[h % 2]
                self.TT(os_[:], on[h % 2][:], grs[b][:, h, :], ALU.mult, [on[h % 2], grs[b]], [os_], eng="gpsimd")
                self.LD(self.OTg[h, :, gs], os_[:], [os_], [mb["OTg"]], eng="gpsimd")
    self.gdeps = []
    P.barrier()


KB.gla = _gla


def _outproj(self, src, dst, w_src, otm_d, otm_b, n_h64, otg_d=None, otg_b=None):
    P = self.P
    hin_v = self.hT[src].rearrange("(fc p) t -> p fc t", p=128)
    hout_v = self.hT[dst].rearrange("(fc p) t -> p fc t", p=128)
    with ExitStack() as st:
        T = lambda shape, dt, n: self.tile(st, shape, dt, n)
        wm = T([64, n_h64, D], BF16, "wm")
        half = n_h64 // 2
        for i in range(2):
            self.LD(wm[:, i * half:(i + 1) * half, :], w_src[i * half * 64:(i + 1) * half * 64, :].rearrange("(h r) n -> r h n", r=64), [], [wm], eng="gpsimd")
        ng = 0
        if otg_d is not None:
            ng = 4
            wg = T([128, 4, D], BF16, "wg")
            self.LD(wg[:], w_src[n_h64 * 64:, :].rearrange("(c p) n -> p c n", p=128), [], [wg], eng="gpsimd")
        hs = [T([128, 8, TG], F32, "hs") for _ in range(2)]
        om = [T([64, n_h64, TG], BF16, "om") for _ in range(2)]
        og = [T([128, 4, TG], BF16, "og") for _ in range(2)] if ng else None
        ho = [T([128, TG], F32, "ho") for _ in range(3)]
        Op = [self.ptile(st, name="O") for _ in range(2)]
        k = 0
        for g in range(NG):
            gs = slice(g * TG, (g + 1) * TG)
            b = g % 2
            self.LD(hs[b][:], hin_v[:, :, gs], [self.hbuf[src]], [hs[b]])
            self.LD(om[b][:], otm_d[:, :, gs].rearrange("h r t -> r h t"), [otm_b], [om[b]])
            if ng:
                self.LD(og[b][:], otg_d[:, :, gs].rearrange("h r t -> r h t"), [otg_b], [og[b]])
            for fc in range(8):
                o = Op[fc % 2]
                fcs = slice(fc * 128, (fc + 1) * 128)
                pairs = [(wm[:, h, fcs], om[b][:, h, :]) for h in range(n_h64)]
                deps = [wm, om[b]]
                if ng:
                    pairs += [(wg[:, c, fcs], og[b][:, c, :]) for c in range(4)]
                    deps += [wg, og[b]]
                self.PROJ(o[:], pairs, deps, [o])
                h_ = ho[k % 3]
                k += 1
                self.TT(h_[:], o[:], hs[b][:, fc, :], ALU.add, [o, hs[b]], [h_])
                self.LD(hout_v[:, fc, gs], h_[:], [h_], [self.hbuf[dst]], eng="gpsimd")
                self.pump(3)
    P.barrier()


KB.outproj = _outproj


def _declare_mix1(self):
    d = self.dram
    self.QN = d("QN", [1024, S], BF16)
    self.KSd = d("KSd", [256, S], BF16)
    self.KWd = d("KWd", [256, S], BF16)
    self.KCd = d("KCd", [256, S], BF16)
    self.VCd = d("VCd", [256, S], BF16)
    self.VSW = d("VSW", [S, 520], BF16)
    self.GT = d("GT", [48, S], F32)
    self.ONT = d("ONT", [16, 64, S], BF16)
    self.m1b = {n: Buf(n) for n in ["QN", "KSd", "KWd", "KCd", "VCd", "VSW", "GT", "ONT"]}


KB.declare_mix1 = _declare_mix1


def _inproj1(self, src):
    P, I = self.P, self.I
    hin_v = self.hT[src].rearrange("(fc p) t -> p fc t", p=128)
    hinb = self.hbuf[src]
    mb = self.m1b
    with ExitStack() as st:
        T = lambda shape, dt, n: self.tile(st, shape, dt, n)
        w_in = T([128, 8, NSA_IN], BF16, "w_in")
        w_sw = T([128, 8, 1536], BF16, "w_sw")
        insrc = I["nsa_w_in"].rearrange("(k p) n -> p k n", p=128)
        with self.nc.allow_non_contiguous_dma("swapped rope halves"):
            for kc in range(8):
                self.LD(w_in[:, kc, :], I["nsa_w_in"][kc * 128:(kc + 1) * 128, :], [], [w_in], eng="gpsimd")
                for (d0, s0, nb) in [(0, 0, 16), (1024, 1536, 4), (1280, 2048, 4)]:
                    dv = w_sw[:, kc, d0:d0 + nb * 64].rearrange("p (b t e) -> p b t e", t=2, e=32)
                    sv = insrc[:, kc, s0:s0 + nb * 64].rearrange("p (b t e) -> p b t e", t=2, e=32)
                    self.LD(dv[:, :, 0, :], sv[:, :, 1, :], [], [w_sw], eng="gpsimd")
                    self.LD(dv[:, :, 1, :], sv[:, :, 0, :], [], [w_sw], eng="gpsimd")
        C, Sg = self.rope_tables(st, 128, 0, 2, 3, "nsa")
        self.nsaC, self.nsaS = C, Sg
        hs = [T([128, 8, TG], F32, "hs") for _ in range(2)]
        uT = T([128, 8, TG], BF16, "uT")
        sq = [T([128, TG], BF16, "sq") for _ in range(2)]
        rstd = T([128, TG], F32, "rstd")
        t1 = [T([128, TG], F32, "t1") for _ in range(2)]
        t2 = [T([128, TG], F32, "t2") for _ in range(2)]
        qst = [T([128, TG], BF16, "qst") for _ in range(3)]
        vst = T([128, 4, 8, 65], BF16, "vst")
        self.MS(vst[:], 1.0, [vst])
        gts = T([48, TG], F32, "gts")
        ss = self.ptile(st, name="ss")
        A = [self.ptile(st, name="A") for _ in range(2)]
        Bp = [self.ptile(st, name="B") for _ in range(2)]
        Tp = [self.ptile(st, name="T") for _ in range(2)]
        self.gdeps = [self.gb]
        gm = [self.gcol[:, 5, fc:fc + 1] for fc in range(8)]
        k = 0
        for g in range(NG):
            gs = slice(g * TG, (g + 1) * TG)
            h_ = hs[g % 2]
            self.LD(h_[:], hin_v[:, :, gs], [hinb], [h_])
            self.norm_ps([(h_[:, fc, :], h_) for fc in range(8)], gm, 1.0 / D, ss, sq, rstd, [uT[:, fc, :] for fc in range(8)], uT)
            jobs = [(c * 128, c * 128, self.QN, c, "QN") for c in range(8)]
            jobs += [(1536 + c * 128, 1024 + c * 128, self.KSd, c, "KSd") for c in range(2)]
            jobs += [(2048 + c * 128, 1280 + c * 128, self.KWd, c, "KWd") for c in range(2)]
            for (ca, cb_, dst, c, nm) in jobs:
                a, b = A[k % 2], Bp[k % 2]
                x1, x2, q_ = t1[k % 2], t2[k % 2], qst[k % 3]
                k += 1
                self.PROJ(a[:], [(w_in[:, kc, ca:ca + 128], uT[:, kc, :]) for kc in range(8)], [w_in, uT], [a])
                self.PROJ(b[:], [(w_sw[:, kc, cb_:cb_ + 128], uT[:, kc, :]) for kc in range(8)], [w_sw, uT], [b])
                self.TT(x1[:], a[:], C[:, gs], ALU.mult, [a, C], [x1])
                self.TT(x2[:], b[:], Sg[:, gs], ALU.mult, [b, Sg], [x2])
                self.TT(q_[:], x1[:], x2[:], ALU.add, [x1, x2], [q_], eng="gpsimd")
                self.LD(dst[c * 128:(c + 1) * 128, gs], q_[:], [q_], [mb[nm]], eng="gpsimd")
            for (ca, dst, c, nm) in [(1024, self.KCd, 0, "KCd"), (1152, self.KCd, 1, "KCd"), (1280, self.VCd, 0, "VCd"), (1408, self.VCd, 1, "VCd")]:
                a = A[k % 2]
                q_ = qst[k % 3]
                k += 1
                self.PROJ(a[:], [(w_in[:, kc, ca:ca + 128], uT[:, kc, :]) for kc in range(8)], [w_in, uT], [a])
                self.CP(q_[:], a[:], [a], [q_], eng="scalar")
                self.LD(dst[c * 128:(c + 1) * 128, gs], q_[:], [q_], [mb[nm]], eng="gpsimd")
            for t in range(4):
                tp = Tp[t % 2]
                self.PROJ(tp[:, 0:256], [(uT[:, kc, t * 128:(t + 1) * 128], w_in[:, kc, 1792:2048]) for kc in range(8)], [w_in, uT], [tp])
                self.PROJ(tp[:, 256:512], [(uT[:, kc, t * 128:(t + 1) * 128], w_in[:, kc, 2304:2560]) for kc in range(8)], [w_in, uT], [tp])
                self.CP(vst[:, t, :, 0:64], tp[:].rearrange("p (h d) -> p h d", d=64), [tp], [vst], eng=("scalar" if t % 2 else "vector"))
            self.LD(self.VSW[gs, :].rearrange("(t p) f -> p t f", p=128), vst[:].rearrange("p t h d -> p t (h d)"), [vst], [mb["VSW"]], eng="gpsimd")
            self.PROJ(A[0][0:48, :], [(w_in[:, kc, 2560:2608], uT[:, kc, :]) for kc in range(8)], [w_in, uT], [A[0]])
            self.ACT(gts[:], A[0][0:48, :], AF.Sigmoid, [A[0]], [gts])
            self.LD(self.GT[:, gs], gts[:], [gts], [mb["GT"]], eng="gpsimd")
    self.gdeps = []
    P.barrier()


KB.inproj1 = _inproj1


def _nsa_attn(self):
    P, I = self.P, self.I
    mb = self.m1b
    SC = 64 ** -0.5
    with ExitStack() as st:
        T = lambda shape, dt, n: self.tile(st, shape, dt, n)
        VSW = T([128, 32, 584], BF16, "VSW")
        self.MS(VSW[:, :, 520:584], 0.0, [VSW])
        for q4 in range(4):
            self.LD(VSW[:, q4 * 8:(q4 + 1) * 8, 0:520], self.VSW[q4 * 1024:(q4 + 1) * 1024, :].rearrange("(n p) f -> p n f", p=128), [mb["VSW"]], [VSW])
        self.vdep = VSW
        cmpm = T([128, 2, S], BF16, "cmpm")
        for i in range(2):
            self.LD(cmpm[:, i, :], I["cmpmask"][i * 128:(i + 1) * 128, :], [], [cmpm], eng="gpsimd")
        ovl = T([128, 2, 65], BF16, "ovl")
        self.LD(ovl[:], I["ovl"].rearrange("(i p) f -> p i f", p=128), [], [ovl], eng="gpsimd")
        eo = CB["E"][0]
        KCMP = T([64, 4, 256], BF16, "KCMP")
        VCMP = T([128, 4, 2, 128], BF16, "VCMP")
        self.MS(KCMP[:], 0.0, [KCMP])
        self.MS(VCMP[:], 0.0, [VCMP])
        Sp = [self.ptile(st, name="S") for _ in range(3)]
        Oc = self.ptile(st, name="Oc")
        Os = self.ptile(st, name="Os")
        Ow = self.ptile(st, name="Ow")
        imp = Os
        M1 = self.ptile(st, name="M1")
        M2 = self.ptile(st, name="M2")
        with ExitStack() as st2:
            T2 = lambda shape, dt, n: self.tile(st2, shape, dt, n)
            Cc, Sc = self.rope_tables(st2, 64, 0, 2, 3, "cmp", pos_ap=I["positions"][0:1, 31:S:16], S=255)
            w1 = [T2([64, 32, 128], BF16, "w1") for _ in range(2)]
            w2 = [T2([128, 64], BF16, "w2") for _ in range(2)]
            w2s = T2([128, 64], BF16, "w2s")
            posT = [T2([64, 32], BF16, "posT") for _ in range(2)]
            with self.nc.allow_non_contiguous_dma("small"):
                for i, (a, b_, pp) in enumerate([("nsa_ck_w1", "nsa_ck_w2", "nsa_pos_k"), ("nsa_cv_w1", "nsa_cv_w2", "nsa_pos_v")]):
                    self.LD(w1[i][:], I[a].rearrange("(l d) n -> d l n", d=64), [], [w1[i]], eng="gpsimd")
                    self.LD(w2[i][:], I[b_], [], [w2[i]], eng="gpsimd")
                    self.LD(posT[i][:], I[pp].rearrange("l d -> d l"), [], [posT[i]], eng="gpsimd", allow_slow_non_contiguous=True)
                self.LD(w2s[:, 0:32], I["nsa_ck_w2"][:, 32:64], [], [w2s], eng="gpsimd")
                self.LD(w2s[:, 32:64], I["nsa_ck_w2"][:, 0:32], [], [w2s], eng="gpsimd")
            cb_ = T2([128, 2], F32, "cbias")
            for i in range(2):
                for l in range(32):
                    self.MM(M1[:, i:i + 1], w1[i][:, l, :], posT[i][:, l:l + 1], l == 0, l == 31, [w1[i], posT[i]], [M1])
            self.CP(cb_[:], M1[:, 0:2], [M1], [cb_])
            src = [T2([64, S], BF16, "csrc") for _ in range(2)]
            hid = [T2([128, 256], BF16, "hid") for _ in range(2)]
            x1 = T2([64, 256], F32, "x1")
            x2 = T2([64, 256], F32, "x2")
            for i in range(2):
                self.MS(hid[i][:], 0.0, [hid[i]])
            k = 0
            for g in range(4):
                for i, (dsrc, nm) in enumerate([(self.KCd, "KCd"), (self.VCd, "VCd")]):
                    s_ = src[k % 2]
                    hd = hid[k % 2]
                    ps_ = Sp[k % 2]
                    k += 1
                    self.LD(s_[:], dsrc[g * 64:(g + 1) * 64, :], [mb[nm]], [s_])
                    for l in range(32):
                        self.MM(ps_[:, 0:255], w1[i][:, l, :], s_[:, l:l + 16 * 254 + 1:16], l == 0, l == 31, [w1[i], s_], [ps_])
                    self.ACT(hd[:, 0:255], ps_[:, 0:255], AF.Silu, [ps_, cb_], [hd], bias=cb_[:, i:i + 1], scale=1.0)
                    if i == 0:
                        self.MM(M1[0:64, 0:255], w2[0][:], hd[:, 0:255], True, True, [w2[0], hd], [M1])
                        self.MM(M2[0:64, 0:255], w2s[:], hd[:, 0:255], True, True, [w2s, hd], [M2])
                        self.TT(x1[:, 0:255], M1[0:64, 0:255], Cc[0:64, :], ALU.mult, [M1, Cc], [x1])
                        self.TT(x2[:, 0:255], M2[0:64, 0:255], Sc[0:64, :], ALU.mult, [M2, Sc], [x2])
                        self.TT(KCMP[:, g, 0:255], x1[:, 0:255], x2[:, 0:255], ALU.add, [x1, x2], [KCMP], eng="gpsimd")
                    else:
                        self.MM(M1[:, 0:64], hd[:, 0:128], w2[1][:], True, True, [w2[1], hd], [M1])
                        self.MM(M1[0:127, 64:128], hd[:, 128:255], w2[1][:], True, True, [w2[1], hd], [M1])
                        self.CP(VCMP[:, g, 0, 0:64], M1[:, 0:64], [M1], [VCMP])
                        self.CP(VCMP[0:127, g, 1, 0:64], M1[0:127, 64:128], [M1], [VCMP])
                        self.MS(VCMP[:, g, 0, 64:65], 1.0, [VCMP])
                        self.MS(VCMP[0:127, g, 1, 64:65], 1.0, [VCMP])
            P.barrier()
        KS = [T([128, S], BF16, "KS") for _ in range(2)]
        for i in range(2):
            self.LD(KS[i][64:128, :], I["consts"][0:64, eo:eo + 32 * 128], [], [KS[i]], eng="gpsimd")
        KW = [T([64, S], BF16, "KW") for _ in range(2)]
        PT = [T([128, TG], BF16, "PT") for _ in range(3)]
        PC = [T([128, 2, TG], BF16, "PC") for _ in range(4)]
        ost = [T([64, TG], BF16, "ost") for _ in range(2)]
        irec = T([128, 4], F32, "irec")
        itmp = T([128, 4, 64], F32, "itmp")
        iacc = T([128, 4, 64], F32, "iacc")
        score = T([128, 4, 64], F32, "score")
        sc2 = T([128, 64], F32, "sc2")
        m8 = T([128, 16], F32, "m8")
        ones32 = self.cst("ones")
        so = CB["sel48"][0]
        wo = CB["wide"][0]
        col0 = self.cst("col0")
        cnt = [0]
        hk = 0
        ak = 0

        A4 = [T([64, TG], F32, "A4") for _ in range(4)]
        Q4 = [T([128, S], BF16, "Q4") for _ in range(4)]
        Q4n = [Tl(q.t, "Q4n") for q in Q4]
        nself = T([128, 4, 128], F32, "nself")
        self.MS(nself[:], 0.0, [nself])
        ident32 = self.cst("ident")

        selbf = T([48, 48 * 64 + 1], BF16, "selbf")
        self.LD(selbf[:], I["consts"][0:48, so:so + 48 * 64 + 1], [], [selbf], eng="gpsimd")
        GTb = T([48, S], BF16, "GTb")
        self.LD(GTb[:], self.GT, [mb["GT"]], [GTb], eng="gpsimd")
        Mb = [M1, M2]
        Ocs = [Oc, Ow]
        rr2 = [T([65, TG], F32, "rr") for _ in range(2)]
        fb2 = [T([65, TG], BF16, "fb") for _ in range(2)]
        bcs2 = [T([64, TG], F32, "bcs") for _ in range(2)]
        tmp2 = [T([64, TG], F32, "tmp") for _ in range(2)]
        fi = [0]

        def finish_unit(o, row, a_, first, qs, pre=None, extra=None, delays=(1, 2)):
            k = fi[0]
            fi[0] += 1
            m, rr_, fb_, bs_, tmp_ = Mb[k % 2], rr2[k % 2], fb2[k % 2], bcs2[k % 2], tmp2[k % 2]

            def f0():
                if pre is not None:
                    pre()
                if first:
                    self.TS(rr_[64:65, :], o[64:65, :], 1e-18, None, ALU.max, None, [o], [rr_])
                    self.ACT(rr_[64:65, :], rr_[64:65, :], AF.Ln, [rr_], [rr_])
                else:
                    self.ACT(rr_[64:65, :], o[64:65, :], AF.Ln, [o], [rr_])
                self.ACT(rr_[64:65, :], rr_[64:65, :], AF.Exp, [rr_], [rr_], scale=-1.0)
                self.MM(m[0:65, :], selbf[0:48, row * 64:row * 64 + 65], GTb[0:48, qs], True, True, [GTb, selbf], [m])

            def f1():
                self.TT(fb_[64:65, :], m[64:65, :], rr_[64:65, :], ALU.mult, [m, rr_], [fb_])
                self.MM(m[0:64, :], self.ones_bf[64:65, 0:64], fb_[64:65, :], True, True, [fb_, self.cbf], [m])

            def f2():
                self.CP(bs_[:], m[0:64, :], [m], [bs_], eng="vector")
                if first:
                    self.TT(a_[:], o[0:64, :], bs_[:], ALU.mult, [o, bs_], [a_])
                else:
                    self.TT(tmp_[:], o[0:64, :], bs_[:], ALU.mult, [o, bs_], [tmp_])
                    self.TT(a_[:], a_[:], tmp_[:], ALU.add, [a_, tmp_], [a_], eng="gpsimd")
                if extra is not None:
                    extra()
            if len(delays) == 3:
                return U(None, None, None, later=[(delays[0], f0), (delays[1], f1), (delays[2], f2)])
            return U(None, None, f0, later=[(delays[0], f1), (delays[1], f2)])

        for g in range(4):
            ks_, kw_ = KS[g % 2], KW[g % 2]
            self.LD(ks_[0:64, :], self.KSd[g * 64:(g + 1) * 64, :], [mb["KSd"]], [ks_])
            self.LD(kw_[:], self.KWd[g * 64:(g + 1) * 64, :], [mb["KWd"]], [kw_])
            for hh in range(4):
                h = g * 4 + hh
                self.LD(Q4[hh][0:64, :], self.QN[h * 64:(h + 1) * 64, :], [mb["QN"]], [Q4[hh]])
            for qg in range(NG):
                qs = slice(qg * TG, (qg + 1) * TG)
                q0 = qg * TG
                ntile = 2 if qg >= 4 else 1
                units = []
                accs = [Oc, Ow, Sp[1], Sp[2]]
                XS = [Sp[0], M1, M2]
                xi = 0
                for hh in range(4):
                    h = g * 4 + hh
                    pc = PC[hh]
                    for i in range(ntile):
                        sp = XS[xi % 3]
                        xi += 1

                        def A(sp=sp, i=i, hh=hh):
                            self.MM(sp[:], KCMP[:, g, i * 128:(i + 1) * 128], Q4[hh][0:64, qs], True, False, [KCMP, Q4[hh]], [sp])
                            self.MM(sp[:], self.ident_bf[:], cmpm[:, i, qs], False, True, [cmpm, self.cbf], [sp])

                        def B(sp=sp, i=i, pc=pc):
                            self.ACT(pc[:, i, :], sp[:], AF.Exp, [sp], [pc], scale=SC)

                        def C(i=i, pc=pc, oc=accs[hh]):
                            self.MM(oc[0:128, :], VCMP[:, g, i, :], pc[:, i, :], i == 0, i == ntile - 1, [VCMP, pc], [oc])
                        units.append(U(A, B, C))
                for hh in range(4):
                    h = g * 4 + hh
                    pc = PC[hh]

                    def pre(hh=hh, pc=pc):
                        for qt in range(4):
                            for i in range(ntile):
                                self.MM(imp[:, qt * 65:(qt + 1) * 65], pc[:, i, qt * 128:(qt + 1) * 128], ovl[:, i, :], i == 0, i == ntile - 1, [pc, ovl], [imp])
                        iv = imp[:, 0:260].rearrange("p (t f) -> p t f", f=65)
                        self.TS(irec[:], iv[:, :, 64], 1e-30, None, ALU.max, None, [imp], [irec])
                        self.P.dve(lambda e: e.reciprocal(out=irec[:], in_=irec[:]), reads=[irec.b], writes=[irec.b])
                        if hh == 0:
                            self.TT(iacc[:], iv[:, :, 0:64], irec[:].unsqueeze(2).broadcast_to([128, 4, 64]), ALU.mult, [imp, irec], [iacc])
                        else:
                            self.TT(itmp[:], iv[:, :, 0:64], irec[:].unsqueeze(2).broadcast_to([128, 4, 64]), ALU.mult, [imp, irec], [itmp])
                            self.TT(iacc[:], iacc[:], itmp[:], ALU.add, [iacc, itmp], [iacc], eng="gpsimd")
                    units.append(finish_unit(accs[hh], 3 * h + 0, A4[hh], True, qs, pre=pre, delays=(1, 1)))
                run_units(units, 2)
                for qt in range(4):
                    tix = qg * 4 + qt
                    self.TT(score[:, qt, :], iacc[:, qt, :], self.c32[:, wo + 64 - 2 * tix:wo + 128 - 2 * tix], ALU.add, [iacc, self.cb], [score])
                    self.TT(score[:, qt, :], score[:, qt, :], col0, ALU.max, [score, self.cb], [score])
                    self.P.dve(lambda e, qt=qt: e.max(out=m8[:, 0:8], in_=score[:, qt, :]), reads=[score.b], writes=[m8.b])
                    self.P.dve(lambda e, qt=qt: e.match_replace(out=sc2[:], in_to_replace=m8[:, 0:8], in_values=score[:, qt, :], imm_value=-3.0e38),
                               reads=[score.b, m8.b], writes=[sc2.b])
                    self.P.dve(lambda e: e.max(out=m8[:, 8:16], in_=sc2[:]), reads=[sc2.b], writes=[m8.b])
                    self.TS(nself[:, qt, 64:128], score[:, qt, :], m8[:, 15:16], -1.0, ALU.is_ge, ALU.add, [score, m8], [nself])
                    self.TR(M2[:, qt * 128:(qt + 1) * 128], nself[:, qt, :], ident32, [nself, self.cb], [M2])
                for hh in range(4):
                    self.CP(Q4[hh][64:128, qs], M2[64:128, :], [M2], [Q4n[hh]], eng="scalar")
                units = []
                for hh in range(4):
                    h = g * 4 + hh
                    qh_ = Q4[hh]
                    nk = 4 * qg + 4
                    for kt in range(nk):
                        d = kt - 4 * qg
                        sp = Sp[cnt[0] % 3]
                        pt = PT[cnt[0] % 3]
                        cnt[0] += 1
                        kc = slice(kt * 128, (kt + 1) * 128)
                        c0 = max(d, 0) * 128

                        def A(sp=sp, kc=kc, d=d, c0=c0, qh_=qh_, qn_=Q4n[hh]):
                            if d < 0:
                                self.MM(sp[:], ks_[:, kc], qh_[:, qs], True, True, [ks_, qh_, qn_], [sp])
                            else:
                                self.MM(sp[:, c0:c0 + 128], ks_[:, kc], qh_[:, q0 + c0:q0 + c0 + 128], True, False, [ks_, qh_, qn_], [sp])
                                self.MM(sp[:, c0:c0 + 128], self.ident_bf[:], self.cpen_bf[:], False, True, [self.cbf], [sp])
                                if c0 + 128 < TG:
                                    self.MM(sp[:, c0 + 128:TG], ks_[:, kc], qh_[:, q0 + c0 + 128:q0 + TG], True, True, [ks_, qh_, qn_], [sp])

                        def B(sp=sp, pt=pt, c0=c0):
                            self.ACT(pt[:, c0:TG], sp[:, c0:TG], AF.Exp, [sp], [pt], scale=SC)

                        def C(pt=pt, c0=c0, kt=kt):
                            self.MM(Os[0:128, c0:TG], VSW[:, kt, g * 65:g * 65 + 128], pt[:, c0:TG], kt == 0, kt == nk - 1, [pt, VSW], [Os])
                        units.append(U(A, B, C))
                    units.append(finish_unit(Os, 3 * h + 1, A4[hh], False, qs, delays=((2, 5, 7) if qg >= 1 else (1, 3, 4))))
                    kts = [4 * qg] + [kt for kt in range(4 * qg - 4, 4 * qg + 4) if kt >= 0 and kt != 4 * qg]
                    for i, kt in enumerate(kts):
                        d = kt - 4 * qg
                        sp = Sp[cnt[0] % 3]
                        pt = PT[cnt[0] % 3]
                        cnt[0] += 1
                        kc = slice(kt * 128, (kt + 1) * 128)
                        lo = max(d, 0) * 128
                        hi = min(d + 5, 4) * 128
                        if d >= 0:
                            pb, pen = lo, self.cpen_bf
                            rest = (lo + 128, hi)
                        else:
                            pb, pen = hi - 128, self.bpen_bf
                            rest = (lo, hi - 128)

                        def A(sp=sp, kc=kc, pb=pb, pen=pen, rest=rest, qh_=qh_):
                            self.MM(sp[:, pb:pb + 128], kw_[:, kc], qh_[0:64, q0 + pb:q0 + pb + 128], True, False, [kw_, qh_], [sp])
                            self.MM(sp[:, pb:pb + 128], self.ident_bf[:], pen[:], False, True, [self.cbf], [sp])
                            if rest[1] > rest[0]:
                                self.MM(sp[:, rest[0]:rest[1]], kw_[:, kc], qh_[0:64, q0 + rest[0]:q0 + rest[1]], True, True, [kw_, qh_], [sp])

                        def B(sp=sp, pt=pt, lo=lo, hi=hi):
                            self.ACT(pt[:, lo:hi], sp[:, lo:hi], AF.Exp, [sp], [pt], scale=SC)

                        def C(pt=pt, lo=lo, hi=hi, kt=kt, i=i, nw=len(kts)):
                            self.MM(Ow[0:128, lo:hi], VSW[:, kt, (4 + g) * 65:(4 + g) * 65 + 128], pt[:, lo:hi], i == 0, i == nw - 1, [pt, VSW], [Ow])
                        units.append(U(A, B, C))

                    def store(h=h, hh=hh, qs=qs):
                        os_ = ost[h % 2]
                        self.CP(os_[:], A4[hh][:], [A4[hh]], [os_], eng="gpsimd")
                        self.LD(self.ONT[h, :, qs], os_[:], [os_], [mb["ONT"]], eng="gpsimd")
                    units.append(finish_unit(Ow, 3 * h + 2, A4[hh], False, qs, extra=store, delays=((2, 5, 7) if qg >= 1 else (1, 3, 4))))
                run_units(units, 2)
    P.barrier()


KB.nsa_attn = _nsa_attn
```
